# Optimizing a Trainium2 kernel written in Bass

```python
import math
import jax, jax.numpy as jnp
from jax import lax
import numpy as np

D_MODEL = 4096
BATCH = 4
SEQ = 4096
DEPTH = 1
DEC_BATCH = 16
DEC_SEQ = 32
PAST_LEN = 1024

CHUNK = 64
Q_BLOCK = 128
HEAD_DIM = 128
ROPE_THETA = 10000.0
EPS = 1e-6
DSA_HEADS = 16
DSA_KV_HEADS = 4
IDX_HEADS = 16
IDX_DIM = 128
DSA_TOPK = 256
DIFF_HEADS = 8
DIFF_D = 128
DSA_WIDTH = DSA_HEADS * HEAD_DIM
DIFF_WIDTH = DIFF_HEADS * 2 * DIFF_D
MIX_WIDTH = DSA_WIDTH + DIFF_WIDTH
IN_SIZES = (DSA_WIDTH, DSA_KV_HEADS * HEAD_DIM, DSA_KV_HEADS * HEAD_DIM, IDX_HEADS * IDX_DIM, IDX_DIM, IDX_HEADS, DIFF_WIDTH, DIFF_WIDTH, DIFF_WIDTH)
IN_WIDTH = sum(IN_SIZES)
PEER_HEADS = 8
PEER_N_KEYS = 128
PEER_EXPERTS = PEER_N_KEYS * PEER_N_KEYS
PEER_DK = 256
PEER_HALF = PEER_DK // 2
PEER_TOPK = 16
PEER_BLOCK = 128

kernel_name = "hybrid_dsa_diffattn_peer_stream_step"


def lambda_init(layer):
    return 0.8 - 0.6 * math.exp(-0.3 * layer)


def rmsnorm(x, g):
    xf = x.astype(jnp.float32)
    y = xf * lax.rsqrt(jnp.mean(xf * xf, axis=-1, keepdims=True) + EPS)
    return (y * g.astype(jnp.float32)).astype(x.dtype)


def rope(x, pos):
    half = x.shape[-1] // 2
    inv = ROPE_THETA ** (-jnp.arange(half, dtype=jnp.float32) / half)
    ang = pos.astype(jnp.float32)[:, None] * inv[None, :]
    ang = ang.reshape(ang.shape[0], *([1] * (x.ndim - 3)), half)
    cos, sin = jnp.cos(ang), jnp.sin(ang)
    x1 = x[..., :half].astype(jnp.float32)
    x2 = x[..., half:].astype(jnp.float32)
    return jnp.concatenate([x1 * cos - x2 * sin, x2 * cos + x1 * sin], axis=-1).astype(x.dtype)


def chunk_visible(q_pos, k_pos):
    return (k_pos[None, :] // CHUNK) <= (q_pos[:, None] // CHUNK)


def map_query_blocks(fn, q_arrays, q_pos):
    n_blk = q_pos.shape[0] // Q_BLOCK

    def split(a):
        a = a.reshape(a.shape[0], n_blk, Q_BLOCK, *a.shape[2:])
        return jnp.moveaxis(a, 1, 0)

    xs = (tuple(split(a) for a in q_arrays), q_pos.reshape(n_blk, Q_BLOCK))
    out = lax.map(lambda xp: fn(*xp[0], xp[1]), xs)
    out = jnp.moveaxis(out, 0, 1)
    return out.reshape(out.shape[0], n_blk * Q_BLOCK, *out.shape[3:])


def dsa_attend(q, qi, wi, q_pos, k, v, ki, k_pos, n_sel):
    b, t = q.shape[0], q.shape[1]
    visible = chunk_visible(q_pos, k_pos)
    logits = jnp.einsum('bthd,bsd->bths', qi, ki, preferred_element_type=jnp.float32) * (IDX_DIM ** -0.5)
    score = jnp.einsum('bths,bth->bts', jax.nn.relu(logits), wi.astype(jnp.float32)) * (IDX_HEADS ** -0.5)
    score = jnp.where(visible[None], score, -jnp.inf)
    _, sel = lax.top_k(score, n_sel)
    sel_ok = (k_pos[sel] // CHUNK) <= (q_pos[None, :, None] // CHUNK)
    gather = jax.vmap(lambda rows, idx: rows[idx])
    k_sel = gather(k, sel)
    v_sel = gather(v, sel)
    qg = q.reshape(b, t, DSA_KV_HEADS, DSA_HEADS // DSA_KV_HEADS, HEAD_DIM)
    s = jnp.einsum('btgrd,btngd->btgrn', qg, k_sel, preferred_element_type=jnp.float32) * (HEAD_DIM ** -0.5)
    s = jnp.where(sel_ok[:, :, None, None, :], s, -jnp.inf)
    p = jax.nn.softmax(s, axis=-1).astype(v.dtype)
    o = jnp.einsum('btgrn,btngd->btgrd', p, v_sel)
    return o.reshape(b, t, DSA_WIDTH)


def diff_attend(dq, q_pos, dk, dv, k_pos, lam):
    visible = chunk_visible(q_pos, k_pos)
    s = jnp.einsum('bthcd,bshcd->bhcts', dq, dk, preferred_element_type=jnp.float32) * (DIFF_D ** -0.5)
    s = jnp.where(visible[None, None, None], s, -jnp.inf)
    p = jax.nn.softmax(s, axis=-1)
    a = (p[:, :, 0] - lam * p[:, :, 1]).astype(dv.dtype)
    return jnp.einsum('bhts,bshe->bthe', a, dv)


def peer_ffn(h, w_query, sub_keys, u, v):
    lead = h.shape[:-1]
    xt = h.reshape(-1, h.shape[-1])
    n_tok = xt.shape[0]
    q = (xt @ w_query).reshape(n_tok, PEER_HEADS, 2, PEER_HALF)
    s = jnp.einsum('thcd,hcnd->thcn', q, sub_keys, preferred_element_type=jnp.float32)
    s1, i1 = lax.top_k(s[:, :, 0], PEER_TOPK)
    s2, i2 = lax.top_k(s[:, :, 1], PEER_TOPK)
    cand = (s1[..., :, None] + s2[..., None, :]).reshape(n_tok, PEER_HEADS, PEER_TOPK * PEER_TOPK)
    best, flat = lax.top_k(cand, PEER_TOPK)
    expert = (jnp.take_along_axis(i1, flat // PEER_TOPK, axis=-1) * PEER_N_KEYS
              + jnp.take_along_axis(i2, flat % PEER_TOPK, axis=-1))
    gate = jax.nn.softmax(best, axis=-1)
    m = PEER_HEADS * PEER_TOPK
    expert = expert.reshape(n_tok, m)
    gate = gate.reshape(n_tok, m)
    n_pad = (-n_tok) % PEER_BLOCK
    n_blk = (n_tok + n_pad) // PEER_BLOCK

    def pad_block(a):
        a = jnp.pad(a, ((0, n_pad),) + ((0, 0),) * (a.ndim - 1))
        return a.reshape(n_blk, PEER_BLOCK, *a.shape[1:])

    def expert_block(args):
        xb, eb, gb = args
        act = jnp.einsum('td,tmd->tm', xb, u[eb], preferred_element_type=jnp.float32)
        wgt = (gb * jax.nn.gelu(act)).astype(v.dtype)
        return jnp.einsum('tm,tmd->td', wgt, v[eb])

    out = lax.map(expert_block, (pad_block(xt), pad_block(expert), pad_block(gate)))
    out = out.reshape(n_blk * PEER_BLOCK, -1)[:n_tok]
    return out.reshape(*lead, -1).astype(h.dtype)


def layer_forward(x, c, past, layer, w_ada, b_ada, g_norm_mix, g_norm_ffn, w_in,
                  lam_q1, lam_k1, lam_q2, lam_k2, g_subln, w_out, peer_wq, peer_keys, peer_u, peer_v):
    b, t = x.shape[0], x.shape[1]
    p_len = 0 if past is None else past[0].shape[1]
    pos = p_len + jnp.arange(t)
    k_pos = jnp.arange(p_len + t)
    n_sel = min(DSA_TOPK, (p_len + t) // 4)

    mod = jnp.einsum('bd,de->be', jax.nn.silu(c), w_ada) + b_ada
    sh1, sc1, ga1, sh2, sc2, ga2 = jnp.split(mod[:, None, :], 6, axis=-1)

    h = rmsnorm(x, g_norm_mix) * (1 + sc1) + sh1
    proj = h @ w_in
    split_at = [int(o) for o in np.cumsum(IN_SIZES)[:-1]]
    q, k, v, qi, ki, wi, dq, dk, dv = jnp.split(proj, split_at, axis=-1)
    q = rope(q.reshape(b, t, DSA_HEADS, HEAD_DIM), pos)
    k = rope(k.reshape(b, t, DSA_KV_HEADS, HEAD_DIM), pos)
    v = v.reshape(b, t, DSA_KV_HEADS, HEAD_DIM)
    qi = rope(qi.reshape(b, t, IDX_HEADS, IDX_DIM), pos)
    ki = rope(ki, pos)
    dq = rope(dq.reshape(b, t, DIFF_HEADS, 2, DIFF_D), pos)
    dk = rope(dk.reshape(b, t, DIFF_HEADS, 2, DIFF_D), pos)
    dv = dv.reshape(b, t, DIFF_HEADS, 2 * DIFF_D)
    new_rows = (k, v, ki, dk, dv)

    if past is None:
        k_all, v_all, ki_all, dk_all, dv_all = new_rows
    else:
        k_all, v_all, ki_all, dk_all, dv_all = (jnp.concatenate([pr, nr], axis=1) for pr, nr in zip(past, new_rows))

    lam = (jnp.exp(jnp.sum(lam_q1.astype(jnp.float32) * lam_k1.astype(jnp.float32)))
           - jnp.exp(jnp.sum(lam_q2.astype(jnp.float32) * lam_k2.astype(jnp.float32)))
           + lambda_init(layer))

    dsa_fn = lambda qb, qib, wib, qp: dsa_attend(qb, qib, wib, qp, k_all, v_all, ki_all, k_pos, n_sel)
    diff_fn = lambda dqb, qp: diff_attend(dqb, qp, dk_all, dv_all, k_pos, lam)
    if past is None:
        a_out = map_query_blocks(dsa_fn, (q, qi, wi), pos)
        d_out = map_query_blocks(diff_fn, (dq,), pos)
    else:
        a_out = dsa_fn(q, qi, wi, pos)
        d_out = diff_fn(dq, pos)
    d_out = (rmsnorm(d_out, g_subln) * (1.0 - lambda_init(layer))).reshape(b, t, DIFF_WIDTH)

    mix = jnp.concatenate([a_out, d_out.astype(a_out.dtype)], axis=-1) @ w_out
    x = x + ga1 * mix
    h2 = rmsnorm(x, g_norm_ffn) * (1 + sc2) + sh2
    x = x + ga2 * peer_ffn(h2, peer_wq, peer_keys, peer_u, peer_v)
    return x, new_rows


def setup_inputs(seed: int = 0) -> dict:
    key = jax.random.key(seed)
    ks = jax.random.split(key, 25)
    f = jnp.float32

    def nrm(k, shape, scale):
        return jax.random.normal(k, shape, f) * scale

    return {
        'x_prompt': nrm(ks[0], (BATCH, SEQ, D_MODEL), 1.0),
        'x_sample': nrm(ks[1], (DEC_BATCH, DEC_SEQ, D_MODEL), 1.0),
        'cache_dsa_k': nrm(ks[2], (DEPTH, DEC_BATCH, PAST_LEN, DSA_KV_HEADS, HEAD_DIM), 1.0),
        'cache_dsa_v': nrm(ks[3], (DEPTH, DEC_BATCH, PAST_LEN, DSA_KV_HEADS, HEAD_DIM), 1.0),
        'cache_idx_k': nrm(ks[4], (DEPTH, DEC_BATCH, PAST_LEN, IDX_DIM), 1.0),
        'cache_diff_k': nrm(ks[5], (DEPTH, DEC_BATCH, PAST_LEN, DIFF_HEADS, 2, DIFF_D), 1.0),
        'cache_diff_v': nrm(ks[6], (DEPTH, DEC_BATCH, PAST_LEN, DIFF_HEADS, 2 * DIFF_D), 1.0),
        'c_prompt': nrm(ks[7], (BATCH, D_MODEL), 1.0),
        'c_sample': nrm(ks[8], (DEC_BATCH, D_MODEL), 1.0),
        'w_ada': nrm(ks[9], (DEPTH, D_MODEL, 6 * D_MODEL), 0.5 * D_MODEL ** -0.5),
        'b_ada': nrm(ks[10], (DEPTH, 6 * D_MODEL), 0.02),
        'g_norm_mix': 1.0 + nrm(ks[11], (DEPTH, D_MODEL), 0.02),
        'g_norm_ffn': 1.0 + nrm(ks[12], (DEPTH, D_MODEL), 0.02),
        'w_in': nrm(ks[13], (DEPTH, D_MODEL, IN_WIDTH), D_MODEL ** -0.5),
        'diff_lambda_q1': nrm(ks[14], (DEPTH, DIFF_D), 0.1),
        'diff_lambda_k1': nrm(ks[15], (DEPTH, DIFF_D), 0.1),
        'diff_lambda_q2': nrm(ks[16], (DEPTH, DIFF_D), 0.1),
        'diff_lambda_k2': nrm(ks[17], (DEPTH, DIFF_D), 0.1),
        'g_diff_subln': 1.0 + nrm(ks[18], (DEPTH, 2 * DIFF_D), 0.02),
        'w_out': nrm(ks[19], (DEPTH, MIX_WIDTH, D_MODEL), MIX_WIDTH ** -0.5),
        'peer_w_query': nrm(ks[20], (DEPTH, D_MODEL, PEER_HEADS * PEER_DK), D_MODEL ** -0.5),
        'peer_sub_keys': nrm(ks[21], (DEPTH, PEER_HEADS, 2, PEER_N_KEYS, PEER_HALF), PEER_HALF ** -0.5),
        'peer_u': nrm(ks[22], (DEPTH, PEER_EXPERTS, D_MODEL), D_MODEL ** -0.5),
        'peer_v': nrm(ks[23], (DEPTH, PEER_EXPERTS, D_MODEL), PEER_HEADS ** -0.5),
        'g_final': 1.0 + nrm(ks[24], (D_MODEL,), 0.02),
    }


def reference(x_prompt, x_sample, cache_dsa_k, cache_dsa_v, cache_idx_k, cache_diff_k, cache_diff_v,
              c_prompt, c_sample, w_ada, b_ada, g_norm_mix, g_norm_ffn, w_in,
              diff_lambda_q1, diff_lambda_k1, diff_lambda_q2, diff_lambda_k2, g_diff_subln, w_out,
              peer_w_query, peer_sub_keys, peer_u, peer_v, g_final):
    hp, hs = x_prompt, x_sample
    rows_p, rows_s = [], []
    for l in range(DEPTH):
        lw = (w_ada[l], b_ada[l], g_norm_mix[l], g_norm_ffn[l], w_in[l],
              diff_lambda_q1[l], diff_lambda_k1[l], diff_lambda_q2[l], diff_lambda_k2[l],
              g_diff_subln[l], w_out[l], peer_w_query[l], peer_sub_keys[l], peer_u[l], peer_v[l])
        hp, rp = layer_forward(hp, c_prompt, None, l, *lw)
        past = (cache_dsa_k[l], cache_dsa_v[l], cache_idx_k[l], cache_diff_k[l], cache_diff_v[l])
        hs, rs = layer_forward(hs, c_sample, past, l, *lw)
        rows_p.append(rp)
        rows_s.append(rs)
    y_prompt = rmsnorm(hp, g_final)
    y_sample = rmsnorm(hs, g_final)
    new_dsa_k_p = jnp.stack([r[0] for r in rows_p])
    new_dsa_v_p = jnp.stack([r[1] for r in rows_p])
    new_idx_k_p = jnp.stack([r[2] for r in rows_p])
    new_diff_k_p = jnp.stack([r[3] for r in rows_p])
    new_diff_v_p = jnp.stack([r[4] for r in rows_p])
    new_dsa_k_s = jnp.stack([r[0] for r in rows_s])
    new_dsa_v_s = jnp.stack([r[1] for r in rows_s])
    new_idx_k_s = jnp.stack([r[2] for r in rows_s])
    new_diff_k_s = jnp.stack([r[3] for r in rows_s])
    new_diff_v_s = jnp.stack([r[4] for r in rows_s])
    return (y_prompt, y_sample, new_dsa_k_p, new_dsa_v_p, new_idx_k_p, new_diff_k_p, new_diff_v_p,
            new_dsa_k_s, new_dsa_v_s, new_idx_k_s, new_diff_k_s, new_diff_v_s)
```

```python
import os
import numpy as np
import concourse.bass as bass
import concourse.mybir as mybir
from concourse.bass_utils import run_bass_kernel_spmd

F32 = mybir.dt.float32
BF16 = mybir.dt.bfloat16
U32 = mybir.dt.uint32
ALU = mybir.AluOpType
AF = mybir.ActivationFunctionType
AX = mybir.AxisListType

ENGS = ("sync", "scalar", "vector", "gpsimd", "tensor")
NDS = 6

D = 4096
NQT = 16
NTOK = NQT * 128 + 64
SEQ = 4096
PAST = 1024
LS = PAST + 32
C_Q, C_K, C_V, C_QI, C_KI, C_WI, C_DQ, C_DK, C_DV, C_END = 0, 2048, 2560, 3072, 5120, 5248, 5264, 7312, 9360, 11408
EPS = 1e-6
NEG = -1.0e30
LAM_INIT = 0.8 - 0.6


class Buf:
    __slots__ = ("t", "lw", "rd")

    def __init__(self, t):
        self.t = t
        self.lw = set()
        self.rd = set()

    def __getitem__(self, k):
        return self.t[k]


class Prog:
    def __init__(self, nc):
        self.nc = nc
        self.eng = {"sync": nc.sync, "scalar": nc.scalar, "vector": nc.vector,
                    "gpsimd": nc.gpsimd, "tensor": nc.tensor}
        self.sem = {}
        self.cnt = {e: 0 for e in ENGS}
        self.known = {e: {} for e in ENGS}
        self.dsem = {}
        self.dcnt = {e: 0 for e in ENGS}
        self._stack = []
        self.ninstr = 0
        self.nwaits = 0

    def open(self):
        nc = self.nc
        for e in ENGS:
            cm = nc.semaphore("s_" + e)
            self.sem[e] = cm.__enter__()
            self._stack.append(cm)
        for e in ("sync", "gpsimd", "scalar"):
            self.dsem[e] = []
            for i in range(NDS):
                cm = nc.semaphore("d_%s%d" % (e, i))
                self.dsem[e].append(cm.__enter__())
                self._stack.append(cm)

    def close(self):
        for cm in reversed(self._stack):
            cm.__exit__(None, None, None)

    def _wait(self, engine, ev):
        if ev[0] == "c":
            _, p, v = ev
            if p == "tensor" and engine == "tensor":
                return
            key = ("c", p)
            if self.known[engine].get(key, 0) >= v:
                return
            self.eng[engine].wait_ge(self.sem[p], v)
        else:
            _, q, slot, v = ev
            key = ("d", q, slot)
            if self.known[engine].get(key, 0) >= v:
                return
            self.eng[engine].wait_ge(self.dsem[q][slot], v)
        self.known[engine][key] = v
        self.nwaits += 1

    def _deps(self, engine, reads, writes):
        deps = set()
        for b in reads:
            deps |= b.lw
        for b in writes:
            deps |= b.lw
            deps |= b.rd
        best = {}
        for ev in deps:
            key = ev[:-1]
            if key not in best or best[key][-1] < ev[-1]:
                best[key] = ev
        for key in sorted(best, key=str):
            self._wait(engine, best[key])

    def _commit(self, ev, reads, writes):
        for b in writes:
            b.lw = {ev}
            b.rd = set()
        for b in reads:
            if b not in writes:
                b.rd.add(ev)

    def op(self, engine, fn, reads=(), writes=()):
        self._deps(engine, reads, writes)
        ins = fn(self.eng[engine])
        self.cnt[engine] += 1
        ins.then_inc(self.sem[engine], 1)
        self._commit(("c", engine, self.cnt[engine]), reads, writes)
        self.ninstr += 1

    def dma(self, engine, out, in_, reads=(), writes=(), **kw):
        self._deps(engine, reads, writes)
        n = self.dcnt[engine]
        self.dcnt[engine] += 1
        slot = n % NDS
        ins = self.eng[engine].dma_start(out=out, in_=in_, **kw)
        ins.then_inc(self.dsem[engine][slot], 16)
        self._commit(("d", engine, slot, 16 * (n // NDS + 1)), reads, writes)
        self.ninstr += 1

    def barrier(self):
        evs = []
        for p in ENGS:
            if self.cnt[p] > 0:
                evs.append(("c", p, self.cnt[p]))
        for q in self.dsem:
            n = self.dcnt[q]
            for slot in range(NDS):
                k = (n - slot + NDS - 1) // NDS if n > slot else 0
                if k > 0:
                    evs.append(("d", q, slot, 16 * k))
        for e in ENGS:
            for ev in evs:
                self._wait(e, ev)


class Ctx:
    def __init__(self, nc):
        self.nc = nc
        self.stack = []
        self.k = 0

    def sb(self, shape, dt, name=None):
        self.k += 1
        cm = self.nc.sbuf_tensor("%s_%d" % (name or "t", self.k), list(shape), dt)
        t = cm.__enter__()
        self.stack.append(cm)
        return Buf(t)

    def ps(self, shape, dt, name=None):
        self.k += 1
        cm = self.nc.psum_tensor("%s_%d" % (name or "p", self.k), list(shape), dt)
        t = cm.__enter__()
        self.stack.append(cm)
        return Buf(t)

    def mark(self):
        return len(self.stack)

    def release(self, mark):
        while len(self.stack) > mark:
            self.stack.pop().__exit__(None, None, None)


class RR:
    def __init__(self, items):
        self.items = list(items)
        self.i = 0

    def next(self):
        x = self.items[self.i % len(self.items)]
        self.i += 1
        return x


def own_tile_index(hh, i):
    g, o = divmod(i, 2)
    if hh == 0:
        return 4 * g + (0 if o == 0 else 3)
    return 4 * g + (1 if o == 0 else 2)


def nchunks_for(i):
    g, o = divmod(i, 2)
    return 4 * g + (2 if o == 0 else 4)


STAGE = int(os.environ.get("MK_STAGE", "9"))
_INPUT_NAMES = []
_LAST = None


def build_program():
    del _INPUT_NAMES[:]
    nc = bass.Bass("TRN2", target_bir_lowering=False)
    P = Prog(nc)
    P.open()
    C = Ctx(nc)
    T = {}

    def din(name, shape, dt=F32):
        _INPUT_NAMES.append(name)
        T[name] = Buf(nc.dram_tensor(name, list(shape), dt, kind="ExternalInput").ap())
        return T[name]

    def dout(name, shape, dt=F32):
        T[name] = Buf(nc.dram_tensor(name, list(shape), dt, kind="ExternalOutput").ap())
        return T[name]

    def dscr(name, shape, dt=BF16):
        kind = "ExternalOutput" if os.environ.get("MK_DEBUG") else "Internal"
        T[name] = Buf(nc.dram_tensor(name, list(shape), dt, kind=kind).ap())
        return T[name]

    xk = din("xk", [SEQ, D])
    xq = din("xq", [NTOK, D])
    cT = din("cT", [128, 96])
    w_ada = din("w_ada", [D, 6 * D])
    badaT = din("badaT", [128, 192])
    gmixT = din("gmixT", [128, 32])
    gffnT = din("gffnT", [128, 32])
    gfinB = din("gfinB", [128, D])
    w_in = din("w_in", [D, C_END])
    cosk = din("cosk", [SEQ, 128])
    sink = din("sink", [SEQ, 128])
    cosq = din("cosq", [NTOK, 128])
    sinq = din("sinq", [NTOK, 128])
    ident_in = din("ident", [128, 128])
    c_k = din("c_k", [2, PAST, 512])
    c_v = din("c_v", [2, PAST, 512])
    c_ki = din("c_ki", [2, PAST, 128])
    c_dk = din("c_dk", [2, PAST, 2048])
    c_dv = din("c_dv", [2, PAST, 2048])
    kcl_in = din("kcl", [128, 512])
    qrel_in = din("qrel", [NTOK, 1])
    lamv_in = din("lamv", [128, 4, 128])
    gsub_in = din("gsub", [128, 256])
    w_out = din("w_out", [D, D])
    pwq_in = din("pwq", [D, 2048])
    pkeys_in = din("pkeys", [8, 2, 128, 128])
    iota_in = din("iota", [128, 128])
    pu_in = din("pu", [16384, D])
    pv_in = din("pv", [16384, D])
    o_kp = dout("o_kp", [SEQ, 512])
    o_vp = dout("o_vp", [SEQ, 512])
    o_kip = dout("o_kip", [SEQ, 128])
    o_dkp = dout("o_dkp", [SEQ, 2048])
    o_dvp = dout("o_dvp", [SEQ, 2048])
    o_ks = dout("o_ks", [64, 512])
    o_vs = dout("o_vs", [64, 512])
    o_kis = dout("o_kis", [64, 128])
    o_dks = dout("o_dks", [64, 2048])
    o_dvs = dout("o_dvs", [64, 2048])
    o_y = dout("o_y", [NTOK, D])
    kT_s = dscr("kT_s", [4, 128, SEQ])
    v_s = dscr("v_s", [SEQ, 512])
    kiT_s = dscr("kiT_s", [128, SEQ])
    dkT_s = dscr("dkT_s", [16, 128, SEQ])
    dv_s = dscr("dv_s", [SEQ, 2048])
    skT_s = dscr("skT_s", [2, 4, 128, LS])
    sv_s = dscr("sv_s", [2, LS, 512])
    skiT_s = dscr("skiT_s", [2, 128, LS])
    sdkT_s = dscr("sdkT_s", [2, 16, 128, LS])
    sdv_s = dscr("sdv_s", [2, LS, 2048])
    qT_s = dscr("qT_s", [16, 128, NTOK])
    qiT_s = dscr("qiT_s", [16, 128, NTOK])
    dqT_s = dscr("dqT_s", [16, 128, NTOK])
    wi_s = dscr("wi_s", [NTOK, 16], F32)
    ga_s = dscr("ga_s", [4, 128, D], F32)

    attnT_s = dscr("attnT_s", [128, 32, NTOK])
    x1_s = dscr("x1_s", [NTOK, D], F32)
    h2T_s = dscr("h2T_s", [128, 32, NTOK])
    s_s = dscr("s_s", [NTOK, 2048], F32)
    G_s = dscr("G_s", [17, 128, 128, 128])
    ident = C.sb([128, 128], F32, "ident")
    identb = C.sb([128, 128], BF16, "identb")
    modT = C.sb([128, 192, 3], F32, "modT")
    A1 = C.sb([128, 32, 3], F32, "A1")
    A2 = C.sb([128, 32, 3], F32, "A2")
    P.dma("sync", ident[:, :], ident_in[:, :], [ident_in], [ident])
    P.op("vector", lambda e: e.tensor_copy(out=identb[:, :], in_=ident[:, :]), [ident], [identb])

    mk = C.mark()
    psF = [C.ps([128, 512], F32, "psF") for _ in range(6)]
    psB = [C.ps([128, 1024], BF16, "psB") for _ in range(2)]
    scT = C.sb([128, 96], F32, "scT")
    wst = [C.sb([128, 12288], F32, "wst") for _ in range(2)]
    pm = [psF[0], psF[1]]
    bada = C.sb([128, 192], F32, "bada")
    gm = C.sb([128, 32], F32, "gm")
    gf = C.sb([128, 32], F32, "gf")
    P.dma("sync", scT[:, :], cT[:, :], [cT], [scT])
    P.dma("sync", bada[:, :], badaT[:, :], [badaT], [bada])
    P.dma("sync", gm[:, :], gmixT[:, :], [gmixT], [gm])
    P.dma("sync", gf[:, :], gffnT[:, :], [gffnT], [gf])
    sg = C.sb([128, 96], F32, "sg")
    P.op("scalar", lambda e: e.activation(out=sg[:, :], in_=scT[:, :], func=AF.Exp, scale=-1.0), [scT], [sg])
    P.op("vector", lambda e: e.tensor_scalar(out=sg[:, :], in0=sg[:, :], scalar1=1.0, scalar2=None, op0=ALU.add), [sg], [sg])
    P.op("vector", lambda e: e.reciprocal(out=sg[:, :], in_=sg[:, :]), [sg], [sg])
    P.op("vector", lambda e: e.tensor_tensor(out=scT[:, :], in0=scT[:, :], in1=sg[:, :], op=ALU.mult), [scT, sg], [scT])
    k = 0
    for cblk in range(64):
        buf = wst[k % 2]
        q = "sync" if k % 2 == 0 else "gpsimd"
        k += 1
        P.dma(q, buf[:, 0:32 * 384].rearrange("p (c w) -> p c w", w=384),
              w_ada[:, cblk * 384:(cblk + 1) * 384].rearrange("(c p) w -> p c w", p=128), [w_ada], [buf])
        for c3 in range(3):
            cb = cblk * 3 + c3
            half, cbl = divmod(cb, 96)
            for dch in range(32):
                P.op("tensor", lambda e, buf=buf, c3=c3, cbl=cbl, half=half, dch=dch: e.matmul(
                    out=pm[half][:, cbl * 3:cbl * 3 + 3], lhsT=buf[:, dch * 384 + c3 * 128:dch * 384 + (c3 + 1) * 128],
                    rhs=scT[:, dch * 3:dch * 3 + 3], start=(dch == 0), stop=(dch == 31)),
                    [buf, scT], [pm[half]])
    for half in range(2):
        P.op("vector", lambda e, half=half: e.tensor_tensor(
            out=modT[:, half * 96:(half + 1) * 96, :],
            in0=pm[half][:, 0:288].rearrange("p (c r) -> p c r", r=3),
            in1=bada[:, half * 96:(half + 1) * 96].unsqueeze(2).to_broadcast([128, 96, 3]), op=ALU.add),
            [pm[half], bada], [modT])
    for (Ax, base, g) in ((A1, 32, gm), (A2, 128, gf)):
        P.op("vector", lambda e, Ax=Ax, base=base: e.tensor_scalar(
            out=Ax[:, :, :], in0=modT[:, base:base + 32, :], scalar1=1.0, scalar2=None, op0=ALU.add), [modT], [Ax])
        P.op("vector", lambda e, Ax=Ax, g=g: e.tensor_tensor(
            out=Ax[:, :, :], in0=Ax[:, :, :], in1=g[:, :].unsqueeze(2).to_broadcast([128, 32, 3]), op=ALU.mult),
            [Ax, g], [Ax])
    Dm = [C.sb([128, D], F32, "Dm") for _ in range(2)]
    L = C.sb([128, 3, 128], F32, "L")
    gat = [C.sb([128, 512], F32, "gat") for _ in range(2)]
    P.op("vector", lambda e: e.memset(L[:, 0, :], 1.0), [], [L])
    P.op("vector", lambda e: e.memset(L[:, 1:3, :], 0.0), [L], [L])
    P.op("vector", lambda e: e.memset(L[:, 1, 0:32], 1.0), [L], [L])
    P.op("vector", lambda e: e.memset(L[:, 2, 32:64], 1.0), [L], [L])
    eng2 = RR(["vector", "gpsimd"])
    kk = 0
    for which, base in ((0, 64), (1, 160)):
        for grp in range(2):
            rs = [0] if grp == 0 else [1, 2]
            for ri, r in enumerate(rs):
                for ch in range(32):
                    P.op(eng2.next(), lambda e, ri=ri, r=r, ch=ch, base=base: e.tensor_scalar(
                        out=Dm[ri][:, ch * 128:(ch + 1) * 128], in0=ident[:, :],
                        scalar1=modT[:, base + ch, r:r + 1], scalar2=None, op0=ALU.mult),
                        [ident, modT], [Dm[ri]])
            for blk in range(8):
                pb = psF[2 + kk % 2]
                gt = gat[kk % 2]
                kk += 1
                for ri, r in enumerate(rs):
                    P.op("tensor", lambda e, ri=ri, r=r, blk=blk, pb=pb, n=len(rs): e.matmul(
                        out=pb[:, :], lhsT=L[:, r, :], rhs=Dm[ri][:, blk * 512:(blk + 1) * 512],
                        start=(ri == 0), stop=(ri == n - 1)), [L, Dm[ri]], [pb])
                P.op("scalar", lambda e, pb=pb, gt=gt: e.copy(out=gt[:, :], in_=pb[:, :]), [pb], [gt])
                P.dma("gpsimd", ga_s[grp * 2 + which, :, blk * 512:(blk + 1) * 512], gt[:, :], [gt], [ga_s])
    P.barrier()
    C.release(mk)

    mk = C.mark()
    psF = [C.ps([128, 512], F32, "psF") for _ in range(6)]
    psB = [C.ps([128, 1024], BF16, "psB") for _ in range(2)]
    xts = RR([C.sb([128, D], F32, "xt") for _ in range(1)])
    junk = C.sb([128, D], BF16, "junk")
    sst = RR([C.sb([128, 2], F32, "ss") for _ in range(2)])
    hTs = [C.sb([128, 32, 128], BF16, "hT") for _ in range(9)]
    wsts = RR([C.sb([128, 4, 512], F32, "wst") for _ in range(2)])
    wbfs = RR([C.sb([128, 32, 512], BF16, "wbf") for _ in range(2)])
    ofs = RR([C.sb([128, 512], F32, "of") for _ in range(3)])
    tmps = RR([C.sb([128, 512], F32, "tmp") for _ in range(2)])
    obs = RR([C.sb([128, 512], BF16, "ob") for _ in range(3)])
    tbs = RR([C.sb([128, 4, 128], BF16, "tb") for _ in range(3)])
    tabs = RR([(C.sb([128, 128], F32, "cs"), C.sb([128, 128], F32, "sn")) for _ in range(3)])
    ps_proj = RR(psF[0:3])
    ps_xT = RR(psF[3:6])
    ps_hT = RR(psB)
    evac = RR(["vector", "scalar"])
    castq = RR(["scalar", "gpsimd"])

    def load_norm_T(src, row0, n, hT, prompt, Ax, shbase):
        xt = xts.next()
        ss = sst.next()
        P.dma("sync", xt[0:n, :], src[row0:row0 + n, :], [src], [xt])
        P.op("vector", lambda e: e.memset(ss[:, :], 0.0), [], [ss])
        P.op("scalar", lambda e: e.activation(out=junk[0:n, :], in_=xt[0:n, :], func=AF.Square,
                                              accum_out=ss[0:n, 0:1]), [xt], [junk, ss])
        P.op("vector", lambda e: e.tensor_scalar(out=ss[0:n, 1:2], in0=ss[0:n, 0:1], scalar1=1.0 / D, scalar2=EPS,
                                                 op0=ALU.mult, op1=ALU.add), [ss], [ss])
        P.op("scalar", lambda e: e.activation(out=ss[0:n, 1:2], in_=ss[0:n, 1:2], func=AF.Sqrt), [ss], [ss])
        P.op("vector", lambda e: e.reciprocal(out=ss[0:n, 1:2], in_=ss[0:n, 1:2]), [ss], [ss])
        P.op("gpsimd", lambda e: e.tensor_scalar(out=xt[0:n, :], in0=xt[0:n, :], scalar1=ss[0:n, 1:2], scalar2=None,
                                                 op0=ALU.mult), [xt, ss], [xt])
        for c4 in range(8):
            pb = ps_xT.next()
            for j in range(4):
                ch = c4 * 4 + j
                P.op("tensor", lambda e, ch=ch, j=j, pb=pb: e.transpose(
                    out=pb[:, j * 128:j * 128 + n], in_=xt[0:n, ch * 128:(ch + 1) * 128], identity=ident[0:n, 0:n]),
                    [xt, ident], [pb])
            for j in range(4):
                ch = c4 * 4 + j
                segs = [(0, n, 0)] if prompt else [(0, 32, 1), (32, 64, 2)]
                for (a, b_, r) in segs:
                    en = evac.next()
                    if en == "vector":
                        P.op("vector", lambda e, ch=ch, j=j, pb=pb, a=a, b_=b_, r=r: e.tensor_scalar(
                            out=hT[:, ch, a:b_], in0=pb[:, j * 128 + a:j * 128 + b_], scalar1=Ax[:, ch, r:r + 1],
                            scalar2=modT[:, shbase + ch, r:r + 1], op0=ALU.mult, op1=ALU.add),
                            [pb, Ax, modT], [hT])
                    else:
                        P.op("scalar", lambda e, ch=ch, j=j, pb=pb, a=a, b_=b_, r=r: e.activation(
                            out=hT[:, ch, a:b_], in_=pb[:, j * 128 + a:j * 128 + b_], func=AF.Identity,
                            scale=Ax[:, ch, r:r + 1], bias=modT[:, shbase + ch, r:r + 1]),
                            [pb, Ax, modT], [hT])
        return xt

    def load_weights(wsrc, c0, W):
        wbf = wbfs.next()
        for p4 in range(8):
            ws = wsts.next()
            P.dma("sync", ws[:, :, 0:W],
                  wsrc[p4 * 512:(p4 + 1) * 512, c0:c0 + W].rearrange("(c p) w -> p c w", p=128), [wsrc], [ws])
            en = castq.next()
            if en == "scalar":
                P.op("scalar", lambda e, ws=ws, p4=p4: e.copy(out=wbf[:, p4 * 4:(p4 + 1) * 4, 0:W], in_=ws[:, :, 0:W]),
                     [ws], [wbf])
            else:
                P.op("gpsimd", lambda e, ws=ws, p4=p4: e.tensor_copy(out=wbf[:, p4 * 4:(p4 + 1) * 4, 0:W], in_=ws[:, :, 0:W]),
                     [ws], [wbf])
        return wbf

    def project(hT, n, wbf, W):
        pb = ps_proj.next()
        for ch in range(32):
            P.op("tensor", lambda e, ch=ch: e.matmul(out=pb[0:n, 0:W], lhsT=hT[:, ch, 0:n], rhs=wbf[:, ch, 0:W],
                                                     start=(ch == 0), stop=(ch == 31)), [hT, wbf], [pb])
        return pb

    def epilogue(src, sbuf, n, W, rope=None, f32dst=None, tokdst=None, featdst=None):
        H = max(W // 128, 1)
        of = ofs.next()
        if rope is not None:
            cs, sn = rope
            tmp = tmps.next()
            s3 = src.rearrange("p (h d) -> p h d", d=128)
            o3 = of[0:n, 0:W].rearrange("p (h d) -> p h d", d=128)
            t3 = tmp[0:n, 0:W].rearrange("p (h d) -> p h d", d=128)
            P.op("vector", lambda e: e.tensor_tensor(out=o3, in0=s3, in1=cs[0:n, :].unsqueeze(1).to_broadcast([n, H, 128]),
                                                     op=ALU.mult), [sbuf, cs], [of])
            P.op("vector", lambda e: e.tensor_tensor(out=t3[:, :, 0:64], in0=s3[:, :, 64:128],
                                                     in1=sn[0:n, 0:64].unsqueeze(1).to_broadcast([n, H, 64]),
                                                     op=ALU.mult), [sbuf, sn], [tmp])
            P.op("vector", lambda e: e.tensor_tensor(out=t3[:, :, 64:128], in0=s3[:, :, 0:64],
                                                     in1=sn[0:n, 64:128].unsqueeze(1).to_broadcast([n, H, 64]),
                                                     op=ALU.mult), [sbuf, sn], [tmp])
            P.op("gpsimd", lambda e: e.tensor_tensor(out=of[0:n, 0:W], in0=of[0:n, 0:W], in1=tmp[0:n, 0:W], op=ALU.add),
                 [of, tmp], [of])
            cur, curb = of[0:n, 0:W], of
        elif f32dst is not None:
            P.op("scalar", lambda e: e.copy(out=of[0:n, 0:W], in_=src), [sbuf], [of])
            cur, curb = of[0:n, 0:W], of
        else:
            cur, curb = src, sbuf
        if f32dst is not None:
            P.dma("gpsimd", f32dst[1], cur, [curb], [f32dst[0]])
        if tokdst is None and featdst is None:
            return
        ob = obs.next()
        P.op("scalar", lambda e: e.copy(out=ob[0:n, 0:W], in_=cur), [curb], [ob])
        if tokdst is not None:
            for (db, dap, r0, r1) in tokdst:
                P.dma("gpsimd", dap, ob[r0:r1, 0:W], [ob], [db])
        if featdst is not None:
            pb = ps_hT.next()
            tb = tbs.next()
            for h in range(H):
                P.op("tensor", lambda e, h=h: e.transpose(out=pb[:, h * 128:h * 128 + n], in_=ob[0:n, h * 128:(h + 1) * 128],
                                                          identity=identb[0:n, 0:n]), [ob, identb], [pb])
            en = evac.next()
            pv = pb[:, 0:H * 128].rearrange("p (h t) -> p h t", t=128)[:, :, 0:n]
            if en == "vector":
                P.op("vector", lambda e: e.tensor_copy(out=tb[:, 0:H, 0:n], in_=pv), [pb], [tb])
            else:
                P.op("scalar", lambda e: e.copy(out=tb[:, 0:H, 0:n], in_=pv), [pb], [tb])
            for (db, apfn, c0, c1) in featdst:
                P.dma("gpsimd", apfn(H), tb[:, 0:H, c0:c1], [tb], [db])

    def featT(buf, h0, t0, ncols, lead=None):
        def fn(H):
            base = buf.t if lead is None else buf.t[lead]
            return base[h0:h0 + H, :, t0:t0 + ncols].rearrange("h d t -> d h t")
        return fn

    def featT1(buf, t0, ncols, lead=None):
        def fn(H):
            base = buf.t if lead is None else buf.t[lead]
            return base[:, t0:t0 + ncols].unsqueeze(1)
        return fn

    KBLOCKS = [("k", C_K, 512, 0), ("v", C_V, 512, 0), ("ki", C_KI, 144, 0)] + \
              [("dk", C_DK + 512 * i, 512, 4 * i) for i in range(4)] + [("dv", C_DV + 512 * i, 512, 4 * i) for i in range(4)]
    QBLOCKS = [("q", C_Q + 512 * i, 512, 4 * i) for i in range(4)] + [("qi", C_QI + 512 * i, 512, 4 * i) for i in range(4)] + \
              [("ki", C_KI, 144, 0)] + [("dq", C_DQ + 512 * i, 512, 4 * i) for i in range(4)]

    def load_tabs(cb, sb_, row0, n):
        cs, sn = tabs.next()
        P.dma("sync", cs[0:n, :], cb[row0:row0 + n, :], [cb], [cs])
        P.dma("sync", sn[0:n, :], sb_[row0:row0 + n, :], [sb_], [sn])
        return cs, sn

    def k_epilogue(kind, pb, n, W, h0, tb_, prompt, tok0):
        src = pb[0:n, 0:W]
        if kind == "ki":
            src = pb[0:n, 0:128]
            W = 128
        ci = C_DK if kind == "dk" else C_DV
        if prompt:
            if kind == "k":
                epilogue(src, pb, n, W, rope=tb_, f32dst=(o_kp, o_kp[tok0:tok0 + n, :]),
                         featdst=[(kT_s, featT(kT_s, 0, tok0, n), 0, n)])
            elif kind == "v":
                epilogue(src, pb, n, W, f32dst=(o_vp, o_vp[tok0:tok0 + n, :]), tokdst=[(v_s, v_s[tok0:tok0 + n, :], 0, n)])
            elif kind == "ki":
                epilogue(src, pb, n, W, rope=tb_, f32dst=(o_kip, o_kip[tok0:tok0 + n, :]),
                         featdst=[(kiT_s, featT1(kiT_s, tok0, n), 0, n)])
            elif kind == "dk":
                epilogue(src, pb, n, W, rope=tb_, f32dst=(o_dkp, o_dkp[tok0:tok0 + n, h0 * 128:h0 * 128 + W]),
                         featdst=[(dkT_s, featT(dkT_s, h0, tok0, n), 0, n)])
            elif kind == "dv":
                epilogue(src, pb, n, W, f32dst=(o_dvp, o_dvp[tok0:tok0 + n, h0 * 128:h0 * 128 + W]),
                         tokdst=[(dv_s, dv_s[tok0:tok0 + n, h0 * 128:h0 * 128 + W], 0, n)])
        else:
            if kind == "k":
                epilogue(src, pb, n, W, rope=tb_, f32dst=(o_ks, o_ks[:, :]),
                         featdst=[(skT_s, featT(skT_s, 0, PAST, 32, lead=s), 32 * s, 32 * s + 32) for s in range(2)])
            elif kind == "v":
                epilogue(src, pb, n, W, f32dst=(o_vs, o_vs[:, :]),
                         tokdst=[(sv_s, sv_s[s, PAST:LS, :], 32 * s, 32 * s + 32) for s in range(2)])
            elif kind == "ki":
                epilogue(src, pb, n, W, rope=tb_, f32dst=(o_kis, o_kis[:, :]),
                         featdst=[(skiT_s, featT1(skiT_s, PAST, 32, lead=s), 32 * s, 32 * s + 32) for s in range(2)])
            elif kind == "dk":
                epilogue(src, pb, n, W, rope=tb_, f32dst=(o_dks, o_dks[:, h0 * 128:h0 * 128 + W]),
                         featdst=[(sdkT_s, featT(sdkT_s, h0, PAST, 32, lead=s), 32 * s, 32 * s + 32) for s in range(2)])
            elif kind == "dv":
                epilogue(src, pb, n, W, f32dst=(o_dvs, o_dvs[:, h0 * 128:h0 * 128 + W]),
                         tokdst=[(sdv_s, sdv_s[s, PAST:LS, h0 * 128:h0 * 128 + W], 32 * s, 32 * s + 32) for s in range(2)])

    def q_epilogue(kind, pb, n, W, h0, tb_, tok0):
        if kind == "ki":
            P.op("scalar", lambda e: e.copy(out=wit[0:n, :], in_=pb[0:n, 128:144]), [pb], [wit])
            P.dma("gpsimd", wi_s[tok0:tok0 + n, :], wit[0:n, :], [wit], [wi_s])
            return
        dst = {"q": qT_s, "qi": qiT_s, "dq": dqT_s}[kind]
        epilogue(pb[0:n, 0:W], pb, n, W, rope=tb_, featdst=[(dst, featT(dst, h0, tok0, n), 0, n)])

    wit = C.sb([128, 16], F32, "wit")

    NKT = int(os.environ.get("MK_NKT", "32"))
    for g0 in range(0, NKT, 8):
        tiles = list(range(g0, min(g0 + 8, NKT)))
        tabl = {}
        for si, j in enumerate(tiles):
            load_norm_T(xk, j * 128, 128, hTs[si], True, A1, 0)
        for (kind, c0, W, h0) in KBLOCKS:
            wbf = load_weights(w_in, c0, W)
            for si, j in enumerate(tiles):
                pb = project(hTs[si], 128, wbf, W)
                tb_ = load_tabs(cosk, sink, j * 128, 128) if kind in ("k", "ki", "dk") else None
                k_epilogue(kind, pb, 128, W, h0, tb_, True, j * 128)
    NQ = int(os.environ.get("MK_NQT", str(NQT)))
    for grp in range(2):
        tiles = list(range(grp * 8, min(grp * 8 + 8, NQ)))
        slots = [(si, i, 128, True) for si, i in enumerate(tiles)]
        if grp == 1:
            slots.append((8, NQT, 64, False))
        for (si, i, n, prompt) in slots:
            load_norm_T(xq, i * 128, n, hTs[si], prompt, A1, 0)
        for (kind, c0, W, h0) in QBLOCKS:
            wbf = load_weights(w_in, c0, W)
            for (si, i, n, prompt) in slots:
                pb = project(hTs[si], n, wbf, W)
                tb_ = load_tabs(cosq, sinq, i * 128, n) if kind != "ki" or not prompt else None
                q_epilogue(kind, pb, n, W, h0, tb_, i * 128)
                if kind == "ki" and not prompt:
                    k_epilogue("ki", pb, n, W, 0, tb_, False, 0)
        if grp == 1:
            for (kind, c0, W, h0) in KBLOCKS:
                if kind == "ki":
                    continue
                wbf = load_weights(w_in, c0, W)
                pb = project(hTs[8], 64, wbf, W)
                tb_ = load_tabs(cosq, sinq, NQT * 128, 64) if kind in ("k", "dk") else None
                k_epilogue(kind, pb, 64, W, h0, tb_, False, 0)
    if STAGE >= 2:
        cin = RR([C.sb([128, 512], F32, "cin") for _ in range(3)])
        for s in range(2):
            for t in range(PAST // 128):
                t0 = t * 128

                def ing(srcb, sap, W, **kw):
                    cb = cin.next()
                    P.dma("sync", cb[:, 0:W], sap, [srcb], [cb])
                    epilogue(cb[:, 0:W], cb, 128, W, **kw)
                ing(c_k, c_k[s, t0:t0 + 128, :], 512, featdst=[(skT_s, featT(skT_s, 0, t0, 128, lead=s), 0, 128)])
                ing(c_v, c_v[s, t0:t0 + 128, :], 512, tokdst=[(sv_s, sv_s[s, t0:t0 + 128, :], 0, 128)])
                ing(c_ki, c_ki[s, t0:t0 + 128, :], 128, featdst=[(skiT_s, featT1(skiT_s, t0, 128, lead=s), 0, 128)])
                for hb in range(4):
                    ing(c_dk, c_dk[s, t0:t0 + 128, hb * 512:(hb + 1) * 512], 512,
                        featdst=[(sdkT_s, featT(sdkT_s, hb * 4, t0, 128, lead=s), 0, 128)])
                    ing(c_dv, c_dv[s, t0:t0 + 128, hb * 512:(hb + 1) * 512], 512,
                        tokdst=[(sdv_s, sdv_s[s, t0:t0 + 128, hb * 512:(hb + 1) * 512], 0, 128)])
    P.barrier()
    C.release(mk)
    mk = C.mark()
    psI = RR([C.ps([128, 512], F32, "psI") for _ in range(3)])
    psO = [C.ps([128, 512], F32, "psO") for _ in range(4)]
    psT = RR([C.ps([128, 1024], BF16, "psT") for _ in range(1)])
    kcl = C.sb([128, 512], F32, "kcl")
    gsubB = C.sb([128, 256], F32, "gsubB")
    lamv = C.sb([128, 4, 128], F32, "lamv")
    lamt = C.sb([128, 4], F32, "lamt")
    neglam = C.sb([128, 1], F32, "neglam")
    P.dma("sync", kcl[:, :], kcl_in[:, :], [kcl_in], [kcl])
    P.dma("sync", gsubB[:, :], gsub_in[:, :], [gsub_in], [gsubB])
    P.dma("sync", lamv[:, :, :], lamv_in[:, :, :], [lamv_in], [lamv])
    P.op("vector", lambda e: e.tensor_scalar(out=gsubB[:, :], in0=gsubB[:, :], scalar1=1.0 - LAM_INIT, scalar2=None,
                                             op0=ALU.mult), [gsubB], [gsubB])
    P.op("vector", lambda e: e.tensor_tensor(out=lamv[:, 0, :], in0=lamv[:, 0, :], in1=lamv[:, 1, :], op=ALU.mult), [lamv], [lamv])
    P.op("vector", lambda e: e.tensor_tensor(out=lamv[:, 2, :], in0=lamv[:, 2, :], in1=lamv[:, 3, :], op=ALU.mult), [lamv], [lamv])
    P.op("vector", lambda e: e.reduce_sum(out=lamt[:, 0:1], in_=lamv[:, 0, :], axis=AX.X), [lamv], [lamt])
    P.op("vector", lambda e: e.reduce_sum(out=lamt[:, 1:2], in_=lamv[:, 2, :], axis=AX.X), [lamv], [lamt])
    P.op("scalar", lambda e: e.activation(out=lamt[:, 2:4], in_=lamt[:, 0:2], func=AF.Exp), [lamt], [lamt])
    P.op("vector", lambda e: e.tensor_tensor(out=neglam[:, :], in0=lamt[:, 3:4], in1=lamt[:, 2:3], op=ALU.subtract), [lamt], [neglam])
    P.op("vector", lambda e: e.tensor_scalar(out=neglam[:, :], in0=neglam[:, :], scalar1=-LAM_INIT, scalar2=None, op0=ALU.add),
         [neglam], [neglam])

    kiT = C.sb([128, SEQ], BF16, "kiT")
    qiT = C.sb([128, 16, 128], BF16, "qiT")
    wiq = C.sb([128, 16], F32, "wiq")
    qrel = C.sb([128, 1], F32, "qrel")
    Iw = C.sb([128, SEQ], F32, "Iw")
    wk = C.sb([128, SEQ], F32, "wk")
    rls = RR([C.sb([128, 512], F32, "rl") for _ in range(2)])
    pen = C.sb([128, 512], F32, "pen")
    m8 = C.sb([128, 8], F32, "m8")
    thr = C.sb([128, 1], F32, "thr")
    maskb = C.sb([128, SEQ], BF16, "maskb")
    maskT = C.sb([128, 32, 128], BF16, "maskT")
    visb = C.sb([128, 512], BF16, "visb")
    visT = C.sb([128, 4, 128], BF16, "visT")
    kTs = RR([C.sb([128, SEQ], BF16, "kTg") for _ in range(2)])
    vts = [C.sb([128, 32, 129], BF16, "vt") for _ in range(2)]
    qgs = RR([C.sb([128, 4, 128], BF16, "qg") for _ in range(2)])
    Pts = RR([C.sb([128, 4, 128], BF16, "Pt") for _ in range(3)])
    Pms = RR([C.sb([128, 4, 128], BF16, "Pm") for _ in range(3)])
    dkTs = RR([C.sb([128, 2, SEQ], BF16, "dkT") for _ in range(2)])
    dvts = [C.sb([128, 32, 257], BF16, "dvt") for _ in range(2)]
    dqs = RR([C.sb([128, 2, 128], BF16, "dq") for _ in range(2)])
    rden = C.sb([128, 8], F32, "rden")
    tmpd = C.sb([128, 256], F32, "tmpd")
    dof = C.sb([128, 256], F32, "dof")
    junkd = C.sb([128, 256], BF16, "junkd")
    ssd = C.sb([128, 2], F32, "ssd")
    attn = C.sb([128, D], BF16, "attn")
    tbc = RR([C.sb([128, 8, 128], BF16, "tbc") for _ in range(2)])
    for vt in vts:
        P.op("vector", lambda e, vt=vt: e.memset(vt[:, :, 128:129], 1.0), [], [vt])
    for dvt in dvts:
        P.op("vector", lambda e, dvt=dvt: e.memset(dvt[:, :, 256:257], 1.0), [], [dvt])
    vti = [0]
    dvi = [0]

    def transposes_to(srcbuf, src_fn, dstbuf, dst_fn, chunks, nq):
        for b0 in range(0, len(chunks), 8):
            grp = chunks[b0:b0 + 8]
            pb = psT.next()
            for s_, (k0, kl) in enumerate(grp):
                P.op("tensor", lambda e, s_=s_, k0=k0, kl=kl: e.transpose(
                    out=pb[0:kl, s_ * 128:s_ * 128 + nq], in_=src_fn(k0, kl), identity=identb[0:nq, 0:nq]),
                    [srcbuf, identb], [pb])
            full = [x for x in grp if x[1] == 128]
            if full:
                nf = len(full)
                P.op("scalar", lambda e, b0=b0, nf=nf: e.copy(
                    out=dst_fn(b0, nf, 128), in_=pb[:, 0:nf * 128].rearrange("p (c t) -> p c t", t=128)[:, :, 0:nq]),
                    [pb], [dstbuf])
            for s_, (k0, kl) in enumerate(grp):
                if kl != 128:
                    P.op("scalar", lambda e, s_=s_, kl=kl, b0=b0: e.copy(
                        out=dst_fn(b0 + s_, 1, kl), in_=pb[0:kl, s_ * 128:s_ * 128 + nq].unsqueeze(1)), [pb], [dstbuf])

    def attn_tile(tok0, nq, nkeys, prompt, S):
        chunks = [(k0, min(128, nkeys - k0)) for k0 in range(0, nkeys, 128)]
        nchk = len(chunks)
        nfull = nkeys // 128
        P.dma("sync", qiT[:, :, 0:nq], qiT_s[:, :, tok0:tok0 + nq].rearrange("h d t -> d h t"), [qiT_s], [qiT])
        P.dma("sync", wiq[0:nq, :], wi_s[tok0:tok0 + nq, :], [wi_s], [wiq])
        for kb0 in range(0, nkeys, 512):
            kw = min(512, nkeys - kb0)
            for h in range(16):
                pb = psI.next()
                P.op("tensor", lambda e, h=h, pb=pb: e.matmul(out=pb[0:nq, 0:kw], lhsT=qiT[:, h, 0:nq], rhs=kiT[:, kb0:kb0 + kw],
                                                              start=True, stop=True), [qiT, kiT], [pb])
                rl = rls.next()
                P.op("scalar", lambda e, pb=pb, rl=rl: e.activation(out=rl[0:nq, 0:kw], in_=pb[0:nq, 0:kw], func=AF.Relu), [pb], [rl])
                if h == 0:
                    P.op("vector", lambda e, rl=rl: e.tensor_scalar(out=Iw[0:nq, kb0:kb0 + kw], in0=rl[0:nq, 0:kw],
                                                                    scalar1=wiq[0:nq, 0:1], scalar2=None, op0=ALU.mult),
                         [rl, wiq], [Iw])
                else:
                    P.op("vector", lambda e, rl=rl, h=h: e.scalar_tensor_tensor(
                        out=Iw[0:nq, kb0:kb0 + kw], in0=rl[0:nq, 0:kw], scalar=wiq[0:nq, h:h + 1], in1=Iw[0:nq, kb0:kb0 + kw],
                        op0=ALU.mult, op1=ALU.add), [rl, wiq, Iw], [Iw])
        nv = 0
        if prompt:
            nv = min(4, nchk)
            v0 = nkeys - nv * 128
            P.dma("sync", qrel[0:nq, :], qrel_in[tok0:tok0 + nq, :], [qrel_in], [qrel])
            P.op("vector", lambda e: e.tensor_scalar(out=pen[0:nq, 0:nv * 128], in0=kcl[0:nq, 0:nv * 128], scalar1=qrel[0:nq, 0:1],
                                                     scalar2=NEG, op0=ALU.is_gt, op1=ALU.mult), [kcl, qrel], [pen])
            P.op("vector", lambda e: e.tensor_tensor(out=Iw[0:nq, v0:nkeys], in0=Iw[0:nq, v0:nkeys], in1=pen[0:nq, 0:nv * 128],
                                                     op=ALU.add), [Iw, pen], [Iw])
            P.op("vector", lambda e: e.tensor_scalar(out=visb[0:nq, 0:nv * 128], in0=kcl[0:nq, 0:nv * 128], scalar1=qrel[0:nq, 0:1],
                                                     scalar2=None, op0=ALU.is_le), [kcl, qrel], [visb])
        if nkeys > 256:
            cur = Iw
            for r in range(32):
                P.op("vector", lambda e, cur=cur: e.max(out=m8[0:nq, :], in_=cur[0:nq, 0:nkeys]), [cur], [m8])
                if r < 31:
                    P.op("vector", lambda e, cur=cur: e.match_replace(out=wk[0:nq, 0:nkeys], in_to_replace=m8[0:nq, :],
                                                                      in_values=cur[0:nq, 0:nkeys], imm_value=-3.0e38),
                         [cur, m8], [wk])
                    cur = wk
            P.op("vector", lambda e: e.tensor_scalar(out=thr[0:nq, :], in0=m8[0:nq, 7:8], scalar1=-1.0e29, scalar2=None,
                                                     op0=ALU.max), [m8], [thr])
        else:
            P.op("vector", lambda e: e.memset(thr[:, :], -1.0e29), [], [thr])
        P.op("vector", lambda e: e.tensor_scalar(out=maskb[0:nq, 0:nkeys], in0=Iw[0:nq, 0:nkeys], scalar1=thr[0:nq, 0:1],
                                                 scalar2=None, op0=ALU.is_ge), [Iw, thr], [maskb])
        transposes_to(maskb, lambda k0, kl: maskb[0:nq, k0:k0 + kl], maskT,
                      lambda c0, n_, kl: maskT[0:kl, c0:c0 + n_, 0:nq], chunks, nq)
        if prompt:
            transposes_to(visb, lambda k0, kl: visb[0:nq, k0:k0 + kl], visT,
                          lambda c0, n_, kl: visT[0:kl, c0:c0 + n_, 0:nq], [(c * 128, 128) for c in range(nv)], nq)
        for g in range(4):
            kT = kTs.next()
            vt = vts[vti[0] % 2]
            vti[0] += 1
            qg = qgs.next()
            P.dma("sync", kT[:, 0:nkeys], S["kT"](g), [S["kTb"]], [kT])
            if nfull:
                P.dma("sync", vt[:, 0:nfull, 0:128], S["v"](0, nfull * 128, g).rearrange("(c p) d -> p c d", p=128), [S["vb"]], [vt])
            if nkeys > nfull * 128:
                P.dma("sync", vt[0:nkeys - nfull * 128, nfull, 0:128], S["v"](nfull * 128, nkeys, g), [S["vb"]], [vt])
            P.dma("sync", qg[:, :, 0:nq], qT_s[4 * g:4 * g + 4, :, tok0:tok0 + nq].rearrange("h d t -> d h t"), [qT_s], [qg])

            def s_mm(c):
                k0, kl = chunks[c]
                pb = psI.next()
                P.op("tensor", lambda e: e.matmul(out=pb[0:kl, :].rearrange("p (r t) -> p r t", t=128)[:, :, 0:nq],
                                                  lhsT=kT[:, k0:k0 + kl], rhs=qg[:, :, 0:nq], start=True, stop=True),
                     [kT, qg], [pb])
                return pb
            pbn = s_mm(0)
            for c, (k0, kl) in enumerate(chunks):
                pb = pbn
                if c + 1 < nchk:
                    pbn = s_mm(c + 1)
                Pt = Pts.next()
                Pm = Pms.next()
                P.op("scalar", lambda e, pb=pb, Pt=Pt, kl=kl: e.activation(
                    out=Pt[0:kl, :, 0:nq], in_=pb[0:kl, :].rearrange("p (r t) -> p r t", t=128)[:, :, 0:nq], func=AF.Exp,
                    scale=float(128 ** -0.5)), [pb], [Pt])
                P.op("gpsimd", lambda e, Pt=Pt, Pm=Pm, kl=kl, c=c: e.tensor_tensor(
                    out=Pm[0:kl, :, 0:nq], in0=Pt[0:kl, :, 0:nq],
                    in1=maskT[0:kl, c, 0:nq].unsqueeze(1).to_broadcast([kl, 4, nq]), op=ALU.mult), [Pt, maskT], [Pm])
                for r in range(4):
                    P.op("tensor", lambda e, r=r, Pm=Pm, kl=kl, c=c: e.matmul(
                        out=psO[r][0:nq, 0:129], lhsT=Pm[0:kl, r, 0:nq], rhs=vt[0:kl, c, 0:129],
                        start=(c == 0), stop=(c == nchk - 1)), [Pm, vt], [psO[r]])
            for r in range(4):
                P.op("vector", lambda e, r=r: e.reciprocal(out=rden[0:nq, r:r + 1], in_=psO[r][0:nq, 128:129]), [psO[r]], [rden])
                col = (4 * g + r) * 128
                P.op("scalar", lambda e, r=r, col=col: e.activation(out=attn[0:nq, col:col + 128], in_=psO[r][0:nq, 0:128],
                                                                    func=AF.Copy, scale=rden[0:nq, r:r + 1]),
                     [psO[r], rden], [attn])
        for hd in range(8):
            dkT = dkTs.next()
            dvt = dvts[dvi[0] % 2]
            dvi[0] += 1
            dq2 = dqs.next()
            P.dma("sync", dkT[:, :, 0:nkeys], S["dkT"](hd), [S["dkTb"]], [dkT])
            if nfull:
                P.dma("sync", dvt[:, 0:nfull, 0:256], S["dv"](0, nfull * 128, hd).rearrange("(c p) d -> p c d", p=128),
                      [S["dvb"]], [dvt])
            if nkeys > nfull * 128:
                P.dma("sync", dvt[0:nkeys - nfull * 128, nfull, 0:256], S["dv"](nfull * 128, nkeys, hd), [S["dvb"]], [dvt])
            P.dma("sync", dq2[:, :, 0:nq], dqT_s[2 * hd:2 * hd + 2, :, tok0:tok0 + nq].rearrange("m d t -> d m t"), [dqT_s], [dq2])

            def d_mm(c):
                k0, kl = chunks[c]
                pb = psI.next()
                for m in range(2):
                    P.op("tensor", lambda e, m=m: e.matmul(out=pb[0:kl, m * 128:m * 128 + nq], lhsT=dkT[:, m, k0:k0 + kl],
                                                           rhs=dq2[:, m, 0:nq], start=True, stop=True), [dkT, dq2], [pb])
                return pb
            pbn = d_mm(0)
            for c, (k0, kl) in enumerate(chunks):
                pb = pbn
                if c + 1 < nchk:
                    pbn = d_mm(c + 1)
                Pt = Pts.next()
                P.op("scalar", lambda e, pb=pb, Pt=Pt, kl=kl: e.activation(
                    out=Pt[0:kl, 0:2, 0:nq], in_=pb[0:kl, 0:256].rearrange("p (r t) -> p r t", t=128)[:, :, 0:nq], func=AF.Exp,
                    scale=float(128 ** -0.5)), [pb], [Pt])
                if prompt and c >= nchk - nv:
                    P.op("gpsimd", lambda e, Pt=Pt, kl=kl, c=c: e.tensor_tensor(
                        out=Pt[0:kl, 0:2, 0:nq], in0=Pt[0:kl, 0:2, 0:nq],
                        in1=visT[0:kl, c - (nchk - nv), 0:nq].unsqueeze(1).to_broadcast([kl, 2, nq]), op=ALU.mult),
                        [Pt, visT], [Pt])
                for m in range(2):
                    P.op("tensor", lambda e, m=m, Pt=Pt, kl=kl, c=c: e.matmul(
                        out=psO[m][0:nq, 0:257], lhsT=Pt[0:kl, m, 0:nq], rhs=dvt[0:kl, c, 0:257],
                        start=(c == 0), stop=(c == nchk - 1)), [Pt, dvt], [psO[m]])
            P.op("vector", lambda e: e.reciprocal(out=rden[0:nq, 4:5], in_=psO[0][0:nq, 256:257]), [psO[0]], [rden])
            P.op("vector", lambda e: e.reciprocal(out=rden[0:nq, 5:6], in_=psO[1][0:nq, 256:257]), [psO[1]], [rden])
            P.op("vector", lambda e: e.tensor_tensor(out=rden[0:nq, 5:6], in0=rden[0:nq, 5:6], in1=neglam[0:nq, 0:1], op=ALU.mult),
                 [rden, neglam], [rden])
            P.op("scalar", lambda e: e.activation(out=tmpd[0:nq, :], in_=psO[0][0:nq, 0:256], func=AF.Copy, scale=rden[0:nq, 4:5]),
                 [psO[0], rden], [tmpd])
            P.op("vector", lambda e: e.scalar_tensor_tensor(out=dof[0:nq, :], in0=psO[1][0:nq, 0:256], scalar=rden[0:nq, 5:6],
                                                            in1=tmpd[0:nq, :], op0=ALU.mult, op1=ALU.add),
                 [psO[1], rden, tmpd], [dof])
            P.op("vector", lambda e: e.memset(ssd[:, :], 0.0), [], [ssd])
            P.op("scalar", lambda e: e.activation(out=junkd[0:nq, :], in_=dof[0:nq, :], func=AF.Square, accum_out=ssd[0:nq, 0:1]),
                 [dof], [junkd, ssd])
            P.op("vector", lambda e: e.tensor_scalar(out=ssd[0:nq, 1:2], in0=ssd[0:nq, 0:1], scalar1=1.0 / 256, scalar2=EPS,
                                                     op0=ALU.mult, op1=ALU.add), [ssd], [ssd])
            P.op("scalar", lambda e: e.activation(out=ssd[0:nq, 1:2], in_=ssd[0:nq, 1:2], func=AF.Sqrt), [ssd], [ssd])
            P.op("vector", lambda e: e.reciprocal(out=ssd[0:nq, 1:2], in_=ssd[0:nq, 1:2]), [ssd], [ssd])
            col = 2048 + hd * 256
            P.op("vector", lambda e, col=col: e.scalar_tensor_tensor(out=attn[0:nq, col:col + 256], in0=dof[0:nq, :],
                                                                     scalar=ssd[0:nq, 1:2], in1=gsubB[0:nq, :],
                                                                     op0=ALU.mult, op1=ALU.mult), [dof, ssd, gsubB], [attn])
        for c8 in range(4):
            pb = psT.next()
            tb = tbc.next()
            for j in range(8):
                ch = c8 * 8 + j
                P.op("tensor", lambda e, j=j, ch=ch: e.transpose(out=pb[:, j * 128:j * 128 + nq], in_=attn[0:nq, ch * 128:(ch + 1) * 128],
                                                                 identity=identb[0:nq, 0:nq]), [attn, identb], [pb])
            P.op("scalar", lambda e: e.copy(out=tb[:, :, 0:nq], in_=pb[:, :].rearrange("p (c t) -> p c t", t=128)[:, :, 0:nq]),
                 [pb], [tb])
            P.dma("gpsimd", attnT_s[:, c8 * 8:(c8 + 1) * 8, tok0:tok0 + nq], tb[:, :, 0:nq], [tb], [attnT_s])

    P.dma("sync", kiT[:, :], kiT_s[:, :], [kiT_s], [kiT])
    Sp = {
        "kT": lambda g: None, "kTb": kT_s, "vb": v_s, "dkTb": dkT_s, "dvb": dv_s,
    }
    NA = int(os.environ.get("MK_NAT", str(NQT)))
    for i in range(NA):
        nkeys = nchunks_for(i) * 128
        S = {
            "kT": (lambda g, nkeys=nkeys: kT_s[g, :, 0:nkeys]), "kTb": kT_s,
            "v": (lambda a, b_, g: v_s[a:b_, g * 128:(g + 1) * 128]), "vb": v_s,
            "dkT": (lambda hd, nkeys=nkeys: dkT_s[2 * hd:2 * hd + 2, :, 0:nkeys].rearrange("m d t -> d m t")), "dkTb": dkT_s,
            "dv": (lambda a, b_, hd: dv_s[a:b_, hd * 256:(hd + 1) * 256]), "dvb": dv_s,
        }
        attn_tile(i * 128, 128, nkeys, True, S)
    for s in range(2):
        P.dma("sync", kiT[:, 0:LS], skiT_s[s, :, :], [skiT_s], [kiT])
        S = {
            "kT": (lambda g, s=s: skT_s[s, g, :, :]), "kTb": skT_s,
            "v": (lambda a, b_, g, s=s: sv_s[s, a:b_, g * 128:(g + 1) * 128]), "vb": sv_s,
            "dkT": (lambda hd, s=s: sdkT_s[s, 2 * hd:2 * hd + 2, :, :].rearrange("m d t -> d m t")), "dkTb": sdkT_s,
            "dv": (lambda a, b_, hd, s=s: sdv_s[s, a:b_, hd * 256:(hd + 1) * 256]), "dvb": sdv_s,
        }
        attn_tile(NQT * 128 + 32 * s, 32, LS, False, S)
    P.barrier()
    C.release(mk)

    mk = C.mark()
    psF = [C.ps([128, 512], F32, "psF") for _ in range(4)]
    ps_proj = RR(psF[0:3])
    hTs = [C.sb([128, 32, 128], BF16, "hT") for _ in range(9)]
    wsts = RR([C.sb([128, 4, 512], F32, "wst") for _ in range(2)])
    wbfs = RR([C.sb([128, 32, 512], BF16, "wbf") for _ in range(2)])
    gaP = C.sb([128, D], F32, "gaP")
    gaS = C.sb([128, D], F32, "gaS")
    xbs = RR([C.sb([128, 512], F32, "xb") for _ in range(3)])
    tms = RR([C.sb([128, 512], F32, "tm") for _ in range(3)])
    P.dma("sync", gaP[:, :], ga_s[0, :, :], [ga_s], [gaP])
    P.dma("sync", gaS[:, :], ga_s[2, :, :], [ga_s], [gaS])
    for grp in range(2):
        slots = [(si, i, 128, True) for si, i in enumerate(range(grp * 8, grp * 8 + 8))]
        if grp == 1:
            slots.append((8, NQT, 64, False))
        for (si, i, n, prompt) in slots:
            P.dma("sync", hTs[si][:, :, 0:n], attnT_s[:, :, i * 128:i * 128 + n], [attnT_s], [hTs[si]])
        for cb in range(8):
            wbf = load_weights(w_out, cb * 512, 512)
            for (si, i, n, prompt) in slots:
                pb = project(hTs[si], n, wbf, 512)
                xb = xbs.next()
                tm = tms.next()
                ga = gaP if prompt else gaS
                P.dma("sync", xb[0:n, :], xq[i * 128:i * 128 + n, cb * 512:(cb + 1) * 512], [xq], [xb])
                P.op("vector", lambda e, pb=pb, tm=tm, ga=ga, n=n, cb=cb: e.tensor_tensor(
                    out=tm[0:n, :], in0=pb[0:n, :], in1=ga[0:n, cb * 512:(cb + 1) * 512], op=ALU.mult), [pb, ga], [tm])
                P.op("gpsimd", lambda e, tm=tm, xb=xb, n=n: e.tensor_tensor(out=tm[0:n, :], in0=tm[0:n, :], in1=xb[0:n, :], op=ALU.add),
                     [tm, xb], [tm])
                P.dma("gpsimd", x1_s[i * 128:i * 128 + n, cb * 512:(cb + 1) * 512], tm[0:n, :], [tm], [x1_s])
    P.barrier()
    C.release(mk)
    mk = C.mark()
    psF = [C.ps([128, 512], F32, "psF") for _ in range(6)]
    psB = [C.ps([128, 1024], BF16, "psB") for _ in range(2)]
    ps_proj = RR(psF[0:2])
    ps_xT = RR(psF[2:4])
    ps_s = RR(psF[4:6])
    xts = RR([C.sb([128, D], F32, "xt") for _ in range(1)])
    junk = C.sb([128, D], BF16, "junk")
    sst = RR([C.sb([128, 2], F32, "ss") for _ in range(2)])
    hTs = [C.sb([128, 32, 128], BF16, "hT") for _ in range(9)]
    wsts = RR([C.sb([128, 4, 512], F32, "wst") for _ in range(2)])
    wbfs = RR([C.sb([128, 32, 512], BF16, "wbf") for _ in range(2)])
    kraw = C.sb([128, 16, 128], F32, "kraw")
    keysT = C.sb([128, 16, 128], F32, "keysT")
    qsbs = RR([C.sb([128, 512], F32, "qsb") for _ in range(2)])
    qT4s = RR([C.sb([128, 4, 128], F32, "qT4") for _ in range(2)])
    sos = RR([C.sb([128, 512], F32, "so") for _ in range(2)])
    P.dma("sync", kraw[:, :, :], pkeys_in[:, :, :, :].rearrange("h c n d -> n (h c) d"), [pkeys_in], [kraw])
    for b4 in range(4):
        pb = ps_xT.next()
        for j in range(4):
            P.op("tensor", lambda e, j=j, b4=b4: e.transpose(out=pb[:, j * 128:(j + 1) * 128], in_=kraw[:, b4 * 4 + j, :],
                                                             identity=ident[:, :]), [kraw, ident], [pb])
        P.op("vector", lambda e, b4=b4: e.tensor_copy(out=keysT[:, b4 * 4:(b4 + 1) * 4, :],
                                                      in_=pb[:, :].rearrange("p (c t) -> p c t", t=128)), [pb], [keysT])
    for grp in range(2):
        slots = [(si, i, 128, True) for si, i in enumerate(range(grp * 8, grp * 8 + 8))]
        if grp == 1:
            slots.append((8, NQT, 64, False))
        for (si, i, n, prompt) in slots:
            load_norm_T(x1_s, i * 128, n, hTs[si], prompt, A2, 96)
            P.dma("gpsimd", h2T_s[:, :, i * 128:i * 128 + n], hTs[si][:, :, 0:n], [hTs[si]], [h2T_s])
        for cb in range(4):
            wbf = load_weights(pwq_in, cb * 512, 512)
            for (si, i, n, prompt) in slots:
                pb = project(hTs[si], n, wbf, 512)
                qsb = qsbs.next()
                P.op("scalar", lambda e, pb=pb, qsb=qsb, n=n: e.copy(out=qsb[0:n, :], in_=pb[0:n, :]), [pb], [qsb])
                pt = ps_xT.next()
                for j in range(4):
                    P.op("tensor", lambda e, j=j, n=n, qsb=qsb, pt=pt: e.transpose(
                        out=pt[:, j * 128:j * 128 + n], in_=qsb[0:n, j * 128:(j + 1) * 128], identity=ident[0:n, 0:n]),
                        [qsb, ident], [pt])
                qT4 = qT4s.next()
                P.op("vector", lambda e, pt=pt, qT4=qT4, n=n: e.tensor_copy(
                    out=qT4[:, :, 0:n], in_=pt[:, :].rearrange("p (c t) -> p c t", t=128)[:, :, 0:n]), [pt], [qT4])
                po = ps_s.next()
                for j in range(4):
                    P.op("tensor", lambda e, j=j, n=n, qT4=qT4, po=po, cb=cb: e.matmul(
                        out=po[0:n, j * 128:(j + 1) * 128], lhsT=qT4[:, j, 0:n], rhs=keysT[:, cb * 4 + j, :],
                        start=True, stop=True), [qT4, keysT], [po])
                so = sos.next()
                P.op("scalar", lambda e, po=po, so=so, n=n: e.copy(out=so[0:n, :], in_=po[0:n, :]), [po], [so])
                P.dma("gpsimd", s_s[i * 128:i * 128 + n, cb * 512:(cb + 1) * 512], so[0:n, :], [so], [s_s])
    P.barrier()
    C.release(mk)

    mk = C.mark()
    psA = RR([C.ps([128, 1024], BF16, "psA") for _ in range(3)])
    psG = RR([C.ps([128, 512], F32, "psG") for _ in range(3)])
    sal = C.sb([128, 16, 128], F32, "sal")
    wk2 = C.sb([128, 16, 128], F32, "wk2")
    top = C.sb([128, 16, 16], F32, "top")
    idx = C.sb([128, 8, 16], U32, "idx")
    idxf = C.sb([128, 8, 16], F32, "idxf")
    cand = C.sb([128, 8, 256], F32, "cand")
    cw = C.sb([128, 8, 256], F32, "cw")
    best = C.sb([128, 8, 16], F32, "best")
    eb = C.sb([128, 8, 16], F32, "eb")
    Zs = C.sb([128, 8], F32, "Zs")
    e1z = C.sb([128, 8, 16], F32, "e1z")
    cth = C.sb([128, 8, 16], F32, "cth")
    e2 = C.sb([128, 8, 128], F32, "e2")
    iot = C.sb([128, 128], F32, "iot")
    AB = C.sb([128, 128, 128], BF16, "AB")
    ABm = C.sb([128, 128, 128], BF16, "ABm")
    AT = C.sb([128, 128, 128], BF16, "AT")
    BT = C.sb([128, 128, 128], BF16, "BT")
    Gall = C.sb([128, 128, 128], BF16, "Gall")
    P.dma("sync", iot[:, :], iota_in[:, :], [iota_in], [iot])
    NE2 = int(os.environ.get("MK_NE2", "17"))
    for ti in range(NE2):
        n = 128 if ti < NQT else 64
        tok0 = ti * 128
        P.dma("sync", sal[0:n, :, :], s_s[tok0:tok0 + n, :].rearrange("t (k m) -> t k m", m=128), [s_s], [sal])
        for hc in range(16):
            h, c = divmod(hc, 2)
            P.op("vector", lambda e, hc=hc: e.max(out=top[0:n, hc, 0:8], in_=sal[0:n, hc, :]), [sal], [top])
            if c == 0:
                P.op("vector", lambda e, hc=hc, h=h: e.max_index(out=idx[0:n, h, 0:8], in_max=top[0:n, hc, 0:8],
                                                                 in_values=sal[0:n, hc, :]), [sal, top], [idx])
            P.op("vector", lambda e, hc=hc: e.match_replace(out=wk2[0:n, hc, :], in_to_replace=top[0:n, hc, 0:8],
                                                            in_values=sal[0:n, hc, :], imm_value=-3.0e38), [sal, top], [wk2])
            P.op("vector", lambda e, hc=hc: e.max(out=top[0:n, hc, 8:16], in_=wk2[0:n, hc, :]), [wk2], [top])
            if c == 0:
                P.op("vector", lambda e, hc=hc, h=h: e.max_index(out=idx[0:n, h, 8:16], in_max=top[0:n, hc, 8:16],
                                                                 in_values=wk2[0:n, hc, :]), [wk2, top], [idx])
        top4 = top[0:n, :, :].rearrange("p (h c) k -> p h c k", c=2)
        sal4 = sal[0:n, :, :].rearrange("p (h c) k -> p h c k", c=2)
        P.op("vector", lambda e: e.tensor_copy(out=idxf[0:n, :, :], in_=idx[0:n, :, :]), [idx], [idxf])
        P.op("vector", lambda e: e.tensor_tensor(
            out=cand[0:n, :, :].rearrange("p h (i j) -> p h i j", j=16),
            in0=top4[:, :, 0, :].unsqueeze(3).to_broadcast([n, 8, 16, 16]),
            in1=top4[:, :, 1, :].unsqueeze(2).to_broadcast([n, 8, 16, 16]), op=ALU.add), [top], [cand])
        for h in range(8):
            P.op("vector", lambda e, h=h: e.max(out=best[0:n, h, 0:8], in_=cand[0:n, h, :]), [cand], [best])
            P.op("vector", lambda e, h=h: e.match_replace(out=cw[0:n, h, :], in_to_replace=best[0:n, h, 0:8],
                                                          in_values=cand[0:n, h, :], imm_value=-3.0e38), [cand, best], [cw])
            P.op("vector", lambda e, h=h: e.max(out=best[0:n, h, 8:16], in_=cw[0:n, h, :]), [cw], [best])
        P.op("vector", lambda e: e.tensor_tensor(out=eb[0:n, :, :], in0=best[0:n, :, :],
                                                 in1=best[0:n, :, 0:1].to_broadcast([n, 8, 16]), op=ALU.subtract), [best], [eb])
        P.op("scalar", lambda e: e.activation(out=eb[0:n, :, :], in_=eb[0:n, :, :], func=AF.Exp), [eb], [eb])
        P.op("vector", lambda e: e.reduce_sum(out=Zs[0:n, :], in_=eb[0:n, :, :], axis=AX.X), [eb], [Zs])
        P.op("vector", lambda e: e.reciprocal(out=Zs[0:n, :], in_=Zs[0:n, :]), [Zs], [Zs])
        P.op("vector", lambda e: e.tensor_tensor(out=e1z[0:n, :, :], in0=top4[:, :, 0, :],
                                                 in1=top4[:, :, 0, 0:1].to_broadcast([n, 8, 16]), op=ALU.subtract), [top], [e1z])
        P.op("scalar", lambda e: e.activation(out=e1z[0:n, :, :], in_=e1z[0:n, :, :], func=AF.Exp), [e1z], [e1z])
        P.op("vector", lambda e: e.tensor_tensor(out=e1z[0:n, :, :], in0=e1z[0:n, :, :],
                                                 in1=Zs[0:n, :].unsqueeze(2).to_broadcast([n, 8, 16]), op=ALU.mult), [e1z, Zs], [e1z])
        P.op("vector", lambda e: e.tensor_tensor(out=cth[0:n, :, :], in0=best[0:n, :, 15:16].to_broadcast([n, 8, 16]),
                                                 in1=top4[:, :, 0, :], op=ALU.subtract), [best, top], [cth])
        P.op("vector", lambda e: e.tensor_scalar(out=cth[0:n, :, :], in0=cth[0:n, :, :], scalar1=-4.0e-6, scalar2=None,
                                                 op0=ALU.add), [cth], [cth])
        P.op("vector", lambda e: e.tensor_tensor(out=e2[0:n, :, :], in0=sal4[:, :, 1, :],
                                                 in1=top4[:, :, 1, 0:1].to_broadcast([n, 8, 128]), op=ALU.subtract), [sal, top], [e2])
        P.op("scalar", lambda e: e.activation(out=e2[0:n, :, :], in_=e2[0:n, :, :], func=AF.Exp), [e2], [e2])
        for which in range(2):
            XT = AT if which == 0 else BT
            if which == 0:
                P.op("vector", lambda e: e.tensor_tensor(
                    out=ABm[0:n, :, :], in0=iot[0:n, :].unsqueeze(1).to_broadcast([n, 128, 128]),
                    in1=idxf[0:n, :, :].rearrange("p h i -> p (h i)").unsqueeze(2).to_broadcast([n, 128, 128]),
                    op=ALU.is_equal), [iot, idxf], [ABm])
                P.op("gpsimd", lambda e: e.tensor_tensor(
                    out=AB[0:n, :, :], in0=ABm[0:n, :, :],
                    in1=e1z[0:n, :, :].rearrange("p h i -> p (h i)").unsqueeze(2).to_broadcast([n, 128, 128]),
                    op=ALU.mult), [ABm, e1z], [AB])
            else:
                P.op("vector", lambda e: e.tensor_tensor(
                    out=ABm[0:n, :, :].rearrange("p (h i) k -> p h i k", i=16),
                    in0=sal4[:, :, 1, :].unsqueeze(2).to_broadcast([n, 8, 16, 128]),
                    in1=cth[0:n, :, :].unsqueeze(3).to_broadcast([n, 8, 16, 128]), op=ALU.is_ge), [sal, cth], [ABm])
                P.op("gpsimd", lambda e: e.tensor_tensor(
                    out=AB[0:n, :, :].rearrange("p (h i) k -> p h i k", i=16),
                    in0=ABm[0:n, :, :].rearrange("p (h i) k -> p h i k", i=16),
                    in1=e2[0:n, :, :].unsqueeze(2).to_broadcast([n, 8, 16, 128]), op=ALU.mult), [ABm, e2], [AB])
            for b8 in range(16):
                pb = psA.next()
                for j in range(8):
                    col = b8 * 8 + j
                    P.op("tensor", lambda e, j=j, col=col, pb=pb: e.transpose(
                        out=pb[:, j * 128:j * 128 + n], in_=AB[0:n, :, col], identity=identb[0:n, 0:n]), [AB, identb], [pb])
                en = "scalar" if b8 % 2 == 0 else "vector"
                src_v = pb[:, :].rearrange("p (c t) -> p c t", t=128)[:, :, 0:n]
                if en == "scalar":
                    P.op("scalar", lambda e, b8=b8, src_v=src_v, XT=XT: e.copy(out=XT[:, b8 * 8:(b8 + 1) * 8, 0:n], in_=src_v), [pb], [XT])
                else:
                    P.op("vector", lambda e, b8=b8, src_v=src_v, XT=XT: e.tensor_copy(out=XT[:, b8 * 8:(b8 + 1) * 8, 0:n], in_=src_v),
                         [pb], [XT])
        for t4 in range(0, n, 4):
            pg = psG.next()
            for j in range(4):
                P.op("tensor", lambda e, j=j, t4=t4, pg=pg: e.matmul(out=pg[:, j * 128:(j + 1) * 128], lhsT=AT[:, :, t4 + j],
                                                                     rhs=BT[:, :, t4 + j], start=True, stop=True), [AT, BT], [pg])
            en = "scalar" if (t4 // 4) % 2 == 0 else "vector"
            src_v = pg[:, :].rearrange("p (t i) -> p i t", i=128)
            if en == "scalar":
                P.op("scalar", lambda e, t4=t4, src_v=src_v: e.copy(out=Gall[:, :, t4:t4 + 4], in_=src_v), [pg], [Gall])
            else:
                P.op("vector", lambda e, t4=t4, src_v=src_v: e.tensor_copy(out=Gall[:, :, t4:t4 + 4], in_=src_v), [pg], [Gall])
        P.dma("gpsimd", G_s[ti, :, :, :], Gall[:, :, :], [Gall], [G_s])
    P.barrier()
    C.release(mk)
    mk = C.mark()
    psAct = RR([C.ps([128, 512], F32, "psAct") for _ in range(2)])
    psUT = RR([C.ps([128, 1024], BF16, "psUT") for _ in range(2)])
    psO2 = RR([C.ps([128, 512], F32, "psO2") for _ in range(3)])
    h2T = C.sb([128, 32, 512], BF16, "h2T")
    oacc = [C.sb([128, D], F32, "oacc") for _ in range(4)]
    stg = RR([C.sb([128, D], F32, "stg") for _ in range(2)])
    ubf = C.sb([128, D], BF16, "ubf")
    uT = C.sb([128, 32, 128], BF16, "uT")
    vbfs = [C.sb([128, D], BF16, "vbf") for _ in range(4)]
    WTs = [C.sb([128, 512], BF16, "WT") for _ in range(4)]
    Gcs = RR([C.sb([128, 4, 128], BF16, "Gc") for _ in range(2)])
    gts = RR([C.sb([128, 512], BF16, "gt") for _ in range(2)])
    xbs = RR([C.sb([128, 512], F32, "xb") for _ in range(2)])
    gbs = RR([C.sb([128, 512], F32, "gb") for _ in range(2)])
    fbs = RR([C.sb([128, 512], F32, "fb") for _ in range(2)])
    ybs = RR([C.sb([128, 512], F32, "yb") for _ in range(2)])
    jk = C.sb([128, 512], BF16, "jk")
    ss8 = C.sb([128, 10], F32, "ss8")
    u3 = pu_in[:, :].rearrange("(a b) d -> a b d", b=128)
    v3 = pv_in[:, :].rearrange("(a b) d -> a b d", b=128)
    BLOCKS = [[0, 1, 2, 3], [4, 5, 6, 7], [8, 9, 10, 11], [12, 13, 14], [15, 16]]
    NBLK = int(os.environ.get("MK_NBLK", "5"))
    NCH = int(os.environ.get("MK_NCH", "128"))
    for blk in BLOCKS[:NBLK]:
        tiles = [(ti, ti * 128, 128 if ti < NQT else 64) for ti in blk]
        tok0 = tiles[0][1]
        T = sum(n for (_, _, n) in tiles)
        P.dma("sync", h2T[:, :, 0:T], h2T_s[:, :, tok0:tok0 + T], [h2T_s], [h2T])
        for g0 in range(0, NCH, 4):
            for k_ in range(4):
                c = g0 + k_
                st = stg.next()
                P.dma("sync", st[:, :], u3[:, c, :], [pu_in], [st])
                P.op("gpsimd", lambda e, st=st: e.tensor_copy(out=ubf[:, :], in_=st[:, :]), [st], [ubf])
                for b8 in range(4):
                    pb = psUT.next()
                    for j in range(8):
                        ch = b8 * 8 + j
                        P.op("tensor", lambda e, j=j, ch=ch, pb=pb: e.transpose(out=pb[:, j * 128:(j + 1) * 128],
                                                                                in_=ubf[:, ch * 128:(ch + 1) * 128],
                                                                                identity=identb[:, :]), [ubf, identb], [pb])
                    if b8 % 2 == 0:
                        P.op("vector", lambda e, b8=b8, pb=pb: e.tensor_copy(
                            out=uT[:, b8 * 8:(b8 + 1) * 8, :], in_=pb[:, :].rearrange("p (c t) -> p c t", t=128)), [pb], [uT])
                    else:
                        P.op("scalar", lambda e, b8=b8, pb=pb: e.copy(
                            out=uT[:, b8 * 8:(b8 + 1) * 8, :], in_=pb[:, :].rearrange("p (c t) -> p c t", t=128)), [pb], [uT])
                Gc = Gcs.next()
                P.dma("sync", Gc[:, 0:len(tiles), :], G_s[blk[0]:blk[0] + len(tiles), :, c, :].rearrange("a p t -> p a t"), [G_s], [Gc])
                pa = psAct.next()
                for ch in range(32):
                    P.op("tensor", lambda e, ch=ch, pa=pa: e.matmul(out=pa[:, 0:T], lhsT=uT[:, ch, :], rhs=h2T[:, ch, 0:T],
                                                                    start=(ch == 0), stop=(ch == 31)), [uT, h2T], [pa])
                gt = gts.next()
                P.op("scalar", lambda e, pa=pa, gt=gt: e.activation(out=gt[:, 0:T], in_=pa[:, 0:T], func=AF.Gelu_apprx_tanh), [pa], [gt])
                WT = WTs[k_]
                P.op("gpsimd", lambda e, gt=gt, WT=WT, Gc=Gc: e.tensor_tensor(
                    out=WT[:, 0:T], in0=gt[:, 0:T], in1=Gc[:, :, :].rearrange("p a t -> p (a t)")[:, 0:T], op=ALU.mult),
                    [gt, Gc], [WT])
                st = stg.next()
                P.dma("sync", st[:, :], v3[:, c, :], [pv_in], [st])
                P.op("scalar", lambda e, st=st, k_=k_: e.copy(out=vbfs[k_][:, :], in_=st[:, :]), [st], [vbfs[k_]])
            off = 0
            for si, (ti, tk0, n) in enumerate(tiles):
                for db in range(8):
                    po = psO2.next()
                    for k_ in range(4):
                        P.op("tensor", lambda e, k_=k_, po=po, off=off, n=n, db=db: e.matmul(
                            out=po[0:n, :], lhsT=WTs[k_][:, off:off + n], rhs=vbfs[k_][:, db * 512:(db + 1) * 512],
                            start=(k_ == 0), stop=(k_ == 3)), [WTs[k_], vbfs[k_]], [po])
                    if g0 == 0:
                        P.op("vector", lambda e, po=po, si=si, n=n, db=db: e.tensor_copy(
                            out=oacc[si][0:n, db * 512:(db + 1) * 512], in_=po[0:n, :]), [po], [oacc[si]])
                    else:
                        P.op("vector", lambda e, po=po, si=si, n=n, db=db: e.tensor_tensor(
                            out=oacc[si][0:n, db * 512:(db + 1) * 512], in0=po[0:n, :], in1=oacc[si][0:n, db * 512:(db + 1) * 512],
                            op=ALU.add), [po, oacc[si]], [oacc[si]])
                off += n
        for si, (ti, tk0, n) in enumerate(tiles):
            prompt = ti < NQT
            P.op("vector", lambda e: e.memset(ss8[:, :], 0.0), [], [ss8])
            for db in range(8):
                xb = xbs.next()
                gb = gbs.next()
                sl = slice(db * 512, (db + 1) * 512)
                P.dma("sync", xb[0:n, :], x1_s[tk0:tk0 + n, sl], [x1_s], [xb])
                P.dma("sync", gb[0:n, :], ga_s[1 if prompt else 3, 0:n, sl], [ga_s], [gb])
                P.op("vector", lambda e, si=si, sl=sl, gb=gb, n=n: e.tensor_tensor(out=oacc[si][0:n, sl], in0=oacc[si][0:n, sl],
                                                                                   in1=gb[0:n, :], op=ALU.mult), [oacc[si], gb], [oacc[si]])
                P.op("gpsimd", lambda e, si=si, sl=sl, xb=xb, n=n: e.tensor_tensor(out=oacc[si][0:n, sl], in0=oacc[si][0:n, sl],
                                                                                   in1=xb[0:n, :], op=ALU.add), [oacc[si], xb], [oacc[si]])
                P.op("scalar", lambda e, si=si, sl=sl, n=n, db=db: e.activation(out=jk[0:n, :], in_=oacc[si][0:n, sl], func=AF.Square,
                                                                                accum_out=ss8[0:n, db:db + 1]), [oacc[si]], [jk, ss8])
            P.op("vector", lambda e, n=n: e.reduce_sum(out=ss8[0:n, 8:9], in_=ss8[0:n, 0:8], axis=AX.X), [ss8], [ss8])
            P.op("vector", lambda e, n=n: e.tensor_scalar(out=ss8[0:n, 9:10], in0=ss8[0:n, 8:9], scalar1=1.0 / D, scalar2=EPS,
                                                          op0=ALU.mult, op1=ALU.add), [ss8], [ss8])
            P.op("scalar", lambda e, n=n: e.activation(out=ss8[0:n, 9:10], in_=ss8[0:n, 9:10], func=AF.Sqrt), [ss8], [ss8])
            P.op("vector", lambda e, n=n: e.reciprocal(out=ss8[0:n, 9:10], in_=ss8[0:n, 9:10]), [ss8], [ss8])
            for db in range(8):
                fb = fbs.next()
                yb = ybs.next()
                sl = slice(db * 512, (db + 1) * 512)
                P.dma("sync", fb[0:n, :], gfinB[0:n, sl], [gfinB], [fb])
                P.op("vector", lambda e, si=si, sl=sl, fb=fb, yb=yb, n=n: e.scalar_tensor_tensor(
                    out=yb[0:n, :], in0=oacc[si][0:n, sl], scalar=ss8[0:n, 9:10], in1=fb[0:n, :], op0=ALU.mult, op1=ALU.mult),
                    [oacc[si], ss8, fb], [yb])
                P.dma("gpsimd", o_y[tk0:tk0 + n, sl], yb[0:n, :], [yb], [o_y])
    P.barrier()
    C.release(mk)
    P.barrier()
    P.close()
    return nc


_ROPE_CACHE = {}


def _rope_tabs(pos):
    half = 64
    inv = (np.float32(10000.0) ** (-np.arange(half, dtype=np.float32) / np.float32(half))).astype(np.float32)
    ang = pos.astype(np.float32)[:, None] * inv[None, :]
    cos, sin = np.cos(ang).astype(np.float32), np.sin(ang).astype(np.float32)
    return (np.ascontiguousarray(np.concatenate([cos, cos], axis=1)),
            np.ascontiguousarray(np.concatenate([-sin, sin], axis=1)))


def _fp(v):
    return np.ascontiguousarray(v.reshape(-1, 128).T)


def kernel(x_prompt, x_sample, cache_dsa_k, cache_dsa_v, cache_idx_k, cache_diff_k, cache_diff_v,
           c_prompt, c_sample, w_ada, b_ada, g_norm_mix, g_norm_ffn, w_in,
           diff_lambda_q1, diff_lambda_k1, diff_lambda_q2, diff_lambda_k2, g_diff_subln, w_out,
           peer_w_query, peer_sub_keys, peer_u, peer_v, g_final):
    f = np.float32
    A = lambda a: np.ascontiguousarray(np.asarray(a, dtype=f))
    x_prompt, x_sample = A(x_prompt), A(x_sample)
    nc = build_program()
    cosk, sink = _rope_tabs(np.arange(SEQ))
    ident = np.eye(128, dtype=f)
    in_maps = []
    own = {}
    shared = {
        "w_ada": A(w_ada[0]), "badaT": _fp(A(b_ada[0])), "gmixT": _fp(A(g_norm_mix[0])), "gffnT": _fp(A(g_norm_ffn[0])),
        "gfinB": np.ascontiguousarray(np.broadcast_to(A(g_final)[None, :], (128, D))),
        "w_in": A(w_in[0]), "cosk": cosk, "sink": sink, "ident": ident,
        "kcl": np.ascontiguousarray(np.broadcast_to((np.arange(512) // 64).astype(f)[None, :], (128, 512))),
        "lamv": np.ascontiguousarray(np.broadcast_to(np.stack([A(diff_lambda_q1[0]), A(diff_lambda_k1[0]),
                                                                A(diff_lambda_q2[0]), A(diff_lambda_k2[0])])[None], (128, 4, 128))),
        "gsub": np.ascontiguousarray(np.broadcast_to(A(g_diff_subln[0])[None, :], (128, 256))),
        "w_out": A(w_out[0]), "pwq": A(peer_w_query[0]), "pkeys": A(peer_sub_keys[0]), "pu": A(peer_u[0]), "pv": A(peer_v[0]),
        "iota": np.ascontiguousarray(np.broadcast_to(np.arange(128, dtype=f)[None, :], (128, 128))),
    }
    for c in range(8):
        b, hh = divmod(c, 2)
        js = [own_tile_index(hh, i) for i in range(NQT)]
        own[c] = js
        xq = np.concatenate([x_prompt[b, j * 128:(j + 1) * 128] for j in js] + [x_sample[2 * c], x_sample[2 * c + 1]], axis=0)
        posq = np.concatenate([np.arange(j * 128, (j + 1) * 128) for j in js] + [np.arange(PAST, LS), np.arange(PAST, LS)])
        cosq, sinq = _rope_tabs(posq)
        qrel = np.zeros((NTOK, 1), f)
        for i, j in enumerate(js):
            nch = nchunks_for(i)
            base64 = 2 * (nch - min(4, nch))
            qrel[i * 128:(i + 1) * 128, 0] = (np.arange(j * 128, (j + 1) * 128) // 64) - base64
        cv = np.stack([A(c_prompt)[b], A(c_sample)[2 * c], A(c_sample)[2 * c + 1]], axis=0)
        cT = np.ascontiguousarray(cv.reshape(3, 32, 128).transpose(2, 1, 0).reshape(128, 96))
        m = dict(shared)
        m.update({
            "xk": x_prompt[b], "xq": np.ascontiguousarray(xq), "cT": cT, "cosq": cosq, "sinq": sinq, "qrel": qrel,
            "c_k": A(cache_dsa_k[0, 2 * c:2 * c + 2]).reshape(2, PAST, 512),
            "c_v": A(cache_dsa_v[0, 2 * c:2 * c + 2]).reshape(2, PAST, 512),
            "c_ki": A(cache_idx_k[0, 2 * c:2 * c + 2]).reshape(2, PAST, 128),
            "c_dk": A(cache_diff_k[0, 2 * c:2 * c + 2]).reshape(2, PAST, 2048),
            "c_dv": A(cache_diff_v[0, 2 * c:2 * c + 2]).reshape(2, PAST, 2048),
        })
        in_maps.append(m)
    names = set(_INPUT_NAMES)
    in_maps = [{k: v for k, v in m.items() if k in names} for m in in_maps]
    ncore = int(os.environ.get("MK_CORES", "8"))
    res = run_bass_kernel_spmd(nc, in_maps[:ncore], core_ids=list(range(ncore)))
    R = list(res.results)
    while len(R) < 8:
        R.append(R[0])
    global _LAST
    _LAST = R
    y_prompt = np.zeros((4, SEQ, D), f)
    y_sample = np.zeros((16, 32, D), f)
    for c in range(8):
        b = c // 2
        oy = R[c]["o_y"]
        for i, j in enumerate(own[c]):
            y_prompt[b, j * 128:(j + 1) * 128] = oy[i * 128:(i + 1) * 128]
        y_sample[2 * c] = oy[NQT * 128:NQT * 128 + 32]
        y_sample[2 * c + 1] = oy[NQT * 128 + 32:NQT * 128 + 64]

    def pk(name, shp):
        return np.stack([R[2 * b][name] for b in range(4)], axis=0).reshape((1, 4, SEQ) + shp)

    def sk(name, shp):
        return np.concatenate([R[c][name].reshape((2, 32) + shp) for c in range(8)], axis=0)[None]

    return (y_prompt, y_sample,
            pk("o_kp", (4, 128)), pk("o_vp", (4, 128)), pk("o_kip", (128,)), pk("o_dkp", (8, 2, 128)), pk("o_dvp", (8, 256)),
            sk("o_ks", (4, 128)), sk("o_vs", (4, 128)), sk("o_kis", (128,)), sk("o_dks", (8, 2, 128)), sk("o_dvs", (8, 256)))
```

```python
import os
import numpy as np
import concourse.bass as bass
import concourse.mybir as mybir
from concourse.bass_utils import run_bass_kernel_spmd

F32 = mybir.dt.float32
BF16 = mybir.dt.bfloat16
U32 = mybir.dt.uint32
ALU = mybir.AluOpType
AF = mybir.ActivationFunctionType
AX = mybir.AxisListType

ENGS = ("sync", "scalar", "vector", "gpsimd", "tensor")
NDS = 6

D = 4096
NQT = 16
NTOK = NQT * 128 + 64
SEQ = 4096
PAST = 1024
LS = PAST + 32
C_Q, C_K, C_V, C_QI, C_KI, C_WI, C_DQ, C_DK, C_DV, C_END = 0, 2048, 2560, 3072, 5120, 5248, 5264, 7312, 9360, 11408
EPS = 1e-6
NEG = -1.0e30
LAM_INIT = 0.8 - 0.6


class Buf:
    __slots__ = ("t", "lw", "rd")

    def __init__(self, t):
        self.t = t
        self.lw = set()
        self.rd = set()

    def __getitem__(self, k):
        return self.t[k]


class Prog:
    def __init__(self, nc):
        self.nc = nc
        self.eng = {"sync": nc.sync, "scalar": nc.scalar, "vector": nc.vector,
                    "gpsimd": nc.gpsimd, "tensor": nc.tensor}
        self.sem = {}
        self.cnt = {e: 0 for e in ENGS}
        self.known = {e: {} for e in ENGS}
        self.dsem = {}
        self.dcnt = {e: 0 for e in ENGS}
        self._stack = []
        self.ninstr = 0
        self.nwaits = 0

    def open(self):
        nc = self.nc
        for e in ENGS:
            cm = nc.semaphore("s_" + e)
            self.sem[e] = cm.__enter__()
            self._stack.append(cm)
        for e in ("sync", "gpsimd", "scalar"):
            self.dsem[e] = []
            for i in range(NDS):
                cm = nc.semaphore("d_%s%d" % (e, i))
                self.dsem[e].append(cm.__enter__())
                self._stack.append(cm)

    def close(self):
        for cm in reversed(self._stack):
            cm.__exit__(None, None, None)

    def _wait(self, engine, ev):
        if ev[0] == "c":
            _, p, v = ev
            if p == "tensor" and engine == "tensor":
                return
            key = ("c", p)
            if self.known[engine].get(key, 0) >= v:
                return
            self.eng[engine].wait_ge(self.sem[p], v)
        else:
            _, q, slot, v = ev
            key = ("d", q, slot)
            if self.known[engine].get(key, 0) >= v:
                return
            self.eng[engine].wait_ge(self.dsem[q][slot], v)
        self.known[engine][key] = v
        self.nwaits += 1

    def _deps(self, engine, reads, writes):
        deps = set()
        for b in reads:
            deps |= b.lw
        for b in writes:
            deps |= b.lw
            deps |= b.rd
        best = {}
        for ev in deps:
            key = ev[:-1]
            if key not in best or best[key][-1] < ev[-1]:
                best[key] = ev
        for key in sorted(best, key=str):
            self._wait(engine, best[key])

    def _commit(self, ev, reads, writes):
        for b in writes:
            b.lw = {ev}
            b.rd = set()
        for b in reads:
            if b not in writes:
                b.rd.add(ev)

    def op(self, engine, fn, reads=(), writes=()):
        self._deps(engine, reads, writes)
        ins = fn(self.eng[engine])
        self.cnt[engine] += 1
        ins.then_inc(self.sem[engine], 1)
        self._commit(("c", engine, self.cnt[engine]), reads, writes)
        self.ninstr += 1

    def dma(self, engine, out, in_, reads=(), writes=(), **kw):
        self._deps(engine, reads, writes)
        n = self.dcnt[engine]
        self.dcnt[engine] += 1
        slot = n % NDS
        ins = self.eng[engine].dma_start(out=out, in_=in_, **kw)
        ins.then_inc(self.dsem[engine][slot], 16)
        self._commit(("d", engine, slot, 16 * (n // NDS + 1)), reads, writes)
        self.ninstr += 1

    def barrier(self):
        evs = []
        for p in ENGS:
            if self.cnt[p] > 0:
                evs.append(("c", p, self.cnt[p]))
        for q in self.dsem:
            n = self.dcnt[q]
            for slot in range(NDS):
                k = (n - slot + NDS - 1) // NDS if n > slot else 0
                if k > 0:
                    evs.append(("d", q, slot, 16 * k))
        for e in ENGS:
            for ev in evs:
                self._wait(e, ev)


class Ctx:
    def __init__(self, nc):
        self.nc = nc
        self.stack = []
        self.k = 0

    def sb(self, shape, dt, name=None):
        self.k += 1
        cm = self.nc.sbuf_tensor("%s_%d" % (name or "t", self.k), list(shape), dt)
        t = cm.__enter__()
        self.stack.append(cm)
        return Buf(t)

    def ps(self, shape, dt, name=None):
        self.k += 1
        cm = self.nc.psum_tensor("%s_%d" % (name or "p", self.k), list(shape), dt)
        t = cm.__enter__()
        self.stack.append(cm)
        return Buf(t)

    def mark(self):
        return len(self.stack)

    def release(self, mark):
        while len(self.stack) > mark:
            self.stack.pop().__exit__(None, None, None)


class RR:
    def __init__(self, items):
        self.items = list(items)
        self.i = 0

    def next(self):
        x = self.items[self.i % len(self.items)]
        self.i += 1
        return x


def own_tile_index(hh, i):
    g, o = divmod(i, 2)
    if hh == 0:
        return 4 * g + (0 if o == 0 else 3)
    return 4 * g + (1 if o == 0 else 2)


def nchunks_for(i):
    g, o = divmod(i, 2)
    return 4 * g + (2 if o == 0 else 4)


STAGE = int(os.environ.get("MK_STAGE", "9"))
_INPUT_NAMES = []
_LAST = None


def build_program():
    del _INPUT_NAMES[:]
    nc = bass.Bass("TRN2", target_bir_lowering=False)
    P = Prog(nc)
    P.open()
    C = Ctx(nc)
    T = {}

    def din(name, shape, dt=F32):
        _INPUT_NAMES.append(name)
        T[name] = Buf(nc.dram_tensor(name, list(shape), dt, kind="ExternalInput").ap())
        return T[name]

    def dout(name, shape, dt=F32):
        T[name] = Buf(nc.dram_tensor(name, list(shape), dt, kind="ExternalOutput").ap())
        return T[name]

    def dscr(name, shape, dt=BF16):
        kind = "ExternalOutput" if os.environ.get("MK_DEBUG") else "Internal"
        T[name] = Buf(nc.dram_tensor(name, list(shape), dt, kind=kind).ap())
        return T[name]

    xk = din("xk", [SEQ, D])
    xq = din("xq", [NTOK, D])
    cT = din("cT", [128, 96])
    w_ada = din("w_ada", [D, 6 * D])
    badaT = din("badaT", [128, 192])
    gmixT = din("gmixT", [128, 32])
    gffnT = din("gffnT", [128, 32])
    gfinB = din("gfinB", [128, D])
    w_in = din("w_in", [D, C_END])
    cosk = din("cosk", [SEQ, 128])
    sink = din("sink", [SEQ, 128])
    cosq = din("cosq", [NTOK, 128])
    sinq = din("sinq", [NTOK, 128])
    ident_in = din("ident", [128, 128])
    c_k = din("c_k", [2, PAST, 512])
    c_v = din("c_v", [2, PAST, 512])
    c_ki = din("c_ki", [2, PAST, 128])
    c_dk = din("c_dk", [2, PAST, 2048])
    c_dv = din("c_dv", [2, PAST, 2048])
    kcl_in = din("kcl", [128, 512])
    qrel_in = din("qrel", [NTOK, 1])
    lamv_in = din("lamv", [128, 4, 128])
    gsub_in = din("gsub", [128, 256])
    w_out = din("w_out", [D, D])
    pwq_in = din("pwq", [D, 2048])
    pkeys_in = din("pkeys", [8, 2, 128, 128])
    iota_in = din("iota", [128, 128])
    pu_in = din("pu", [16384, D])
    pv_in = din("pv", [16384, D])
    o_kp = dout("o_kp", [SEQ, 512])
    o_vp = dout("o_vp", [SEQ, 512])
    o_kip = dout("o_kip", [SEQ, 128])
    o_dkp = dout("o_dkp", [SEQ, 2048])
    o_dvp = dout("o_dvp", [SEQ, 2048])
    o_ks = dout("o_ks", [64, 512])
    o_vs = dout("o_vs", [64, 512])
    o_kis = dout("o_kis", [64, 128])
    o_dks = dout("o_dks", [64, 2048])
    o_dvs = dout("o_dvs", [64, 2048])
    o_y = dout("o_y", [NTOK, D])
    kT_s = dscr("kT_s", [4, 128, SEQ])
    v_s = dscr("v_s", [SEQ, 512])
    kiT_s = dscr("kiT_s", [128, SEQ])
    dkT_s = dscr("dkT_s", [16, 128, SEQ])
    dv_s = dscr("dv_s", [SEQ, 2048])
    skT_s = dscr("skT_s", [2, 4, 128, LS])
    sv_s = dscr("sv_s", [2, LS, 512])
    skiT_s = dscr("skiT_s", [2, 128, LS])
    sdkT_s = dscr("sdkT_s", [2, 16, 128, LS])
    sdv_s = dscr("sdv_s", [2, LS, 2048])
    qT_s = dscr("qT_s", [16, 128, NTOK])
    qiT_s = dscr("qiT_s", [16, 128, NTOK])
    dqT_s = dscr("dqT_s", [16, 128, NTOK])
    wi_s = dscr("wi_s", [NTOK, 16], F32)
    ga_s = dscr("ga_s", [4, 128, D], F32)

    attnT_s = dscr("attnT_s", [128, 32, NTOK])
    x1_s = dscr("x1_s", [NTOK, D], F32)
    h2T_s = dscr("h2T_s", [128, 32, NTOK])
    s_s = dscr("s_s", [NTOK, 2048], F32)
    G_s = dscr("G_s", [17, 128, 128, 128])
    ident = C.sb([128, 128], F32, "ident")
    identb = C.sb([128, 128], BF16, "identb")
    modT = C.sb([128, 192, 3], F32, "modT")
    A1 = C.sb([128, 32, 3], F32, "A1")
    A2 = C.sb([128, 32, 3], F32, "A2")
    P.dma("sync", ident[:, :], ident_in[:, :], [ident_in], [ident])
    P.op("vector", lambda e: e.tensor_copy(out=identb[:, :], in_=ident[:, :]), [ident], [identb])

    mk = C.mark()
    psF = [C.ps([128, 512], F32, "psF") for _ in range(6)]
    psB = [C.ps([128, 1024], BF16, "psB") for _ in range(2)]
    scT = C.sb([128, 96], F32, "scT")
    wst = [C.sb([128, 12288], F32, "wst") for _ in range(2)]
    pm = [psF[0], psF[1]]
    bada = C.sb([128, 192], F32, "bada")
    gm = C.sb([128, 32], F32, "gm")
    gf = C.sb([128, 32], F32, "gf")
    P.dma("sync", scT[:, :], cT[:, :], [cT], [scT])
    P.dma("sync", bada[:, :], badaT[:, :], [badaT], [bada])
    P.dma("sync", gm[:, :], gmixT[:, :], [gmixT], [gm])
    P.dma("sync", gf[:, :], gffnT[:, :], [gffnT], [gf])
    sg = C.sb([128, 96], F32, "sg")
    P.op("scalar", lambda e: e.activation(out=sg[:, :], in_=scT[:, :], func=AF.Exp, scale=-1.0), [scT], [sg])
    P.op("vector", lambda e: e.tensor_scalar(out=sg[:, :], in0=sg[:, :], scalar1=1.0, scalar2=None, op0=ALU.add), [sg], [sg])
    P.op("vector", lambda e: e.reciprocal(out=sg[:, :], in_=sg[:, :]), [sg], [sg])
    P.op("vector", lambda e: e.tensor_tensor(out=scT[:, :], in0=scT[:, :], in1=sg[:, :], op=ALU.mult), [scT, sg], [scT])
    k = 0
    for cblk in range(64):
        buf = wst[k % 2]
        q = "sync" if k % 2 == 0 else "gpsimd"
        k += 1
        P.dma(q, buf[:, 0:32 * 384].rearrange("p (c w) -> p c w", w=384),
              w_ada[:, cblk * 384:(cblk + 1) * 384].rearrange("(c p) w -> p c w", p=128), [w_ada], [buf])
        for c3 in range(3):
            cb = cblk * 3 + c3
            half, cbl = divmod(cb, 96)
            for dch in range(32):
                P.op("tensor", lambda e, buf=buf, c3=c3, cbl=cbl, half=half, dch=dch: e.matmul(
                    out=pm[half][:, cbl * 3:cbl * 3 + 3], lhsT=buf[:, dch * 384 + c3 * 128:dch * 384 + (c3 + 1) * 128],
                    rhs=scT[:, dch * 3:dch * 3 + 3], start=(dch == 0), stop=(dch == 31)),
                    [buf, scT], [pm[half]])
    for half in range(2):
        P.op("vector", lambda e, half=half: e.tensor_tensor(
            out=modT[:, half * 96:(half + 1) * 96, :],
            in0=pm[half][:, 0:288].rearrange("p (c r) -> p c r", r=3),
            in1=bada[:, half * 96:(half + 1) * 96].unsqueeze(2).to_broadcast([128, 96, 3]), op=ALU.add),
            [pm[half], bada], [modT])
    for (Ax, base, g) in ((A1, 32, gm), (A2, 128, gf)):
        P.op("vector", lambda e, Ax=Ax, base=base: e.tensor_scalar(
            out=Ax[:, :, :], in0=modT[:, base:base + 32, :], scalar1=1.0, scalar2=None, op0=ALU.add), [modT], [Ax])
        P.op("vector", lambda e, Ax=Ax, g=g: e.tensor_tensor(
            out=Ax[:, :, :], in0=Ax[:, :, :], in1=g[:, :].unsqueeze(2).to_broadcast([128, 32, 3]), op=ALU.mult),
            [Ax, g], [Ax])
    Dm = [C.sb([128, D], F32, "Dm") for _ in range(2)]
    L = C.sb([128, 3, 128], F32, "L")
    gat = [C.sb([128, 512], F32, "gat") for _ in range(2)]
    P.op("vector", lambda e: e.memset(L[:, 0, :], 1.0), [], [L])
    P.op("vector", lambda e: e.memset(L[:, 1:3, :], 0.0), [L], [L])
    P.op("vector", lambda e: e.memset(L[:, 1, 0:32], 1.0), [L], [L])
    P.op("vector", lambda e: e.memset(L[:, 2, 32:64], 1.0), [L], [L])
    eng2 = RR(["vector", "gpsimd"])
    kk = 0
    for which, base in ((0, 64), (1, 160)):
        for grp in range(2):
            rs = [0] if grp == 0 else [1, 2]
            for ri, r in enumerate(rs):
                for ch in range(32):
                    P.op(eng2.next(), lambda e, ri=ri, r=r, ch=ch, base=base: e.tensor_scalar(
                        out=Dm[ri][:, ch * 128:(ch + 1) * 128], in0=ident[:, :],
                        scalar1=modT[:, base + ch, r:r + 1], scalar2=None, op0=ALU.mult),
                        [ident, modT], [Dm[ri]])
            for blk in range(8):
                pb = psF[2 + kk % 2]
                gt = gat[kk % 2]
                kk += 1
                for ri, r in enumerate(rs):
                    P.op("tensor", lambda e, ri=ri, r=r, blk=blk, pb=pb, n=len(rs): e.matmul(
                        out=pb[:, :], lhsT=L[:, r, :], rhs=Dm[ri][:, blk * 512:(blk + 1) * 512],
                        start=(ri == 0), stop=(ri == n - 1)), [L, Dm[ri]], [pb])
                P.op("scalar", lambda e, pb=pb, gt=gt: e.copy(out=gt[:, :], in_=pb[:, :]), [pb], [gt])
                P.dma("gpsimd", ga_s[grp * 2 + which, :, blk * 512:(blk + 1) * 512], gt[:, :], [gt], [ga_s])
    P.barrier()
    C.release(mk)

    mk = C.mark()
    psF = [C.ps([128, 512], F32, "psF") for _ in range(6)]
    psB = [C.ps([128, 1024], BF16, "psB") for _ in range(2)]
    xts = RR([C.sb([128, D], F32, "xt") for _ in range(2)])
    sst = RR([C.sb([128, 2], F32, "ss") for _ in range(2)])
    hTs = [C.sb([128, 32, 128], BF16, "hT") for _ in range(9)]
    wsts = RR([C.sb([128, 4, 512], F32, "wst") for _ in range(2)])
    wbfs = RR([C.sb([128, 32, 512], BF16, "wbf") for _ in range(2)])
    ofs = RR([C.sb([128, 512], F32, "of") for _ in range(2)])
    tmps = RR([C.sb([128, 512], F32, "tmp") for _ in range(2)])
    obs = RR([C.sb([128, 512], BF16, "ob") for _ in range(3)])
    tbs = RR([C.sb([128, 4, 128], BF16, "tb") for _ in range(3)])
    tabs = RR([(C.sb([128, 128], F32, "cs"), C.sb([128, 128], F32, "sn")) for _ in range(3)])
    ps_proj = RR(psF[0:3])
    ps_xT = RR(psF[3:6])
    ps_hT = RR(psB)
    evac = RR(["vector", "scalar"])
    castq = RR(["gpsimd"])

    def weight_stream(specs):
        nxt = load_weights(*specs[0]) if specs else None
        for i_ in range(len(specs)):
            cur = nxt
            nxt = load_weights(*specs[i_ + 1]) if i_ + 1 < len(specs) else None
            yield cur

    def load_norm_T(src, row0, n, hT, prompt, Ax, shbase):
        xt = xts.next()
        ss = sst.next()
        P.dma("sync", xt[0:n, :], src[row0:row0 + n, :], [src], [xt])
        P.op("vector", lambda e: e.memset(ss[:, :], 0.0), [], [ss])
        P.op("scalar", lambda e: e.activation(out=hT[0:n, :, :].rearrange("p c t -> p (c t)"), in_=xt[0:n, :], func=AF.Square,
                                              accum_out=ss[0:n, 0:1]), [xt], [hT, ss])
        P.op("vector", lambda e: e.tensor_scalar(out=ss[0:n, 1:2], in0=ss[0:n, 0:1], scalar1=1.0 / D, scalar2=EPS,
                                                 op0=ALU.mult, op1=ALU.add), [ss], [ss])
        P.op("scalar", lambda e: e.activation(out=ss[0:n, 1:2], in_=ss[0:n, 1:2], func=AF.Sqrt), [ss], [ss])
        P.op("vector", lambda e: e.reciprocal(out=ss[0:n, 1:2], in_=ss[0:n, 1:2]), [ss], [ss])
        P.op("scalar", lambda e: e.activation(out=xt[0:n, :], in_=xt[0:n, :], func=AF.Copy, scale=ss[0:n, 1:2]), [xt, ss], [xt])
        for c4 in range(8):
            pb = ps_xT.next()
            for j in range(4):
                ch = c4 * 4 + j
                P.op("tensor", lambda e, ch=ch, j=j, pb=pb: e.transpose(
                    out=pb[:, j * 128:j * 128 + n], in_=xt[0:n, ch * 128:(ch + 1) * 128], identity=ident[0:n, 0:n]),
                    [xt, ident], [pb])
            for j in range(4):
                ch = c4 * 4 + j
                segs = [(0, n, 0)] if prompt else [(0, 32, 1), (32, 64, 2)]
                for (a, b_, r) in segs:
                    en = evac.next()
                    if en == "vector":
                        P.op("vector", lambda e, ch=ch, j=j, pb=pb, a=a, b_=b_, r=r: e.tensor_scalar(
                            out=hT[:, ch, a:b_], in0=pb[:, j * 128 + a:j * 128 + b_], scalar1=Ax[:, ch, r:r + 1],
                            scalar2=modT[:, shbase + ch, r:r + 1], op0=ALU.mult, op1=ALU.add),
                            [pb, Ax, modT], [hT])
                    else:
                        P.op("scalar", lambda e, ch=ch, j=j, pb=pb, a=a, b_=b_, r=r: e.activation(
                            out=hT[:, ch, a:b_], in_=pb[:, j * 128 + a:j * 128 + b_], func=AF.Identity,
                            scale=Ax[:, ch, r:r + 1], bias=modT[:, shbase + ch, r:r + 1]),
                            [pb, Ax, modT], [hT])
        return xt

    def load_weights(wsrc, c0, W):
        wbf = wbfs.next()
        for p4 in range(8):
            ws = wsts.next()
            P.dma("sync", ws[:, :, 0:W],
                  wsrc[p4 * 512:(p4 + 1) * 512, c0:c0 + W].rearrange("(c p) w -> p c w", p=128), [wsrc], [ws])
            en = castq.next()
            if en == "scalar":
                P.op("scalar", lambda e, ws=ws, p4=p4: e.copy(out=wbf[:, p4 * 4:(p4 + 1) * 4, 0:W], in_=ws[:, :, 0:W]),
                     [ws], [wbf])
            else:
                P.op("gpsimd", lambda e, ws=ws, p4=p4: e.tensor_copy(out=wbf[:, p4 * 4:(p4 + 1) * 4, 0:W], in_=ws[:, :, 0:W]),
                     [ws], [wbf])
        return wbf

    def project(hT, n, wbf, W):
        pb = ps_proj.next()
        for ch in range(32):
            P.op("tensor", lambda e, ch=ch: e.matmul(out=pb[0:n, 0:W], lhsT=hT[:, ch, 0:n], rhs=wbf[:, ch, 0:W],
                                                     start=(ch == 0), stop=(ch == 31)), [hT, wbf], [pb])
        return pb

    def epilogue(src, sbuf, n, W, rope=None, f32dst=None, tokdst=None, featdst=None):
        H = max(W // 128, 1)
        of = ofs.next()
        if rope is not None:
            cs, sn = rope
            tmp = tmps.next()
            s3 = src.rearrange("p (h d) -> p h d", d=128)
            o3 = of[0:n, 0:W].rearrange("p (h d) -> p h d", d=128)
            t3 = tmp[0:n, 0:W].rearrange("p (h d) -> p h d", d=128)
            P.op("vector", lambda e: e.tensor_tensor(out=o3, in0=s3, in1=cs[0:n, :].unsqueeze(1).to_broadcast([n, H, 128]),
                                                     op=ALU.mult), [sbuf, cs], [of])
            P.op("vector", lambda e: e.tensor_tensor(out=t3[:, :, 0:64], in0=s3[:, :, 64:128],
                                                     in1=sn[0:n, 0:64].unsqueeze(1).to_broadcast([n, H, 64]),
                                                     op=ALU.mult), [sbuf, sn], [tmp])
            P.op("vector", lambda e: e.tensor_tensor(out=t3[:, :, 64:128], in0=s3[:, :, 0:64],
                                                     in1=sn[0:n, 64:128].unsqueeze(1).to_broadcast([n, H, 64]),
                                                     op=ALU.mult), [sbuf, sn], [tmp])
            P.op("vector", lambda e: e.tensor_tensor(out=of[0:n, 0:W], in0=of[0:n, 0:W], in1=tmp[0:n, 0:W], op=ALU.add),
                 [of, tmp], [of])
            cur, curb = of[0:n, 0:W], of
        elif f32dst is not None:
            P.op("scalar", lambda e: e.copy(out=of[0:n, 0:W], in_=src), [sbuf], [of])
            cur, curb = of[0:n, 0:W], of
        else:
            cur, curb = src, sbuf
        if f32dst is not None:
            P.dma("scalar", f32dst[1], cur, [curb], [f32dst[0]])
        if tokdst is None and featdst is None:
            return
        ob = obs.next()
        P.op("scalar", lambda e: e.copy(out=ob[0:n, 0:W], in_=cur), [curb], [ob])
        if tokdst is not None:
            for (db, dap, r0, r1) in tokdst:
                P.dma("scalar", dap, ob[r0:r1, 0:W], [ob], [db])
        if featdst is not None:
            pb = ps_hT.next()
            tb = tbs.next()
            for h in range(H):
                P.op("tensor", lambda e, h=h: e.transpose(out=pb[:, h * 128:h * 128 + n], in_=ob[0:n, h * 128:(h + 1) * 128],
                                                          identity=identb[0:n, 0:n]), [ob, identb], [pb])
            en = evac.next()
            pv = pb[:, 0:H * 128].rearrange("p (h t) -> p h t", t=128)[:, :, 0:n]
            if en == "vector":
                P.op("vector", lambda e: e.tensor_copy(out=tb[:, 0:H, 0:n], in_=pv), [pb], [tb])
            else:
                P.op("scalar", lambda e: e.copy(out=tb[:, 0:H, 0:n], in_=pv), [pb], [tb])
            for (db, apfn, c0, c1) in featdst:
                P.dma("scalar", apfn(H), tb[:, 0:H, c0:c1], [tb], [db])

    def featT(buf, h0, t0, ncols, lead=None):
        def fn(H):
            base = buf.t if lead is None else buf.t[lead]
            return base[h0:h0 + H, :, t0:t0 + ncols].rearrange("h d t -> d h t")
        return fn

    def featT1(buf, t0, ncols, lead=None):
        def fn(H):
            base = buf.t if lead is None else buf.t[lead]
            return base[:, t0:t0 + ncols].unsqueeze(1)
        return fn

    KBLOCKS = [("k", C_K, 512, 0), ("v", C_V, 512, 0), ("ki", C_KI, 144, 0)] + \
              [("dk", C_DK + 512 * i, 512, 4 * i) for i in range(4)] + [("dv", C_DV + 512 * i, 512, 4 * i) for i in range(4)]
    QBLOCKS = [("q", C_Q + 512 * i, 512, 4 * i) for i in range(4)] + [("qi", C_QI + 512 * i, 512, 4 * i) for i in range(4)] + \
              [("ki", C_KI, 144, 0)] + [("dq", C_DQ + 512 * i, 512, 4 * i) for i in range(4)]

    def load_tabs(cb, sb_, row0, n):
        cs, sn = tabs.next()
        P.dma("sync", cs[0:n, :], cb[row0:row0 + n, :], [cb], [cs])
        P.dma("sync", sn[0:n, :], sb_[row0:row0 + n, :], [sb_], [sn])
        return cs, sn

    def k_epilogue(kind, pb, n, W, h0, tb_, prompt, tok0):
        src = pb[0:n, 0:W]
        if kind == "ki":
            src = pb[0:n, 0:128]
            W = 128
        ci = C_DK if kind == "dk" else C_DV
        if prompt:
            if kind == "k":
                epilogue(src, pb, n, W, rope=tb_, f32dst=(o_kp, o_kp[tok0:tok0 + n, :]),
                         featdst=[(kT_s, featT(kT_s, 0, tok0, n), 0, n)])
            elif kind == "v":
                epilogue(src, pb, n, W, f32dst=(o_vp, o_vp[tok0:tok0 + n, :]), tokdst=[(v_s, v_s[tok0:tok0 + n, :], 0, n)])
            elif kind == "ki":
                epilogue(src, pb, n, W, rope=tb_, f32dst=(o_kip, o_kip[tok0:tok0 + n, :]),
                         featdst=[(kiT_s, featT1(kiT_s, tok0, n), 0, n)])
            elif kind == "dk":
                epilogue(src, pb, n, W, rope=tb_, f32dst=(o_dkp, o_dkp[tok0:tok0 + n, h0 * 128:h0 * 128 + W]),
                         featdst=[(dkT_s, featT(dkT_s, h0, tok0, n), 0, n)])
            elif kind == "dv":
                epilogue(src, pb, n, W, f32dst=(o_dvp, o_dvp[tok0:tok0 + n, h0 * 128:h0 * 128 + W]),
                         tokdst=[(dv_s, dv_s[tok0:tok0 + n, h0 * 128:h0 * 128 + W], 0, n)])
        else:
            if kind == "k":
                epilogue(src, pb, n, W, rope=tb_, f32dst=(o_ks, o_ks[:, :]),
                         featdst=[(skT_s, featT(skT_s, 0, PAST, 32, lead=s), 32 * s, 32 * s + 32) for s in range(2)])
            elif kind == "v":
                epilogue(src, pb, n, W, f32dst=(o_vs, o_vs[:, :]),
                         tokdst=[(sv_s, sv_s[s, PAST:LS, :], 32 * s, 32 * s + 32) for s in range(2)])
            elif kind == "ki":
                epilogue(src, pb, n, W, rope=tb_, f32dst=(o_kis, o_kis[:, :]),
                         featdst=[(skiT_s, featT1(skiT_s, PAST, 32, lead=s), 32 * s, 32 * s + 32) for s in range(2)])
            elif kind == "dk":
                epilogue(src, pb, n, W, rope=tb_, f32dst=(o_dks, o_dks[:, h0 * 128:h0 * 128 + W]),
                         featdst=[(sdkT_s, featT(sdkT_s, h0, PAST, 32, lead=s), 32 * s, 32 * s + 32) for s in range(2)])
            elif kind == "dv":
                epilogue(src, pb, n, W, f32dst=(o_dvs, o_dvs[:, h0 * 128:h0 * 128 + W]),
                         tokdst=[(sdv_s, sdv_s[s, PAST:LS, h0 * 128:h0 * 128 + W], 32 * s, 32 * s + 32) for s in range(2)])

    def q_epilogue(kind, pb, n, W, h0, tb_, tok0):
        if kind == "ki":
            P.op("scalar", lambda e: e.copy(out=wit[0:n, :], in_=pb[0:n, 128:144]), [pb], [wit])
            P.dma("gpsimd", wi_s[tok0:tok0 + n, :], wit[0:n, :], [wit], [wi_s])
            return
        dst = {"q": qT_s, "qi": qiT_s, "dq": dqT_s}[kind]
        epilogue(pb[0:n, 0:W], pb, n, W, rope=tb_, featdst=[(dst, featT(dst, h0, tok0, n), 0, n)])

    wit = C.sb([128, 16], F32, "wit")

    NKT = int(os.environ.get("MK_NKT", "32"))
    for g0 in range(0, NKT, 8):
        tiles = list(range(g0, min(g0 + 8, NKT)))
        tabl = {}
        for si, j in enumerate(tiles):
            load_norm_T(xk, j * 128, 128, hTs[si], True, A1, 0)
        for (kind, c0, W, h0), wbf in zip(KBLOCKS, weight_stream([(w_in, b_[1], b_[2]) for b_ in KBLOCKS])):
            pend = None
            for si, j in enumerate(tiles):
                pb = project(hTs[si], 128, wbf, W)
                if pend is not None:
                    pend()
                tb_ = load_tabs(cosk, sink, j * 128, 128) if kind in ("k", "ki", "dk") else None
                pend = (lambda kind=kind, pb=pb, W=W, h0=h0, tb_=tb_, j=j: k_epilogue(kind, pb, 128, W, h0, tb_, True, j * 128))
            pend()
    NQ = int(os.environ.get("MK_NQT", str(NQT)))
    for grp in range(2):
        tiles = list(range(grp * 8, min(grp * 8 + 8, NQ)))
        slots = [(si, i, 128, True) for si, i in enumerate(tiles)]
        if grp == 1:
            slots.append((8, NQT, 64, False))
        for (si, i, n, prompt) in slots:
            load_norm_T(xq, i * 128, n, hTs[si], prompt, A1, 0)
        for (kind, c0, W, h0), wbf in zip(QBLOCKS, weight_stream([(w_in, b_[1], b_[2]) for b_ in QBLOCKS])):
            pend = None
            for (si, i, n, prompt) in slots:
                pb = project(hTs[si], n, wbf, W)
                if pend is not None:
                    pend()
                tb_ = load_tabs(cosq, sinq, i * 128, n) if kind != "ki" or not prompt else None

                def pend(kind=kind, pb=pb, n=n, W=W, h0=h0, tb_=tb_, i=i, prompt=prompt):
                    q_epilogue(kind, pb, n, W, h0, tb_, i * 128)
                    if kind == "ki" and not prompt:
                        k_epilogue("ki", pb, n, W, 0, tb_, False, 0)
            pend()
        if grp == 1:
            for (kind, c0, W, h0) in KBLOCKS:
                if kind == "ki":
                    continue
                wbf = load_weights(w_in, c0, W)
                pb = project(hTs[8], 64, wbf, W)
                tb_ = load_tabs(cosq, sinq, NQT * 128, 64) if kind in ("k", "dk") else None
                k_epilogue(kind, pb, 64, W, h0, tb_, False, 0)
    if STAGE >= 2:
        cin = RR([C.sb([128, 512], F32, "cin") for _ in range(1)])
        for s in range(2):
            for t in range(PAST // 128):
                t0 = t * 128

                def ing(srcb, sap, W, **kw):
                    cb = cin.next()
                    P.dma("sync", cb[:, 0:W], sap, [srcb], [cb])
                    epilogue(cb[:, 0:W], cb, 128, W, **kw)
                ing(c_k, c_k[s, t0:t0 + 128, :], 512, featdst=[(skT_s, featT(skT_s, 0, t0, 128, lead=s), 0, 128)])
                ing(c_v, c_v[s, t0:t0 + 128, :], 512, tokdst=[(sv_s, sv_s[s, t0:t0 + 128, :], 0, 128)])
                ing(c_ki, c_ki[s, t0:t0 + 128, :], 128, featdst=[(skiT_s, featT1(skiT_s, t0, 128, lead=s), 0, 128)])
                for hb in range(4):
                    ing(c_dk, c_dk[s, t0:t0 + 128, hb * 512:(hb + 1) * 512], 512,
                        featdst=[(sdkT_s, featT(sdkT_s, hb * 4, t0, 128, lead=s), 0, 128)])
                    ing(c_dv, c_dv[s, t0:t0 + 128, hb * 512:(hb + 1) * 512], 512,
                        tokdst=[(sdv_s, sdv_s[s, t0:t0 + 128, hb * 512:(hb + 1) * 512], 0, 128)])
    P.barrier()
    C.release(mk)
    mk = C.mark()
    psI = RR([C.ps([128, 512], F32, "psI") for _ in range(3)])
    psO = [C.ps([128, 512], F32, "psO") for _ in range(4)]
    psT = RR([C.ps([128, 1024], BF16, "psT") for _ in range(1)])
    kcl = C.sb([128, 512], F32, "kcl")
    gsubB = C.sb([128, 256], F32, "gsubB")
    lamv = C.sb([128, 4, 128], F32, "lamv")
    lamt = C.sb([128, 4], F32, "lamt")
    neglam = C.sb([128, 1], F32, "neglam")
    P.dma("sync", kcl[:, :], kcl_in[:, :], [kcl_in], [kcl])
    P.dma("sync", gsubB[:, :], gsub_in[:, :], [gsub_in], [gsubB])
    P.dma("sync", lamv[:, :, :], lamv_in[:, :, :], [lamv_in], [lamv])
    P.op("vector", lambda e: e.tensor_scalar(out=gsubB[:, :], in0=gsubB[:, :], scalar1=1.0 - LAM_INIT, scalar2=None,
                                             op0=ALU.mult), [gsubB], [gsubB])
    P.op("vector", lambda e: e.tensor_tensor(out=lamv[:, 0, :], in0=lamv[:, 0, :], in1=lamv[:, 1, :], op=ALU.mult), [lamv], [lamv])
    P.op("vector", lambda e: e.tensor_tensor(out=lamv[:, 2, :], in0=lamv[:, 2, :], in1=lamv[:, 3, :], op=ALU.mult), [lamv], [lamv])
    P.op("vector", lambda e: e.reduce_sum(out=lamt[:, 0:1], in_=lamv[:, 0, :], axis=AX.X), [lamv], [lamt])
    P.op("vector", lambda e: e.reduce_sum(out=lamt[:, 1:2], in_=lamv[:, 2, :], axis=AX.X), [lamv], [lamt])
    P.op("scalar", lambda e: e.activation(out=lamt[:, 2:4], in_=lamt[:, 0:2], func=AF.Exp), [lamt], [lamt])
    P.op("vector", lambda e: e.tensor_tensor(out=neglam[:, :], in0=lamt[:, 3:4], in1=lamt[:, 2:3], op=ALU.subtract), [lamt], [neglam])
    P.op("vector", lambda e: e.tensor_scalar(out=neglam[:, :], in0=neglam[:, :], scalar1=-LAM_INIT, scalar2=None, op0=ALU.add),
         [neglam], [neglam])

    kiT = C.sb([128, SEQ], BF16, "kiT")
    qiT = C.sb([128, 16, 128], BF16, "qiT")
    wiq = C.sb([128, 16], F32, "wiq")
    qrel = C.sb([128, 1], F32, "qrel")
    Iw = C.sb([128, SEQ], F32, "Iw")
    wk = C.sb([128, SEQ], F32, "wk")
    rls = RR([C.sb([128, 512], F32, "rl") for _ in range(2)])
    pen = C.sb([128, 512], F32, "pen")
    m8 = C.sb([128, 8], F32, "m8")
    thr = C.sb([128, 1], F32, "thr")
    maskb = C.sb([128, SEQ], BF16, "maskb")
    maskT = C.sb([128, 32, 128], BF16, "maskT")
    visb = C.sb([128, 512], BF16, "visb")
    visT = C.sb([128, 4, 128], BF16, "visT")
    kTs = RR([C.sb([128, SEQ], BF16, "kTg") for _ in range(2)])
    vts = [C.sb([128, 32, 129], BF16, "vt") for _ in range(2)]
    qgs = RR([C.sb([128, 4, 128], BF16, "qg") for _ in range(2)])
    Pts = RR([C.sb([128, 4, 128], BF16, "Pt") for _ in range(3)])
    Pms = RR([C.sb([128, 4, 128], BF16, "Pm") for _ in range(3)])
    dkTs = RR([C.sb([128, 2, SEQ], BF16, "dkT") for _ in range(2)])
    dvts = [C.sb([128, 32, 257], BF16, "dvt") for _ in range(2)]
    dqs = RR([C.sb([128, 2, 128], BF16, "dq") for _ in range(2)])
    rden = C.sb([128, 8], F32, "rden")
    Osb = [C.sb([128, 129], F32, "Osb") for _ in range(4)]
    Od = [C.sb([128, 257], F32, "Od") for _ in range(2)]
    negc = C.sb([128, 2], F32, "negc")
    P.op("vector", lambda e: e.memset(negc[:, 0:1], -1.0), [], [negc])
    P.op("vector", lambda e: e.memset(negc[:, 1:2], -0.5), [negc], [negc])
    tmpd = C.sb([128, 256], F32, "tmpd")
    dof = C.sb([128, 256], F32, "dof")
    junkd = C.sb([128, 256], BF16, "junkd")
    ssd = C.sb([128, 2], F32, "ssd")
    attn = C.sb([128, D], BF16, "attn")
    tbc = RR([C.sb([128, 8, 128], BF16, "tbc") for _ in range(2)])
    for vt in vts:
        P.op("vector", lambda e, vt=vt: e.memset(vt[:, :, 128:129], 1.0), [], [vt])
    for dvt in dvts:
        P.op("vector", lambda e, dvt=dvt: e.memset(dvt[:, :, 256:257], 1.0), [], [dvt])
    vti = [0]
    dvi = [0]

    def transposes_to(srcbuf, src_fn, dstbuf, dst_fn, chunks, nq):
        for b0 in range(0, len(chunks), 8):
            grp = chunks[b0:b0 + 8]
            pb = psT.next()
            for s_, (k0, kl) in enumerate(grp):
                P.op("tensor", lambda e, s_=s_, k0=k0, kl=kl: e.transpose(
                    out=pb[0:kl, s_ * 128:s_ * 128 + nq], in_=src_fn(k0, kl), identity=identb[0:nq, 0:nq]),
                    [srcbuf, identb], [pb])
            full = [x for x in grp if x[1] == 128]
            if full:
                nf = len(full)
                P.op("scalar", lambda e, b0=b0, nf=nf: e.copy(
                    out=dst_fn(b0, nf, 128), in_=pb[:, 0:nf * 128].rearrange("p (c t) -> p c t", t=128)[:, :, 0:nq]),
                    [pb], [dstbuf])
            for s_, (k0, kl) in enumerate(grp):
                if kl != 128:
                    P.op("scalar", lambda e, s_=s_, kl=kl, b0=b0: e.copy(
                        out=dst_fn(b0 + s_, 1, kl), in_=pb[0:kl, s_ * 128:s_ * 128 + nq].unsqueeze(1)), [pb], [dstbuf])

    def geom(nkeys, prompt):
        chunks = [(k0, min(128, nkeys - k0)) for k0 in range(0, nkeys, 128)]
        nchk = len(chunks)
        nfull = nkeys // 128
        nv = min(4, nchk) if prompt else 0
        return chunks, nchk, nfull, nv

    def idx_topk(tok0, nq, nkeys, prompt, S):
        chunks, nchk, nfull, nv = geom(nkeys, prompt)
        P.dma("sync", qiT[:, :, 0:nq], qiT_s[:, :, tok0:tok0 + nq].rearrange("h d t -> d h t"), [qiT_s], [qiT])
        P.dma("sync", wiq[0:nq, :], wi_s[tok0:tok0 + nq, :], [wi_s], [wiq])
        for kb0 in range(0, nkeys, 512):
            kw = min(512, nkeys - kb0)
            for h in range(16):
                pb = psI.next()
                P.op("tensor", lambda e, h=h, pb=pb: e.matmul(out=pb[0:nq, 0:kw], lhsT=qiT[:, h, 0:nq], rhs=kiT[:, kb0:kb0 + kw],
                                                              start=True, stop=True), [qiT, kiT], [pb])
                rl = rls.next()
                P.op("scalar", lambda e, pb=pb, rl=rl: e.activation(out=rl[0:nq, 0:kw], in_=pb[0:nq, 0:kw], func=AF.Relu), [pb], [rl])
                if h == 0:
                    P.op("vector", lambda e, rl=rl: e.tensor_scalar(out=Iw[0:nq, kb0:kb0 + kw], in0=rl[0:nq, 0:kw],
                                                                    scalar1=wiq[0:nq, 0:1], scalar2=None, op0=ALU.mult),
                         [rl, wiq], [Iw])
                else:
                    P.op("vector", lambda e, rl=rl, h=h: e.scalar_tensor_tensor(
                        out=Iw[0:nq, kb0:kb0 + kw], in0=rl[0:nq, 0:kw], scalar=wiq[0:nq, h:h + 1], in1=Iw[0:nq, kb0:kb0 + kw],
                        op0=ALU.mult, op1=ALU.add), [rl, wiq, Iw], [Iw])
        if prompt:
            v0 = nkeys - nv * 128
            P.dma("sync", qrel[0:nq, :], qrel_in[tok0:tok0 + nq, :], [qrel_in], [qrel])
            P.op("vector", lambda e: e.tensor_scalar(out=pen[0:nq, 0:nv * 128], in0=kcl[0:nq, 0:nv * 128], scalar1=qrel[0:nq, 0:1],
                                                     scalar2=NEG, op0=ALU.is_gt, op1=ALU.mult), [kcl, qrel], [pen])
            P.op("vector", lambda e: e.tensor_tensor(out=Iw[0:nq, v0:nkeys], in0=Iw[0:nq, v0:nkeys], in1=pen[0:nq, 0:nv * 128],
                                                     op=ALU.add), [Iw, pen], [Iw])
            P.op("vector", lambda e: e.tensor_scalar(out=visb[0:nq, 0:nv * 128], in0=kcl[0:nq, 0:nv * 128], scalar1=qrel[0:nq, 0:1],
                                                     scalar2=None, op0=ALU.is_le), [kcl, qrel], [visb])
        if nkeys > 256:
            cur = Iw
            for r in range(32):
                P.op("vector", lambda e, cur=cur: e.max(out=m8[0:nq, :], in_=cur[0:nq, 0:nkeys]), [cur], [m8])
                if r < 31:
                    P.op("vector", lambda e, cur=cur: e.match_replace(out=wk[0:nq, 0:nkeys], in_to_replace=m8[0:nq, :],
                                                                      in_values=cur[0:nq, 0:nkeys], imm_value=-3.0e38),
                         [cur, m8], [wk])
                    cur = wk
            P.op("vector", lambda e: e.tensor_scalar(out=thr[0:nq, :], in0=m8[0:nq, 7:8], scalar1=-1.0e29, scalar2=None,
                                                     op0=ALU.max), [m8], [thr])
        else:
            P.op("vector", lambda e: e.memset(thr[:, :], -1.0e29), [], [thr])
        P.op("vector", lambda e: e.tensor_scalar(out=maskb[0:nq, 0:nkeys], in0=Iw[0:nq, 0:nkeys], scalar1=thr[0:nq, 0:1],
                                                 scalar2=None, op0=ALU.is_ge), [Iw, thr], [maskb])

    def mask_T(tok0, nq, nkeys, prompt, S):
        chunks, nchk, nfull, nv = geom(nkeys, prompt)
        transposes_to(maskb, lambda k0, kl: maskb[0:nq, k0:k0 + kl], maskT,
                      lambda c0, n_, kl: maskT[0:kl, c0:c0 + n_, 0:nq], chunks, nq)
        if prompt:
            transposes_to(visb, lambda k0, kl: visb[0:nq, k0:k0 + kl], visT,
                          lambda c0, n_, kl: visT[0:kl, c0:c0 + n_, 0:nq], [(c * 128, 128) for c in range(nv)], nq)
        P.op("gpsimd", lambda e: e.tensor_scalar(out=maskT[:, 0:nchk, 0:nq], in0=maskT[:, 0:nchk, 0:nq], scalar1=30000.0,
                                                 scalar2=-30000.0, op0=ALU.mult, op1=ALU.add), [maskT], [maskT])

    def attend(tok0, nq, nkeys, prompt, S):
        chunks, nchk, nfull, nv = geom(nkeys, prompt)
        for g in range(4):
            kT = kTs.next()
            vt = vts[vti[0] % 2]
            vti[0] += 1
            qg = qgs.next()
            P.dma("sync", kT[:, 0:nkeys], S["kT"](g), [S["kTb"]], [kT])
            if nfull:
                P.dma("sync", vt[:, 0:nfull, 0:128], S["v"](0, nfull * 128, g).rearrange("(c p) d -> p c d", p=128), [S["vb"]], [vt])
            if nkeys > nfull * 128:
                P.dma("sync", vt[0:nkeys - nfull * 128, nfull, 0:128], S["v"](nfull * 128, nkeys, g), [S["vb"]], [vt])
            P.dma("sync", qg[:, :, 0:nq], qT_s[4 * g:4 * g + 4, :, tok0:tok0 + nq].rearrange("h d t -> d h t"), [qT_s], [qg])

            def s_mm(c):
                k0, kl = chunks[c]
                pb = psI.next()
                P.op("tensor", lambda e: e.matmul(out=pb[0:kl, :].rearrange("p (r t) -> p r t", t=128)[:, :, 0:nq],
                                                  lhsT=kT[:, k0:k0 + kl], rhs=qg[:, :, 0:nq], start=True, stop=False),
                     [kT, qg], [pb])
                P.op("tensor", lambda e: e.matmul(out=pb[0:kl, :].rearrange("p (r t) -> p r t", t=128)[:, :, 0:nq],
                                                  lhsT=identb[0:kl, 0:kl],
                                                  rhs=maskT[0:kl, c, 0:nq].unsqueeze(1).to_broadcast([kl, 4, nq]),
                                                  start=False, stop=True), [identb, maskT], [pb])
                return pb
            pbq = [s_mm(0)] + ([s_mm(1)] if nchk > 1 else [])
            for c, (k0, kl) in enumerate(chunks):
                pb = pbq.pop(0)
                if c + 2 < nchk:
                    pbq.append(s_mm(c + 2))
                Pm = Pms.next()
                P.op("scalar", lambda e, pb=pb, Pm=Pm, kl=kl: e.activation(
                    out=Pm[0:kl, :, 0:nq], in_=pb[0:kl, :].rearrange("p (r t) -> p r t", t=128)[:, :, 0:nq], func=AF.Exp,
                    scale=float(128 ** -0.5)), [pb], [Pm])
                for r in range(4):
                    P.op("tensor", lambda e, r=r, Pm=Pm, kl=kl, c=c: e.matmul(
                        out=psO[r][0:nq, 0:129], lhsT=Pm[0:kl, r, 0:nq], rhs=vt[0:kl, c, 0:129],
                        start=(c == 0), stop=(c == nchk - 1)), [Pm, vt], [psO[r]])
            for r in range(4):
                ob_ = Osb[r]
                col = (4 * g + r) * 128
                P.op("scalar", lambda e, r=r, ob_=ob_: e.copy(out=ob_[0:nq, 0:129], in_=psO[r][0:nq, 0:129]), [psO[r]], [ob_])
                P.op("gpsimd", lambda e, ob_=ob_, r=r: e.tensor_tensor(out=rden[0:nq, r:r + 1], in0=ob_[0:nq, 128:129],
                                                                       in1=negc[0:nq, 0:1], op=ALU.pow), [ob_, negc], [rden])
                P.op("gpsimd", lambda e, ob_=ob_, col=col, r=r: e.tensor_scalar(out=attn[0:nq, col:col + 128], in0=ob_[0:nq, 0:128],
                                                                                scalar1=rden[0:nq, r:r + 1], scalar2=None, op0=ALU.mult),
                     [ob_, rden], [attn])
        for hd in range(8):
            dkT = dkTs.next()
            dvt = dvts[dvi[0] % 2]
            dvi[0] += 1
            dq2 = dqs.next()
            P.dma("sync", dkT[:, :, 0:nkeys], S["dkT"](hd), [S["dkTb"]], [dkT])
            if nfull:
                P.dma("sync", dvt[:, 0:nfull, 0:256], S["dv"](0, nfull * 128, hd).rearrange("(c p) d -> p c d", p=128),
                      [S["dvb"]], [dvt])
            if nkeys > nfull * 128:
                P.dma("sync", dvt[0:nkeys - nfull * 128, nfull, 0:256], S["dv"](nfull * 128, nkeys, hd), [S["dvb"]], [dvt])
            P.dma("sync", dq2[:, :, 0:nq], dqT_s[2 * hd:2 * hd + 2, :, tok0:tok0 + nq].rearrange("m d t -> d m t"), [dqT_s], [dq2])

            def d_mm(c):
                k0, kl = chunks[c]
                pb = psI.next()
                for m in range(2):
                    P.op("tensor", lambda e, m=m: e.matmul(out=pb[0:kl, m * 128:m * 128 + nq], lhsT=dkT[:, m, k0:k0 + kl],
                                                           rhs=dq2[:, m, 0:nq], start=True, stop=True), [dkT, dq2], [pb])
                return pb
            pbq = [d_mm(0)] + ([d_mm(1)] if nchk > 1 else [])
            for c, (k0, kl) in enumerate(chunks):
                pb = pbq.pop(0)
                if c + 2 < nchk:
                    pbq.append(d_mm(c + 2))
                Pt = Pts.next()
                P.op("scalar", lambda e, pb=pb, Pt=Pt, kl=kl: e.activation(
                    out=Pt[0:kl, 0:2, 0:nq], in_=pb[0:kl, 0:256].rearrange("p (r t) -> p r t", t=128)[:, :, 0:nq], func=AF.Exp,
                    scale=float(128 ** -0.5)), [pb], [Pt])
                if prompt and c >= nchk - nv:
                    P.op("gpsimd", lambda e, Pt=Pt, kl=kl, c=c: e.tensor_tensor(
                        out=Pt[0:kl, 0:2, 0:nq], in0=Pt[0:kl, 0:2, 0:nq],
                        in1=visT[0:kl, c - (nchk - nv), 0:nq].unsqueeze(1).to_broadcast([kl, 2, nq]), op=ALU.mult),
                        [Pt, visT], [Pt])
                for m in range(2):
                    P.op("tensor", lambda e, m=m, Pt=Pt, kl=kl, c=c: e.matmul(
                        out=psO[m][0:nq, 0:257], lhsT=Pt[0:kl, m, 0:nq], rhs=dvt[0:kl, c, 0:257],
                        start=(c == 0), stop=(c == nchk - 1)), [Pt, dvt], [psO[m]])
            P.op("scalar", lambda e: e.copy(out=Od[0][0:nq, :], in_=psO[0][0:nq, 0:257]), [psO[0]], [Od[0]])
            P.op("scalar", lambda e: e.copy(out=Od[1][0:nq, :], in_=psO[1][0:nq, 0:257]), [psO[1]], [Od[1]])
            P.op("gpsimd", lambda e: e.tensor_tensor(out=rden[0:nq, 4:5], in0=Od[0][0:nq, 256:257], in1=negc[0:nq, 0:1], op=ALU.pow),
                 [Od[0], negc], [rden])
            P.op("gpsimd", lambda e: e.tensor_tensor(out=rden[0:nq, 5:6], in0=Od[1][0:nq, 256:257], in1=negc[0:nq, 0:1], op=ALU.pow),
                 [Od[1], negc], [rden])
            P.op("gpsimd", lambda e: e.tensor_tensor(out=rden[0:nq, 5:6], in0=rden[0:nq, 5:6], in1=neglam[0:nq, 0:1], op=ALU.mult),
                 [rden, neglam], [rden])
            P.op("gpsimd", lambda e: e.tensor_scalar(out=tmpd[0:nq, :], in0=Od[0][0:nq, 0:256], scalar1=rden[0:nq, 4:5],
                                                     scalar2=None, op0=ALU.mult), [Od[0], rden], [tmpd])
            P.op("gpsimd", lambda e: e.tensor_scalar(out=dof[0:nq, :], in0=Od[1][0:nq, 0:256], scalar1=rden[0:nq, 5:6],
                                                     scalar2=None, op0=ALU.mult), [Od[1], rden], [dof])
            P.op("gpsimd", lambda e: e.tensor_tensor(out=dof[0:nq, :], in0=dof[0:nq, :], in1=tmpd[0:nq, :], op=ALU.add), [dof, tmpd], [dof])
            P.op("gpsimd", lambda e: e.memset(ssd[:, :], 0.0), [], [ssd])
            P.op("scalar", lambda e: e.activation(out=junkd[0:nq, :], in_=dof[0:nq, :], func=AF.Square, accum_out=ssd[0:nq, 0:1]),
                 [dof], [junkd, ssd])
            P.op("gpsimd", lambda e: e.tensor_scalar(out=ssd[0:nq, 1:2], in0=ssd[0:nq, 0:1], scalar1=1.0 / 256, scalar2=EPS,
                                                     op0=ALU.mult, op1=ALU.add), [ssd], [ssd])
            P.op("gpsimd", lambda e: e.tensor_tensor(out=ssd[0:nq, 1:2], in0=ssd[0:nq, 1:2], in1=negc[0:nq, 1:2], op=ALU.pow),
                 [ssd, negc], [ssd])
            col = 2048 + hd * 256
            P.op("gpsimd", lambda e: e.tensor_scalar(out=tmpd[0:nq, :], in0=dof[0:nq, :], scalar1=ssd[0:nq, 1:2], scalar2=None,
                                                     op0=ALU.mult), [dof, ssd], [tmpd])
            P.op("gpsimd", lambda e, col=col: e.tensor_tensor(out=attn[0:nq, col:col + 256], in0=tmpd[0:nq, :], in1=gsubB[0:nq, :],
                                                              op=ALU.mult), [tmpd, gsubB], [attn])
        for c8 in range(4):
            pb = psT.next()
            tb = tbc.next()
            for j in range(8):
                ch = c8 * 8 + j
                P.op("tensor", lambda e, j=j, ch=ch: e.transpose(out=pb[:, j * 128:j * 128 + nq], in_=attn[0:nq, ch * 128:(ch + 1) * 128],
                                                                 identity=identb[0:nq, 0:nq]), [attn, identb], [pb])
            P.op("scalar", lambda e: e.copy(out=tb[:, :, 0:nq], in_=pb[:, :].rearrange("p (c t) -> p c t", t=128)[:, :, 0:nq]),
                 [pb], [tb])
            P.dma("gpsimd", attnT_s[:, c8 * 8:(c8 + 1) * 8, tok0:tok0 + nq], tb[:, :, 0:nq], [tb], [attnT_s])

    NA = int(os.environ.get("MK_NAT", str(NQT)))
    jobs = []
    for i in range(NA):
        nkeys = nchunks_for(i) * 128
        S = {
            "kT": (lambda g, nkeys=nkeys: kT_s[g, :, 0:nkeys]), "kTb": kT_s,
            "v": (lambda a, b_, g: v_s[a:b_, g * 128:(g + 1) * 128]), "vb": v_s,
            "dkT": (lambda hd, nkeys=nkeys: dkT_s[2 * hd:2 * hd + 2, :, 0:nkeys].rearrange("m d t -> d m t")), "dkTb": dkT_s,
            "dv": (lambda a, b_, hd: dv_s[a:b_, hd * 256:(hd + 1) * 256]), "dvb": dv_s,
            "ki": None,
        }
        jobs.append((i * 128, 128, nkeys, True, S))
    for s in range(2):
        S = {
            "kT": (lambda g, s=s: skT_s[s, g, :, :]), "kTb": skT_s,
            "v": (lambda a, b_, g, s=s: sv_s[s, a:b_, g * 128:(g + 1) * 128]), "vb": sv_s,
            "dkT": (lambda hd, s=s: sdkT_s[s, 2 * hd:2 * hd + 2, :, :].rearrange("m d t -> d m t")), "dkTb": sdkT_s,
            "dv": (lambda a, b_, hd, s=s: sdv_s[s, a:b_, hd * 256:(hd + 1) * 256]), "dvb": sdv_s,
            "ki": s,
        }
        jobs.append((NQT * 128 + 32 * s, 32, LS, False, S))

    def stage1(job):
        if job[4]["ki"] is not None:
            s_ = job[4]["ki"]
            P.dma("sync", kiT[:, 0:LS], skiT_s[s_, :, :], [skiT_s], [kiT])
        idx_topk(*job)

    P.dma("sync", kiT[:, :], kiT_s[:, :], [kiT_s], [kiT])
    stage1(jobs[0])
    mask_T(*jobs[0])
    for ji, job in enumerate(jobs):
        if ji + 1 < len(jobs):
            stage1(jobs[ji + 1])
        attend(*job)
        if ji + 1 < len(jobs):
            mask_T(*jobs[ji + 1])
    P.barrier()
    C.release(mk)

    mk = C.mark()
    psF = [C.ps([128, 512], F32, "psF") for _ in range(4)]
    ps_proj = RR(psF[0:3])
    hTs = [C.sb([128, 32, 128], BF16, "hT") for _ in range(9)]
    wsts = RR([C.sb([128, 4, 512], F32, "wst") for _ in range(2)])
    wbfs = RR([C.sb([128, 32, 512], BF16, "wbf") for _ in range(2)])
    gaP = C.sb([128, D], F32, "gaP")
    gaS = C.sb([128, D], F32, "gaS")
    xbs = RR([C.sb([128, 512], F32, "xb") for _ in range(3)])
    tms = RR([C.sb([128, 512], F32, "tm") for _ in range(3)])
    P.dma("sync", gaP[:, :], ga_s[0, :, :], [ga_s], [gaP])
    P.dma("sync", gaS[:, :], ga_s[2, :, :], [ga_s], [gaS])
    for grp in range(2):
        slots = [(si, i, 128, True) for si, i in enumerate(range(grp * 8, grp * 8 + 8))]
        if grp == 1:
            slots.append((8, NQT, 64, False))
        for (si, i, n, prompt) in slots:
            P.dma("sync", hTs[si][:, :, 0:n], attnT_s[:, :, i * 128:i * 128 + n], [attnT_s], [hTs[si]])
        for cb, wbf in zip(range(8), weight_stream([(w_out, cb_ * 512, 512) for cb_ in range(8)])):
            for (si, i, n, prompt) in slots:
                pb = project(hTs[si], n, wbf, 512)
                xb = xbs.next()
                tm = tms.next()
                ga = gaP if prompt else gaS
                P.dma("sync", xb[0:n, :], xq[i * 128:i * 128 + n, cb * 512:(cb + 1) * 512], [xq], [xb])
                P.op("vector", lambda e, pb=pb, tm=tm, ga=ga, n=n, cb=cb: e.tensor_tensor(
                    out=tm[0:n, :], in0=pb[0:n, :], in1=ga[0:n, cb * 512:(cb + 1) * 512], op=ALU.mult), [pb, ga], [tm])
                P.op("gpsimd", lambda e, tm=tm, xb=xb, n=n: e.tensor_tensor(out=tm[0:n, :], in0=tm[0:n, :], in1=xb[0:n, :], op=ALU.add),
                     [tm, xb], [tm])
                P.dma("gpsimd", x1_s[i * 128:i * 128 + n, cb * 512:(cb + 1) * 512], tm[0:n, :], [tm], [x1_s])
    P.barrier()
    C.release(mk)
    mk = C.mark()
    psF = [C.ps([128, 512], F32, "psF") for _ in range(6)]
    psB = [C.ps([128, 1024], BF16, "psB") for _ in range(2)]
    ps_proj = RR(psF[0:2])
    ps_xT = RR(psF[2:4])
    ps_s = RR(psF[4:6])
    xts = RR([C.sb([128, D], F32, "xt") for _ in range(1)])
    sst = RR([C.sb([128, 2], F32, "ss") for _ in range(2)])
    hTs = [C.sb([128, 32, 128], BF16, "hT") for _ in range(9)]
    wsts = RR([C.sb([128, 4, 512], F32, "wst") for _ in range(2)])
    wbfs = RR([C.sb([128, 32, 512], BF16, "wbf") for _ in range(2)])
    kraw = C.sb([128, 16, 128], F32, "kraw")
    keysT = C.sb([128, 16, 128], F32, "keysT")
    qsbs = RR([C.sb([128, 512], F32, "qsb") for _ in range(2)])
    qT4s = RR([C.sb([128, 4, 128], F32, "qT4") for _ in range(2)])
    sos = RR([C.sb([128, 512], F32, "so") for _ in range(2)])
    P.dma("sync", kraw[:, :, :], pkeys_in[:, :, :, :].rearrange("h c n d -> n (h c) d"), [pkeys_in], [kraw])
    for b4 in range(4):
        pb = ps_xT.next()
        for j in range(4):
            P.op("tensor", lambda e, j=j, b4=b4: e.transpose(out=pb[:, j * 128:(j + 1) * 128], in_=kraw[:, b4 * 4 + j, :],
                                                             identity=ident[:, :]), [kraw, ident], [pb])
        P.op("vector", lambda e, b4=b4: e.tensor_copy(out=keysT[:, b4 * 4:(b4 + 1) * 4, :],
                                                      in_=pb[:, :].rearrange("p (c t) -> p c t", t=128)), [pb], [keysT])
    for grp in range(2):
        slots = [(si, i, 128, True) for si, i in enumerate(range(grp * 8, grp * 8 + 8))]
        if grp == 1:
            slots.append((8, NQT, 64, False))
        for (si, i, n, prompt) in slots:
            load_norm_T(x1_s, i * 128, n, hTs[si], prompt, A2, 96)
            P.dma("gpsimd", h2T_s[:, :, i * 128:i * 128 + n], hTs[si][:, :, 0:n], [hTs[si]], [h2T_s])
        for cb, wbf in zip(range(4), weight_stream([(pwq_in, cb_ * 512, 512) for cb_ in range(4)])):
            for (si, i, n, prompt) in slots:
                pb = project(hTs[si], n, wbf, 512)
                qsb = qsbs.next()
                P.op("scalar", lambda e, pb=pb, qsb=qsb, n=n: e.copy(out=qsb[0:n, :], in_=pb[0:n, :]), [pb], [qsb])
                pt = ps_xT.next()
                for j in range(4):
                    P.op("tensor", lambda e, j=j, n=n, qsb=qsb, pt=pt: e.transpose(
                        out=pt[:, j * 128:j * 128 + n], in_=qsb[0:n, j * 128:(j + 1) * 128], identity=ident[0:n, 0:n]),
                        [qsb, ident], [pt])
                qT4 = qT4s.next()
                P.op("vector", lambda e, pt=pt, qT4=qT4, n=n: e.tensor_copy(
                    out=qT4[:, :, 0:n], in_=pt[:, :].rearrange("p (c t) -> p c t", t=128)[:, :, 0:n]), [pt], [qT4])
                po = ps_s.next()
                for j in range(4):
                    P.op("tensor", lambda e, j=j, n=n, qT4=qT4, po=po, cb=cb: e.matmul(
                        out=po[0:n, j * 128:(j + 1) * 128], lhsT=qT4[:, j, 0:n], rhs=keysT[:, cb * 4 + j, :],
                        start=True, stop=True), [qT4, keysT], [po])
                so = sos.next()
                P.op("scalar", lambda e, po=po, so=so, n=n: e.copy(out=so[0:n, :], in_=po[0:n, :]), [po], [so])
                P.dma("gpsimd", s_s[i * 128:i * 128 + n, cb * 512:(cb + 1) * 512], so[0:n, :], [so], [s_s])
    P.barrier()
    C.release(mk)

    mk = C.mark()
    psA = RR([C.ps([128, 1024], BF16, "psA") for _ in range(3)])
    psG = RR([C.ps([128, 512], F32, "psG") for _ in range(3)])
    sal = C.sb([128, 16, 128], F32, "sal")
    wk2 = C.sb([128, 16, 128], F32, "wk2")
    top = C.sb([128, 16, 16], F32, "top")
    idx = C.sb([128, 8, 16], U32, "idx")
    idxf = C.sb([128, 8, 16], F32, "idxf")
    cand = C.sb([128, 8, 256], F32, "cand")
    cw = C.sb([128, 8, 256], F32, "cw")
    best = C.sb([128, 8, 16], F32, "best")
    eb = C.sb([128, 8, 16], F32, "eb")
    Zs = C.sb([128, 8], F32, "Zs")
    e1z = C.sb([128, 8, 16], F32, "e1z")
    cth = C.sb([128, 8, 16], F32, "cth")
    e2 = C.sb([128, 8, 128], F32, "e2")
    iot = C.sb([128, 128], F32, "iot")
    ABa = C.sb([128, 128, 128], BF16, "ABa")
    ABb = C.sb([128, 128, 128], BF16, "ABb")
    AT = C.sb([128, 128, 128], BF16, "AT")
    BT = C.sb([128, 128, 128], BF16, "BT")
    Gall = C.sb([128, 128, 128], BF16, "Gall")
    P.dma("sync", iot[:, :], iota_in[:, :], [iota_in], [iot])
    NE2 = int(os.environ.get("MK_NE2", "17"))
    for ti in range(NE2):
        n = 128 if ti < NQT else 64
        tok0 = ti * 128
        P.dma("sync", sal[0:n, :, :], s_s[tok0:tok0 + n, :].rearrange("t (k m) -> t k m", m=128), [s_s], [sal])
        for hc in range(16):
            h, c = divmod(hc, 2)
            P.op("vector", lambda e, hc=hc: e.max(out=top[0:n, hc, 0:8], in_=sal[0:n, hc, :]), [sal], [top])
            if c == 0:
                P.op("vector", lambda e, hc=hc, h=h: e.max_index(out=idx[0:n, h, 0:8], in_max=top[0:n, hc, 0:8],
                                                                 in_values=sal[0:n, hc, :]), [sal, top], [idx])
            P.op("vector", lambda e, hc=hc: e.match_replace(out=wk2[0:n, hc, :], in_to_replace=top[0:n, hc, 0:8],
                                                            in_values=sal[0:n, hc, :], imm_value=-3.0e38), [sal, top], [wk2])
            P.op("vector", lambda e, hc=hc: e.max(out=top[0:n, hc, 8:16], in_=wk2[0:n, hc, :]), [wk2], [top])
            if c == 0:
                P.op("vector", lambda e, hc=hc, h=h: e.max_index(out=idx[0:n, h, 8:16], in_max=top[0:n, hc, 8:16],
                                                                 in_values=wk2[0:n, hc, :]), [wk2, top], [idx])
        top4 = top[0:n, :, :].rearrange("p (h c) k -> p h c k", c=2)
        sal4 = sal[0:n, :, :].rearrange("p (h c) k -> p h c k", c=2)
        P.op("vector", lambda e: e.tensor_copy(out=idxf[0:n, :, :], in_=idx[0:n, :, :]), [idx], [idxf])
        P.op("vector", lambda e: e.tensor_tensor(
            out=cand[0:n, :, :].rearrange("p h (i j) -> p h i j", j=16),
            in0=top4[:, :, 0, :].unsqueeze(3).to_broadcast([n, 8, 16, 16]),
            in1=top4[:, :, 1, :].unsqueeze(2).to_broadcast([n, 8, 16, 16]), op=ALU.add), [top], [cand])
        for h in range(8):
            P.op("vector", lambda e, h=h: e.max(out=best[0:n, h, 0:8], in_=cand[0:n, h, :]), [cand], [best])
            P.op("vector", lambda e, h=h: e.match_replace(out=cw[0:n, h, :], in_to_replace=best[0:n, h, 0:8],
                                                          in_values=cand[0:n, h, :], imm_value=-3.0e38), [cand, best], [cw])
            P.op("vector", lambda e, h=h: e.max(out=best[0:n, h, 8:16], in_=cw[0:n, h, :]), [cw], [best])
        P.op("vector", lambda e: e.tensor_tensor(out=eb[0:n, :, :], in0=best[0:n, :, :],
                                                 in1=best[0:n, :, 0:1].to_broadcast([n, 8, 16]), op=ALU.subtract), [best], [eb])
        P.op("scalar", lambda e: e.activation(out=eb[0:n, :, :], in_=eb[0:n, :, :], func=AF.Exp), [eb], [eb])
        P.op("vector", lambda e: e.reduce_sum(out=Zs[0:n, :], in_=eb[0:n, :, :], axis=AX.X), [eb], [Zs])
        P.op("vector", lambda e: e.reciprocal(out=Zs[0:n, :], in_=Zs[0:n, :]), [Zs], [Zs])
        P.op("vector", lambda e: e.tensor_tensor(out=e1z[0:n, :, :], in0=top4[:, :, 0, :],
                                                 in1=top4[:, :, 0, 0:1].to_broadcast([n, 8, 16]), op=ALU.subtract), [top], [e1z])
        P.op("scalar", lambda e: e.activation(out=e1z[0:n, :, :], in_=e1z[0:n, :, :], func=AF.Exp), [e1z], [e1z])
        P.op("vector", lambda e: e.tensor_tensor(out=e1z[0:n, :, :], in0=e1z[0:n, :, :],
                                                 in1=Zs[0:n, :].unsqueeze(2).to_broadcast([n, 8, 16]), op=ALU.mult), [e1z, Zs], [e1z])
        P.op("vector", lambda e: e.tensor_tensor(out=cth[0:n, :, :], in0=best[0:n, :, 15:16].to_broadcast([n, 8, 16]),
                                                 in1=top4[:, :, 0, :], op=ALU.subtract), [best, top], [cth])
        P.op("vector", lambda e: e.tensor_scalar(out=cth[0:n, :, :], in0=cth[0:n, :, :], scalar1=-4.0e-6, scalar2=None,
                                                 op0=ALU.add), [cth], [cth])
        P.op("vector", lambda e: e.tensor_tensor(out=e2[0:n, :, :], in0=sal4[:, :, 1, :],
                                                 in1=top4[:, :, 1, 0:1].to_broadcast([n, 8, 128]), op=ALU.subtract), [sal, top], [e2])
        P.op("scalar", lambda e: e.activation(out=e2[0:n, :, :], in_=e2[0:n, :, :], func=AF.Exp), [e2], [e2])
        for which in range(2):
            XT = AT if which == 0 else BT
            AB = ABa if which == 0 else ABb
            if which == 0:
                P.op("vector", lambda e, AB=AB: e.tensor_tensor(
                    out=AB[0:n, :, :], in0=iot[0:n, :].unsqueeze(1).to_broadcast([n, 128, 128]),
                    in1=idxf[0:n, :, :].rearrange("p h i -> p (h i)").unsqueeze(2).to_broadcast([n, 128, 128]),
                    op=ALU.is_equal), [iot, idxf], [AB])
                P.op("vector", lambda e, AB=AB: e.tensor_tensor(
                    out=AB[0:n, :, :], in0=AB[0:n, :, :],
                    in1=e1z[0:n, :, :].rearrange("p h i -> p (h i)").unsqueeze(2).to_broadcast([n, 128, 128]),
                    op=ALU.mult), [AB, e1z], [AB])
            else:
                P.op("vector", lambda e, AB=AB: e.tensor_tensor(
                    out=AB[0:n, :, :].rearrange("p (h i) k -> p h i k", i=16),
                    in0=sal4[:, :, 1, :].unsqueeze(2).to_broadcast([n, 8, 16, 128]),
                    in1=cth[0:n, :, :].unsqueeze(3).to_broadcast([n, 8, 16, 128]), op=ALU.is_ge), [sal, cth], [AB])
                P.op("gpsimd", lambda e, AB=AB: e.tensor_tensor(
                    out=AB[0:n, :, :].rearrange("p (h i) k -> p h i k", i=16),
                    in0=AB[0:n, :, :].rearrange("p (h i) k -> p h i k", i=16),
                    in1=e2[0:n, :, :].unsqueeze(2).to_broadcast([n, 8, 16, 128]), op=ALU.mult), [AB, e2], [AB])
            for b8 in range(16):
                pb = psA.next()
                for j in range(8):
                    col = b8 * 8 + j
                    P.op("tensor", lambda e, j=j, col=col, pb=pb, AB=AB: e.transpose(
                        out=pb[:, j * 128:j * 128 + n], in_=AB[0:n, :, col], identity=identb[0:n, 0:n]), [AB, identb], [pb])
                en = "scalar" if b8 % 2 == 0 else "vector"
                src_v = pb[:, :].rearrange("p (c t) -> p c t", t=128)[:, :, 0:n]
                if en == "scalar":
                    P.op("scalar", lambda e, b8=b8, src_v=src_v, XT=XT: e.copy(out=XT[:, b8 * 8:(b8 + 1) * 8, 0:n], in_=src_v), [pb], [XT])
                else:
                    P.op("vector", lambda e, b8=b8, src_v=src_v, XT=XT: e.tensor_copy(out=XT[:, b8 * 8:(b8 + 1) * 8, 0:n], in_=src_v),
                         [pb], [XT])
        for t4 in range(0, n, 4):
            pg = psG.next()
            for j in range(4):
                P.op("tensor", lambda e, j=j, t4=t4, pg=pg: e.matmul(out=pg[:, j * 128:(j + 1) * 128], lhsT=AT[:, :, t4 + j],
                                                                     rhs=BT[:, :, t4 + j], start=True, stop=True), [AT, BT], [pg])
            en = "scalar" if (t4 // 4) % 2 == 0 else "vector"
            src_v = pg[:, :].rearrange("p (t i) -> p i t", i=128)
            if en == "scalar":
                P.op("scalar", lambda e, t4=t4, src_v=src_v: e.copy(out=Gall[:, :, t4:t4 + 4], in_=src_v), [pg], [Gall])
            else:
                P.op("vector", lambda e, t4=t4, src_v=src_v: e.tensor_copy(out=Gall[:, :, t4:t4 + 4], in_=src_v), [pg], [Gall])
        P.dma("gpsimd", G_s[ti, :, :, :], Gall[:, :, :], [Gall], [G_s])
    P.barrier()
    C.release(mk)
    mk = C.mark()
    psAct = RR([C.ps([128, 512], F32, "psAct") for _ in range(2)])
    psUT = RR([C.ps([128, 1024], BF16, "psUT") for _ in range(2)])
    psO2 = RR([C.ps([128, 512], F32, "psO2") for _ in range(3)])
    h2T = C.sb([128, 32, 512], BF16, "h2T")
    oacc = [C.sb([128, D], F32, "oacc") for _ in range(4)]
    stg = RR([C.sb([128, D], F32, "stg") for _ in range(2)])
    ubfs = RR([C.sb([128, D], BF16, "ubf") for _ in range(2)])
    uTs = RR([C.sb([128, 32, 128], BF16, "uT") for _ in range(2)])
    vbfs = [C.sb([128, D], BF16, "vbf") for _ in range(4)]
    WTs = [C.sb([128, 512], BF16, "WT") for _ in range(4)]
    Gcs = RR([C.sb([128, 4, 128], BF16, "Gc") for _ in range(2)])
    gts = RR([C.sb([128, 512], BF16, "gt") for _ in range(2)])
    _s0 = stg.items[0].t
    xbs = RR([Buf(_s0[:, 0:512]), Buf(_s0[:, 512:1024])])
    gbs = RR([Buf(_s0[:, 1024:1536]), Buf(_s0[:, 1536:2048])])
    fbs = RR([Buf(_s0[:, 2048:2560]), Buf(_s0[:, 2560:3072])])
    ybs = RR([Buf(_s0[:, 3072:3584]), Buf(_s0[:, 3584:4096])])
    jk = C.sb([128, 512], BF16, "jk")
    ss8 = C.sb([128, 10], F32, "ss8")
    u3 = pu_in[:, :].rearrange("(a b) d -> a b d", b=128)
    v3 = pv_in[:, :].rearrange("(a b) d -> a b d", b=128)
    BLOCKS = [[0, 1, 2, 3], [4, 5, 6, 7], [8, 9, 10, 11], [12, 13, 14], [15, 16]]
    NBLK = int(os.environ.get("MK_NBLK", "5"))
    NCH = int(os.environ.get("MK_NCH", "128"))
    for blk in BLOCKS[:NBLK]:
        tiles = [(ti, ti * 128, 128 if ti < NQT else 64) for ti in blk]
        tok0 = tiles[0][1]
        T = sum(n for (_, _, n) in tiles)
        P.dma("sync", h2T[:, :, 0:T], h2T_s[:, :, tok0:tok0 + T], [h2T_s], [h2T])
        for g0 in range(0, NCH, 4):
            for k_ in range(4):
                c = g0 + k_
                st = stg.next()
                P.dma("sync", st[:, :], u3[:, c, :], [pu_in], [st])
                ubf = ubfs.next()
                uT = uTs.next()
                P.op("vector", lambda e, st=st, ubf=ubf: e.tensor_copy(out=ubf[:, :], in_=st[:, :]), [st], [ubf])
                for b8 in range(4):
                    pb = psUT.next()
                    for j in range(8):
                        ch = b8 * 8 + j
                        P.op("tensor", lambda e, j=j, ch=ch, pb=pb, ubf=ubf: e.transpose(out=pb[:, j * 128:(j + 1) * 128],
                                                                                         in_=ubf[:, ch * 128:(ch + 1) * 128],
                                                                                         identity=identb[:, :]), [ubf, identb], [pb])
                    P.op("scalar", lambda e, b8=b8, pb=pb, uT=uT: e.copy(
                        out=uT[:, b8 * 8:(b8 + 1) * 8, :], in_=pb[:, :].rearrange("p (c t) -> p c t", t=128)), [pb], [uT])
                Gc = Gcs.next()
                P.dma("sync", Gc[:, 0:len(tiles), :], G_s[blk[0]:blk[0] + len(tiles), :, c, :].rearrange("a p t -> p a t"), [G_s], [Gc])
                pa = psAct.next()
                for ch in range(32):
                    P.op("tensor", lambda e, ch=ch, pa=pa, uT=uT: e.matmul(out=pa[:, 0:T], lhsT=uT[:, ch, :], rhs=h2T[:, ch, 0:T],
                                                                    start=(ch == 0), stop=(ch == 31)), [uT, h2T], [pa])
                gt = gts.next()
                P.op("scalar", lambda e, pa=pa, gt=gt: e.activation(out=gt[:, 0:T], in_=pa[:, 0:T], func=AF.Gelu_apprx_tanh), [pa], [gt])
                WT = WTs[k_]
                P.op("gpsimd", lambda e, gt=gt, WT=WT, Gc=Gc: e.tensor_tensor(
                    out=WT[:, 0:T], in0=gt[:, 0:T], in1=Gc[:, :, :].rearrange("p a t -> p (a t)")[:, 0:T], op=ALU.mult),
                    [gt, Gc], [WT])
                st = stg.next()
                P.dma("sync", st[:, :], v3[:, c, :], [pv_in], [st])
                P.op("scalar", lambda e, st=st, k_=k_: e.copy(out=vbfs[k_][:, :], in_=st[:, :]), [st], [vbfs[k_]])
            off = 0
            for si, (ti, tk0, n) in enumerate(tiles):
                for db in range(8):
                    po = psO2.next()
                    for k_ in range(4):
                        P.op("tensor", lambda e, k_=k_, po=po, off=off, n=n, db=db: e.matmul(
                            out=po[0:n, :], lhsT=WTs[k_][:, off:off + n], rhs=vbfs[k_][:, db * 512:(db + 1) * 512],
                            start=(k_ == 0), stop=(k_ == 3)), [WTs[k_], vbfs[k_]], [po])
                    if g0 == 0:
                        P.op("vector", lambda e, po=po, si=si, n=n, db=db: e.tensor_copy(
                            out=oacc[si][0:n, db * 512:(db + 1) * 512], in_=po[0:n, :]), [po], [oacc[si]])
                    else:
                        P.op("vector", lambda e, po=po, si=si, n=n, db=db: e.tensor_tensor(
                            out=oacc[si][0:n, db * 512:(db + 1) * 512], in0=po[0:n, :], in1=oacc[si][0:n, db * 512:(db + 1) * 512],
                            op=ALU.add), [po, oacc[si]], [oacc[si]])
                off += n
        P.barrier()
        for si, (ti, tk0, n) in enumerate(tiles):
            prompt = ti < NQT
            P.op("vector", lambda e: e.memset(ss8[:, :], 0.0), [], [ss8])
            for db in range(8):
                xb = xbs.next()
                gb = gbs.next()
                sl = slice(db * 512, (db + 1) * 512)
                P.dma("sync", xb[0:n, :], x1_s[tk0:tk0 + n, sl], [x1_s], [xb])
                P.dma("sync", gb[0:n, :], ga_s[1 if prompt else 3, 0:n, sl], [ga_s], [gb])
                P.op("vector", lambda e, si=si, sl=sl, gb=gb, n=n: e.tensor_tensor(out=oacc[si][0:n, sl], in0=oacc[si][0:n, sl],
                                                                                   in1=gb[0:n, :], op=ALU.mult), [oacc[si], gb], [oacc[si]])
                P.op("gpsimd", lambda e, si=si, sl=sl, xb=xb, n=n: e.tensor_tensor(out=oacc[si][0:n, sl], in0=oacc[si][0:n, sl],
                                                                                   in1=xb[0:n, :], op=ALU.add), [oacc[si], xb], [oacc[si]])
                P.op("scalar", lambda e, si=si, sl=sl, n=n, db=db: e.activation(out=jk[0:n, :], in_=oacc[si][0:n, sl], func=AF.Square,
                                                                                accum_out=ss8[0:n, db:db + 1]), [oacc[si]], [jk, ss8])
            P.op("vector", lambda e, n=n: e.reduce_sum(out=ss8[0:n, 8:9], in_=ss8[0:n, 0:8], axis=AX.X), [ss8], [ss8])
            P.op("vector", lambda e, n=n: e.tensor_scalar(out=ss8[0:n, 9:10], in0=ss8[0:n, 8:9], scalar1=1.0 / D, scalar2=EPS,
                                                          op0=ALU.mult, op1=ALU.add), [ss8], [ss8])
            P.op("scalar", lambda e, n=n: e.activation(out=ss8[0:n, 9:10], in_=ss8[0:n, 9:10], func=AF.Sqrt), [ss8], [ss8])
            P.op("vector", lambda e, n=n: e.reciprocal(out=ss8[0:n, 9:10], in_=ss8[0:n, 9:10]), [ss8], [ss8])
            for db in range(8):
                fb = fbs.next()
                yb = ybs.next()
                sl = slice(db * 512, (db + 1) * 512)
                P.dma("sync", fb[0:n, :], gfinB[0:n, sl], [gfinB], [fb])
                P.op("vector", lambda e, si=si, sl=sl, fb=fb, yb=yb, n=n: e.scalar_tensor_tensor(
                    out=yb[0:n, :], in0=oacc[si][0:n, sl], scalar=ss8[0:n, 9:10], in1=fb[0:n, :], op0=ALU.mult, op1=ALU.mult),
                    [oacc[si], ss8, fb], [yb])
                P.dma("gpsimd", o_y[tk0:tk0 + n, sl], yb[0:n, :], [yb], [o_y])
        P.barrier()
    P.barrier()
    C.release(mk)
    P.barrier()
    P.close()
    return nc


_ROPE_CACHE = {}


def _rope_tabs(pos):
    half = 64
    inv = (np.float32(10000.0) ** (-np.arange(half, dtype=np.float32) / np.float32(half))).astype(np.float32)
    ang = pos.astype(np.float32)[:, None] * inv[None, :]
    cos, sin = np.cos(ang).astype(np.float32), np.sin(ang).astype(np.float32)
    return (np.ascontiguousarray(np.concatenate([cos, cos], axis=1)),
            np.ascontiguousarray(np.concatenate([-sin, sin], axis=1)))


def _fp(v):
    return np.ascontiguousarray(v.reshape(-1, 128).T)


def kernel(x_prompt, x_sample, cache_dsa_k, cache_dsa_v, cache_idx_k, cache_diff_k, cache_diff_v,
           c_prompt, c_sample, w_ada, b_ada, g_norm_mix, g_norm_ffn, w_in,
           diff_lambda_q1, diff_lambda_k1, diff_lambda_q2, diff_lambda_k2, g_diff_subln, w_out,
           peer_w_query, peer_sub_keys, peer_u, peer_v, g_final):
    f = np.float32
    A = lambda a: np.ascontiguousarray(np.asarray(a, dtype=f))
    x_prompt, x_sample = A(x_prompt), A(x_sample)
    nc = build_program()
    cosk, sink = _rope_tabs(np.arange(SEQ))
    ident = np.eye(128, dtype=f)
    in_maps = []
    own = {}
    shared = {
        "w_ada": A(w_ada[0]), "badaT": _fp(A(b_ada[0])), "gmixT": _fp(A(g_norm_mix[0])), "gffnT": _fp(A(g_norm_ffn[0])),
        "gfinB": np.ascontiguousarray(np.broadcast_to(A(g_final)[None, :], (128, D))),
        "w_in": A(w_in[0]), "cosk": cosk, "sink": sink, "ident": ident,
        "kcl": np.ascontiguousarray(np.broadcast_to((np.arange(512) // 64).astype(f)[None, :], (128, 512))),
        "lamv": np.ascontiguousarray(np.broadcast_to(np.stack([A(diff_lambda_q1[0]), A(diff_lambda_k1[0]),
                                                                A(diff_lambda_q2[0]), A(diff_lambda_k2[0])])[None], (128, 4, 128))),
        "gsub": np.ascontiguousarray(np.broadcast_to(A(g_diff_subln[0])[None, :], (128, 256))),
        "w_out": A(w_out[0]), "pwq": A(peer_w_query[0]), "pkeys": A(peer_sub_keys[0]), "pu": A(peer_u[0]), "pv": A(peer_v[0]),
        "iota": np.ascontiguousarray(np.broadcast_to(np.arange(128, dtype=f)[None, :], (128, 128))),
    }
    for c in range(8):
        b, hh = divmod(c, 2)
        js = [own_tile_index(hh, i) for i in range(NQT)]
        own[c] = js
        xq = np.concatenate([x_prompt[b, j * 128:(j + 1) * 128] for j in js] + [x_sample[2 * c], x_sample[2 * c + 1]], axis=0)
        posq = np.concatenate([np.arange(j * 128, (j + 1) * 128) for j in js] + [np.arange(PAST, LS), np.arange(PAST, LS)])
        cosq, sinq = _rope_tabs(posq)
        qrel = np.zeros((NTOK, 1), f)
        for i, j in enumerate(js):
            nch = nchunks_for(i)
            base64 = 2 * (nch - min(4, nch))
            qrel[i * 128:(i + 1) * 128, 0] = (np.arange(j * 128, (j + 1) * 128) // 64) - base64
        cv = np.stack([A(c_prompt)[b], A(c_sample)[2 * c], A(c_sample)[2 * c + 1]], axis=0)
        cT = np.ascontiguousarray(cv.reshape(3, 32, 128).transpose(2, 1, 0).reshape(128, 96))
        m = dict(shared)
        m.update({
            "xk": x_prompt[b], "xq": np.ascontiguousarray(xq), "cT": cT, "cosq": cosq, "sinq": sinq, "qrel": qrel,
            "c_k": A(cache_dsa_k[0, 2 * c:2 * c + 2]).reshape(2, PAST, 512),
            "c_v": A(cache_dsa_v[0, 2 * c:2 * c + 2]).reshape(2, PAST, 512),
            "c_ki": A(cache_idx_k[0, 2 * c:2 * c + 2]).reshape(2, PAST, 128),
            "c_dk": A(cache_diff_k[0, 2 * c:2 * c + 2]).reshape(2, PAST, 2048),
            "c_dv": A(cache_diff_v[0, 2 * c:2 * c + 2]).reshape(2, PAST, 2048),
        })
        in_maps.append(m)
    names = set(_INPUT_NAMES)
    in_maps = [{k: v for k, v in m.items() if k in names} for m in in_maps]
    ncore = int(os.environ.get("MK_CORES", "8"))
    res = run_bass_kernel_spmd(nc, in_maps[:ncore], core_ids=list(range(ncore)))
    R = list(res.results)
    while len(R) < 8:
        R.append(R[0])
    global _LAST
    _LAST = R
    y_prompt = np.zeros((4, SEQ, D), f)
    y_sample = np.zeros((16, 32, D), f)
    for c in range(8):
        b = c // 2
        oy = R[c]["o_y"]
        for i, j in enumerate(own[c]):
            y_prompt[b, j * 128:(j + 1) * 128] = oy[i * 128:(i + 1) * 128]
        y_sample[2 * c] = oy[NQT * 128:NQT * 128 + 32]
        y_sample[2 * c + 1] = oy[NQT * 128 + 32:NQT * 128 + 64]

    def pk(name, shp):
        return np.stack([R[2 * b][name] for b in range(4)], axis=0).reshape((1, 4, SEQ) + shp)

    def sk(name, shp):
        return np.concatenate([R[c][name].reshape((2, 32) + shp) for c in range(8)], axis=0)[None]

    return (y_prompt, y_sample,
            pk("o_kp", (4, 128)), pk("o_vp", (4, 128)), pk("o_kip", (128,)), pk("o_dkp", (8, 2, 128)), pk("o_dvp", (8, 256)),
            sk("o_ks", (4, 128)), sk("o_vs", (4, 128)), sk("o_kis", (128,)), sk("o_dks", (8, 2, 128)), sk("o_dvs", (8, 256)))
```

```python
import os
import numpy as np
import concourse.bass as bass
import concourse.mybir as mybir
from concourse.bass_utils import run_bass_kernel_spmd

F32 = mybir.dt.float32
BF16 = mybir.dt.bfloat16
U32 = mybir.dt.uint32
ALU = mybir.AluOpType
AF = mybir.ActivationFunctionType
AX = mybir.AxisListType

ENGS = ("sync", "scalar", "vector", "gpsimd", "tensor")
NDS = 6

D = 4096
NQT = 16
NTOK = NQT * 128 + 64
SEQ = 4096
PAST = 1024
LS = PAST + 32
C_Q, C_K, C_V, C_QI, C_KI, C_WI, C_DQ, C_DK, C_DV, C_END = 0, 2048, 2560, 3072, 5120, 5248, 5264, 7312, 9360, 11408
EPS = 1e-6
NEG = -1.0e30
LAM_INIT = 0.8 - 0.6


class Buf:
    __slots__ = ("t", "lw", "rd")

    def __init__(self, t):
        self.t = t
        self.lw = set()
        self.rd = set()

    def __getitem__(self, k):
        return self.t[k]


class Prog:
    def __init__(self, nc):
        self.nc = nc
        self.eng = {"sync": nc.sync, "scalar": nc.scalar, "vector": nc.vector,
                    "gpsimd": nc.gpsimd, "tensor": nc.tensor}
        self.sem = {}
        self.cnt = {e: 0 for e in ENGS}
        self.known = {e: {} for e in ENGS}
        self.dsem = {}
        self.dcnt = {e: 0 for e in ENGS}
        self._stack = []
        self.ninstr = 0
        self.nwaits = 0

    def open(self):
        nc = self.nc
        for e in ENGS:
            cm = nc.semaphore("s_" + e)
            self.sem[e] = cm.__enter__()
            self._stack.append(cm)
        for e in ("sync", "gpsimd", "scalar"):
            self.dsem[e] = []
            for i in range(NDS):
                cm = nc.semaphore("d_%s%d" % (e, i))
                self.dsem[e].append(cm.__enter__())
                self._stack.append(cm)

    def close(self):
        for cm in reversed(self._stack):
            cm.__exit__(None, None, None)

    def _wait(self, engine, ev):
        if ev[0] == "c":
            _, p, v = ev
            if p == "tensor" and engine == "tensor":
                return
            key = ("c", p)
            if self.known[engine].get(key, 0) >= v:
                return
            self.eng[engine].wait_ge(self.sem[p], v)
        else:
            _, q, slot, v = ev
            key = ("d", q, slot)
            if self.known[engine].get(key, 0) >= v:
                return
            self.eng[engine].wait_ge(self.dsem[q][slot], v)
        self.known[engine][key] = v
        self.nwaits += 1

    def _deps(self, engine, reads, writes):
        deps = set()
        for b in reads:
            deps |= b.lw
        for b in writes:
            deps |= b.lw
            deps |= b.rd
        best = {}
        for ev in deps:
            key = ev[:-1]
            if key not in best or best[key][-1] < ev[-1]:
                best[key] = ev
        for key in sorted(best, key=str):
            self._wait(engine, best[key])

    def _commit(self, ev, reads, writes):
        for b in writes:
            b.lw = {ev}
            b.rd = set()
        for b in reads:
            if b not in writes:
                b.rd.add(ev)

    def op(self, engine, fn, reads=(), writes=()):
        self._deps(engine, reads, writes)
        ins = fn(self.eng[engine])
        self.cnt[engine] += 1
        ins.then_inc(self.sem[engine], 1)
        self._commit(("c", engine, self.cnt[engine]), reads, writes)
        self.ninstr += 1

    def dma(self, engine, out, in_, reads=(), writes=(), **kw):
        self._deps(engine, reads, writes)
        n = self.dcnt[engine]
        self.dcnt[engine] += 1
        slot = n % NDS
        ins = self.eng[engine].dma_start(out=out, in_=in_, **kw)
        ins.then_inc(self.dsem[engine][slot], 16)
        self._commit(("d", engine, slot, 16 * (n // NDS + 1)), reads, writes)
        self.ninstr += 1

    def barrier(self):
        evs = []
        for p in ENGS:
            if self.cnt[p] > 0:
                evs.append(("c", p, self.cnt[p]))
        for q in self.dsem:
            n = self.dcnt[q]
            for slot in range(NDS):
                k = (n - slot + NDS - 1) // NDS if n > slot else 0
                if k > 0:
                    evs.append(("d", q, slot, 16 * k))
        for e in ENGS:
            for ev in evs:
                self._wait(e, ev)


class Ctx:
    def __init__(self, nc):
        self.nc = nc
        self.stack = []
        self.k = 0

    def sb(self, shape, dt, name=None):
        self.k += 1
        cm = self.nc.sbuf_tensor("%s_%d" % (name or "t", self.k), list(shape), dt)
        t = cm.__enter__()
        self.stack.append(cm)
        return Buf(t)

    def ps(self, shape, dt, name=None):
        self.k += 1
        cm = self.nc.psum_tensor("%s_%d" % (name or "p", self.k), list(shape), dt)
        t = cm.__enter__()
        self.stack.append(cm)
        return Buf(t)

    def mark(self):
        return len(self.stack)

    def release(self, mark):
        while len(self.stack) > mark:
            self.stack.pop().__exit__(None, None, None)


class RR:
    def __init__(self, items):
        self.items = list(items)
        self.i = 0

    def next(self):
        x = self.items[self.i % len(self.items)]
        self.i += 1
        return x


def own_tile_index(hh, i):
    g, o = divmod(i, 2)
    if hh == 0:
        return 4 * g + (0 if o == 0 else 3)
    return 4 * g + (1 if o == 0 else 2)


def nchunks_for(i):
    g, o = divmod(i, 2)
    return 4 * g + (2 if o == 0 else 4)


STAGE = int(os.environ.get("MK_STAGE", "9"))
_INPUT_NAMES = []
_LAST = None


def build_program():
    del _INPUT_NAMES[:]
    nc = bass.Bass("TRN2", target_bir_lowering=False)
    P = Prog(nc)
    P.open()
    C = Ctx(nc)
    T = {}

    def din(name, shape, dt=F32):
        _INPUT_NAMES.append(name)
        T[name] = Buf(nc.dram_tensor(name, list(shape), dt, kind="ExternalInput").ap())
        return T[name]

    def dout(name, shape, dt=F32):
        T[name] = Buf(nc.dram_tensor(name, list(shape), dt, kind="ExternalOutput").ap())
        return T[name]

    def dscr(name, shape, dt=BF16):
        kind = "ExternalOutput" if os.environ.get("MK_DEBUG") else "Internal"
        T[name] = Buf(nc.dram_tensor(name, list(shape), dt, kind=kind).ap())
        return T[name]

    xk = din("xk", [SEQ, D])
    xq = din("xq", [NTOK, D])
    cT = din("cT", [128, 96])
    w_ada = din("w_ada", [D, 6 * D])
    badaT = din("badaT", [128, 192])
    gmixT = din("gmixT", [128, 32])
    gffnT = din("gffnT", [128, 32])
    gfinB = din("gfinB", [128, D])
    w_in = din("w_in", [D, C_END])
    cosk = din("cosk", [SEQ, 128])
    sink = din("sink", [SEQ, 128])
    cosq = din("cosq", [NTOK, 128])
    sinq = din("sinq", [NTOK, 128])
    ident_in = din("ident", [128, 128])
    c_k = din("c_k", [2, PAST, 512])
    c_v = din("c_v", [2, PAST, 512])
    c_ki = din("c_ki", [2, PAST, 128])
    c_dk = din("c_dk", [2, PAST, 2048])
    c_dv = din("c_dv", [2, PAST, 2048])
    kcl_in = din("kcl", [128, 512])
    qrel_in = din("qrel", [NTOK, 1])
    lamv_in = din("lamv", [128, 4, 128])
    gsub_in = din("gsub", [128, 256])
    w_out = din("w_out", [D, D])
    pwq_in = din("pwq", [D, 2048])
    pkeys_in = din("pkeys", [8, 2, 128, 128])
    iota_in = din("iota", [128, 128])
    pu_in = din("pu", [16384, D])
    pv_in = din("pv", [16384, D])
    o_kp = dout("o_kp", [SEQ, 512])
    o_vp = dout("o_vp", [SEQ, 512])
    o_kip = dout("o_kip", [SEQ, 128])
    o_dkp = dout("o_dkp", [SEQ, 2048])
    o_dvp = dout("o_dvp", [SEQ, 2048])
    o_ks = dout("o_ks", [64, 512])
    o_vs = dout("o_vs", [64, 512])
    o_kis = dout("o_kis", [64, 128])
    o_dks = dout("o_dks", [64, 2048])
    o_dvs = dout("o_dvs", [64, 2048])
    o_y = dout("o_y", [NTOK, D])
    kT_s = dscr("kT_s", [4, 128, SEQ])
    v_s = dscr("v_s", [SEQ, 512])
    kiT_s = dscr("kiT_s", [128, SEQ])
    dkT_s = dscr("dkT_s", [16, 128, SEQ])
    dv_s = dscr("dv_s", [SEQ, 2048])
    skT_s = dscr("skT_s", [2, 4, 128, LS])
    sv_s = dscr("sv_s", [2, LS, 512])
    skiT_s = dscr("skiT_s", [2, 128, LS])
    sdkT_s = dscr("sdkT_s", [2, 16, 128, LS])
    sdv_s = dscr("sdv_s", [2, LS, 2048])
    qT_s = dscr("qT_s", [16, 128, NTOK])
    qiT_s = dscr("qiT_s", [16, 128, NTOK])
    dqT_s = dscr("dqT_s", [16, 128, NTOK])
    wi_s = dscr("wi_s", [NTOK, 16], F32)
    ga_s = dscr("ga_s", [4, 128, D], F32)

    attnT_s = dscr("attnT_s", [128, 32, NTOK])
    x1_s = dscr("x1_s", [NTOK, D], F32)
    h2T_s = dscr("h2T_s", [128, 32, NTOK])
    s_s = dscr("s_s", [NTOK, 2048], F32)
    G_s = dscr("G_s", [17, 128, 128, 128])
    uT_c = dscr("uT_c", [128, 128, D])
    v_c = dscr("v_c", [128, 128, D])
    ident = C.sb([128, 128], F32, "ident")
    identb = C.sb([128, 128], BF16, "identb")
    modT = C.sb([128, 192, 3], F32, "modT")
    A1 = C.sb([128, 32, 3], F32, "A1")
    A2 = C.sb([128, 32, 3], F32, "A2")
    P.dma("sync", ident[:, :], ident_in[:, :], [ident_in], [ident])
    P.op("vector", lambda e: e.tensor_copy(out=identb[:, :], in_=ident[:, :]), [ident], [identb])

    mk = C.mark()
    psF = [C.ps([128, 512], F32, "psF") for _ in range(6)]
    psB = [C.ps([128, 1024], BF16, "psB") for _ in range(2)]
    scT = C.sb([128, 96], F32, "scT")
    wst = [C.sb([128, 12288], F32, "wst") for _ in range(2)]
    pm = [psF[0], psF[1]]
    bada = C.sb([128, 192], F32, "bada")
    gm = C.sb([128, 32], F32, "gm")
    gf = C.sb([128, 32], F32, "gf")
    P.dma("sync", scT[:, :], cT[:, :], [cT], [scT])
    P.dma("sync", bada[:, :], badaT[:, :], [badaT], [bada])
    P.dma("sync", gm[:, :], gmixT[:, :], [gmixT], [gm])
    P.dma("sync", gf[:, :], gffnT[:, :], [gffnT], [gf])
    sg = C.sb([128, 96], F32, "sg")
    P.op("scalar", lambda e: e.activation(out=sg[:, :], in_=scT[:, :], func=AF.Exp, scale=-1.0), [scT], [sg])
    P.op("vector", lambda e: e.tensor_scalar(out=sg[:, :], in0=sg[:, :], scalar1=1.0, scalar2=None, op0=ALU.add), [sg], [sg])
    P.op("vector", lambda e: e.reciprocal(out=sg[:, :], in_=sg[:, :]), [sg], [sg])
    P.op("vector", lambda e: e.tensor_tensor(out=scT[:, :], in0=scT[:, :], in1=sg[:, :], op=ALU.mult), [scT, sg], [scT])
    k = 0
    for cblk in range(64):
        buf = wst[k % 2]
        q = "sync" if k % 2 == 0 else "gpsimd"
        k += 1
        P.dma(q, buf[:, 0:32 * 384].rearrange("p (c w) -> p c w", w=384),
              w_ada[:, cblk * 384:(cblk + 1) * 384].rearrange("(c p) w -> p c w", p=128), [w_ada], [buf])
        for c3 in range(3):
            cb = cblk * 3 + c3
            half, cbl = divmod(cb, 96)
            for dch in range(32):
                P.op("tensor", lambda e, buf=buf, c3=c3, cbl=cbl, half=half, dch=dch: e.matmul(
                    out=pm[half][:, cbl * 3:cbl * 3 + 3], lhsT=buf[:, dch * 384 + c3 * 128:dch * 384 + (c3 + 1) * 128],
                    rhs=scT[:, dch * 3:dch * 3 + 3], start=(dch == 0), stop=(dch == 31)),
                    [buf, scT], [pm[half]])
    for half in range(2):
        P.op("vector", lambda e, half=half: e.tensor_tensor(
            out=modT[:, half * 96:(half + 1) * 96, :],
            in0=pm[half][:, 0:288].rearrange("p (c r) -> p c r", r=3),
            in1=bada[:, half * 96:(half + 1) * 96].unsqueeze(2).to_broadcast([128, 96, 3]), op=ALU.add),
            [pm[half], bada], [modT])
    for (Ax, base, g) in ((A1, 32, gm), (A2, 128, gf)):
        P.op("vector", lambda e, Ax=Ax, base=base: e.tensor_scalar(
            out=Ax[:, :, :], in0=modT[:, base:base + 32, :], scalar1=1.0, scalar2=None, op0=ALU.add), [modT], [Ax])
        P.op("vector", lambda e, Ax=Ax, g=g: e.tensor_tensor(
            out=Ax[:, :, :], in0=Ax[:, :, :], in1=g[:, :].unsqueeze(2).to_broadcast([128, 32, 3]), op=ALU.mult),
            [Ax, g], [Ax])
    Dm = [C.sb([128, D], F32, "Dm") for _ in range(2)]
    L = C.sb([128, 3, 128], F32, "L")
    gat = [C.sb([128, 512], F32, "gat") for _ in range(2)]
    P.op("vector", lambda e: e.memset(L[:, 0, :], 1.0), [], [L])
    P.op("vector", lambda e: e.memset(L[:, 1:3, :], 0.0), [L], [L])
    P.op("vector", lambda e: e.memset(L[:, 1, 0:32], 1.0), [L], [L])
    P.op("vector", lambda e: e.memset(L[:, 2, 32:64], 1.0), [L], [L])
    eng2 = RR(["vector", "gpsimd"])
    kk = 0
    for which, base in ((0, 64), (1, 160)):
        for grp in range(2):
            rs = [0] if grp == 0 else [1, 2]
            for ri, r in enumerate(rs):
                for ch in range(32):
                    P.op(eng2.next(), lambda e, ri=ri, r=r, ch=ch, base=base: e.tensor_scalar(
                        out=Dm[ri][:, ch * 128:(ch + 1) * 128], in0=ident[:, :],
                        scalar1=modT[:, base + ch, r:r + 1], scalar2=None, op0=ALU.mult),
                        [ident, modT], [Dm[ri]])
            for blk in range(8):
                pb = psF[2 + kk % 2]
                gt = gat[kk % 2]
                kk += 1
                for ri, r in enumerate(rs):
                    P.op("tensor", lambda e, ri=ri, r=r, blk=blk, pb=pb, n=len(rs): e.matmul(
                        out=pb[:, :], lhsT=L[:, r, :], rhs=Dm[ri][:, blk * 512:(blk + 1) * 512],
                        start=(ri == 0), stop=(ri == n - 1)), [L, Dm[ri]], [pb])
                P.op("scalar", lambda e, pb=pb, gt=gt: e.copy(out=gt[:, :], in_=pb[:, :]), [pb], [gt])
                P.dma("gpsimd", ga_s[grp * 2 + which, :, blk * 512:(blk + 1) * 512], gt[:, :], [gt], [ga_s])
    P.barrier()
    C.release(mk)

    mk = C.mark()
    psF = [C.ps([128, 512], F32, "psF") for _ in range(6)]
    psB = [C.ps([128, 1024], BF16, "psB") for _ in range(2)]
    xts = RR([C.sb([128, D], F32, "xt") for _ in range(2)])
    sst = RR([C.sb([128, 2], F32, "ss") for _ in range(2)])
    hTs = [C.sb([128, 32, 128], BF16, "hT") for _ in range(9)]
    wsts = RR([C.sb([128, 4, 512], F32, "wst") for _ in range(2)])
    wbfs = RR([C.sb([128, 32, 512], BF16, "wbf") for _ in range(2)])
    ofs = RR([C.sb([128, 512], F32, "of") for _ in range(2)])
    tmps = RR([C.sb([128, 512], F32, "tmp") for _ in range(2)])
    obs = RR([C.sb([128, 512], BF16, "ob") for _ in range(3)])
    tbs = RR([C.sb([128, 4, 128], BF16, "tb") for _ in range(3)])
    tabs = RR([(C.sb([128, 128], F32, "cs"), C.sb([128, 128], F32, "sn")) for _ in range(3)])
    ps_proj = RR(psF[0:3])
    ps_xT = RR(psF[3:6])
    ps_hT = RR(psB)
    evac = RR(["vector", "scalar"])
    castq = RR(["gpsimd"])

    def weight_stream(specs):
        nxt = load_weights(*specs[0]) if specs else None
        for i_ in range(len(specs)):
            cur = nxt
            nxt = load_weights(*specs[i_ + 1]) if i_ + 1 < len(specs) else None
            yield cur

    def load_norm_T(src, row0, n, hT, prompt, Ax, shbase):
        xt = xts.next()
        ss = sst.next()
        P.dma("sync", xt[0:n, :], src[row0:row0 + n, :], [src], [xt])
        P.op("vector", lambda e: e.memset(ss[:, :], 0.0), [], [ss])
        P.op("scalar", lambda e: e.activation(out=hT[0:n, :, :].rearrange("p c t -> p (c t)"), in_=xt[0:n, :], func=AF.Square,
                                              accum_out=ss[0:n, 0:1]), [xt], [hT, ss])
        P.op("vector", lambda e: e.tensor_scalar(out=ss[0:n, 1:2], in0=ss[0:n, 0:1], scalar1=1.0 / D, scalar2=EPS,
                                                 op0=ALU.mult, op1=ALU.add), [ss], [ss])
        P.op("scalar", lambda e: e.activation(out=ss[0:n, 1:2], in_=ss[0:n, 1:2], func=AF.Sqrt), [ss], [ss])
        P.op("vector", lambda e: e.reciprocal(out=ss[0:n, 1:2], in_=ss[0:n, 1:2]), [ss], [ss])
        P.op("scalar", lambda e: e.activation(out=xt[0:n, :], in_=xt[0:n, :], func=AF.Copy, scale=ss[0:n, 1:2]), [xt, ss], [xt])
        for c4 in range(8):
            pb = ps_xT.next()
            for j in range(4):
                ch = c4 * 4 + j
                P.op("tensor", lambda e, ch=ch, j=j, pb=pb: e.transpose(
                    out=pb[:, j * 128:j * 128 + n], in_=xt[0:n, ch * 128:(ch + 1) * 128], identity=ident[0:n, 0:n]),
                    [xt, ident], [pb])
            for j in range(4):
                ch = c4 * 4 + j
                segs = [(0, n, 0)] if prompt else [(0, 32, 1), (32, 64, 2)]
                for (a, b_, r) in segs:
                    en = evac.next()
                    if en == "vector":
                        P.op("vector", lambda e, ch=ch, j=j, pb=pb, a=a, b_=b_, r=r: e.tensor_scalar(
                            out=hT[:, ch, a:b_], in0=pb[:, j * 128 + a:j * 128 + b_], scalar1=Ax[:, ch, r:r + 1],
                            scalar2=modT[:, shbase + ch, r:r + 1], op0=ALU.mult, op1=ALU.add),
                            [pb, Ax, modT], [hT])
                    else:
                        P.op("scalar", lambda e, ch=ch, j=j, pb=pb, a=a, b_=b_, r=r: e.activation(
                            out=hT[:, ch, a:b_], in_=pb[:, j * 128 + a:j * 128 + b_], func=AF.Identity,
                            scale=Ax[:, ch, r:r + 1], bias=modT[:, shbase + ch, r:r + 1]),
                            [pb, Ax, modT], [hT])
        return xt

    def load_weights(wsrc, c0, W):
        wbf = wbfs.next()
        for p4 in range(8):
            ws = wsts.next()
            P.dma("sync", ws[:, :, 0:W],
                  wsrc[p4 * 512:(p4 + 1) * 512, c0:c0 + W].rearrange("(c p) w -> p c w", p=128), [wsrc], [ws])
            en = castq.next()
            if en == "scalar":
                P.op("scalar", lambda e, ws=ws, p4=p4: e.copy(out=wbf[:, p4 * 4:(p4 + 1) * 4, 0:W], in_=ws[:, :, 0:W]),
                     [ws], [wbf])
            else:
                P.op("gpsimd", lambda e, ws=ws, p4=p4: e.tensor_copy(out=wbf[:, p4 * 4:(p4 + 1) * 4, 0:W], in_=ws[:, :, 0:W]),
                     [ws], [wbf])
        return wbf

    def project(hT, n, wbf, W):
        pb = ps_proj.next()
        for ch in range(32):
            P.op("tensor", lambda e, ch=ch: e.matmul(out=pb[0:n, 0:W], lhsT=hT[:, ch, 0:n], rhs=wbf[:, ch, 0:W],
                                                     start=(ch == 0), stop=(ch == 31)), [hT, wbf], [pb])
        return pb

    def epilogue(src, sbuf, n, W, rope=None, f32dst=None, tokdst=None, featdst=None):
        H = max(W // 128, 1)
        of = ofs.next()
        if rope is not None:
            cs, sn = rope
            tmp = tmps.next()
            s3 = src.rearrange("p (h d) -> p h d", d=128)
            o3 = of[0:n, 0:W].rearrange("p (h d) -> p h d", d=128)
            t3 = tmp[0:n, 0:W].rearrange("p (h d) -> p h d", d=128)
            P.op("vector", lambda e: e.tensor_tensor(out=o3, in0=s3, in1=cs[0:n, :].unsqueeze(1).to_broadcast([n, H, 128]),
                                                     op=ALU.mult), [sbuf, cs], [of])
            P.op("vector", lambda e: e.tensor_tensor(out=t3[:, :, 0:64], in0=s3[:, :, 64:128],
                                                     in1=sn[0:n, 0:64].unsqueeze(1).to_broadcast([n, H, 64]),
                                                     op=ALU.mult), [sbuf, sn], [tmp])
            P.op("vector", lambda e: e.tensor_tensor(out=t3[:, :, 64:128], in0=s3[:, :, 0:64],
                                                     in1=sn[0:n, 64:128].unsqueeze(1).to_broadcast([n, H, 64]),
                                                     op=ALU.mult), [sbuf, sn], [tmp])
            P.op("vector", lambda e: e.tensor_tensor(out=of[0:n, 0:W], in0=of[0:n, 0:W], in1=tmp[0:n, 0:W], op=ALU.add),
                 [of, tmp], [of])
            cur, curb = of[0:n, 0:W], of
        elif f32dst is not None:
            P.op("scalar", lambda e: e.copy(out=of[0:n, 0:W], in_=src), [sbuf], [of])
            cur, curb = of[0:n, 0:W], of
        else:
            cur, curb = src, sbuf
        if f32dst is not None:
            P.dma("scalar", f32dst[1], cur, [curb], [f32dst[0]])
        if tokdst is None and featdst is None:
            return
        ob = obs.next()
        P.op("scalar", lambda e: e.copy(out=ob[0:n, 0:W], in_=cur), [curb], [ob])
        if tokdst is not None:
            for (db, dap, r0, r1) in tokdst:
                P.dma("scalar", dap, ob[r0:r1, 0:W], [ob], [db])
        if featdst is not None:
            pb = ps_hT.next()
            tb = tbs.next()
            for h in range(H):
                P.op("tensor", lambda e, h=h: e.transpose(out=pb[:, h * 128:h * 128 + n], in_=ob[0:n, h * 128:(h + 1) * 128],
                                                          identity=identb[0:n, 0:n]), [ob, identb], [pb])
            en = evac.next()
            pv = pb[:, 0:H * 128].rearrange("p (h t) -> p h t", t=128)[:, :, 0:n]
            if en == "vector":
                P.op("vector", lambda e: e.tensor_copy(out=tb[:, 0:H, 0:n], in_=pv), [pb], [tb])
            else:
                P.op("scalar", lambda e: e.copy(out=tb[:, 0:H, 0:n], in_=pv), [pb], [tb])
            for (db, apfn, c0, c1) in featdst:
                P.dma("scalar", apfn(H), tb[:, 0:H, c0:c1], [tb], [db])

    def featT(buf, h0, t0, ncols, lead=None):
        def fn(H):
            base = buf.t if lead is None else buf.t[lead]
            return base[h0:h0 + H, :, t0:t0 + ncols].rearrange("h d t -> d h t")
        return fn

    def featT1(buf, t0, ncols, lead=None):
        def fn(H):
            base = buf.t if lead is None else buf.t[lead]
            return base[:, t0:t0 + ncols].unsqueeze(1)
        return fn

    KBLOCKS = [("k", C_K, 512, 0), ("v", C_V, 512, 0), ("ki", C_KI, 144, 0)] + \
              [("dk", C_DK + 512 * i, 512, 4 * i) for i in range(4)] + [("dv", C_DV + 512 * i, 512, 4 * i) for i in range(4)]
    QBLOCKS = [("q", C_Q + 512 * i, 512, 4 * i) for i in range(4)] + [("qi", C_QI + 512 * i, 512, 4 * i) for i in range(4)] + \
              [("ki", C_KI, 144, 0)] + [("dq", C_DQ + 512 * i, 512, 4 * i) for i in range(4)]

    def load_tabs(cb, sb_, row0, n):
        cs, sn = tabs.next()
        P.dma("sync", cs[0:n, :], cb[row0:row0 + n, :], [cb], [cs])
        P.dma("sync", sn[0:n, :], sb_[row0:row0 + n, :], [sb_], [sn])
        return cs, sn

    def k_epilogue(kind, pb, n, W, h0, tb_, prompt, tok0):
        src = pb[0:n, 0:W]
        if kind == "ki":
            src = pb[0:n, 0:128]
            W = 128
        ci = C_DK if kind == "dk" else C_DV
        if prompt:
            if kind == "k":
                epilogue(src, pb, n, W, rope=tb_, f32dst=(o_kp, o_kp[tok0:tok0 + n, :]),
                         featdst=[(kT_s, featT(kT_s, 0, tok0, n), 0, n)])
            elif kind == "v":
                epilogue(src, pb, n, W, f32dst=(o_vp, o_vp[tok0:tok0 + n, :]), tokdst=[(v_s, v_s[tok0:tok0 + n, :], 0, n)])
            elif kind == "ki":
                epilogue(src, pb, n, W, rope=tb_, f32dst=(o_kip, o_kip[tok0:tok0 + n, :]),
                         featdst=[(kiT_s, featT1(kiT_s, tok0, n), 0, n)])
            elif kind == "dk":
                epilogue(src, pb, n, W, rope=tb_, f32dst=(o_dkp, o_dkp[tok0:tok0 + n, h0 * 128:h0 * 128 + W]),
                         featdst=[(dkT_s, featT(dkT_s, h0, tok0, n), 0, n)])
            elif kind == "dv":
                epilogue(src, pb, n, W, f32dst=(o_dvp, o_dvp[tok0:tok0 + n, h0 * 128:h0 * 128 + W]),
                         tokdst=[(dv_s, dv_s[tok0:tok0 + n, h0 * 128:h0 * 128 + W], 0, n)])
        else:
            if kind == "k":
                epilogue(src, pb, n, W, rope=tb_, f32dst=(o_ks, o_ks[:, :]),
                         featdst=[(skT_s, featT(skT_s, 0, PAST, 32, lead=s), 32 * s, 32 * s + 32) for s in range(2)])
            elif kind == "v":
                epilogue(src, pb, n, W, f32dst=(o_vs, o_vs[:, :]),
                         tokdst=[(sv_s, sv_s[s, PAST:LS, :], 32 * s, 32 * s + 32) for s in range(2)])
            elif kind == "ki":
                epilogue(src, pb, n, W, rope=tb_, f32dst=(o_kis, o_kis[:, :]),
                         featdst=[(skiT_s, featT1(skiT_s, PAST, 32, lead=s), 32 * s, 32 * s + 32) for s in range(2)])
            elif kind == "dk":
                epilogue(src, pb, n, W, rope=tb_, f32dst=(o_dks, o_dks[:, h0 * 128:h0 * 128 + W]),
                         featdst=[(sdkT_s, featT(sdkT_s, h0, PAST, 32, lead=s), 32 * s, 32 * s + 32) for s in range(2)])
            elif kind == "dv":
                epilogue(src, pb, n, W, f32dst=(o_dvs, o_dvs[:, h0 * 128:h0 * 128 + W]),
                         tokdst=[(sdv_s, sdv_s[s, PAST:LS, h0 * 128:h0 * 128 + W], 32 * s, 32 * s + 32) for s in range(2)])

    def q_epilogue(kind, pb, n, W, h0, tb_, tok0):
        if kind == "ki":
            P.op("scalar", lambda e: e.copy(out=wit[0:n, :], in_=pb[0:n, 128:144]), [pb], [wit])
            P.dma("gpsimd", wi_s[tok0:tok0 + n, :], wit[0:n, :], [wit], [wi_s])
            return
        dst = {"q": qT_s, "qi": qiT_s, "dq": dqT_s}[kind]
        epilogue(pb[0:n, 0:W], pb, n, W, rope=tb_, featdst=[(dst, featT(dst, h0, tok0, n), 0, n)])

    wit = C.sb([128, 16], F32, "wit")

    NKT = int(os.environ.get("MK_NKT", "32"))
    for g0 in range(0, NKT, 8):
        tiles = list(range(g0, min(g0 + 8, NKT)))
        tabl = {}
        for si, j in enumerate(tiles):
            load_norm_T(xk, j * 128, 128, hTs[si], True, A1, 0)
        for (kind, c0, W, h0), wbf in zip(KBLOCKS, weight_stream([(w_in, b_[1], b_[2]) for b_ in KBLOCKS])):
            pend = None
            for si, j in enumerate(tiles):
                pb = project(hTs[si], 128, wbf, W)
                if pend is not None:
                    pend()
                tb_ = load_tabs(cosk, sink, j * 128, 128) if kind in ("k", "ki", "dk") else None
                pend = (lambda kind=kind, pb=pb, W=W, h0=h0, tb_=tb_, j=j: k_epilogue(kind, pb, 128, W, h0, tb_, True, j * 128))
            pend()
    NQ = int(os.environ.get("MK_NQT", str(NQT)))
    for grp in range(2):
        tiles = list(range(grp * 8, min(grp * 8 + 8, NQ)))
        slots = [(si, i, 128, True) for si, i in enumerate(tiles)]
        if grp == 1:
            slots.append((8, NQT, 64, False))
        for (si, i, n, prompt) in slots:
            load_norm_T(xq, i * 128, n, hTs[si], prompt, A1, 0)
        for (kind, c0, W, h0), wbf in zip(QBLOCKS, weight_stream([(w_in, b_[1], b_[2]) for b_ in QBLOCKS])):
            pend = None
            for (si, i, n, prompt) in slots:
                pb = project(hTs[si], n, wbf, W)
                if pend is not None:
                    pend()
                tb_ = load_tabs(cosq, sinq, i * 128, n) if kind != "ki" or not prompt else None

                def pend(kind=kind, pb=pb, n=n, W=W, h0=h0, tb_=tb_, i=i, prompt=prompt):
                    q_epilogue(kind, pb, n, W, h0, tb_, i * 128)
                    if kind == "ki" and not prompt:
                        k_epilogue("ki", pb, n, W, 0, tb_, False, 0)
            pend()
        if grp == 1:
            for (kind, c0, W, h0) in KBLOCKS:
                if kind == "ki":
                    continue
                wbf = load_weights(w_in, c0, W)
                pb = project(hTs[8], 64, wbf, W)
                tb_ = load_tabs(cosq, sinq, NQT * 128, 64) if kind in ("k", "dk") else None
                k_epilogue(kind, pb, 64, W, h0, tb_, False, 0)
    if STAGE >= 2:
        cin = RR([C.sb([128, 512], F32, "cin") for _ in range(1)])
        for s in range(2):
            for t in range(PAST // 128):
                t0 = t * 128

                def ing(srcb, sap, W, **kw):
                    cb = cin.next()
                    P.dma("sync", cb[:, 0:W], sap, [srcb], [cb])
                    epilogue(cb[:, 0:W], cb, 128, W, **kw)
                ing(c_k, c_k[s, t0:t0 + 128, :], 512, featdst=[(skT_s, featT(skT_s, 0, t0, 128, lead=s), 0, 128)])
                ing(c_v, c_v[s, t0:t0 + 128, :], 512, tokdst=[(sv_s, sv_s[s, t0:t0 + 128, :], 0, 128)])
                ing(c_ki, c_ki[s, t0:t0 + 128, :], 128, featdst=[(skiT_s, featT1(skiT_s, t0, 128, lead=s), 0, 128)])
                for hb in range(4):
                    ing(c_dk, c_dk[s, t0:t0 + 128, hb * 512:(hb + 1) * 512], 512,
                        featdst=[(sdkT_s, featT(sdkT_s, hb * 4, t0, 128, lead=s), 0, 128)])
                    ing(c_dv, c_dv[s, t0:t0 + 128, hb * 512:(hb + 1) * 512], 512,
                        tokdst=[(sdv_s, sdv_s[s, t0:t0 + 128, hb * 512:(hb + 1) * 512], 0, 128)])
    P.barrier()
    C.release(mk)
    mk = C.mark()
    psI = RR([C.ps([128, 512], F32, "psI") for _ in range(3)])
    psO = [C.ps([128, 512], F32, "psO") for _ in range(4)]
    psT = RR([C.ps([128, 1024], BF16, "psT") for _ in range(1)])
    kcl = C.sb([128, 512], F32, "kcl")
    gsubB = C.sb([128, 256], F32, "gsubB")
    lamv = C.sb([128, 4, 128], F32, "lamv")
    lamt = C.sb([128, 4], F32, "lamt")
    neglam = C.sb([128, 1], F32, "neglam")
    P.dma("sync", kcl[:, :], kcl_in[:, :], [kcl_in], [kcl])
    P.dma("sync", gsubB[:, :], gsub_in[:, :], [gsub_in], [gsubB])
    P.dma("sync", lamv[:, :, :], lamv_in[:, :, :], [lamv_in], [lamv])
    P.op("vector", lambda e: e.tensor_scalar(out=gsubB[:, :], in0=gsubB[:, :], scalar1=1.0 - LAM_INIT, scalar2=None,
                                             op0=ALU.mult), [gsubB], [gsubB])
    P.op("vector", lambda e: e.tensor_tensor(out=lamv[:, 0, :], in0=lamv[:, 0, :], in1=lamv[:, 1, :], op=ALU.mult), [lamv], [lamv])
    P.op("vector", lambda e: e.tensor_tensor(out=lamv[:, 2, :], in0=lamv[:, 2, :], in1=lamv[:, 3, :], op=ALU.mult), [lamv], [lamv])
    P.op("vector", lambda e: e.reduce_sum(out=lamt[:, 0:1], in_=lamv[:, 0, :], axis=AX.X), [lamv], [lamt])
    P.op("vector", lambda e: e.reduce_sum(out=lamt[:, 1:2], in_=lamv[:, 2, :], axis=AX.X), [lamv], [lamt])
    P.op("scalar", lambda e: e.activation(out=lamt[:, 2:4], in_=lamt[:, 0:2], func=AF.Exp), [lamt], [lamt])
    P.op("vector", lambda e: e.tensor_tensor(out=neglam[:, :], in0=lamt[:, 3:4], in1=lamt[:, 2:3], op=ALU.subtract), [lamt], [neglam])
    P.op("vector", lambda e: e.tensor_scalar(out=neglam[:, :], in0=neglam[:, :], scalar1=-LAM_INIT, scalar2=None, op0=ALU.add),
         [neglam], [neglam])

    kiT = C.sb([128, SEQ], BF16, "kiT")
    qiT = C.sb([128, 16, 128], BF16, "qiT")
    wiq = C.sb([128, 16], F32, "wiq")
    qrel = C.sb([128, 1], F32, "qrel")
    Iw = C.sb([128, SEQ], F32, "Iw")
    wk = C.sb([128, SEQ], F32, "wk")
    rls = RR([C.sb([128, 512], F32, "rl") for _ in range(2)])
    pen = C.sb([128, 512], F32, "pen")
    m8 = C.sb([128, 8], F32, "m8")
    thr = C.sb([128, 1], F32, "thr")
    maskb = C.sb([128, SEQ], BF16, "maskb")
    maskT = C.sb([128, 32, 128], BF16, "maskT")
    visb = C.sb([128, 512], BF16, "visb")
    visT = C.sb([128, 4, 128], BF16, "visT")
    kTs = RR([C.sb([128, SEQ], BF16, "kTg") for _ in range(2)])
    vts = [C.sb([128, 32, 129], BF16, "vt") for _ in range(2)]
    qgs = RR([C.sb([128, 4, 128], BF16, "qg") for _ in range(2)])
    Pts = RR([C.sb([128, 4, 128], BF16, "Pt") for _ in range(3)])
    Pms = RR([C.sb([128, 4, 128], BF16, "Pm") for _ in range(3)])
    dkTs = RR([C.sb([128, 2, SEQ], BF16, "dkT") for _ in range(2)])
    dvts = [C.sb([128, 32, 257], BF16, "dvt") for _ in range(2)]
    dqs = RR([C.sb([128, 2, 128], BF16, "dq") for _ in range(2)])
    rden = C.sb([128, 8], F32, "rden")
    Osb = [C.sb([128, 129], F32, "Osb") for _ in range(4)]
    Od = [C.sb([128, 257], F32, "Od") for _ in range(2)]
    negc = C.sb([128, 2], F32, "negc")
    P.op("vector", lambda e: e.memset(negc[:, 0:1], -1.0), [], [negc])
    P.op("vector", lambda e: e.memset(negc[:, 1:2], -0.5), [negc], [negc])
    tmpd = C.sb([128, 256], F32, "tmpd")
    dof = C.sb([128, 256], F32, "dof")
    junkd = C.sb([128, 256], BF16, "junkd")
    ssd = C.sb([128, 2], F32, "ssd")
    attn = C.sb([128, D], BF16, "attn")
    tbc = RR([C.sb([128, 8, 128], BF16, "tbc") for _ in range(2)])
    for vt in vts:
        P.op("vector", lambda e, vt=vt: e.memset(vt[:, :, 128:129], 1.0), [], [vt])
    for dvt in dvts:
        P.op("vector", lambda e, dvt=dvt: e.memset(dvt[:, :, 256:257], 1.0), [], [dvt])
    vti = [0]
    dvi = [0]

    def transposes_to(srcbuf, src_fn, dstbuf, dst_fn, chunks, nq):
        for b0 in range(0, len(chunks), 8):
            grp = chunks[b0:b0 + 8]
            pb = psT.next()
            for s_, (k0, kl) in enumerate(grp):
                P.op("tensor", lambda e, s_=s_, k0=k0, kl=kl: e.transpose(
                    out=pb[0:kl, s_ * 128:s_ * 128 + nq], in_=src_fn(k0, kl), identity=identb[0:nq, 0:nq]),
                    [srcbuf, identb], [pb])
            full = [x for x in grp if x[1] == 128]
            if full:
                nf = len(full)
                P.op("scalar", lambda e, b0=b0, nf=nf: e.copy(
                    out=dst_fn(b0, nf, 128), in_=pb[:, 0:nf * 128].rearrange("p (c t) -> p c t", t=128)[:, :, 0:nq]),
                    [pb], [dstbuf])
            for s_, (k0, kl) in enumerate(grp):
                if kl != 128:
                    P.op("scalar", lambda e, s_=s_, kl=kl, b0=b0: e.copy(
                        out=dst_fn(b0 + s_, 1, kl), in_=pb[0:kl, s_ * 128:s_ * 128 + nq].unsqueeze(1)), [pb], [dstbuf])

    def geom(nkeys, prompt):
        chunks = [(k0, min(128, nkeys - k0)) for k0 in range(0, nkeys, 128)]
        nchk = len(chunks)
        nfull = nkeys // 128
        nv = min(4, nchk) if prompt else 0
        return chunks, nchk, nfull, nv

    def idx_topk(tok0, nq, nkeys, prompt, S):
        chunks, nchk, nfull, nv = geom(nkeys, prompt)
        P.dma("sync", qiT[:, :, 0:nq], qiT_s[:, :, tok0:tok0 + nq].rearrange("h d t -> d h t"), [qiT_s], [qiT])
        P.dma("sync", wiq[0:nq, :], wi_s[tok0:tok0 + nq, :], [wi_s], [wiq])
        for kb0 in range(0, nkeys, 512):
            kw = min(512, nkeys - kb0)
            for h in range(16):
                pb = psI.next()
                P.op("tensor", lambda e, h=h, pb=pb: e.matmul(out=pb[0:nq, 0:kw], lhsT=qiT[:, h, 0:nq], rhs=kiT[:, kb0:kb0 + kw],
                                                              start=True, stop=True), [qiT, kiT], [pb])
                rl = rls.next()
                P.op("scalar", lambda e, pb=pb, rl=rl: e.activation(out=rl[0:nq, 0:kw], in_=pb[0:nq, 0:kw], func=AF.Relu), [pb], [rl])
                if h == 0:
                    P.op("vector", lambda e, rl=rl: e.tensor_scalar(out=Iw[0:nq, kb0:kb0 + kw], in0=rl[0:nq, 0:kw],
                                                                    scalar1=wiq[0:nq, 0:1], scalar2=None, op0=ALU.mult),
                         [rl, wiq], [Iw])
                else:
                    P.op("vector", lambda e, rl=rl, h=h: e.scalar_tensor_tensor(
                        out=Iw[0:nq, kb0:kb0 + kw], in0=rl[0:nq, 0:kw], scalar=wiq[0:nq, h:h + 1], in1=Iw[0:nq, kb0:kb0 + kw],
                        op0=ALU.mult, op1=ALU.add), [rl, wiq, Iw], [Iw])
        if prompt:
            v0 = nkeys - nv * 128
            P.dma("sync", qrel[0:nq, :], qrel_in[tok0:tok0 + nq, :], [qrel_in], [qrel])
            P.op("vector", lambda e: e.tensor_scalar(out=pen[0:nq, 0:nv * 128], in0=kcl[0:nq, 0:nv * 128], scalar1=qrel[0:nq, 0:1],
                                                     scalar2=NEG, op0=ALU.is_gt, op1=ALU.mult), [kcl, qrel], [pen])
            P.op("vector", lambda e: e.tensor_tensor(out=Iw[0:nq, v0:nkeys], in0=Iw[0:nq, v0:nkeys], in1=pen[0:nq, 0:nv * 128],
                                                     op=ALU.add), [Iw, pen], [Iw])
            P.op("vector", lambda e: e.tensor_scalar(out=visb[0:nq, 0:nv * 128], in0=kcl[0:nq, 0:nv * 128], scalar1=qrel[0:nq, 0:1],
                                                     scalar2=None, op0=ALU.is_le), [kcl, qrel], [visb])
        if nkeys > 256:
            cur = Iw
            for r in range(32):
                P.op("vector", lambda e, cur=cur: e.max(out=m8[0:nq, :], in_=cur[0:nq, 0:nkeys]), [cur], [m8])
                if r < 31:
                    P.op("vector", lambda e, cur=cur: e.match_replace(out=wk[0:nq, 0:nkeys], in_to_replace=m8[0:nq, :],
                                                                      in_values=cur[0:nq, 0:nkeys], imm_value=-3.0e38),
                         [cur, m8], [wk])
                    cur = wk
            P.op("vector", lambda e: e.tensor_scalar(out=thr[0:nq, :], in0=m8[0:nq, 7:8], scalar1=-1.0e29, scalar2=None,
                                                     op0=ALU.max), [m8], [thr])
        else:
            P.op("vector", lambda e: e.memset(thr[:, :], -1.0e29), [], [thr])
        P.op("vector", lambda e: e.tensor_scalar(out=maskb[0:nq, 0:nkeys], in0=Iw[0:nq, 0:nkeys], scalar1=thr[0:nq, 0:1],
                                                 scalar2=None, op0=ALU.is_ge), [Iw, thr], [maskb])

    def mask_T(tok0, nq, nkeys, prompt, S):
        chunks, nchk, nfull, nv = geom(nkeys, prompt)
        transposes_to(maskb, lambda k0, kl: maskb[0:nq, k0:k0 + kl], maskT,
                      lambda c0, n_, kl: maskT[0:kl, c0:c0 + n_, 0:nq], chunks, nq)
        if prompt:
            transposes_to(visb, lambda k0, kl: visb[0:nq, k0:k0 + kl], visT,
                          lambda c0, n_, kl: visT[0:kl, c0:c0 + n_, 0:nq], [(c * 128, 128) for c in range(nv)], nq)
        P.op("gpsimd", lambda e: e.tensor_scalar(out=maskT[:, 0:nchk, 0:nq], in0=maskT[:, 0:nchk, 0:nq], scalar1=30000.0,
                                                 scalar2=-30000.0, op0=ALU.mult, op1=ALU.add), [maskT], [maskT])

    def attend(tok0, nq, nkeys, prompt, S):
        chunks, nchk, nfull, nv = geom(nkeys, prompt)
        for g in range(4):
            kT = kTs.next()
            vt = vts[vti[0] % 2]
            vti[0] += 1
            qg = qgs.next()
            P.dma("sync", kT[:, 0:nkeys], S["kT"](g), [S["kTb"]], [kT])
            if nfull:
                P.dma("sync", vt[:, 0:nfull, 0:128], S["v"](0, nfull * 128, g).rearrange("(c p) d -> p c d", p=128), [S["vb"]], [vt])
            if nkeys > nfull * 128:
                P.dma("sync", vt[0:nkeys - nfull * 128, nfull, 0:128], S["v"](nfull * 128, nkeys, g), [S["vb"]], [vt])
            P.dma("sync", qg[:, :, 0:nq], qT_s[4 * g:4 * g + 4, :, tok0:tok0 + nq].rearrange("h d t -> d h t"), [qT_s], [qg])

            def s_mm(c):
                k0, kl = chunks[c]
                pb = psI.next()
                P.op("tensor", lambda e: e.matmul(out=pb[0:kl, :].rearrange("p (r t) -> p r t", t=128)[:, :, 0:nq],
                                                  lhsT=kT[:, k0:k0 + kl], rhs=qg[:, :, 0:nq], start=True, stop=False),
                     [kT, qg], [pb])
                P.op("tensor", lambda e: e.matmul(out=pb[0:kl, :].rearrange("p (r t) -> p r t", t=128)[:, :, 0:nq],
                                                  lhsT=identb[0:kl, 0:kl],
                                                  rhs=maskT[0:kl, c, 0:nq].unsqueeze(1).to_broadcast([kl, 4, nq]),
                                                  start=False, stop=True), [identb, maskT], [pb])
                return pb
            pbq = [s_mm(0)] + ([s_mm(1)] if nchk > 1 else [])
            for c, (k0, kl) in enumerate(chunks):
                pb = pbq.pop(0)
                if c + 2 < nchk:
                    pbq.append(s_mm(c + 2))
                Pm = Pms.next()
                P.op("scalar", lambda e, pb=pb, Pm=Pm, kl=kl: e.activation(
                    out=Pm[0:kl, :, 0:nq], in_=pb[0:kl, :].rearrange("p (r t) -> p r t", t=128)[:, :, 0:nq], func=AF.Exp,
                    scale=float(128 ** -0.5)), [pb], [Pm])
                for r in range(4):
                    P.op("tensor", lambda e, r=r, Pm=Pm, kl=kl, c=c: e.matmul(
                        out=psO[r][0:nq, 0:129], lhsT=Pm[0:kl, r, 0:nq], rhs=vt[0:kl, c, 0:129],
                        start=(c == 0), stop=(c == nchk - 1)), [Pm, vt], [psO[r]])
            for r in range(4):
                ob_ = Osb[r]
                col = (4 * g + r) * 128
                P.op("scalar", lambda e, r=r, ob_=ob_: e.copy(out=ob_[0:nq, 0:129], in_=psO[r][0:nq, 0:129]), [psO[r]], [ob_])
                P.op("gpsimd", lambda e, ob_=ob_, r=r: e.tensor_tensor(out=rden[0:nq, r:r + 1], in0=ob_[0:nq, 128:129],
                                                                       in1=negc[0:nq, 0:1], op=ALU.pow), [ob_, negc], [rden])
                P.op("gpsimd", lambda e, ob_=ob_, col=col, r=r: e.tensor_scalar(out=attn[0:nq, col:col + 128], in0=ob_[0:nq, 0:128],
                                                                                scalar1=rden[0:nq, r:r + 1], scalar2=None, op0=ALU.mult),
                     [ob_, rden], [attn])
        for hd in range(8):
            dkT = dkTs.next()
            dvt = dvts[dvi[0] % 2]
            dvi[0] += 1
            dq2 = dqs.next()
            P.dma("sync", dkT[:, :, 0:nkeys], S["dkT"](hd), [S["dkTb"]], [dkT])
            if nfull:
                P.dma("sync", dvt[:, 0:nfull, 0:256], S["dv"](0, nfull * 128, hd).rearrange("(c p) d -> p c d", p=128),
                      [S["dvb"]], [dvt])
            if nkeys > nfull * 128:
                P.dma("sync", dvt[0:nkeys - nfull * 128, nfull, 0:256], S["dv"](nfull * 128, nkeys, hd), [S["dvb"]], [dvt])
            P.dma("sync", dq2[:, :, 0:nq], dqT_s[2 * hd:2 * hd + 2, :, tok0:tok0 + nq].rearrange("m d t -> d m t"), [dqT_s], [dq2])

            def d_mm(c):
                k0, kl = chunks[c]
                pb = psI.next()
                for m in range(2):
                    P.op("tensor", lambda e, m=m: e.matmul(out=pb[0:kl, m * 128:m * 128 + nq], lhsT=dkT[:, m, k0:k0 + kl],
                                                           rhs=dq2[:, m, 0:nq], start=True, stop=True), [dkT, dq2], [pb])
                return pb
            pbq = [d_mm(0)] + ([d_mm(1)] if nchk > 1 else [])
            for c, (k0, kl) in enumerate(chunks):
                pb = pbq.pop(0)
                if c + 2 < nchk:
                    pbq.append(d_mm(c + 2))
                Pt = Pts.next()
                P.op("scalar", lambda e, pb=pb, Pt=Pt, kl=kl: e.activation(
                    out=Pt[0:kl, 0:2, 0:nq], in_=pb[0:kl, 0:256].rearrange("p (r t) -> p r t", t=128)[:, :, 0:nq], func=AF.Exp,
                    scale=float(128 ** -0.5)), [pb], [Pt])
                if prompt and c >= nchk - nv:
                    P.op("gpsimd", lambda e, Pt=Pt, kl=kl, c=c: e.tensor_tensor(
                        out=Pt[0:kl, 0:2, 0:nq], in0=Pt[0:kl, 0:2, 0:nq],
                        in1=visT[0:kl, c - (nchk - nv), 0:nq].unsqueeze(1).to_broadcast([kl, 2, nq]), op=ALU.mult),
                        [Pt, visT], [Pt])
                for m in range(2):
                    P.op("tensor", lambda e, m=m, Pt=Pt, kl=kl, c=c: e.matmul(
                        out=psO[m][0:nq, 0:257], lhsT=Pt[0:kl, m, 0:nq], rhs=dvt[0:kl, c, 0:257],
                        start=(c == 0), stop=(c == nchk - 1)), [Pt, dvt], [psO[m]])
            P.op("scalar", lambda e: e.copy(out=Od[0][0:nq, :], in_=psO[0][0:nq, 0:257]), [psO[0]], [Od[0]])
            P.op("scalar", lambda e: e.copy(out=Od[1][0:nq, :], in_=psO[1][0:nq, 0:257]), [psO[1]], [Od[1]])
            P.op("gpsimd", lambda e: e.tensor_tensor(out=rden[0:nq, 4:5], in0=Od[0][0:nq, 256:257], in1=negc[0:nq, 0:1], op=ALU.pow),
                 [Od[0], negc], [rden])
            P.op("gpsimd", lambda e: e.tensor_tensor(out=rden[0:nq, 5:6], in0=Od[1][0:nq, 256:257], in1=negc[0:nq, 0:1], op=ALU.pow),
                 [Od[1], negc], [rden])
            P.op("gpsimd", lambda e: e.tensor_tensor(out=rden[0:nq, 5:6], in0=rden[0:nq, 5:6], in1=neglam[0:nq, 0:1], op=ALU.mult),
                 [rden, neglam], [rden])
            P.op("gpsimd", lambda e: e.tensor_scalar(out=tmpd[0:nq, :], in0=Od[0][0:nq, 0:256], scalar1=rden[0:nq, 4:5],
                                                     scalar2=None, op0=ALU.mult), [Od[0], rden], [tmpd])
            P.op("gpsimd", lambda e: e.tensor_scalar(out=dof[0:nq, :], in0=Od[1][0:nq, 0:256], scalar1=rden[0:nq, 5:6],
                                                     scalar2=None, op0=ALU.mult), [Od[1], rden], [dof])
            P.op("gpsimd", lambda e: e.tensor_tensor(out=dof[0:nq, :], in0=dof[0:nq, :], in1=tmpd[0:nq, :], op=ALU.add), [dof, tmpd], [dof])
            P.op("gpsimd", lambda e: e.memset(ssd[:, :], 0.0), [], [ssd])
            P.op("scalar", lambda e: e.activation(out=junkd[0:nq, :], in_=dof[0:nq, :], func=AF.Square, accum_out=ssd[0:nq, 0:1]),
                 [dof], [junkd, ssd])
            P.op("gpsimd", lambda e: e.tensor_scalar(out=ssd[0:nq, 1:2], in0=ssd[0:nq, 0:1], scalar1=1.0 / 256, scalar2=EPS,
                                                     op0=ALU.mult, op1=ALU.add), [ssd], [ssd])
            P.op("gpsimd", lambda e: e.tensor_tensor(out=ssd[0:nq, 1:2], in0=ssd[0:nq, 1:2], in1=negc[0:nq, 1:2], op=ALU.pow),
                 [ssd, negc], [ssd])
            col = 2048 + hd * 256
            P.op("gpsimd", lambda e: e.tensor_scalar(out=tmpd[0:nq, :], in0=dof[0:nq, :], scalar1=ssd[0:nq, 1:2], scalar2=None,
                                                     op0=ALU.mult), [dof, ssd], [tmpd])
            P.op("gpsimd", lambda e, col=col: e.tensor_tensor(out=attn[0:nq, col:col + 256], in0=tmpd[0:nq, :], in1=gsubB[0:nq, :],
                                                              op=ALU.mult), [tmpd, gsubB], [attn])
        for c8 in range(4):
            pb = psT.next()
            tb = tbc.next()
            for j in range(8):
                ch = c8 * 8 + j
                P.op("tensor", lambda e, j=j, ch=ch: e.transpose(out=pb[:, j * 128:j * 128 + nq], in_=attn[0:nq, ch * 128:(ch + 1) * 128],
                                                                 identity=identb[0:nq, 0:nq]), [attn, identb], [pb])
            P.op("scalar", lambda e: e.copy(out=tb[:, :, 0:nq], in_=pb[:, :].rearrange("p (c t) -> p c t", t=128)[:, :, 0:nq]),
                 [pb], [tb])
            P.dma("gpsimd", attnT_s[:, c8 * 8:(c8 + 1) * 8, tok0:tok0 + nq], tb[:, :, 0:nq], [tb], [attnT_s])

    NA = int(os.environ.get("MK_NAT", str(NQT)))
    jobs = []
    for i in range(NA):
        nkeys = nchunks_for(i) * 128
        S = {
            "kT": (lambda g, nkeys=nkeys: kT_s[g, :, 0:nkeys]), "kTb": kT_s,
            "v": (lambda a, b_, g: v_s[a:b_, g * 128:(g + 1) * 128]), "vb": v_s,
            "dkT": (lambda hd, nkeys=nkeys: dkT_s[2 * hd:2 * hd + 2, :, 0:nkeys].rearrange("m d t -> d m t")), "dkTb": dkT_s,
            "dv": (lambda a, b_, hd: dv_s[a:b_, hd * 256:(hd + 1) * 256]), "dvb": dv_s,
            "ki": None,
        }
        jobs.append((i * 128, 128, nkeys, True, S))
    for s in range(2):
        S = {
            "kT": (lambda g, s=s: skT_s[s, g, :, :]), "kTb": skT_s,
            "v": (lambda a, b_, g, s=s: sv_s[s, a:b_, g * 128:(g + 1) * 128]), "vb": sv_s,
            "dkT": (lambda hd, s=s: sdkT_s[s, 2 * hd:2 * hd + 2, :, :].rearrange("m d t -> d m t")), "dkTb": sdkT_s,
            "dv": (lambda a, b_, hd, s=s: sdv_s[s, a:b_, hd * 256:(hd + 1) * 256]), "dvb": sdv_s,
            "ki": s,
        }
        jobs.append((NQT * 128 + 32 * s, 32, LS, False, S))

    def stage1(job):
        if job[4]["ki"] is not None:
            s_ = job[4]["ki"]
            P.dma("sync", kiT[:, 0:LS], skiT_s[s_, :, :], [skiT_s], [kiT])
        idx_topk(*job)

    P.dma("sync", kiT[:, :], kiT_s[:, :], [kiT_s], [kiT])
    stage1(jobs[0])
    mask_T(*jobs[0])
    for ji, job in enumerate(jobs):
        if ji + 1 < len(jobs):
            stage1(jobs[ji + 1])
        attend(*job)
        if ji + 1 < len(jobs):
            mask_T(*jobs[ji + 1])
    P.barrier()
    C.release(mk)

    mk = C.mark()
    psF = [C.ps([128, 512], F32, "psF") for _ in range(4)]
    ps_proj = RR(psF[0:3])
    hTs = [C.sb([128, 32, 128], BF16, "hT") for _ in range(9)]
    wsts = RR([C.sb([128, 4, 512], F32, "wst") for _ in range(2)])
    wbfs = RR([C.sb([128, 32, 512], BF16, "wbf") for _ in range(2)])
    gaP = C.sb([128, D], F32, "gaP")
    gaS = C.sb([128, D], F32, "gaS")
    xbs = RR([C.sb([128, 512], F32, "xb") for _ in range(3)])
    tms = RR([C.sb([128, 512], F32, "tm") for _ in range(3)])
    P.dma("sync", gaP[:, :], ga_s[0, :, :], [ga_s], [gaP])
    P.dma("sync", gaS[:, :], ga_s[2, :, :], [ga_s], [gaS])
    for grp in range(2):
        slots = [(si, i, 128, True) for si, i in enumerate(range(grp * 8, grp * 8 + 8))]
        if grp == 1:
            slots.append((8, NQT, 64, False))
        for (si, i, n, prompt) in slots:
            P.dma("sync", hTs[si][:, :, 0:n], attnT_s[:, :, i * 128:i * 128 + n], [attnT_s], [hTs[si]])
        for cb, wbf in zip(range(8), weight_stream([(w_out, cb_ * 512, 512) for cb_ in range(8)])):
            for (si, i, n, prompt) in slots:
                pb = project(hTs[si], n, wbf, 512)
                xb = xbs.next()
                tm = tms.next()
                ga = gaP if prompt else gaS
                P.dma("sync", xb[0:n, :], xq[i * 128:i * 128 + n, cb * 512:(cb + 1) * 512], [xq], [xb])
                P.op("vector", lambda e, pb=pb, tm=tm, ga=ga, n=n, cb=cb: e.tensor_tensor(
                    out=tm[0:n, :], in0=pb[0:n, :], in1=ga[0:n, cb * 512:(cb + 1) * 512], op=ALU.mult), [pb, ga], [tm])
                P.op("gpsimd", lambda e, tm=tm, xb=xb, n=n: e.tensor_tensor(out=tm[0:n, :], in0=tm[0:n, :], in1=xb[0:n, :], op=ALU.add),
                     [tm, xb], [tm])
                P.dma("gpsimd", x1_s[i * 128:i * 128 + n, cb * 512:(cb + 1) * 512], tm[0:n, :], [tm], [x1_s])
    P.barrier()
    C.release(mk)
    mk = C.mark()
    psF = [C.ps([128, 512], F32, "psF") for _ in range(6)]
    psB = [C.ps([128, 1024], BF16, "psB") for _ in range(2)]
    ps_proj = RR(psF[0:2])
    ps_xT = RR(psF[2:4])
    ps_s = RR(psF[4:6])
    xts = RR([C.sb([128, D], F32, "xt") for _ in range(1)])
    sst = RR([C.sb([128, 2], F32, "ss") for _ in range(2)])
    hTs = [C.sb([128, 32, 128], BF16, "hT") for _ in range(9)]
    wsts = RR([C.sb([128, 4, 512], F32, "wst") for _ in range(2)])
    wbfs = RR([C.sb([128, 32, 512], BF16, "wbf") for _ in range(2)])
    kraw = C.sb([128, 16, 128], F32, "kraw")
    keysT = C.sb([128, 16, 128], F32, "keysT")
    qsbs = RR([C.sb([128, 512], F32, "qsb") for _ in range(2)])
    qT4s = RR([C.sb([128, 4, 128], F32, "qT4") for _ in range(2)])
    sos = RR([C.sb([128, 512], F32, "so") for _ in range(2)])
    P.dma("sync", kraw[:, :, :], pkeys_in[:, :, :, :].rearrange("h c n d -> n (h c) d"), [pkeys_in], [kraw])
    for b4 in range(4):
        pb = ps_xT.next()
        for j in range(4):
            P.op("tensor", lambda e, j=j, b4=b4: e.transpose(out=pb[:, j * 128:(j + 1) * 128], in_=kraw[:, b4 * 4 + j, :],
                                                             identity=ident[:, :]), [kraw, ident], [pb])
        P.op("vector", lambda e, b4=b4: e.tensor_copy(out=keysT[:, b4 * 4:(b4 + 1) * 4, :],
                                                      in_=pb[:, :].rearrange("p (c t) -> p c t", t=128)), [pb], [keysT])
    for grp in range(2):
        slots = [(si, i, 128, True) for si, i in enumerate(range(grp * 8, grp * 8 + 8))]
        if grp == 1:
            slots.append((8, NQT, 64, False))
        for (si, i, n, prompt) in slots:
            load_norm_T(x1_s, i * 128, n, hTs[si], prompt, A2, 96)
            P.dma("gpsimd", h2T_s[:, :, i * 128:i * 128 + n], hTs[si][:, :, 0:n], [hTs[si]], [h2T_s])
        for cb, wbf in zip(range(4), weight_stream([(pwq_in, cb_ * 512, 512) for cb_ in range(4)])):
            for (si, i, n, prompt) in slots:
                pb = project(hTs[si], n, wbf, 512)
                qsb = qsbs.next()
                P.op("scalar", lambda e, pb=pb, qsb=qsb, n=n: e.copy(out=qsb[0:n, :], in_=pb[0:n, :]), [pb], [qsb])
                pt = ps_xT.next()
                for j in range(4):
                    P.op("tensor", lambda e, j=j, n=n, qsb=qsb, pt=pt: e.transpose(
                        out=pt[:, j * 128:j * 128 + n], in_=qsb[0:n, j * 128:(j + 1) * 128], identity=ident[0:n, 0:n]),
                        [qsb, ident], [pt])
                qT4 = qT4s.next()
                P.op("vector", lambda e, pt=pt, qT4=qT4, n=n: e.tensor_copy(
                    out=qT4[:, :, 0:n], in_=pt[:, :].rearrange("p (c t) -> p c t", t=128)[:, :, 0:n]), [pt], [qT4])
                po = ps_s.next()
                for j in range(4):
                    P.op("tensor", lambda e, j=j, n=n, qT4=qT4, po=po, cb=cb: e.matmul(
                        out=po[0:n, j * 128:(j + 1) * 128], lhsT=qT4[:, j, 0:n], rhs=keysT[:, cb * 4 + j, :],
                        start=True, stop=True), [qT4, keysT], [po])
                so = sos.next()
                P.op("scalar", lambda e, po=po, so=so, n=n: e.copy(out=so[0:n, :], in_=po[0:n, :]), [po], [so])
                P.dma("gpsimd", s_s[i * 128:i * 128 + n, cb * 512:(cb + 1) * 512], so[0:n, :], [so], [s_s])
    P.barrier()
    C.release(mk)

    mk = C.mark()
    psA = RR([C.ps([128, 1024], BF16, "psA") for _ in range(3)])
    psG = RR([C.ps([128, 512], F32, "psG") for _ in range(3)])
    sal = C.sb([128, 16, 128], F32, "sal")
    wk2 = C.sb([128, 16, 128], F32, "wk2")
    top = C.sb([128, 16, 16], F32, "top")
    idx = C.sb([128, 8, 16], U32, "idx")
    idxf = C.sb([128, 8, 16], F32, "idxf")
    cand = C.sb([128, 8, 256], F32, "cand")
    cw = C.sb([128, 8, 256], F32, "cw")
    best = C.sb([128, 8, 16], F32, "best")
    eb = C.sb([128, 8, 16], F32, "eb")
    Zs = C.sb([128, 8], F32, "Zs")
    e1z = C.sb([128, 8, 16], F32, "e1z")
    cth = C.sb([128, 8, 16], F32, "cth")
    e2 = C.sb([128, 8, 128], F32, "e2")
    iot = C.sb([128, 128], F32, "iot")
    ABa = C.sb([128, 128, 128], BF16, "ABa")
    ABb = C.sb([128, 128, 128], BF16, "ABb")
    AT = C.sb([128, 128, 128], BF16, "AT")
    BT = C.sb([128, 128, 128], BF16, "BT")
    Gall = C.sb([128, 128, 128], BF16, "Gall")
    P.dma("sync", iot[:, :], iota_in[:, :], [iota_in], [iot])
    NE2 = int(os.environ.get("MK_NE2", "17"))
    for ti in range(NE2):
        n = 128 if ti < NQT else 64
        tok0 = ti * 128
        P.dma("sync", sal[0:n, :, :], s_s[tok0:tok0 + n, :].rearrange("t (k m) -> t k m", m=128), [s_s], [sal])
        for hc in range(16):
            h, c = divmod(hc, 2)
            P.op("vector", lambda e, hc=hc: e.max(out=top[0:n, hc, 0:8], in_=sal[0:n, hc, :]), [sal], [top])
            if c == 0:
                P.op("vector", lambda e, hc=hc, h=h: e.max_index(out=idx[0:n, h, 0:8], in_max=top[0:n, hc, 0:8],
                                                                 in_values=sal[0:n, hc, :]), [sal, top], [idx])
            P.op("vector", lambda e, hc=hc: e.match_replace(out=wk2[0:n, hc, :], in_to_replace=top[0:n, hc, 0:8],
                                                            in_values=sal[0:n, hc, :], imm_value=-3.0e38), [sal, top], [wk2])
            P.op("vector", lambda e, hc=hc: e.max(out=top[0:n, hc, 8:16], in_=wk2[0:n, hc, :]), [wk2], [top])
            if c == 0:
                P.op("vector", lambda e, hc=hc, h=h: e.max_index(out=idx[0:n, h, 8:16], in_max=top[0:n, hc, 8:16],
                                                                 in_values=wk2[0:n, hc, :]), [wk2, top], [idx])
        top4 = top[0:n, :, :].rearrange("p (h c) k -> p h c k", c=2)
        sal4 = sal[0:n, :, :].rearrange("p (h c) k -> p h c k", c=2)
        P.op("vector", lambda e: e.tensor_copy(out=idxf[0:n, :, :], in_=idx[0:n, :, :]), [idx], [idxf])
        P.op("vector", lambda e: e.tensor_tensor(
            out=cand[0:n, :, :].rearrange("p h (i j) -> p h i j", j=16),
            in0=top4[:, :, 0, :].unsqueeze(3).to_broadcast([n, 8, 16, 16]),
            in1=top4[:, :, 1, :].unsqueeze(2).to_broadcast([n, 8, 16, 16]), op=ALU.add), [top], [cand])
        for h in range(8):
            P.op("vector", lambda e, h=h: e.max(out=best[0:n, h, 0:8], in_=cand[0:n, h, :]), [cand], [best])
            P.op("vector", lambda e, h=h: e.match_replace(out=cw[0:n, h, :], in_to_replace=best[0:n, h, 0:8],
                                                          in_values=cand[0:n, h, :], imm_value=-3.0e38), [cand, best], [cw])
            P.op("vector", lambda e, h=h: e.max(out=best[0:n, h, 8:16], in_=cw[0:n, h, :]), [cw], [best])
        P.op("vector", lambda e: e.tensor_tensor(out=eb[0:n, :, :], in0=best[0:n, :, :],
                                                 in1=best[0:n, :, 0:1].to_broadcast([n, 8, 16]), op=ALU.subtract), [best], [eb])
        P.op("scalar", lambda e: e.activation(out=eb[0:n, :, :], in_=eb[0:n, :, :], func=AF.Exp), [eb], [eb])
        P.op("vector", lambda e: e.reduce_sum(out=Zs[0:n, :], in_=eb[0:n, :, :], axis=AX.X), [eb], [Zs])
        P.op("vector", lambda e: e.reciprocal(out=Zs[0:n, :], in_=Zs[0:n, :]), [Zs], [Zs])
        P.op("vector", lambda e: e.tensor_tensor(out=e1z[0:n, :, :], in0=top4[:, :, 0, :],
                                                 in1=top4[:, :, 0, 0:1].to_broadcast([n, 8, 16]), op=ALU.subtract), [top], [e1z])
        P.op("scalar", lambda e: e.activation(out=e1z[0:n, :, :], in_=e1z[0:n, :, :], func=AF.Exp), [e1z], [e1z])
        P.op("vector", lambda e: e.tensor_tensor(out=e1z[0:n, :, :], in0=e1z[0:n, :, :],
                                                 in1=Zs[0:n, :].unsqueeze(2).to_broadcast([n, 8, 16]), op=ALU.mult), [e1z, Zs], [e1z])
        P.op("vector", lambda e: e.tensor_tensor(out=cth[0:n, :, :], in0=best[0:n, :, 15:16].to_broadcast([n, 8, 16]),
                                                 in1=top4[:, :, 0, :], op=ALU.subtract), [best, top], [cth])
        P.op("vector", lambda e: e.tensor_scalar(out=cth[0:n, :, :], in0=cth[0:n, :, :], scalar1=-4.0e-6, scalar2=None,
                                                 op0=ALU.add), [cth], [cth])
        P.op("vector", lambda e: e.tensor_tensor(out=e2[0:n, :, :], in0=sal4[:, :, 1, :],
                                                 in1=top4[:, :, 1, 0:1].to_broadcast([n, 8, 128]), op=ALU.subtract), [sal, top], [e2])
        P.op("scalar", lambda e: e.activation(out=e2[0:n, :, :], in_=e2[0:n, :, :], func=AF.Exp), [e2], [e2])
        for which in range(2):
            XT = AT if which == 0 else BT
            AB = ABa if which == 0 else ABb
            if which == 0:
                P.op("vector", lambda e, AB=AB: e.tensor_tensor(
                    out=AB[0:n, :, :], in0=iot[0:n, :].unsqueeze(1).to_broadcast([n, 128, 128]),
                    in1=idxf[0:n, :, :].rearrange("p h i -> p (h i)").unsqueeze(2).to_broadcast([n, 128, 128]),
                    op=ALU.is_equal), [iot, idxf], [AB])
                P.op("vector", lambda e, AB=AB: e.tensor_tensor(
                    out=AB[0:n, :, :], in0=AB[0:n, :, :],
                    in1=e1z[0:n, :, :].rearrange("p h i -> p (h i)").unsqueeze(2).to_broadcast([n, 128, 128]),
                    op=ALU.mult), [AB, e1z], [AB])
            else:
                P.op("vector", lambda e, AB=AB: e.tensor_tensor(
                    out=AB[0:n, :, :].rearrange("p (h i) k -> p h i k", i=16),
                    in0=sal4[:, :, 1, :].unsqueeze(2).to_broadcast([n, 8, 16, 128]),
                    in1=cth[0:n, :, :].unsqueeze(3).to_broadcast([n, 8, 16, 128]), op=ALU.is_ge), [sal, cth], [AB])
                P.op("gpsimd", lambda e, AB=AB: e.tensor_tensor(
                    out=AB[0:n, :, :].rearrange("p (h i) k -> p h i k", i=16),
                    in0=AB[0:n, :, :].rearrange("p (h i) k -> p h i k", i=16),
                    in1=e2[0:n, :, :].unsqueeze(2).to_broadcast([n, 8, 16, 128]), op=ALU.mult), [AB, e2], [AB])
            for b8 in range(16):
                pb = psA.next()
                for j in range(8):
                    col = b8 * 8 + j
                    P.op("tensor", lambda e, j=j, col=col, pb=pb, AB=AB: e.transpose(
                        out=pb[:, j * 128:j * 128 + n], in_=AB[0:n, :, col], identity=identb[0:n, 0:n]), [AB, identb], [pb])
                en = "scalar" if b8 % 2 == 0 else "vector"
                src_v = pb[:, :].rearrange("p (c t) -> p c t", t=128)[:, :, 0:n]
                if en == "scalar":
                    P.op("scalar", lambda e, b8=b8, src_v=src_v, XT=XT: e.copy(out=XT[:, b8 * 8:(b8 + 1) * 8, 0:n], in_=src_v), [pb], [XT])
                else:
                    P.op("vector", lambda e, b8=b8, src_v=src_v, XT=XT: e.tensor_copy(out=XT[:, b8 * 8:(b8 + 1) * 8, 0:n], in_=src_v),
                         [pb], [XT])
        for t4 in range(0, n, 4):
            pg = psG.next()
            for j in range(4):
                P.op("tensor", lambda e, j=j, t4=t4, pg=pg: e.matmul(out=pg[:, j * 128:(j + 1) * 128], lhsT=AT[:, :, t4 + j],
                                                                     rhs=BT[:, :, t4 + j], start=True, stop=True), [AT, BT], [pg])
            en = "scalar" if (t4 // 4) % 2 == 0 else "vector"
            src_v = pg[:, :].rearrange("p (t i) -> p i t", i=128)
            if en == "scalar":
                P.op("scalar", lambda e, t4=t4, src_v=src_v: e.copy(out=Gall[:, :, t4:t4 + 4], in_=src_v), [pg], [Gall])
            else:
                P.op("vector", lambda e, t4=t4, src_v=src_v: e.tensor_copy(out=Gall[:, :, t4:t4 + 4], in_=src_v), [pg], [Gall])
        P.dma("gpsimd", G_s[ti, :, :, :], Gall[:, :, :], [Gall], [G_s])
    P.barrier()
    C.release(mk)
    mk = C.mark()
    psAct = RR([C.ps([128, 512], F32, "psAct") for _ in range(2)])
    psUT = RR([C.ps([128, 1024], BF16, "psUT") for _ in range(2)])
    psO2 = RR([C.ps([128, 512], F32, "psO2") for _ in range(3)])
    h2T = C.sb([128, 32, 512], BF16, "h2T")
    oacc = [C.sb([128, D], F32, "oacc") for _ in range(4)]
    stg = RR([C.sb([128, D], F32, "stg") for _ in range(2)])
    ubfs = RR([C.sb([128, D], BF16, "ubf") for _ in range(2)])
    uTs = RR([C.sb([128, 32, 128], BF16, "uT") for _ in range(2)])
    vbfs = [C.sb([128, D], BF16, "vbf") for _ in range(4)]
    WTs = [C.sb([128, 512], BF16, "WT") for _ in range(4)]
    Gcs = RR([C.sb([128, 4, 128], BF16, "Gc") for _ in range(2)])
    gts = RR([C.sb([128, 512], BF16, "gt") for _ in range(2)])
    _s0 = stg.items[0].t
    xbs = RR([Buf(_s0[:, 0:512]), Buf(_s0[:, 512:1024])])
    gbs = RR([Buf(_s0[:, 1024:1536]), Buf(_s0[:, 1536:2048])])
    fbs = RR([Buf(_s0[:, 2048:2560]), Buf(_s0[:, 2560:3072])])
    ybs = RR([Buf(_s0[:, 3072:3584]), Buf(_s0[:, 3584:4096])])
    jk = C.sb([128, 512], BF16, "jk")
    ss8 = C.sb([128, 10], F32, "ss8")
    u3 = pu_in[:, :].rearrange("(a b) d -> a b d", b=128)
    v3 = pv_in[:, :].rearrange("(a b) d -> a b d", b=128)
    uT_cb = [Buf(uT_c.t[c_]) for c_ in range(128)]
    v_cb = [Buf(v_c.t[c_]) for c_ in range(128)]
    BLOCKS = [[15, 16], [0, 1, 2, 3], [4, 5, 6, 7], [8, 9, 10, 11], [12, 13, 14]]
    NBLK = int(os.environ.get("MK_NBLK", "5"))
    NCH = int(os.environ.get("MK_NCH", "128"))
    for bi, blk in enumerate(BLOCKS[:NBLK]):
        tiles = [(ti, ti * 128, 128 if ti < NQT else 64) for ti in blk]
        tok0 = tiles[0][1]
        T = sum(n for (_, _, n) in tiles)
        P.dma("sync", h2T[:, :, 0:T], h2T_s[:, :, tok0:tok0 + T], [h2T_s], [h2T])
        for g0 in range(0, NCH, 4):
            for k_ in range(4):
                c = g0 + k_
                uT = uTs.next()
                if bi == 0:
                    st = stg.next()
                    P.dma("sync", st[:, :], u3[:, c, :], [pu_in], [st])
                    ubf = ubfs.next()
                    P.op("vector", lambda e, st=st, ubf=ubf: e.tensor_copy(out=ubf[:, :], in_=st[:, :]), [st], [ubf])
                    for b8 in range(4):
                        pb = psUT.next()
                        for j in range(8):
                            ch = b8 * 8 + j
                            P.op("tensor", lambda e, j=j, ch=ch, pb=pb, ubf=ubf: e.transpose(out=pb[:, j * 128:(j + 1) * 128],
                                                                                             in_=ubf[:, ch * 128:(ch + 1) * 128],
                                                                                             identity=identb[:, :]), [ubf, identb], [pb])
                        P.op("scalar", lambda e, b8=b8, pb=pb, uT=uT: e.copy(
                            out=uT[:, b8 * 8:(b8 + 1) * 8, :], in_=pb[:, :].rearrange("p (c t) -> p c t", t=128)), [pb], [uT])
                    P.dma("gpsimd", uT_cb[c][:, :], uT[:, :, :].rearrange("p c t -> p (c t)"), [uT], [uT_cb[c]])
                else:
                    P.dma("sync", uT[:, :, :].rearrange("p c t -> p (c t)"), uT_cb[c][:, :], [uT_cb[c]], [uT])
                Gc = Gcs.next()
                P.dma("sync", Gc[:, 0:len(tiles), :], G_s[blk[0]:blk[0] + len(tiles), :, c, :].rearrange("a p t -> p a t"), [G_s], [Gc])
                pa = psAct.next()
                for ch in range(32):
                    P.op("tensor", lambda e, ch=ch, pa=pa, uT=uT: e.matmul(out=pa[:, 0:T], lhsT=uT[:, ch, :], rhs=h2T[:, ch, 0:T],
                                                                    start=(ch == 0), stop=(ch == 31)), [uT, h2T], [pa])
                gt = gts.next()
                P.op("scalar", lambda e, pa=pa, gt=gt: e.activation(out=gt[:, 0:T], in_=pa[:, 0:T], func=AF.Gelu_apprx_tanh), [pa], [gt])
                WT = WTs[k_]
                P.op("gpsimd", lambda e, gt=gt, WT=WT, Gc=Gc: e.tensor_tensor(
                    out=WT[:, 0:T], in0=gt[:, 0:T], in1=Gc[:, :, :].rearrange("p a t -> p (a t)")[:, 0:T], op=ALU.mult),
                    [gt, Gc], [WT])
                if bi == 0:
                    st = stg.next()
                    P.dma("sync", st[:, :], v3[:, c, :], [pv_in], [st])
                    P.op("scalar", lambda e, st=st, k_=k_: e.copy(out=vbfs[k_][:, :], in_=st[:, :]), [st], [vbfs[k_]])
                    P.dma("gpsimd", v_cb[c][:, :], vbfs[k_][:, :], [vbfs[k_]], [v_cb[c]])
                else:
                    P.dma("sync", vbfs[k_][:, :], v_cb[c][:, :], [v_cb[c]], [vbfs[k_]])
            off = 0
            for si, (ti, tk0, n) in enumerate(tiles):
                for db in range(8):
                    po = psO2.next()
                    for k_ in range(4):
                        P.op("tensor", lambda e, k_=k_, po=po, off=off, n=n, db=db: e.matmul(
                            out=po[0:n, :], lhsT=WTs[k_][:, off:off + n], rhs=vbfs[k_][:, db * 512:(db + 1) * 512],
                            start=(k_ == 0), stop=(k_ == 3)), [WTs[k_], vbfs[k_]], [po])
                    if g0 == 0:
                        P.op("vector", lambda e, po=po, si=si, n=n, db=db: e.tensor_copy(
                            out=oacc[si][0:n, db * 512:(db + 1) * 512], in_=po[0:n, :]), [po], [oacc[si]])
                    else:
                        P.op("vector", lambda e, po=po, si=si, n=n, db=db: e.tensor_tensor(
                            out=oacc[si][0:n, db * 512:(db + 1) * 512], in0=po[0:n, :], in1=oacc[si][0:n, db * 512:(db + 1) * 512],
                            op=ALU.add), [po, oacc[si]], [oacc[si]])
                off += n
        P.barrier()
        for si, (ti, tk0, n) in enumerate(tiles):
            prompt = ti < NQT
            P.op("vector", lambda e: e.memset(ss8[:, :], 0.0), [], [ss8])
            for db in range(8):
                xb = xbs.next()
                gb = gbs.next()
                sl = slice(db * 512, (db + 1) * 512)
                P.dma("sync", xb[0:n, :], x1_s[tk0:tk0 + n, sl], [x1_s], [xb])
                P.dma("sync", gb[0:n, :], ga_s[1 if prompt else 3, 0:n, sl], [ga_s], [gb])
                P.op("vector", lambda e, si=si, sl=sl, gb=gb, n=n: e.tensor_tensor(out=oacc[si][0:n, sl], in0=oacc[si][0:n, sl],
                                                                                   in1=gb[0:n, :], op=ALU.mult), [oacc[si], gb], [oacc[si]])
                P.op("gpsimd", lambda e, si=si, sl=sl, xb=xb, n=n: e.tensor_tensor(out=oacc[si][0:n, sl], in0=oacc[si][0:n, sl],
                                                                                   in1=xb[0:n, :], op=ALU.add), [oacc[si], xb], [oacc[si]])
                P.op("scalar", lambda e, si=si, sl=sl, n=n, db=db: e.activation(out=jk[0:n, :], in_=oacc[si][0:n, sl], func=AF.Square,
                                                                                accum_out=ss8[0:n, db:db + 1]), [oacc[si]], [jk, ss8])
            P.op("vector", lambda e, n=n: e.reduce_sum(out=ss8[0:n, 8:9], in_=ss8[0:n, 0:8], axis=AX.X), [ss8], [ss8])
            P.op("vector", lambda e, n=n: e.tensor_scalar(out=ss8[0:n, 9:10], in0=ss8[0:n, 8:9], scalar1=1.0 / D, scalar2=EPS,
                                                          op0=ALU.mult, op1=ALU.add), [ss8], [ss8])
            P.op("scalar", lambda e, n=n: e.activation(out=ss8[0:n, 9:10], in_=ss8[0:n, 9:10], func=AF.Sqrt), [ss8], [ss8])
            P.op("vector", lambda e, n=n: e.reciprocal(out=ss8[0:n, 9:10], in_=ss8[0:n, 9:10]), [ss8], [ss8])
            for db in range(8):
                fb = fbs.next()
                yb = ybs.next()
                sl = slice(db * 512, (db + 1) * 512)
                P.dma("sync", fb[0:n, :], gfinB[0:n, sl], [gfinB], [fb])
                P.op("vector", lambda e, si=si, sl=sl, fb=fb, yb=yb, n=n: e.scalar_tensor_tensor(
                    out=yb[0:n, :], in0=oacc[si][0:n, sl], scalar=ss8[0:n, 9:10], in1=fb[0:n, :], op0=ALU.mult, op1=ALU.mult),
                    [oacc[si], ss8, fb], [yb])
                P.dma("gpsimd", o_y[tk0:tk0 + n, sl], yb[0:n, :], [yb], [o_y])
        P.barrier()
    P.barrier()
    C.release(mk)
    P.barrier()
    P.close()
    return nc


_ROPE_CACHE = {}


def _rope_tabs(pos):
    half = 64
    inv = (np.float32(10000.0) ** (-np.arange(half, dtype=np.float32) / np.float32(half))).astype(np.float32)
    ang = pos.astype(np.float32)[:, None] * inv[None, :]
    cos, sin = np.cos(ang).astype(np.float32), np.sin(ang).astype(np.float32)
    return (np.ascontiguousarray(np.concatenate([cos, cos], axis=1)),
            np.ascontiguousarray(np.concatenate([-sin, sin], axis=1)))


def _fp(v):
    return np.ascontiguousarray(v.reshape(-1, 128).T)


def kernel(x_prompt, x_sample, cache_dsa_k, cache_dsa_v, cache_idx_k, cache_diff_k, cache_diff_v,
           c_prompt, c_sample, w_ada, b_ada, g_norm_mix, g_norm_ffn, w_in,
           diff_lambda_q1, diff_lambda_k1, diff_lambda_q2, diff_lambda_k2, g_diff_subln, w_out,
           peer_w_query, peer_sub_keys, peer_u, peer_v, g_final):
    f = np.float32
    A = lambda a: np.ascontiguousarray(np.asarray(a, dtype=f))
    x_prompt, x_sample = A(x_prompt), A(x_sample)
    nc = build_program()
    cosk, sink = _rope_tabs(np.arange(SEQ))
    ident = np.eye(128, dtype=f)
    in_maps = []
    own = {}
    shared = {
        "w_ada": A(w_ada[0]), "badaT": _fp(A(b_ada[0])), "gmixT": _fp(A(g_norm_mix[0])), "gffnT": _fp(A(g_norm_ffn[0])),
        "gfinB": np.ascontiguousarray(np.broadcast_to(A(g_final)[None, :], (128, D))),
        "w_in": A(w_in[0]), "cosk": cosk, "sink": sink, "ident": ident,
        "kcl": np.ascontiguousarray(np.broadcast_to((np.arange(512) // 64).astype(f)[None, :], (128, 512))),
        "lamv": np.ascontiguousarray(np.broadcast_to(np.stack([A(diff_lambda_q1[0]), A(diff_lambda_k1[0]),
                                                                A(diff_lambda_q2[0]), A(diff_lambda_k2[0])])[None], (128, 4, 128))),
        "gsub": np.ascontiguousarray(np.broadcast_to(A(g_diff_subln[0])[None, :], (128, 256))),
        "w_out": A(w_out[0]), "pwq": A(peer_w_query[0]), "pkeys": A(peer_sub_keys[0]), "pu": A(peer_u[0]), "pv": A(peer_v[0]),
        "iota": np.ascontiguousarray(np.broadcast_to(np.arange(128, dtype=f)[None, :], (128, 128))),
    }
    for c in range(8):
        b, hh = divmod(c, 2)
        js = [own_tile_index(hh, i) for i in range(NQT)]
        own[c] = js
        xq = np.concatenate([x_prompt[b, j * 128:(j + 1) * 128] for j in js] + [x_sample[2 * c], x_sample[2 * c + 1]], axis=0)
        posq = np.concatenate([np.arange(j * 128, (j + 1) * 128) for j in js] + [np.arange(PAST, LS), np.arange(PAST, LS)])
        cosq, sinq = _rope_tabs(posq)
        qrel = np.zeros((NTOK, 1), f)
        for i, j in enumerate(js):
            nch = nchunks_for(i)
            base64 = 2 * (nch - min(4, nch))
            qrel[i * 128:(i + 1) * 128, 0] = (np.arange(j * 128, (j + 1) * 128) // 64) - base64
        cv = np.stack([A(c_prompt)[b], A(c_sample)[2 * c], A(c_sample)[2 * c + 1]], axis=0)
        cT = np.ascontiguousarray(cv.reshape(3, 32, 128).transpose(2, 1, 0).reshape(128, 96))
        m = dict(shared)
        m.update({
            "xk": x_prompt[b], "xq": np.ascontiguousarray(xq), "cT": cT, "cosq": cosq, "sinq": sinq, "qrel": qrel,
            "c_k": A(cache_dsa_k[0, 2 * c:2 * c + 2]).reshape(2, PAST, 512),
            "c_v": A(cache_dsa_v[0, 2 * c:2 * c + 2]).reshape(2, PAST, 512),
            "c_ki": A(cache_idx_k[0, 2 * c:2 * c + 2]).reshape(2, PAST, 128),
            "c_dk": A(cache_diff_k[0, 2 * c:2 * c + 2]).reshape(2, PAST, 2048),
            "c_dv": A(cache_diff_v[0, 2 * c:2 * c + 2]).reshape(2, PAST, 2048),
        })
        in_maps.append(m)
    names = set(_INPUT_NAMES)
    in_maps = [{k: v for k, v in m.items() if k in names} for m in in_maps]
    ncore = int(os.environ.get("MK_CORES", "8"))
    res = run_bass_kernel_spmd(nc, in_maps[:ncore], core_ids=list(range(ncore)))
    R = list(res.results)
    while len(R) < 8:
        R.append(R[0])
    global _LAST
    _LAST = R
    y_prompt = np.zeros((4, SEQ, D), f)
    y_sample = np.zeros((16, 32, D), f)
    for c in range(8):
        b = c // 2
        oy = R[c]["o_y"]
        for i, j in enumerate(own[c]):
            y_prompt[b, j * 128:(j + 1) * 128] = oy[i * 128:(i + 1) * 128]
        y_sample[2 * c] = oy[NQT * 128:NQT * 128 + 32]
        y_sample[2 * c + 1] = oy[NQT * 128 + 32:NQT * 128 + 64]

    def pk(name, shp):
        return np.stack([R[2 * b][name] for b in range(4)], axis=0).reshape((1, 4, SEQ) + shp)

    def sk(name, shp):
        return np.concatenate([R[c][name].reshape((2, 32) + shp) for c in range(8)], axis=0)[None]

    return (y_prompt, y_sample,
            pk("o_kp", (4, 128)), pk("o_vp", (4, 128)), pk("o_kip", (128,)), pk("o_dkp", (8, 2, 128)), pk("o_dvp", (8, 256)),
            sk("o_ks", (4, 128)), sk("o_vs", (4, 128)), sk("o_kis", (128,)), sk("o_dks", (8, 2, 128)), sk("o_dvs", (8, 256)))
```

```python
import os
import numpy as np
import concourse.bass as bass
import concourse.mybir as mybir
from concourse.bass_utils import run_bass_kernel_spmd

F32 = mybir.dt.float32
BF16 = mybir.dt.bfloat16
U32 = mybir.dt.uint32
ALU = mybir.AluOpType
AF = mybir.ActivationFunctionType
AX = mybir.AxisListType

ENGS = ("sync", "scalar", "vector", "gpsimd", "tensor")
NDS = 6

D = 4096
NQT = 16
NTOK = NQT * 128 + 64
SEQ = 4096
PAST = 1024
LS = PAST + 32
C_Q, C_K, C_V, C_QI, C_KI, C_WI, C_DQ, C_DK, C_DV, C_END = 0, 2048, 2560, 3072, 5120, 5248, 5264, 7312, 9360, 11408
EPS = 1e-6
NEG = -1.0e30
LAM_INIT = 0.8 - 0.6


class Buf:
    __slots__ = ("t", "lw", "rd")

    def __init__(self, t):
        self.t = t
        self.lw = set()
        self.rd = set()

    def __getitem__(self, k):
        return self.t[k]


class Prog:
    def __init__(self, nc):
        self.nc = nc
        self.eng = {"sync": nc.sync, "scalar": nc.scalar, "vector": nc.vector,
                    "gpsimd": nc.gpsimd, "tensor": nc.tensor}
        self.sem = {}
        self.cnt = {e: 0 for e in ENGS}
        self.known = {e: {} for e in ENGS}
        self.dsem = {}
        self.dcnt = {e: 0 for e in ENGS}
        self._stack = []
        self.ninstr = 0
        self.nwaits = 0

    def open(self):
        nc = self.nc
        for e in ENGS:
            cm = nc.semaphore("s_" + e)
            self.sem[e] = cm.__enter__()
            self._stack.append(cm)
        for e in ("sync", "gpsimd", "scalar"):
            self.dsem[e] = []
            for i in range(NDS):
                cm = nc.semaphore("d_%s%d" % (e, i))
                self.dsem[e].append(cm.__enter__())
                self._stack.append(cm)

    def close(self):
        for cm in reversed(self._stack):
            cm.__exit__(None, None, None)

    def _wait(self, engine, ev):
        if ev[0] == "c":
            _, p, v = ev
            if p == "tensor" and engine == "tensor":
                return
            key = ("c", p)
            if self.known[engine].get(key, 0) >= v:
                return
            self.eng[engine].wait_ge(self.sem[p], v)
        else:
            _, q, slot, v = ev
            key = ("d", q, slot)
            if self.known[engine].get(key, 0) >= v:
                return
            self.eng[engine].wait_ge(self.dsem[q][slot], v)
        self.known[engine][key] = v
        self.nwaits += 1

    def _deps(self, engine, reads, writes):
        deps = set()
        for b in reads:
            deps |= b.lw
        for b in writes:
            deps |= b.lw
            deps |= b.rd
        best = {}
        for ev in deps:
            key = ev[:-1]
            if key not in best or best[key][-1] < ev[-1]:
                best[key] = ev
        for key in sorted(best, key=str):
            self._wait(engine, best[key])

    def _commit(self, ev, reads, writes):
        for b in writes:
            b.lw = {ev}
            b.rd = set()
        for b in reads:
            if b not in writes:
                b.rd.add(ev)

    def op(self, engine, fn, reads=(), writes=()):
        self._deps(engine, reads, writes)
        ins = fn(self.eng[engine])
        self.cnt[engine] += 1
        ins.then_inc(self.sem[engine], 1)
        self._commit(("c", engine, self.cnt[engine]), reads, writes)
        self.ninstr += 1

    def dma(self, engine, out, in_, reads=(), writes=(), **kw):
        self._deps(engine, reads, writes)
        n = self.dcnt[engine]
        self.dcnt[engine] += 1
        slot = n % NDS
        ins = self.eng[engine].dma_start(out=out, in_=in_, **kw)
        ins.then_inc(self.dsem[engine][slot], 16)
        self._commit(("d", engine, slot, 16 * (n // NDS + 1)), reads, writes)
        self.ninstr += 1

    def barrier(self):
        evs = []
        for p in ENGS:
            if self.cnt[p] > 0:
                evs.append(("c", p, self.cnt[p]))
        for q in self.dsem:
            n = self.dcnt[q]
            for slot in range(NDS):
                k = (n - slot + NDS - 1) // NDS if n > slot else 0
                if k > 0:
                    evs.append(("d", q, slot, 16 * k))
        for e in ENGS:
            for ev in evs:
                self._wait(e, ev)


class Ctx:
    def __init__(self, nc):
        self.nc = nc
        self.stack = []
        self.k = 0

    def sb(self, shape, dt, name=None):
        self.k += 1
        cm = self.nc.sbuf_tensor("%s_%d" % (name or "t", self.k), list(shape), dt)
        t = cm.__enter__()
        self.stack.append(cm)
        return Buf(t)

    def ps(self, shape, dt, name=None):
        self.k += 1
        cm = self.nc.psum_tensor("%s_%d" % (name or "p", self.k), list(shape), dt)
        t = cm.__enter__()
        self.stack.append(cm)
        return Buf(t)

    def mark(self):
        return len(self.stack)

    def release(self, mark):
        while len(self.stack) > mark:
            self.stack.pop().__exit__(None, None, None)


class RR:
    def __init__(self, items):
        self.items = list(items)
        self.i = 0

    def next(self):
        x = self.items[self.i % len(self.items)]
        self.i += 1
        return x


def own_tile_index(hh, i):
    g, o = divmod(i, 2)
    if hh == 0:
        return 4 * g + (0 if o == 0 else 3)
    return 4 * g + (1 if o == 0 else 2)


def nchunks_for(i):
    g, o = divmod(i, 2)
    return 4 * g + (2 if o == 0 else 4)


STAGE = int(os.environ.get("MK_STAGE", "9"))
_INPUT_NAMES = []
_LAST = None


def build_program():
    del _INPUT_NAMES[:]
    nc = bass.Bass("TRN2", target_bir_lowering=False)
    P = Prog(nc)
    P.open()
    C = Ctx(nc)
    T = {}

    def din(name, shape, dt=F32):
        _INPUT_NAMES.append(name)
        T[name] = Buf(nc.dram_tensor(name, list(shape), dt, kind="ExternalInput").ap())
        return T[name]

    def dout(name, shape, dt=F32):
        T[name] = Buf(nc.dram_tensor(name, list(shape), dt, kind="ExternalOutput").ap())
        return T[name]

    def dscr(name, shape, dt=BF16):
        kind = "ExternalOutput" if os.environ.get("MK_DEBUG") else "Internal"
        T[name] = Buf(nc.dram_tensor(name, list(shape), dt, kind=kind).ap())
        return T[name]

    xk = din("xk", [SEQ, D])
    xq = din("xq", [NTOK, D])
    cT = din("cT", [128, 96])
    w_ada = din("w_ada", [D, 6 * D])
    badaT = din("badaT", [128, 192])
    gmixT = din("gmixT", [128, 32])
    gffnT = din("gffnT", [128, 32])
    gfinB = din("gfinB", [128, D])
    w_in = din("w_in", [D, C_END])
    cosk = din("cosk", [SEQ, 128])
    sink = din("sink", [SEQ, 128])
    cosq = din("cosq", [NTOK, 128])
    sinq = din("sinq", [NTOK, 128])
    ident_in = din("ident", [128, 128])
    c_k = din("c_k", [2, PAST, 512])
    c_v = din("c_v", [2, PAST, 512])
    c_ki = din("c_ki", [2, PAST, 128])
    c_dk = din("c_dk", [2, PAST, 2048])
    c_dv = din("c_dv", [2, PAST, 2048])
    kcl_in = din("kcl", [128, 512])
    qrel_in = din("qrel", [NTOK, 1])
    lamv_in = din("lamv", [128, 4, 128])
    gsub_in = din("gsub", [128, 256])
    w_out = din("w_out", [D, D])
    pwq_in = din("pwq", [D, 2048])
    pkeys_in = din("pkeys", [8, 2, 128, 128])
    iota_in = din("iota", [128, 128])
    pu_in = din("pu", [16384, D])
    pv_in = din("pv", [16384, D])
    o_kp = dout("o_kp", [SEQ, 512])
    o_vp = dout("o_vp", [SEQ, 512])
    o_kip = dout("o_kip", [SEQ, 128])
    o_dkp = dout("o_dkp", [SEQ, 2048])
    o_dvp = dout("o_dvp", [SEQ, 2048])
    o_ks = dout("o_ks", [64, 512])
    o_vs = dout("o_vs", [64, 512])
    o_kis = dout("o_kis", [64, 128])
    o_dks = dout("o_dks", [64, 2048])
    o_dvs = dout("o_dvs", [64, 2048])
    o_y = dout("o_y", [NTOK, D])
    kT_s = dscr("kT_s", [4, 128, SEQ])
    v_s = dscr("v_s", [SEQ, 512])
    kiT_s = dscr("kiT_s", [128, SEQ])
    dkT_s = dscr("dkT_s", [16, 128, SEQ])
    dv_s = dscr("dv_s", [SEQ, 2048])
    skT_s = dscr("skT_s", [2, 4, 128, LS])
    sv_s = dscr("sv_s", [2, LS, 512])
    skiT_s = dscr("skiT_s", [2, 128, LS])
    sdkT_s = dscr("sdkT_s", [2, 16, 128, LS])
    sdv_s = dscr("sdv_s", [2, LS, 2048])
    qT_s = dscr("qT_s", [16, 128, NTOK])
    qiT_s = dscr("qiT_s", [16, 128, NTOK])
    dqT_s = dscr("dqT_s", [16, 128, NTOK])
    wi_s = dscr("wi_s", [NTOK, 16], F32)
    ga_s = dscr("ga_s", [4, 128, D], F32)

    attnT_s = dscr("attnT_s", [128, 32, NTOK])
    x1_s = dscr("x1_s", [NTOK, D], F32)
    h2T_s = dscr("h2T_s", [128, 32, NTOK])
    s_s = dscr("s_s", [NTOK, 2048], F32)
    G_s = dscr("G_s", [17, 128, 128, 128])
    uT_c = dscr("uT_c", [128, 128, D])
    v_c = dscr("v_c", [128, 128, D])
    ident = C.sb([128, 128], F32, "ident")
    identb = C.sb([128, 128], BF16, "identb")
    modT = C.sb([128, 192, 3], F32, "modT")
    A1 = C.sb([128, 32, 3], F32, "A1")
    A2 = C.sb([128, 32, 3], F32, "A2")
    P.dma("sync", ident[:, :], ident_in[:, :], [ident_in], [ident])
    P.op("vector", lambda e: e.tensor_copy(out=identb[:, :], in_=ident[:, :]), [ident], [identb])

    mk = C.mark()
    psF = [C.ps([128, 512], F32, "psF") for _ in range(6)]
    psB = [C.ps([128, 1024], BF16, "psB") for _ in range(2)]
    scT = C.sb([128, 96], F32, "scT")
    wst = [C.sb([128, 12288], F32, "wst") for _ in range(2)]
    pm = [psF[0], psF[1]]
    bada = C.sb([128, 192], F32, "bada")
    gm = C.sb([128, 32], F32, "gm")
    gf = C.sb([128, 32], F32, "gf")
    P.dma("sync", scT[:, :], cT[:, :], [cT], [scT])
    P.dma("sync", bada[:, :], badaT[:, :], [badaT], [bada])
    P.dma("sync", gm[:, :], gmixT[:, :], [gmixT], [gm])
    P.dma("sync", gf[:, :], gffnT[:, :], [gffnT], [gf])
    sg = C.sb([128, 96], F32, "sg")
    P.op("scalar", lambda e: e.activation(out=sg[:, :], in_=scT[:, :], func=AF.Exp, scale=-1.0), [scT], [sg])
    P.op("vector", lambda e: e.tensor_scalar(out=sg[:, :], in0=sg[:, :], scalar1=1.0, scalar2=None, op0=ALU.add), [sg], [sg])
    P.op("vector", lambda e: e.reciprocal(out=sg[:, :], in_=sg[:, :]), [sg], [sg])
    P.op("vector", lambda e: e.tensor_tensor(out=scT[:, :], in0=scT[:, :], in1=sg[:, :], op=ALU.mult), [scT, sg], [scT])
    scTb = C.sb([128, 96], BF16, "scTb")
    wbA = [C.sb([128, 12288], BF16, "wbA") for _ in range(2)]
    wbS = [[Buf(wb_.t[:, i_ * 4096:(i_ + 1) * 4096]) for i_ in range(3)] for wb_ in wbA]
    P.op("vector", lambda e: e.tensor_copy(out=scTb[:, :], in_=scT[:, :]), [scT], [scTb])
    k = 0
    for cblk in range(64):
        buf = wst[k % 2]
        sls = wbS[k % 2]
        k += 1
        P.dma("sync", buf[:, 0:32 * 384].rearrange("p (c w) -> p c w", w=384),
              w_ada[:, cblk * 384:(cblk + 1) * 384].rearrange("(c p) w -> p c w", p=128), [w_ada], [buf])
        for i_, en in enumerate(("vector", "scalar", "gpsimd")):
            sb_ = sls[i_]
            if en == "scalar":
                P.op(en, lambda e, i_=i_, sb_=sb_, buf=buf: e.copy(out=sb_[:, :], in_=buf[:, i_ * 4096:(i_ + 1) * 4096]), [buf], [sb_])
            else:
                P.op(en, lambda e, i_=i_, sb_=sb_, buf=buf: e.tensor_copy(out=sb_[:, :], in_=buf[:, i_ * 4096:(i_ + 1) * 4096]),
                     [buf], [sb_])
        for c3 in range(3):
            cb = cblk * 3 + c3
            half, cbl = divmod(cb, 96)
            for dch in range(32):
                col = dch * 384 + c3 * 128
                sb_ = sls[col // 4096]
                lc = col % 4096
                P.op("tensor", lambda e, sb_=sb_, lc=lc, cbl=cbl, half=half, dch=dch: e.matmul(
                    out=pm[half][:, cbl * 3:cbl * 3 + 3], lhsT=sb_[:, lc:lc + 128],
                    rhs=scTb[:, dch * 3:dch * 3 + 3], start=(dch == 0), stop=(dch == 31)),
                    [sb_, scTb], [pm[half]])
    for half in range(2):
        P.op("vector", lambda e, half=half: e.tensor_tensor(
            out=modT[:, half * 96:(half + 1) * 96, :],
            in0=pm[half][:, 0:288].rearrange("p (c r) -> p c r", r=3),
            in1=bada[:, half * 96:(half + 1) * 96].unsqueeze(2).to_broadcast([128, 96, 3]), op=ALU.add),
            [pm[half], bada], [modT])
    for (Ax, base, g) in ((A1, 32, gm), (A2, 128, gf)):
        P.op("vector", lambda e, Ax=Ax, base=base: e.tensor_scalar(
            out=Ax[:, :, :], in0=modT[:, base:base + 32, :], scalar1=1.0, scalar2=None, op0=ALU.add), [modT], [Ax])
        P.op("vector", lambda e, Ax=Ax, g=g: e.tensor_tensor(
            out=Ax[:, :, :], in0=Ax[:, :, :], in1=g[:, :].unsqueeze(2).to_broadcast([128, 32, 3]), op=ALU.mult),
            [Ax, g], [Ax])
    Dm = [C.sb([128, D], F32, "Dm") for _ in range(2)]
    L = C.sb([128, 3, 128], F32, "L")
    gat = [C.sb([128, 512], F32, "gat") for _ in range(2)]
    P.op("vector", lambda e: e.memset(L[:, 0, :], 1.0), [], [L])
    P.op("vector", lambda e: e.memset(L[:, 1:3, :], 0.0), [L], [L])
    P.op("vector", lambda e: e.memset(L[:, 1, 0:32], 1.0), [L], [L])
    P.op("vector", lambda e: e.memset(L[:, 2, 32:64], 1.0), [L], [L])
    eng2 = RR(["vector", "gpsimd"])
    kk = 0
    for which, base in ((0, 64), (1, 160)):
        for grp in range(2):
            rs = [0] if grp == 0 else [1, 2]
            for ri, r in enumerate(rs):
                for ch in range(32):
                    P.op(eng2.next(), lambda e, ri=ri, r=r, ch=ch, base=base: e.tensor_scalar(
                        out=Dm[ri][:, ch * 128:(ch + 1) * 128], in0=ident[:, :],
                        scalar1=modT[:, base + ch, r:r + 1], scalar2=None, op0=ALU.mult),
                        [ident, modT], [Dm[ri]])
            for blk in range(8):
                pb = psF[2 + kk % 2]
                gt = gat[kk % 2]
                kk += 1
                for ri, r in enumerate(rs):
                    P.op("tensor", lambda e, ri=ri, r=r, blk=blk, pb=pb, n=len(rs): e.matmul(
                        out=pb[:, :], lhsT=L[:, r, :], rhs=Dm[ri][:, blk * 512:(blk + 1) * 512],
                        start=(ri == 0), stop=(ri == n - 1)), [L, Dm[ri]], [pb])
                P.op("scalar", lambda e, pb=pb, gt=gt: e.copy(out=gt[:, :], in_=pb[:, :]), [pb], [gt])
                P.dma("gpsimd", ga_s[grp * 2 + which, :, blk * 512:(blk + 1) * 512], gt[:, :], [gt], [ga_s])
    P.barrier()
    C.release(mk)

    mk = C.mark()
    psF = [C.ps([128, 512], F32, "psF") for _ in range(6)]
    psB = [C.ps([128, 1024], BF16, "psB") for _ in range(2)]
    xts = RR([C.sb([128, D], F32, "xt") for _ in range(2)])
    sst = RR([C.sb([128, 2], F32, "ss") for _ in range(2)])
    hTs = [C.sb([128, 32, 128], BF16, "hT") for _ in range(9)]
    wsts = RR([C.sb([128, 4, 512], F32, "wst") for _ in range(2)])
    wbfs = RR([C.sb([128, 32, 512], BF16, "wbf") for _ in range(2)])
    ofs = RR([C.sb([128, 512], F32, "of") for _ in range(2)])
    tmps = RR([C.sb([128, 512], F32, "tmp") for _ in range(2)])
    obs = RR([C.sb([128, 512], BF16, "ob") for _ in range(3)])
    tbs = RR([C.sb([128, 4, 128], BF16, "tb") for _ in range(3)])
    tabs = RR([(C.sb([128, 128], F32, "cs"), C.sb([128, 128], F32, "sn")) for _ in range(3)])
    ps_proj = RR(psF[0:3])
    ps_xT = RR(psF[3:6])
    ps_hT = RR(psB)
    evac = RR(["vector", "scalar"])
    castq = RR(["gpsimd"])

    def weight_stream(specs):
        nxt = load_weights(*specs[0]) if specs else None
        for i_ in range(len(specs)):
            cur = nxt
            nxt = load_weights(*specs[i_ + 1]) if i_ + 1 < len(specs) else None
            yield cur

    def load_norm_T(src, row0, n, hT, prompt, Ax, shbase):
        xt = xts.next()
        ss = sst.next()
        P.dma("sync", xt[0:n, :], src[row0:row0 + n, :], [src], [xt])
        P.op("vector", lambda e: e.memset(ss[:, :], 0.0), [], [ss])
        P.op("scalar", lambda e: e.activation(out=hT[0:n, :, :].rearrange("p c t -> p (c t)"), in_=xt[0:n, :], func=AF.Square,
                                              accum_out=ss[0:n, 0:1]), [xt], [hT, ss])
        P.op("vector", lambda e: e.tensor_scalar(out=ss[0:n, 1:2], in0=ss[0:n, 0:1], scalar1=1.0 / D, scalar2=EPS,
                                                 op0=ALU.mult, op1=ALU.add), [ss], [ss])
        P.op("scalar", lambda e: e.activation(out=ss[0:n, 1:2], in_=ss[0:n, 1:2], func=AF.Sqrt), [ss], [ss])
        P.op("vector", lambda e: e.reciprocal(out=ss[0:n, 1:2], in_=ss[0:n, 1:2]), [ss], [ss])
        P.op("scalar", lambda e: e.activation(out=xt[0:n, :], in_=xt[0:n, :], func=AF.Copy, scale=ss[0:n, 1:2]), [xt, ss], [xt])
        for c4 in range(8):
            pb = ps_xT.next()
            for j in range(4):
                ch = c4 * 4 + j
                P.op("tensor", lambda e, ch=ch, j=j, pb=pb: e.transpose(
                    out=pb[:, j * 128:j * 128 + n], in_=xt[0:n, ch * 128:(ch + 1) * 128], identity=ident[0:n, 0:n]),
                    [xt, ident], [pb])
            for j in range(4):
                ch = c4 * 4 + j
                segs = [(0, n, 0)] if prompt else [(0, 32, 1), (32, 64, 2)]
                for (a, b_, r) in segs:
                    en = evac.next()
                    if en == "vector":
                        P.op("vector", lambda e, ch=ch, j=j, pb=pb, a=a, b_=b_, r=r: e.tensor_scalar(
                            out=hT[:, ch, a:b_], in0=pb[:, j * 128 + a:j * 128 + b_], scalar1=Ax[:, ch, r:r + 1],
                            scalar2=modT[:, shbase + ch, r:r + 1], op0=ALU.mult, op1=ALU.add),
                            [pb, Ax, modT], [hT])
                    else:
                        P.op("scalar", lambda e, ch=ch, j=j, pb=pb, a=a, b_=b_, r=r: e.activation(
                            out=hT[:, ch, a:b_], in_=pb[:, j * 128 + a:j * 128 + b_], func=AF.Identity,
                            scale=Ax[:, ch, r:r + 1], bias=modT[:, shbase + ch, r:r + 1]),
                            [pb, Ax, modT], [hT])
        return xt

    def load_weights(wsrc, c0, W):
        wbf = wbfs.next()
        for p4 in range(8):
            ws = wsts.next()
            P.dma("sync", ws[:, :, 0:W],
                  wsrc[p4 * 512:(p4 + 1) * 512, c0:c0 + W].rearrange("(c p) w -> p c w", p=128), [wsrc], [ws])
            en = castq.next()
            if en == "scalar":
                P.op("scalar", lambda e, ws=ws, p4=p4: e.copy(out=wbf[:, p4 * 4:(p4 + 1) * 4, 0:W], in_=ws[:, :, 0:W]),
                     [ws], [wbf])
            else:
                P.op("gpsimd", lambda e, ws=ws, p4=p4: e.tensor_copy(out=wbf[:, p4 * 4:(p4 + 1) * 4, 0:W], in_=ws[:, :, 0:W]),
                     [ws], [wbf])
        return wbf

    def project(hT, n, wbf, W):
        pb = ps_proj.next()
        for ch in range(32):
            P.op("tensor", lambda e, ch=ch: e.matmul(out=pb[0:n, 0:W], lhsT=hT[:, ch, 0:n], rhs=wbf[:, ch, 0:W],
                                                     start=(ch == 0), stop=(ch == 31)), [hT, wbf], [pb])
        return pb

    def epilogue(src, sbuf, n, W, rope=None, f32dst=None, tokdst=None, featdst=None):
        H = max(W // 128, 1)
        of = ofs.next()
        if rope is not None:
            cs, sn = rope
            tmp = tmps.next()
            s3 = src.rearrange("p (h d) -> p h d", d=128)
            o3 = of[0:n, 0:W].rearrange("p (h d) -> p h d", d=128)
            t3 = tmp[0:n, 0:W].rearrange("p (h d) -> p h d", d=128)
            P.op("vector", lambda e: e.tensor_tensor(out=o3, in0=s3, in1=cs[0:n, :].unsqueeze(1).to_broadcast([n, H, 128]),
                                                     op=ALU.mult), [sbuf, cs], [of])
            P.op("vector", lambda e: e.tensor_tensor(out=t3[:, :, 0:64], in0=s3[:, :, 64:128],
                                                     in1=sn[0:n, 0:64].unsqueeze(1).to_broadcast([n, H, 64]),
                                                     op=ALU.mult), [sbuf, sn], [tmp])
            P.op("vector", lambda e: e.tensor_tensor(out=t3[:, :, 64:128], in0=s3[:, :, 0:64],
                                                     in1=sn[0:n, 64:128].unsqueeze(1).to_broadcast([n, H, 64]),
                                                     op=ALU.mult), [sbuf, sn], [tmp])
            P.op("vector", lambda e: e.tensor_tensor(out=of[0:n, 0:W], in0=of[0:n, 0:W], in1=tmp[0:n, 0:W], op=ALU.add),
                 [of, tmp], [of])
            cur, curb = of[0:n, 0:W], of
        elif f32dst is not None:
            P.op("scalar", lambda e: e.copy(out=of[0:n, 0:W], in_=src), [sbuf], [of])
            cur, curb = of[0:n, 0:W], of
        else:
            cur, curb = src, sbuf
        if f32dst is not None:
            P.dma("scalar", f32dst[1], cur, [curb], [f32dst[0]])
        if tokdst is None and featdst is None:
            return
        ob = obs.next()
        P.op("scalar", lambda e: e.copy(out=ob[0:n, 0:W], in_=cur), [curb], [ob])
        if tokdst is not None:
            for (db, dap, r0, r1) in tokdst:
                P.dma("scalar", dap, ob[r0:r1, 0:W], [ob], [db])
        if featdst is not None:
            pb = ps_hT.next()
            tb = tbs.next()
            for h in range(H):
                P.op("tensor", lambda e, h=h: e.transpose(out=pb[:, h * 128:h * 128 + n], in_=ob[0:n, h * 128:(h + 1) * 128],
                                                          identity=identb[0:n, 0:n]), [ob, identb], [pb])
            en = evac.next()
            pv = pb[:, 0:H * 128].rearrange("p (h t) -> p h t", t=128)[:, :, 0:n]
            if en == "vector":
                P.op("vector", lambda e: e.tensor_copy(out=tb[:, 0:H, 0:n], in_=pv), [pb], [tb])
            else:
                P.op("scalar", lambda e: e.copy(out=tb[:, 0:H, 0:n], in_=pv), [pb], [tb])
            for (db, apfn, c0, c1) in featdst:
                P.dma("scalar", apfn(H), tb[:, 0:H, c0:c1], [tb], [db])

    def featT(buf, h0, t0, ncols, lead=None):
        def fn(H):
            base = buf.t if lead is None else buf.t[lead]
            return base[h0:h0 + H, :, t0:t0 + ncols].rearrange("h d t -> d h t")
        return fn

    def featT1(buf, t0, ncols, lead=None):
        def fn(H):
            base = buf.t if lead is None else buf.t[lead]
            return base[:, t0:t0 + ncols].unsqueeze(1)
        return fn

    KBLOCKS = [("k", C_K, 512, 0), ("v", C_V, 512, 0), ("ki", C_KI, 144, 0)] + \
              [("dk", C_DK + 512 * i, 512, 4 * i) for i in range(4)] + [("dv", C_DV + 512 * i, 512, 4 * i) for i in range(4)]
    QBLOCKS = [("q", C_Q + 512 * i, 512, 4 * i) for i in range(4)] + [("qi", C_QI + 512 * i, 512, 4 * i) for i in range(4)] + \
              [("ki", C_KI, 144, 0)] + [("dq", C_DQ + 512 * i, 512, 4 * i) for i in range(4)]

    def load_tabs(cb, sb_, row0, n):
        cs, sn = tabs.next()
        P.dma("sync", cs[0:n, :], cb[row0:row0 + n, :], [cb], [cs])
        P.dma("sync", sn[0:n, :], sb_[row0:row0 + n, :], [sb_], [sn])
        return cs, sn

    def k_epilogue(kind, pb, n, W, h0, tb_, prompt, tok0):
        src = pb[0:n, 0:W]
        if kind == "ki":
            src = pb[0:n, 0:128]
            W = 128
        ci = C_DK if kind == "dk" else C_DV
        if prompt:
            if kind == "k":
                epilogue(src, pb, n, W, rope=tb_, f32dst=(o_kp, o_kp[tok0:tok0 + n, :]),
                         featdst=[(kT_s, featT(kT_s, 0, tok0, n), 0, n)])
            elif kind == "v":
                epilogue(src, pb, n, W, f32dst=(o_vp, o_vp[tok0:tok0 + n, :]), tokdst=[(v_s, v_s[tok0:tok0 + n, :], 0, n)])
            elif kind == "ki":
                epilogue(src, pb, n, W, rope=tb_, f32dst=(o_kip, o_kip[tok0:tok0 + n, :]),
                         featdst=[(kiT_s, featT1(kiT_s, tok0, n), 0, n)])
            elif kind == "dk":
                epilogue(src, pb, n, W, rope=tb_, f32dst=(o_dkp, o_dkp[tok0:tok0 + n, h0 * 128:h0 * 128 + W]),
                         featdst=[(dkT_s, featT(dkT_s, h0, tok0, n), 0, n)])
            elif kind == "dv":
                epilogue(src, pb, n, W, f32dst=(o_dvp, o_dvp[tok0:tok0 + n, h0 * 128:h0 * 128 + W]),
                         tokdst=[(dv_s, dv_s[tok0:tok0 + n, h0 * 128:h0 * 128 + W], 0, n)])
        else:
            if kind == "k":
                epilogue(src, pb, n, W, rope=tb_, f32dst=(o_ks, o_ks[:, :]),
                         featdst=[(skT_s, featT(skT_s, 0, PAST, 32, lead=s), 32 * s, 32 * s + 32) for s in range(2)])
            elif kind == "v":
                epilogue(src, pb, n, W, f32dst=(o_vs, o_vs[:, :]),
                         tokdst=[(sv_s, sv_s[s, PAST:LS, :], 32 * s, 32 * s + 32) for s in range(2)])
            elif kind == "ki":
                epilogue(src, pb, n, W, rope=tb_, f32dst=(o_kis, o_kis[:, :]),
                         featdst=[(skiT_s, featT1(skiT_s, PAST, 32, lead=s), 32 * s, 32 * s + 32) for s in range(2)])
            elif kind == "dk":
                epilogue(src, pb, n, W, rope=tb_, f32dst=(o_dks, o_dks[:, h0 * 128:h0 * 128 + W]),
                         featdst=[(sdkT_s, featT(sdkT_s, h0, PAST, 32, lead=s), 32 * s, 32 * s + 32) for s in range(2)])
            elif kind == "dv":
                epilogue(src, pb, n, W, f32dst=(o_dvs, o_dvs[:, h0 * 128:h0 * 128 + W]),
                         tokdst=[(sdv_s, sdv_s[s, PAST:LS, h0 * 128:h0 * 128 + W], 32 * s, 32 * s + 32) for s in range(2)])

    def q_epilogue(kind, pb, n, W, h0, tb_, tok0):
        if kind == "ki":
            P.op("scalar", lambda e: e.copy(out=wit[0:n, :], in_=pb[0:n, 128:144]), [pb], [wit])
            P.dma("gpsimd", wi_s[tok0:tok0 + n, :], wit[0:n, :], [wit], [wi_s])
            return
        dst = {"q": qT_s, "qi": qiT_s, "dq": dqT_s}[kind]
        epilogue(pb[0:n, 0:W], pb, n, W, rope=tb_, featdst=[(dst, featT(dst, h0, tok0, n), 0, n)])

    wit = C.sb([128, 16], F32, "wit")

    NKT = int(os.environ.get("MK_NKT", "32"))
    for g0 in range(0, NKT, 8):
        tiles = list(range(g0, min(g0 + 8, NKT)))
        tabl = {}
        for si, j in enumerate(tiles):
            load_norm_T(xk, j * 128, 128, hTs[si], True, A1, 0)
        for (kind, c0, W, h0), wbf in zip(KBLOCKS, weight_stream([(w_in, b_[1], b_[2]) for b_ in KBLOCKS])):
            pend = None
            for si, j in enumerate(tiles):
                pb = project(hTs[si], 128, wbf, W)
                if pend is not None:
                    pend()
                tb_ = load_tabs(cosk, sink, j * 128, 128) if kind in ("k", "ki", "dk") else None
                pend = (lambda kind=kind, pb=pb, W=W, h0=h0, tb_=tb_, j=j: k_epilogue(kind, pb, 128, W, h0, tb_, True, j * 128))
            pend()
    NQ = int(os.environ.get("MK_NQT", str(NQT)))
    for grp in range(2):
        tiles = list(range(grp * 8, min(grp * 8 + 8, NQ)))
        slots = [(si, i, 128, True) for si, i in enumerate(tiles)]
        if grp == 1:
            slots.append((8, NQT, 64, False))
        for (si, i, n, prompt) in slots:
            load_norm_T(xq, i * 128, n, hTs[si], prompt, A1, 0)
        for (kind, c0, W, h0), wbf in zip(QBLOCKS, weight_stream([(w_in, b_[1], b_[2]) for b_ in QBLOCKS])):
            pend = None
            for (si, i, n, prompt) in slots:
                pb = project(hTs[si], n, wbf, W)
                if pend is not None:
                    pend()
                tb_ = load_tabs(cosq, sinq, i * 128, n) if kind != "ki" or not prompt else None

                def pend(kind=kind, pb=pb, n=n, W=W, h0=h0, tb_=tb_, i=i, prompt=prompt):
                    q_epilogue(kind, pb, n, W, h0, tb_, i * 128)
                    if kind == "ki" and not prompt:
                        k_epilogue("ki", pb, n, W, 0, tb_, False, 0)
            pend()
        if grp == 1:
            for (kind, c0, W, h0) in KBLOCKS:
                if kind == "ki":
                    continue
                wbf = load_weights(w_in, c0, W)
                pb = project(hTs[8], 64, wbf, W)
                tb_ = load_tabs(cosq, sinq, NQT * 128, 64) if kind in ("k", "dk") else None
                k_epilogue(kind, pb, 64, W, h0, tb_, False, 0)
    if STAGE >= 2:
        cin = RR([C.sb([128, 512], F32, "cin") for _ in range(1)])
        for s in range(2):
            for t in range(PAST // 128):
                t0 = t * 128

                def ing(srcb, sap, W, **kw):
                    cb = cin.next()
                    P.dma("sync", cb[:, 0:W], sap, [srcb], [cb])
                    epilogue(cb[:, 0:W], cb, 128, W, **kw)
                ing(c_k, c_k[s, t0:t0 + 128, :], 512, featdst=[(skT_s, featT(skT_s, 0, t0, 128, lead=s), 0, 128)])
                ing(c_v, c_v[s, t0:t0 + 128, :], 512, tokdst=[(sv_s, sv_s[s, t0:t0 + 128, :], 0, 128)])
                ing(c_ki, c_ki[s, t0:t0 + 128, :], 128, featdst=[(skiT_s, featT1(skiT_s, t0, 128, lead=s), 0, 128)])
                for hb in range(4):
                    ing(c_dk, c_dk[s, t0:t0 + 128, hb * 512:(hb + 1) * 512], 512,
                        featdst=[(sdkT_s, featT(sdkT_s, hb * 4, t0, 128, lead=s), 0, 128)])
                    ing(c_dv, c_dv[s, t0:t0 + 128, hb * 512:(hb + 1) * 512], 512,
                        tokdst=[(sdv_s, sdv_s[s, t0:t0 + 128, hb * 512:(hb + 1) * 512], 0, 128)])
    P.barrier()
    C.release(mk)
    mk = C.mark()
    psI = RR([C.ps([128, 512], F32, "psI") for _ in range(3)])
    psO = [C.ps([128, 512], F32, "psO") for _ in range(4)]
    psT = RR([C.ps([128, 1024], BF16, "psT") for _ in range(1)])
    kcl = C.sb([128, 512], F32, "kcl")
    gsubB = C.sb([128, 256], F32, "gsubB")
    lamv = C.sb([128, 4, 128], F32, "lamv")
    lamt = C.sb([128, 4], F32, "lamt")
    neglam = C.sb([128, 1], F32, "neglam")
    P.dma("sync", kcl[:, :], kcl_in[:, :], [kcl_in], [kcl])
    P.dma("sync", gsubB[:, :], gsub_in[:, :], [gsub_in], [gsubB])
    P.dma("sync", lamv[:, :, :], lamv_in[:, :, :], [lamv_in], [lamv])
    P.op("vector", lambda e: e.tensor_scalar(out=gsubB[:, :], in0=gsubB[:, :], scalar1=1.0 - LAM_INIT, scalar2=None,
                                             op0=ALU.mult), [gsubB], [gsubB])
    P.op("vector", lambda e: e.tensor_tensor(out=lamv[:, 0, :], in0=lamv[:, 0, :], in1=lamv[:, 1, :], op=ALU.mult), [lamv], [lamv])
    P.op("vector", lambda e: e.tensor_tensor(out=lamv[:, 2, :], in0=lamv[:, 2, :], in1=lamv[:, 3, :], op=ALU.mult), [lamv], [lamv])
    P.op("vector", lambda e: e.reduce_sum(out=lamt[:, 0:1], in_=lamv[:, 0, :], axis=AX.X), [lamv], [lamt])
    P.op("vector", lambda e: e.reduce_sum(out=lamt[:, 1:2], in_=lamv[:, 2, :], axis=AX.X), [lamv], [lamt])
    P.op("scalar", lambda e: e.activation(out=lamt[:, 2:4], in_=lamt[:, 0:2], func=AF.Exp), [lamt], [lamt])
    P.op("vector", lambda e: e.tensor_tensor(out=neglam[:, :], in0=lamt[:, 3:4], in1=lamt[:, 2:3], op=ALU.subtract), [lamt], [neglam])
    P.op("vector", lambda e: e.tensor_scalar(out=neglam[:, :], in0=neglam[:, :], scalar1=-LAM_INIT, scalar2=None, op0=ALU.add),
         [neglam], [neglam])

    kiT = C.sb([128, SEQ], BF16, "kiT")
    qiT = C.sb([128, 16, 128], BF16, "qiT")
    wiq = C.sb([128, 16], F32, "wiq")
    qrel = C.sb([128, 1], F32, "qrel")
    Iw = C.sb([128, SEQ], F32, "Iw")
    wk = C.sb([128, SEQ], F32, "wk")
    rls = RR([C.sb([128, 512], F32, "rl") for _ in range(2)])
    pen = C.sb([128, 512], F32, "pen")
    m8 = C.sb([128, 8], F32, "m8")
    thr = C.sb([128, 1], F32, "thr")
    maskb = C.sb([128, SEQ], BF16, "maskb")
    maskT = C.sb([128, 32, 128], BF16, "maskT")
    visb = C.sb([128, 512], BF16, "visb")
    visT = C.sb([128, 4, 128], BF16, "visT")
    kTs = RR([C.sb([128, SEQ], BF16, "kTg") for _ in range(2)])
    vts = [C.sb([128, 32, 129], BF16, "vt") for _ in range(2)]
    qgs = RR([C.sb([128, 4, 128], BF16, "qg") for _ in range(2)])
    Pts = RR([C.sb([128, 4, 128], BF16, "Pt") for _ in range(3)])
    Pms = RR([C.sb([128, 4, 128], BF16, "Pm") for _ in range(3)])
    dkTs = RR([C.sb([128, 2, SEQ], BF16, "dkT") for _ in range(2)])
    dvts = [C.sb([128, 32, 257], BF16, "dvt") for _ in range(2)]
    dqs = RR([C.sb([128, 2, 128], BF16, "dq") for _ in range(2)])
    rden = C.sb([128, 8], F32, "rden")
    Osb = [C.sb([128, 129], F32, "Osb") for _ in range(4)]
    Od = [C.sb([128, 257], F32, "Od") for _ in range(2)]
    negc = C.sb([128, 2], F32, "negc")
    P.op("vector", lambda e: e.memset(negc[:, 0:1], -1.0), [], [negc])
    P.op("vector", lambda e: e.memset(negc[:, 1:2], -0.5), [negc], [negc])
    tmpd = C.sb([128, 256], F32, "tmpd")
    dof = C.sb([128, 256], F32, "dof")
    junkd = C.sb([128, 256], BF16, "junkd")
    ssd = C.sb([128, 2], F32, "ssd")
    attn = C.sb([128, D], BF16, "attn")
    tbc = RR([C.sb([128, 8, 128], BF16, "tbc") for _ in range(2)])
    for vt in vts:
        P.op("vector", lambda e, vt=vt: e.memset(vt[:, :, 128:129], 1.0), [], [vt])
    for dvt in dvts:
        P.op("vector", lambda e, dvt=dvt: e.memset(dvt[:, :, 256:257], 1.0), [], [dvt])
    vti = [0]
    dvi = [0]

    def transposes_to(srcbuf, src_fn, dstbuf, dst_fn, chunks, nq):
        for b0 in range(0, len(chunks), 8):
            grp = chunks[b0:b0 + 8]
            pb = psT.next()
            for s_, (k0, kl) in enumerate(grp):
                P.op("tensor", lambda e, s_=s_, k0=k0, kl=kl: e.transpose(
                    out=pb[0:kl, s_ * 128:s_ * 128 + nq], in_=src_fn(k0, kl), identity=identb[0:nq, 0:nq]),
                    [srcbuf, identb], [pb])
            full = [x for x in grp if x[1] == 128]
            if full:
                nf = len(full)
                P.op("scalar", lambda e, b0=b0, nf=nf: e.copy(
                    out=dst_fn(b0, nf, 128), in_=pb[:, 0:nf * 128].rearrange("p (c t) -> p c t", t=128)[:, :, 0:nq]),
                    [pb], [dstbuf])
            for s_, (k0, kl) in enumerate(grp):
                if kl != 128:
                    P.op("scalar", lambda e, s_=s_, kl=kl, b0=b0: e.copy(
                        out=dst_fn(b0 + s_, 1, kl), in_=pb[0:kl, s_ * 128:s_ * 128 + nq].unsqueeze(1)), [pb], [dstbuf])

    def geom(nkeys, prompt):
        chunks = [(k0, min(128, nkeys - k0)) for k0 in range(0, nkeys, 128)]
        nchk = len(chunks)
        nfull = nkeys // 128
        nv = min(4, nchk) if prompt else 0
        return chunks, nchk, nfull, nv

    def idx_topk(tok0, nq, nkeys, prompt, S):
        chunks, nchk, nfull, nv = geom(nkeys, prompt)
        P.dma("sync", qiT[:, :, 0:nq], qiT_s[:, :, tok0:tok0 + nq].rearrange("h d t -> d h t"), [qiT_s], [qiT])
        P.dma("sync", wiq[0:nq, :], wi_s[tok0:tok0 + nq, :], [wi_s], [wiq])
        for kb0 in range(0, nkeys, 512):
            kw = min(512, nkeys - kb0)
            for h in range(16):
                pb = psI.next()
                P.op("tensor", lambda e, h=h, pb=pb: e.matmul(out=pb[0:nq, 0:kw], lhsT=qiT[:, h, 0:nq], rhs=kiT[:, kb0:kb0 + kw],
                                                              start=True, stop=True), [qiT, kiT], [pb])
                rl = rls.next()
                P.op("scalar", lambda e, pb=pb, rl=rl: e.activation(out=rl[0:nq, 0:kw], in_=pb[0:nq, 0:kw], func=AF.Relu), [pb], [rl])
                if h == 0:
                    P.op("vector", lambda e, rl=rl: e.tensor_scalar(out=Iw[0:nq, kb0:kb0 + kw], in0=rl[0:nq, 0:kw],
                                                                    scalar1=wiq[0:nq, 0:1], scalar2=None, op0=ALU.mult),
                         [rl, wiq], [Iw])
                else:
                    P.op("vector", lambda e, rl=rl, h=h: e.scalar_tensor_tensor(
                        out=Iw[0:nq, kb0:kb0 + kw], in0=rl[0:nq, 0:kw], scalar=wiq[0:nq, h:h + 1], in1=Iw[0:nq, kb0:kb0 + kw],
                        op0=ALU.mult, op1=ALU.add), [rl, wiq, Iw], [Iw])
        if prompt:
            v0 = nkeys - nv * 128
            P.dma("sync", qrel[0:nq, :], qrel_in[tok0:tok0 + nq, :], [qrel_in], [qrel])
            P.op("vector", lambda e: e.tensor_scalar(out=pen[0:nq, 0:nv * 128], in0=kcl[0:nq, 0:nv * 128], scalar1=qrel[0:nq, 0:1],
                                                     scalar2=NEG, op0=ALU.is_gt, op1=ALU.mult), [kcl, qrel], [pen])
            P.op("vector", lambda e: e.tensor_tensor(out=Iw[0:nq, v0:nkeys], in0=Iw[0:nq, v0:nkeys], in1=pen[0:nq, 0:nv * 128],
                                                     op=ALU.add), [Iw, pen], [Iw])
            P.op("vector", lambda e: e.tensor_scalar(out=visb[0:nq, 0:nv * 128], in0=kcl[0:nq, 0:nv * 128], scalar1=qrel[0:nq, 0:1],
                                                     scalar2=None, op0=ALU.is_le), [kcl, qrel], [visb])
        if nkeys > 256:
            cur = Iw
            for r in range(32):
                P.op("vector", lambda e, cur=cur: e.max(out=m8[0:nq, :], in_=cur[0:nq, 0:nkeys]), [cur], [m8])
                if r < 31:
                    P.op("vector", lambda e, cur=cur: e.match_replace(out=wk[0:nq, 0:nkeys], in_to_replace=m8[0:nq, :],
                                                                      in_values=cur[0:nq, 0:nkeys], imm_value=-3.0e38),
                         [cur, m8], [wk])
                    cur = wk
            P.op("vector", lambda e: e.tensor_scalar(out=thr[0:nq, :], in0=m8[0:nq, 7:8], scalar1=-1.0e29, scalar2=None,
                                                     op0=ALU.max), [m8], [thr])
        else:
            P.op("vector", lambda e: e.memset(thr[:, :], -1.0e29), [], [thr])
        P.op("vector", lambda e: e.tensor_scalar(out=maskb[0:nq, 0:nkeys], in0=Iw[0:nq, 0:nkeys], scalar1=thr[0:nq, 0:1],
                                                 scalar2=None, op0=ALU.is_ge), [Iw, thr], [maskb])

    def mask_T(tok0, nq, nkeys, prompt, S):
        chunks, nchk, nfull, nv = geom(nkeys, prompt)
        transposes_to(maskb, lambda k0, kl: maskb[0:nq, k0:k0 + kl], maskT,
                      lambda c0, n_, kl: maskT[0:kl, c0:c0 + n_, 0:nq], chunks, nq)
        if prompt:
            transposes_to(visb, lambda k0, kl: visb[0:nq, k0:k0 + kl], visT,
                          lambda c0, n_, kl: visT[0:kl, c0:c0 + n_, 0:nq], [(c * 128, 128) for c in range(nv)], nq)
        P.op("gpsimd", lambda e: e.tensor_scalar(out=maskT[:, 0:nchk, 0:nq], in0=maskT[:, 0:nchk, 0:nq], scalar1=30000.0,
                                                 scalar2=-30000.0, op0=ALU.mult, op1=ALU.add), [maskT], [maskT])

    def attend(tok0, nq, nkeys, prompt, S):
        chunks, nchk, nfull, nv = geom(nkeys, prompt)
        for g in range(4):
            kT = kTs.next()
            vt = vts[vti[0] % 2]
            vti[0] += 1
            qg = qgs.next()
            P.dma("sync", kT[:, 0:nkeys], S["kT"](g), [S["kTb"]], [kT])
            if nfull:
                P.dma("sync", vt[:, 0:nfull, 0:128], S["v"](0, nfull * 128, g).rearrange("(c p) d -> p c d", p=128), [S["vb"]], [vt])
            if nkeys > nfull * 128:
                P.dma("sync", vt[0:nkeys - nfull * 128, nfull, 0:128], S["v"](nfull * 128, nkeys, g), [S["vb"]], [vt])
            P.dma("sync", qg[:, :, 0:nq], qT_s[4 * g:4 * g + 4, :, tok0:tok0 + nq].rearrange("h d t -> d h t"), [qT_s], [qg])

            def s_mm(c):
                k0, kl = chunks[c]
                pb = psI.next()
                P.op("tensor", lambda e: e.matmul(out=pb[0:kl, :].rearrange("p (r t) -> p r t", t=128)[:, :, 0:nq],
                                                  lhsT=kT[:, k0:k0 + kl], rhs=qg[:, :, 0:nq], start=True, stop=False),
                     [kT, qg], [pb])
                P.op("tensor", lambda e: e.matmul(out=pb[0:kl, :].rearrange("p (r t) -> p r t", t=128)[:, :, 0:nq],
                                                  lhsT=identb[0:kl, 0:kl],
                                                  rhs=maskT[0:kl, c, 0:nq].unsqueeze(1).to_broadcast([kl, 4, nq]),
                                                  start=False, stop=True), [identb, maskT], [pb])
                return pb
            pbq = [s_mm(0)] + ([s_mm(1)] if nchk > 1 else [])
            for c, (k0, kl) in enumerate(chunks):
                pb = pbq.pop(0)
                if c + 2 < nchk:
                    pbq.append(s_mm(c + 2))
                Pm = Pms.next()
                P.op("scalar", lambda e, pb=pb, Pm=Pm, kl=kl: e.activation(
                    out=Pm[0:kl, :, 0:nq], in_=pb[0:kl, :].rearrange("p (r t) -> p r t", t=128)[:, :, 0:nq], func=AF.Exp,
                    scale=float(128 ** -0.5)), [pb], [Pm])
                for r in range(4):
                    P.op("tensor", lambda e, r=r, Pm=Pm, kl=kl, c=c: e.matmul(
                        out=psO[r][0:nq, 0:129], lhsT=Pm[0:kl, r, 0:nq], rhs=vt[0:kl, c, 0:129],
                        start=(c == 0), stop=(c == nchk - 1)), [Pm, vt], [psO[r]])
            for r in range(4):
                ob_ = Osb[r]
                col = (4 * g + r) * 128
                P.op("scalar", lambda e, r=r, ob_=ob_: e.copy(out=ob_[0:nq, 0:129], in_=psO[r][0:nq, 0:129]), [psO[r]], [ob_])
                P.op("gpsimd", lambda e, ob_=ob_, r=r: e.tensor_tensor(out=rden[0:nq, r:r + 1], in0=ob_[0:nq, 128:129],
                                                                       in1=negc[0:nq, 0:1], op=ALU.pow), [ob_, negc], [rden])
                P.op("gpsimd", lambda e, ob_=ob_, col=col, r=r: e.tensor_scalar(out=attn[0:nq, col:col + 128], in0=ob_[0:nq, 0:128],
                                                                                scalar1=rden[0:nq, r:r + 1], scalar2=None, op0=ALU.mult),
                     [ob_, rden], [attn])
        for hd in range(8):
            dkT = dkTs.next()
            dvt = dvts[dvi[0] % 2]
            dvi[0] += 1
            dq2 = dqs.next()
            P.dma("sync", dkT[:, :, 0:nkeys], S["dkT"](hd), [S["dkTb"]], [dkT])
            if nfull:
                P.dma("sync", dvt[:, 0:nfull, 0:256], S["dv"](0, nfull * 128, hd).rearrange("(c p) d -> p c d", p=128),
                      [S["dvb"]], [dvt])
            if nkeys > nfull * 128:
                P.dma("sync", dvt[0:nkeys - nfull * 128, nfull, 0:256], S["dv"](nfull * 128, nkeys, hd), [S["dvb"]], [dvt])
            P.dma("sync", dq2[:, :, 0:nq], dqT_s[2 * hd:2 * hd + 2, :, tok0:tok0 + nq].rearrange("m d t -> d m t"), [dqT_s], [dq2])

            def d_mm(c):
                k0, kl = chunks[c]
                pb = psI.next()
                for m in range(2):
                    P.op("tensor", lambda e, m=m: e.matmul(out=pb[0:kl, m * 128:m * 128 + nq], lhsT=dkT[:, m, k0:k0 + kl],
                                                           rhs=dq2[:, m, 0:nq], start=True, stop=True), [dkT, dq2], [pb])
                return pb
            pbq = [d_mm(0)] + ([d_mm(1)] if nchk > 1 else [])
            for c, (k0, kl) in enumerate(chunks):
                pb = pbq.pop(0)
                if c + 2 < nchk:
                    pbq.append(d_mm(c + 2))
                Pt = Pts.next()
                P.op("scalar", lambda e, pb=pb, Pt=Pt, kl=kl: e.activation(
                    out=Pt[0:kl, 0:2, 0:nq], in_=pb[0:kl, 0:256].rearrange("p (r t) -> p r t", t=128)[:, :, 0:nq], func=AF.Exp,
                    scale=float(128 ** -0.5)), [pb], [Pt])
                if prompt and c >= nchk - nv:
                    P.op("gpsimd", lambda e, Pt=Pt, kl=kl, c=c: e.tensor_tensor(
                        out=Pt[0:kl, 0:2, 0:nq], in0=Pt[0:kl, 0:2, 0:nq],
                        in1=visT[0:kl, c - (nchk - nv), 0:nq].unsqueeze(1).to_broadcast([kl, 2, nq]), op=ALU.mult),
                        [Pt, visT], [Pt])
                for m in range(2):
                    P.op("tensor", lambda e, m=m, Pt=Pt, kl=kl, c=c: e.matmul(
                        out=psO[m][0:nq, 0:257], lhsT=Pt[0:kl, m, 0:nq], rhs=dvt[0:kl, c, 0:257],
                        start=(c == 0), stop=(c == nchk - 1)), [Pt, dvt], [psO[m]])
            P.op("scalar", lambda e: e.copy(out=Od[0][0:nq, :], in_=psO[0][0:nq, 0:257]), [psO[0]], [Od[0]])
            P.op("scalar", lambda e: e.copy(out=Od[1][0:nq, :], in_=psO[1][0:nq, 0:257]), [psO[1]], [Od[1]])
            P.op("gpsimd", lambda e: e.tensor_tensor(out=rden[0:nq, 4:5], in0=Od[0][0:nq, 256:257], in1=negc[0:nq, 0:1], op=ALU.pow),
                 [Od[0], negc], [rden])
            P.op("gpsimd", lambda e: e.tensor_tensor(out=rden[0:nq, 5:6], in0=Od[1][0:nq, 256:257], in1=negc[0:nq, 0:1], op=ALU.pow),
                 [Od[1], negc], [rden])
            P.op("gpsimd", lambda e: e.tensor_tensor(out=rden[0:nq, 5:6], in0=rden[0:nq, 5:6], in1=neglam[0:nq, 0:1], op=ALU.mult),
                 [rden, neglam], [rden])
            P.op("gpsimd", lambda e: e.tensor_scalar(out=tmpd[0:nq, :], in0=Od[0][0:nq, 0:256], scalar1=rden[0:nq, 4:5],
                                                     scalar2=None, op0=ALU.mult), [Od[0], rden], [tmpd])
            P.op("gpsimd", lambda e: e.tensor_scalar(out=dof[0:nq, :], in0=Od[1][0:nq, 0:256], scalar1=rden[0:nq, 5:6],
                                                     scalar2=None, op0=ALU.mult), [Od[1], rden], [dof])
            P.op("gpsimd", lambda e: e.tensor_tensor(out=dof[0:nq, :], in0=dof[0:nq, :], in1=tmpd[0:nq, :], op=ALU.add), [dof, tmpd], [dof])
            P.op("gpsimd", lambda e: e.memset(ssd[:, :], 0.0), [], [ssd])
            P.op("scalar", lambda e: e.activation(out=junkd[0:nq, :], in_=dof[0:nq, :], func=AF.Square, accum_out=ssd[0:nq, 0:1]),
                 [dof], [junkd, ssd])
            P.op("gpsimd", lambda e: e.tensor_scalar(out=ssd[0:nq, 1:2], in0=ssd[0:nq, 0:1], scalar1=1.0 / 256, scalar2=EPS,
                                                     op0=ALU.mult, op1=ALU.add), [ssd], [ssd])
            P.op("gpsimd", lambda e: e.tensor_tensor(out=ssd[0:nq, 1:2], in0=ssd[0:nq, 1:2], in1=negc[0:nq, 1:2], op=ALU.pow),
                 [ssd, negc], [ssd])
            col = 2048 + hd * 256
            P.op("gpsimd", lambda e: e.tensor_scalar(out=tmpd[0:nq, :], in0=dof[0:nq, :], scalar1=ssd[0:nq, 1:2], scalar2=None,
                                                     op0=ALU.mult), [dof, ssd], [tmpd])
            P.op("gpsimd", lambda e, col=col: e.tensor_tensor(out=attn[0:nq, col:col + 256], in0=tmpd[0:nq, :], in1=gsubB[0:nq, :],
                                                              op=ALU.mult), [tmpd, gsubB], [attn])
        for c8 in range(4):
            pb = psT.next()
            tb = tbc.next()
            for j in range(8):
                ch = c8 * 8 + j
                P.op("tensor", lambda e, j=j, ch=ch: e.transpose(out=pb[:, j * 128:j * 128 + nq], in_=attn[0:nq, ch * 128:(ch + 1) * 128],
                                                                 identity=identb[0:nq, 0:nq]), [attn, identb], [pb])
            P.op("scalar", lambda e: e.copy(out=tb[:, :, 0:nq], in_=pb[:, :].rearrange("p (c t) -> p c t", t=128)[:, :, 0:nq]),
                 [pb], [tb])
            P.dma("gpsimd", attnT_s[:, c8 * 8:(c8 + 1) * 8, tok0:tok0 + nq], tb[:, :, 0:nq], [tb], [attnT_s])

    NA = int(os.environ.get("MK_NAT", str(NQT)))
    jobs = []
    for i in range(NA):
        nkeys = nchunks_for(i) * 128
        S = {
            "kT": (lambda g, nkeys=nkeys: kT_s[g, :, 0:nkeys]), "kTb": kT_s,
            "v": (lambda a, b_, g: v_s[a:b_, g * 128:(g + 1) * 128]), "vb": v_s,
            "dkT": (lambda hd, nkeys=nkeys: dkT_s[2 * hd:2 * hd + 2, :, 0:nkeys].rearrange("m d t -> d m t")), "dkTb": dkT_s,
            "dv": (lambda a, b_, hd: dv_s[a:b_, hd * 256:(hd + 1) * 256]), "dvb": dv_s,
            "ki": None,
        }
        jobs.append((i * 128, 128, nkeys, True, S))
    for s in range(2):
        S = {
            "kT": (lambda g, s=s: skT_s[s, g, :, :]), "kTb": skT_s,
            "v": (lambda a, b_, g, s=s: sv_s[s, a:b_, g * 128:(g + 1) * 128]), "vb": sv_s,
            "dkT": (lambda hd, s=s: sdkT_s[s, 2 * hd:2 * hd + 2, :, :].rearrange("m d t -> d m t")), "dkTb": sdkT_s,
            "dv": (lambda a, b_, hd, s=s: sdv_s[s, a:b_, hd * 256:(hd + 1) * 256]), "dvb": sdv_s,
            "ki": s,
        }
        jobs.append((NQT * 128 + 32 * s, 32, LS, False, S))

    def stage1(job):
        if job[4]["ki"] is not None:
            s_ = job[4]["ki"]
            P.dma("sync", kiT[:, 0:LS], skiT_s[s_, :, :], [skiT_s], [kiT])
        idx_topk(*job)

    P.dma("sync", kiT[:, :], kiT_s[:, :], [kiT_s], [kiT])
    stage1(jobs[0])
    mask_T(*jobs[0])
    for ji, job in enumerate(jobs):
        if ji + 1 < len(jobs):
            stage1(jobs[ji + 1])
        attend(*job)
        if ji + 1 < len(jobs):
            mask_T(*jobs[ji + 1])
    P.barrier()
    C.release(mk)

    mk = C.mark()
    psF = [C.ps([128, 512], F32, "psF") for _ in range(4)]
    ps_proj = RR(psF[0:3])
    hTs = [C.sb([128, 32, 128], BF16, "hT") for _ in range(9)]
    wsts = RR([C.sb([128, 4, 512], F32, "wst") for _ in range(2)])
    wbfs = RR([C.sb([128, 32, 512], BF16, "wbf") for _ in range(2)])
    gaP = C.sb([128, D], F32, "gaP")
    gaS = C.sb([128, D], F32, "gaS")
    xbs = RR([C.sb([128, 512], F32, "xb") for _ in range(3)])
    tms = RR([C.sb([128, 512], F32, "tm") for _ in range(3)])
    P.dma("sync", gaP[:, :], ga_s[0, :, :], [ga_s], [gaP])
    P.dma("sync", gaS[:, :], ga_s[2, :, :], [ga_s], [gaS])
    for grp in range(2):
        slots = [(si, i, 128, True) for si, i in enumerate(range(grp * 8, grp * 8 + 8))]
        if grp == 1:
            slots.append((8, NQT, 64, False))
        for (si, i, n, prompt) in slots:
            P.dma("sync", hTs[si][:, :, 0:n], attnT_s[:, :, i * 128:i * 128 + n], [attnT_s], [hTs[si]])
        for cb, wbf in zip(range(8), weight_stream([(w_out, cb_ * 512, 512) for cb_ in range(8)])):
            for (si, i, n, prompt) in slots:
                pb = project(hTs[si], n, wbf, 512)
                xb = xbs.next()
                tm = tms.next()
                ga = gaP if prompt else gaS
                P.dma("sync", xb[0:n, :], xq[i * 128:i * 128 + n, cb * 512:(cb + 1) * 512], [xq], [xb])
                P.op("vector", lambda e, pb=pb, tm=tm, ga=ga, n=n, cb=cb: e.tensor_tensor(
                    out=tm[0:n, :], in0=pb[0:n, :], in1=ga[0:n, cb * 512:(cb + 1) * 512], op=ALU.mult), [pb, ga], [tm])
                P.op("gpsimd", lambda e, tm=tm, xb=xb, n=n: e.tensor_tensor(out=tm[0:n, :], in0=tm[0:n, :], in1=xb[0:n, :], op=ALU.add),
                     [tm, xb], [tm])
                P.dma("gpsimd", x1_s[i * 128:i * 128 + n, cb * 512:(cb + 1) * 512], tm[0:n, :], [tm], [x1_s])
    P.barrier()
    C.release(mk)
    mk = C.mark()
    psF = [C.ps([128, 512], F32, "psF") for _ in range(6)]
    psB = [C.ps([128, 1024], BF16, "psB") for _ in range(2)]
    ps_proj = RR(psF[0:2])
    ps_xT = RR(psF[2:4])
    ps_s = RR(psF[4:6])
    xts = RR([C.sb([128, D], F32, "xt") for _ in range(1)])
    sst = RR([C.sb([128, 2], F32, "ss") for _ in range(2)])
    hTs = [C.sb([128, 32, 128], BF16, "hT") for _ in range(9)]
    wsts = RR([C.sb([128, 4, 512], F32, "wst") for _ in range(2)])
    wbfs = RR([C.sb([128, 32, 512], BF16, "wbf") for _ in range(2)])
    kraw = C.sb([128, 16, 128], F32, "kraw")
    keysT = C.sb([128, 16, 128], F32, "keysT")
    qsbs = RR([C.sb([128, 512], F32, "qsb") for _ in range(2)])
    qT4s = RR([C.sb([128, 4, 128], F32, "qT4") for _ in range(2)])
    sos = RR([C.sb([128, 512], F32, "so") for _ in range(2)])
    P.dma("sync", kraw[:, :, :], pkeys_in[:, :, :, :].rearrange("h c n d -> n (h c) d"), [pkeys_in], [kraw])
    for b4 in range(4):
        pb = ps_xT.next()
        for j in range(4):
            P.op("tensor", lambda e, j=j, b4=b4: e.transpose(out=pb[:, j * 128:(j + 1) * 128], in_=kraw[:, b4 * 4 + j, :],
                                                             identity=ident[:, :]), [kraw, ident], [pb])
        P.op("vector", lambda e, b4=b4: e.tensor_copy(out=keysT[:, b4 * 4:(b4 + 1) * 4, :],
                                                      in_=pb[:, :].rearrange("p (c t) -> p c t", t=128)), [pb], [keysT])
    for grp in range(2):
        slots = [(si, i, 128, True) for si, i in enumerate(range(grp * 8, grp * 8 + 8))]
        if grp == 1:
            slots.append((8, NQT, 64, False))
        for (si, i, n, prompt) in slots:
            load_norm_T(x1_s, i * 128, n, hTs[si], prompt, A2, 96)
            P.dma("gpsimd", h2T_s[:, :, i * 128:i * 128 + n], hTs[si][:, :, 0:n], [hTs[si]], [h2T_s])
        for cb, wbf in zip(range(4), weight_stream([(pwq_in, cb_ * 512, 512) for cb_ in range(4)])):
            for (si, i, n, prompt) in slots:
                pb = project(hTs[si], n, wbf, 512)
                qsb = qsbs.next()
                P.op("scalar", lambda e, pb=pb, qsb=qsb, n=n: e.copy(out=qsb[0:n, :], in_=pb[0:n, :]), [pb], [qsb])
                pt = ps_xT.next()
                for j in range(4):
                    P.op("tensor", lambda e, j=j, n=n, qsb=qsb, pt=pt: e.transpose(
                        out=pt[:, j * 128:j * 128 + n], in_=qsb[0:n, j * 128:(j + 1) * 128], identity=ident[0:n, 0:n]),
                        [qsb, ident], [pt])
                qT4 = qT4s.next()
                P.op("vector", lambda e, pt=pt, qT4=qT4, n=n: e.tensor_copy(
                    out=qT4[:, :, 0:n], in_=pt[:, :].rearrange("p (c t) -> p c t", t=128)[:, :, 0:n]), [pt], [qT4])
                po = ps_s.next()
                for j in range(4):
                    P.op("tensor", lambda e, j=j, n=n, qT4=qT4, po=po, cb=cb: e.matmul(
                        out=po[0:n, j * 128:(j + 1) * 128], lhsT=qT4[:, j, 0:n], rhs=keysT[:, cb * 4 + j, :],
                        start=True, stop=True), [qT4, keysT], [po])
                so = sos.next()
                P.op("scalar", lambda e, po=po, so=so, n=n: e.copy(out=so[0:n, :], in_=po[0:n, :]), [po], [so])
                P.dma("gpsimd", s_s[i * 128:i * 128 + n, cb * 512:(cb + 1) * 512], so[0:n, :], [so], [s_s])
    P.barrier()
    C.release(mk)

    mk = C.mark()
    psA = RR([C.ps([128, 1024], BF16, "psA") for _ in range(3)])
    psG = RR([C.ps([128, 512], F32, "psG") for _ in range(3)])
    sal = C.sb([128, 16, 128], F32, "sal")
    wk2 = C.sb([128, 16, 128], F32, "wk2")
    top = C.sb([128, 16, 16], F32, "top")
    idx = C.sb([128, 8, 16], U32, "idx")
    idxf = C.sb([128, 8, 16], F32, "idxf")
    cand = C.sb([128, 8, 256], F32, "cand")
    cw = C.sb([128, 8, 256], F32, "cw")
    best = C.sb([128, 8, 16], F32, "best")
    eb = C.sb([128, 8, 16], F32, "eb")
    Zs = C.sb([128, 8], F32, "Zs")
    e1z = C.sb([128, 8, 16], F32, "e1z")
    cth = C.sb([128, 8, 16], F32, "cth")
    e2 = C.sb([128, 8, 128], F32, "e2")
    iot = C.sb([128, 128], F32, "iot")
    ABa = C.sb([128, 128, 128], BF16, "ABa")
    ABb = C.sb([128, 128, 128], BF16, "ABb")
    AT = C.sb([128, 128, 128], BF16, "AT")
    BT = C.sb([128, 128, 128], BF16, "BT")
    Gall = C.sb([128, 128, 128], BF16, "Gall")
    P.dma("sync", iot[:, :], iota_in[:, :], [iota_in], [iot])
    NE2 = int(os.environ.get("MK_NE2", "17"))
    for ti in range(NE2):
        n = 128 if ti < NQT else 64
        tok0 = ti * 128
        P.dma("sync", sal[0:n, :, :], s_s[tok0:tok0 + n, :].rearrange("t (k m) -> t k m", m=128), [s_s], [sal])
        for hc in range(16):
            h, c = divmod(hc, 2)
            P.op("vector", lambda e, hc=hc: e.max(out=top[0:n, hc, 0:8], in_=sal[0:n, hc, :]), [sal], [top])
            if c == 0:
                P.op("vector", lambda e, hc=hc, h=h: e.max_index(out=idx[0:n, h, 0:8], in_max=top[0:n, hc, 0:8],
                                                                 in_values=sal[0:n, hc, :]), [sal, top], [idx])
            P.op("vector", lambda e, hc=hc: e.match_replace(out=wk2[0:n, hc, :], in_to_replace=top[0:n, hc, 0:8],
                                                            in_values=sal[0:n, hc, :], imm_value=-3.0e38), [sal, top], [wk2])
            P.op("vector", lambda e, hc=hc: e.max(out=top[0:n, hc, 8:16], in_=wk2[0:n, hc, :]), [wk2], [top])
            if c == 0:
                P.op("vector", lambda e, hc=hc, h=h: e.max_index(out=idx[0:n, h, 8:16], in_max=top[0:n, hc, 8:16],
                                                                 in_values=wk2[0:n, hc, :]), [wk2, top], [idx])
        top4 = top[0:n, :, :].rearrange("p (h c) k -> p h c k", c=2)
        sal4 = sal[0:n, :, :].rearrange("p (h c) k -> p h c k", c=2)
        P.op("vector", lambda e: e.tensor_copy(out=idxf[0:n, :, :], in_=idx[0:n, :, :]), [idx], [idxf])
        P.op("vector", lambda e: e.tensor_tensor(
            out=cand[0:n, :, :].rearrange("p h (i j) -> p h i j", j=16),
            in0=top4[:, :, 0, :].unsqueeze(3).to_broadcast([n, 8, 16, 16]),
            in1=top4[:, :, 1, :].unsqueeze(2).to_broadcast([n, 8, 16, 16]), op=ALU.add), [top], [cand])
        for h in range(8):
            P.op("vector", lambda e, h=h: e.max(out=best[0:n, h, 0:8], in_=cand[0:n, h, :]), [cand], [best])
            P.op("vector", lambda e, h=h: e.match_replace(out=cw[0:n, h, :], in_to_replace=best[0:n, h, 0:8],
                                                          in_values=cand[0:n, h, :], imm_value=-3.0e38), [cand, best], [cw])
            P.op("vector", lambda e, h=h: e.max(out=best[0:n, h, 8:16], in_=cw[0:n, h, :]), [cw], [best])
        P.op("vector", lambda e: e.tensor_tensor(out=eb[0:n, :, :], in0=best[0:n, :, :],
                                                 in1=best[0:n, :, 0:1].to_broadcast([n, 8, 16]), op=ALU.subtract), [best], [eb])
        P.op("scalar", lambda e: e.activation(out=eb[0:n, :, :], in_=eb[0:n, :, :], func=AF.Exp), [eb], [eb])
        P.op("vector", lambda e: e.reduce_sum(out=Zs[0:n, :], in_=eb[0:n, :, :], axis=AX.X), [eb], [Zs])
        P.op("vector", lambda e: e.reciprocal(out=Zs[0:n, :], in_=Zs[0:n, :]), [Zs], [Zs])
        P.op("vector", lambda e: e.tensor_tensor(out=e1z[0:n, :, :], in0=top4[:, :, 0, :],
                                                 in1=top4[:, :, 0, 0:1].to_broadcast([n, 8, 16]), op=ALU.subtract), [top], [e1z])
        P.op("scalar", lambda e: e.activation(out=e1z[0:n, :, :], in_=e1z[0:n, :, :], func=AF.Exp), [e1z], [e1z])
        P.op("vector", lambda e: e.tensor_tensor(out=e1z[0:n, :, :], in0=e1z[0:n, :, :],
                                                 in1=Zs[0:n, :].unsqueeze(2).to_broadcast([n, 8, 16]), op=ALU.mult), [e1z, Zs], [e1z])
        P.op("vector", lambda e: e.tensor_tensor(out=cth[0:n, :, :], in0=best[0:n, :, 15:16].to_broadcast([n, 8, 16]),
                                                 in1=top4[:, :, 0, :], op=ALU.subtract), [best, top], [cth])
        P.op("vector", lambda e: e.tensor_scalar(out=cth[0:n, :, :], in0=cth[0:n, :, :], scalar1=-4.0e-6, scalar2=None,
                                                 op0=ALU.add), [cth], [cth])
        P.op("vector", lambda e: e.tensor_tensor(out=e2[0:n, :, :], in0=sal4[:, :, 1, :],
                                                 in1=top4[:, :, 1, 0:1].to_broadcast([n, 8, 128]), op=ALU.subtract), [sal, top], [e2])
        P.op("scalar", lambda e: e.activation(out=e2[0:n, :, :], in_=e2[0:n, :, :], func=AF.Exp), [e2], [e2])
        for which in range(2):
            XT = AT if which == 0 else BT
            AB = ABa if which == 0 else ABb
            if which == 0:
                P.op("vector", lambda e, AB=AB: e.tensor_tensor(
                    out=AB[0:n, :, :], in0=iot[0:n, :].unsqueeze(1).to_broadcast([n, 128, 128]),
                    in1=idxf[0:n, :, :].rearrange("p h i -> p (h i)").unsqueeze(2).to_broadcast([n, 128, 128]),
                    op=ALU.is_equal), [iot, idxf], [AB])
                P.op("gpsimd", lambda e, AB=AB: e.tensor_tensor(
                    out=AB[0:n, :, :], in0=AB[0:n, :, :],
                    in1=e1z[0:n, :, :].rearrange("p h i -> p (h i)").unsqueeze(2).to_broadcast([n, 128, 128]),
                    op=ALU.mult), [AB, e1z], [AB])
            else:
                P.op("vector", lambda e, AB=AB: e.tensor_tensor(
                    out=AB[0:n, :, :].rearrange("p (h i) k -> p h i k", i=16),
                    in0=sal4[:, :, 1, :].unsqueeze(2).to_broadcast([n, 8, 16, 128]),
                    in1=cth[0:n, :, :].unsqueeze(3).to_broadcast([n, 8, 16, 128]), op=ALU.is_ge), [sal, cth], [AB])
                P.op("gpsimd", lambda e, AB=AB: e.tensor_tensor(
                    out=AB[0:n, :, :].rearrange("p (h i) k -> p h i k", i=16),
                    in0=AB[0:n, :, :].rearrange("p (h i) k -> p h i k", i=16),
                    in1=e2[0:n, :, :].unsqueeze(2).to_broadcast([n, 8, 16, 128]), op=ALU.mult), [AB, e2], [AB])
            for b8 in range(16):
                pb = psA.next()
                for j in range(8):
                    col = b8 * 8 + j
                    P.op("tensor", lambda e, j=j, col=col, pb=pb, AB=AB: e.transpose(
                        out=pb[:, j * 128:j * 128 + n], in_=AB[0:n, :, col], identity=identb[0:n, 0:n]), [AB, identb], [pb])
                en = "scalar" if b8 % 2 == 0 else "vector"
                src_v = pb[:, :].rearrange("p (c t) -> p c t", t=128)[:, :, 0:n]
                if en == "scalar":
                    P.op("scalar", lambda e, b8=b8, src_v=src_v, XT=XT: e.copy(out=XT[:, b8 * 8:(b8 + 1) * 8, 0:n], in_=src_v), [pb], [XT])
                else:
                    P.op("vector", lambda e, b8=b8, src_v=src_v, XT=XT: e.tensor_copy(out=XT[:, b8 * 8:(b8 + 1) * 8, 0:n], in_=src_v),
                         [pb], [XT])
        for t4 in range(0, n, 4):
            pg = psG.next()
            for j in range(4):
                P.op("tensor", lambda e, j=j, t4=t4, pg=pg: e.matmul(out=pg[:, j * 128:(j + 1) * 128], lhsT=AT[:, :, t4 + j],
                                                                     rhs=BT[:, :, t4 + j], start=True, stop=True), [AT, BT], [pg])
            en = "scalar" if (t4 // 4) % 2 == 0 else "vector"
            src_v = pg[:, :].rearrange("p (t i) -> p i t", i=128)
            if en == "scalar":
                P.op("scalar", lambda e, t4=t4, src_v=src_v: e.copy(out=Gall[:, :, t4:t4 + 4], in_=src_v), [pg], [Gall])
            else:
                P.op("vector", lambda e, t4=t4, src_v=src_v: e.tensor_copy(out=Gall[:, :, t4:t4 + 4], in_=src_v), [pg], [Gall])
        P.dma("gpsimd", G_s[ti, :, :, :], Gall[:, :, :], [Gall], [G_s])
    P.barrier()
    C.release(mk)
    mk = C.mark()
    psAct = RR([C.ps([128, 512], F32, "psAct") for _ in range(2)])
    psUT = RR([C.ps([128, 1024], BF16, "psUT") for _ in range(2)])
    psO2 = RR([C.ps([128, 512], F32, "psO2") for _ in range(3)])
    h2T = C.sb([128, 32, 512], BF16, "h2T")
    oacc = [C.sb([128, D], F32, "oacc") for _ in range(4)]
    stg = RR([C.sb([128, D], F32, "stg") for _ in range(2)])
    ubfs = RR([C.sb([128, D], BF16, "ubf") for _ in range(2)])
    uTs = RR([C.sb([128, 32, 128], BF16, "uT") for _ in range(2)])
    vbfs = [C.sb([128, D], BF16, "vbf") for _ in range(4)]
    WTs = [C.sb([128, 512], BF16, "WT") for _ in range(4)]
    Gcs = RR([C.sb([128, 4, 128], BF16, "Gc") for _ in range(2)])
    gts = RR([C.sb([128, 512], BF16, "gt") for _ in range(2)])
    _s0 = stg.items[0].t
    xbs = RR([Buf(_s0[:, 0:512]), Buf(_s0[:, 512:1024])])
    gbs = RR([Buf(_s0[:, 1024:1536]), Buf(_s0[:, 1536:2048])])
    fbs = RR([Buf(_s0[:, 2048:2560]), Buf(_s0[:, 2560:3072])])
    ybs = RR([Buf(_s0[:, 3072:3584]), Buf(_s0[:, 3584:4096])])
    jk = C.sb([128, 512], BF16, "jk")
    ss8 = C.sb([128, 10], F32, "ss8")
    u3 = pu_in[:, :].rearrange("(a b) d -> a b d", b=128)
    v3 = pv_in[:, :].rearrange("(a b) d -> a b d", b=128)
    uT_cb = [Buf(uT_c.t[c_]) for c_ in range(128)]
    v_cb = [Buf(v_c.t[c_]) for c_ in range(128)]
    BLOCKS = [[15, 16], [0, 1, 2, 3], [4, 5, 6, 7], [8, 9, 10, 11], [12, 13, 14]]
    NBLK = int(os.environ.get("MK_NBLK", "5"))
    NCH = int(os.environ.get("MK_NCH", "128"))
    for bi, blk in enumerate(BLOCKS[:NBLK]):
        tiles = [(ti, ti * 128, 128 if ti < NQT else 64) for ti in blk]
        tok0 = tiles[0][1]
        T = sum(n for (_, _, n) in tiles)
        P.dma("sync", h2T[:, :, 0:T], h2T_s[:, :, tok0:tok0 + T], [h2T_s], [h2T])
        for g0 in range(0, NCH, 4):
            for k_ in range(4):
                c = g0 + k_
                uT = uTs.next()
                if bi == 0:
                    st = stg.next()
                    P.dma("sync", st[:, :], u3[:, c, :], [pu_in], [st])
                    ubf = ubfs.next()
                    P.op("vector", lambda e, st=st, ubf=ubf: e.tensor_copy(out=ubf[:, :], in_=st[:, :]), [st], [ubf])
                    for b8 in range(4):
                        pb = psUT.next()
                        for j in range(8):
                            ch = b8 * 8 + j
                            P.op("tensor", lambda e, j=j, ch=ch, pb=pb, ubf=ubf: e.transpose(out=pb[:, j * 128:(j + 1) * 128],
                                                                                             in_=ubf[:, ch * 128:(ch + 1) * 128],
                                                                                             identity=identb[:, :]), [ubf, identb], [pb])
                        P.op("scalar", lambda e, b8=b8, pb=pb, uT=uT: e.copy(
                            out=uT[:, b8 * 8:(b8 + 1) * 8, :], in_=pb[:, :].rearrange("p (c t) -> p c t", t=128)), [pb], [uT])
                    P.dma("gpsimd", uT_cb[c][:, :], uT[:, :, :].rearrange("p c t -> p (c t)"), [uT], [uT_cb[c]])
                else:
                    P.dma("sync", uT[:, :, :].rearrange("p c t -> p (c t)"), uT_cb[c][:, :], [uT_cb[c]], [uT])
                Gc = Gcs.next()
                P.dma("sync", Gc[:, 0:len(tiles), :], G_s[blk[0]:blk[0] + len(tiles), :, c, :].rearrange("a p t -> p a t"), [G_s], [Gc])
                pa = psAct.next()
                for ch in range(32):
                    P.op("tensor", lambda e, ch=ch, pa=pa, uT=uT: e.matmul(out=pa[:, 0:T], lhsT=uT[:, ch, :], rhs=h2T[:, ch, 0:T],
                                                                    start=(ch == 0), stop=(ch == 31)), [uT, h2T], [pa])
                gt = gts.next()
                P.op("scalar", lambda e, pa=pa, gt=gt: e.activation(out=gt[:, 0:T], in_=pa[:, 0:T], func=AF.Gelu_apprx_tanh), [pa], [gt])
                WT = WTs[k_]
                P.op("gpsimd", lambda e, gt=gt, WT=WT, Gc=Gc: e.tensor_tensor(
                    out=WT[:, 0:T], in0=gt[:, 0:T], in1=Gc[:, :, :].rearrange("p a t -> p (a t)")[:, 0:T], op=ALU.mult),
                    [gt, Gc], [WT])
                if bi == 0:
                    st = stg.next()
                    P.dma("sync", st[:, :], v3[:, c, :], [pv_in], [st])
                    P.op("scalar", lambda e, st=st, k_=k_: e.copy(out=vbfs[k_][:, :], in_=st[:, :]), [st], [vbfs[k_]])
                    P.dma("gpsimd", v_cb[c][:, :], vbfs[k_][:, :], [vbfs[k_]], [v_cb[c]])
                else:
                    P.dma("sync", vbfs[k_][:, :], v_cb[c][:, :], [v_cb[c]], [vbfs[k_]])
            off = 0
            for si, (ti, tk0, n) in enumerate(tiles):
                for db in range(8):
                    po = psO2.next()
                    for k_ in range(4):
                        P.op("tensor", lambda e, k_=k_, po=po, off=off, n=n, db=db: e.matmul(
                            out=po[0:n, :], lhsT=WTs[k_][:, off:off + n], rhs=vbfs[k_][:, db * 512:(db + 1) * 512],
                            start=(k_ == 0), stop=(k_ == 3)), [WTs[k_], vbfs[k_]], [po])
                    if g0 == 0:
                        P.op("vector", lambda e, po=po, si=si, n=n, db=db: e.tensor_copy(
                            out=oacc[si][0:n, db * 512:(db + 1) * 512], in_=po[0:n, :]), [po], [oacc[si]])
                    else:
                        P.op("vector", lambda e, po=po, si=si, n=n, db=db: e.tensor_tensor(
                            out=oacc[si][0:n, db * 512:(db + 1) * 512], in0=po[0:n, :], in1=oacc[si][0:n, db * 512:(db + 1) * 512],
                            op=ALU.add), [po, oacc[si]], [oacc[si]])
                off += n
        P.barrier()
        for si, (ti, tk0, n) in enumerate(tiles):
            prompt = ti < NQT
            P.op("vector", lambda e: e.memset(ss8[:, :], 0.0), [], [ss8])
            for db in range(8):
                xb = xbs.next()
                gb = gbs.next()
                sl = slice(db * 512, (db + 1) * 512)
                P.dma("sync", xb[0:n, :], x1_s[tk0:tk0 + n, sl], [x1_s], [xb])
                P.dma("sync", gb[0:n, :], ga_s[1 if prompt else 3, 0:n, sl], [ga_s], [gb])
                P.op("vector", lambda e, si=si, sl=sl, gb=gb, n=n: e.tensor_tensor(out=oacc[si][0:n, sl], in0=oacc[si][0:n, sl],
                                                                                   in1=gb[0:n, :], op=ALU.mult), [oacc[si], gb], [oacc[si]])
                P.op("gpsimd", lambda e, si=si, sl=sl, xb=xb, n=n: e.tensor_tensor(out=oacc[si][0:n, sl], in0=oacc[si][0:n, sl],
                                                                                   in1=xb[0:n, :], op=ALU.add), [oacc[si], xb], [oacc[si]])
                P.op("scalar", lambda e, si=si, sl=sl, n=n, db=db: e.activation(out=jk[0:n, :], in_=oacc[si][0:n, sl], func=AF.Square,
                                                                                accum_out=ss8[0:n, db:db + 1]), [oacc[si]], [jk, ss8])
            P.op("vector", lambda e, n=n: e.reduce_sum(out=ss8[0:n, 8:9], in_=ss8[0:n, 0:8], axis=AX.X), [ss8], [ss8])
            P.op("vector", lambda e, n=n: e.tensor_scalar(out=ss8[0:n, 9:10], in0=ss8[0:n, 8:9], scalar1=1.0 / D, scalar2=EPS,
                                                          op0=ALU.mult, op1=ALU.add), [ss8], [ss8])
            P.op("scalar", lambda e, n=n: e.activation(out=ss8[0:n, 9:10], in_=ss8[0:n, 9:10], func=AF.Sqrt), [ss8], [ss8])
            P.op("vector", lambda e, n=n: e.reciprocal(out=ss8[0:n, 9:10], in_=ss8[0:n, 9:10]), [ss8], [ss8])
            for db in range(8):
                fb = fbs.next()
                yb = ybs.next()
                sl = slice(db * 512, (db + 1) * 512)
                P.dma("sync", fb[0:n, :], gfinB[0:n, sl], [gfinB], [fb])
                P.op("vector", lambda e, si=si, sl=sl, fb=fb, yb=yb, n=n: e.scalar_tensor_tensor(
                    out=yb[0:n, :], in0=oacc[si][0:n, sl], scalar=ss8[0:n, 9:10], in1=fb[0:n, :], op0=ALU.mult, op1=ALU.mult),
                    [oacc[si], ss8, fb], [yb])
                P.dma("gpsimd", o_y[tk0:tk0 + n, sl], yb[0:n, :], [yb], [o_y])
        P.barrier()
    P.barrier()
    C.release(mk)
    P.barrier()
    P.close()
    return nc


_ROPE_CACHE = {}


def _rope_tabs(pos):
    half = 64
    inv = (np.float32(10000.0) ** (-np.arange(half, dtype=np.float32) / np.float32(half))).astype(np.float32)
    ang = pos.astype(np.float32)[:, None] * inv[None, :]
    cos, sin = np.cos(ang).astype(np.float32), np.sin(ang).astype(np.float32)
    return (np.ascontiguousarray(np.concatenate([cos, cos], axis=1)),
            np.ascontiguousarray(np.concatenate([-sin, sin], axis=1)))


def _fp(v):
    return np.ascontiguousarray(v.reshape(-1, 128).T)


def kernel(x_prompt, x_sample, cache_dsa_k, cache_dsa_v, cache_idx_k, cache_diff_k, cache_diff_v,
           c_prompt, c_sample, w_ada, b_ada, g_norm_mix, g_norm_ffn, w_in,
           diff_lambda_q1, diff_lambda_k1, diff_lambda_q2, diff_lambda_k2, g_diff_subln, w_out,
           peer_w_query, peer_sub_keys, peer_u, peer_v, g_final):
    f = np.float32
    A = lambda a: np.ascontiguousarray(np.asarray(a, dtype=f))
    x_prompt, x_sample = A(x_prompt), A(x_sample)
    nc = build_program()
    cosk, sink = _rope_tabs(np.arange(SEQ))
    ident = np.eye(128, dtype=f)
    in_maps = []
    own = {}
    shared = {
        "w_ada": A(w_ada[0]), "badaT": _fp(A(b_ada[0])), "gmixT": _fp(A(g_norm_mix[0])), "gffnT": _fp(A(g_norm_ffn[0])),
        "gfinB": np.ascontiguousarray(np.broadcast_to(A(g_final)[None, :], (128, D))),
        "w_in": A(w_in[0]), "cosk": cosk, "sink": sink, "ident": ident,
        "kcl": np.ascontiguousarray(np.broadcast_to((np.arange(512) // 64).astype(f)[None, :], (128, 512))),
        "lamv": np.ascontiguousarray(np.broadcast_to(np.stack([A(diff_lambda_q1[0]), A(diff_lambda_k1[0]),
                                                                A(diff_lambda_q2[0]), A(diff_lambda_k2[0])])[None], (128, 4, 128))),
        "gsub": np.ascontiguousarray(np.broadcast_to(A(g_diff_subln[0])[None, :], (128, 256))),
        "w_out": A(w_out[0]), "pwq": A(peer_w_query[0]), "pkeys": A(peer_sub_keys[0]), "pu": A(peer_u[0]), "pv": A(peer_v[0]),
        "iota": np.ascontiguousarray(np.broadcast_to(np.arange(128, dtype=f)[None, :], (128, 128))),
    }
    for c in range(8):
        b, hh = divmod(c, 2)
        js = [own_tile_index(hh, i) for i in range(NQT)]
        own[c] = js
        xq = np.concatenate([x_prompt[b, j * 128:(j + 1) * 128] for j in js] + [x_sample[2 * c], x_sample[2 * c + 1]], axis=0)
        posq = np.concatenate([np.arange(j * 128, (j + 1) * 128) for j in js] + [np.arange(PAST, LS), np.arange(PAST, LS)])
        cosq, sinq = _rope_tabs(posq)
        qrel = np.zeros((NTOK, 1), f)
        for i, j in enumerate(js):
            nch = nchunks_for(i)
            base64 = 2 * (nch - min(4, nch))
            qrel[i * 128:(i + 1) * 128, 0] = (np.arange(j * 128, (j + 1) * 128) // 64) - base64
        cv = np.stack([A(c_prompt)[b], A(c_sample)[2 * c], A(c_sample)[2 * c + 1]], axis=0)
        cT = np.ascontiguousarray(cv.reshape(3, 32, 128).transpose(2, 1, 0).reshape(128, 96))
        m = dict(shared)
        m.update({
            "xk": x_prompt[b], "xq": np.ascontiguousarray(xq), "cT": cT, "cosq": cosq, "sinq": sinq, "qrel": qrel,
            "c_k": A(cache_dsa_k[0, 2 * c:2 * c + 2]).reshape(2, PAST, 512),
            "c_v": A(cache_dsa_v[0, 2 * c:2 * c + 2]).reshape(2, PAST, 512),
            "c_ki": A(cache_idx_k[0, 2 * c:2 * c + 2]).reshape(2, PAST, 128),
            "c_dk": A(cache_diff_k[0, 2 * c:2 * c + 2]).reshape(2, PAST, 2048),
            "c_dv": A(cache_diff_v[0, 2 * c:2 * c + 2]).reshape(2, PAST, 2048),
        })
        in_maps.append(m)
    names = set(_INPUT_NAMES)
    in_maps = [{k: v for k, v in m.items() if k in names} for m in in_maps]
    ncore = int(os.environ.get("MK_CORES", "8"))
    res = run_bass_kernel_spmd(nc, in_maps[:ncore], core_ids=list(range(ncore)))
    R = list(res.results)
    while len(R) < 8:
        R.append(R[0])
    global _LAST
    _LAST = R
    y_prompt = np.zeros((4, SEQ, D), f)
    y_sample = np.zeros((16, 32, D), f)
    for c in range(8):
        b = c // 2
        oy = R[c]["o_y"]
        for i, j in enumerate(own[c]):
            y_prompt[b, j * 128:(j + 1) * 128] = oy[i * 128:(i + 1) * 128]
        y_sample[2 * c] = oy[NQT * 128:NQT * 128 + 32]
        y_sample[2 * c + 1] = oy[NQT * 128 + 32:NQT * 128 + 64]

    def pk(name, shp):
        return np.stack([R[2 * b][name] for b in range(4)], axis=0).reshape((1, 4, SEQ) + shp)

    def sk(name, shp):
        return np.concatenate([R[c][name].reshape((2, 32) + shp) for c in range(8)], axis=0)[None]

    return (y_prompt, y_sample,
            pk("o_kp", (4, 128)), pk("o_vp", (4, 128)), pk("o_kip", (128,)), pk("o_dkp", (8, 2, 128)), pk("o_dvp", (8, 256)),
            sk("o_ks", (4, 128)), sk("o_vs", (4, 128)), sk("o_kis", (128,)), sk("o_dks", (8, 2, 128)), sk("o_dvs", (8, 256)))
```

```python
import os
import numpy as np
import concourse.bass as bass
import concourse.mybir as mybir
from concourse.bass_utils import run_bass_kernel_spmd

F32 = mybir.dt.float32
BF16 = mybir.dt.bfloat16
U32 = mybir.dt.uint32
ALU = mybir.AluOpType
AF = mybir.ActivationFunctionType
AX = mybir.AxisListType

ENGS = ("sync", "scalar", "vector", "gpsimd", "tensor")
NDS = 6

D = 4096
NQT = 16
NTOK = NQT * 128 + 64
SEQ = 4096
PAST = 1024
LS = PAST + 32
C_Q, C_K, C_V, C_QI, C_KI, C_WI, C_DQ, C_DK, C_DV, C_END = 0, 2048, 2560, 3072, 5120, 5248, 5264, 7312, 9360, 11408
EPS = 1e-6
NEG = -1.0e30
LAM_INIT = 0.8 - 0.6


class Buf:
    __slots__ = ("t", "lw", "rd")

    def __init__(self, t):
        self.t = t
        self.lw = set()
        self.rd = set()

    def __getitem__(self, k):
        return self.t[k]


class Prog:
    def __init__(self, nc):
        self.nc = nc
        self.eng = {"sync": nc.sync, "scalar": nc.scalar, "vector": nc.vector,
                    "gpsimd": nc.gpsimd, "tensor": nc.tensor}
        self.sem = {}
        self.cnt = {e: 0 for e in ENGS}
        self.known = {e: {} for e in ENGS}
        self.dsem = {}
        self.dcnt = {e: 0 for e in ENGS}
        self._stack = []
        self.ninstr = 0
        self.nwaits = 0

    def open(self):
        nc = self.nc
        for e in ENGS:
            cm = nc.semaphore("s_" + e)
            self.sem[e] = cm.__enter__()
            self._stack.append(cm)
        for e in ("sync", "gpsimd", "scalar"):
            self.dsem[e] = []
            for i in range(NDS):
                cm = nc.semaphore("d_%s%d" % (e, i))
                self.dsem[e].append(cm.__enter__())
                self._stack.append(cm)

    def close(self):
        for cm in reversed(self._stack):
            cm.__exit__(None, None, None)

    def _wait(self, engine, ev):
        if ev[0] == "c":
            _, p, v = ev
            if p == "tensor" and engine == "tensor":
                return
            key = ("c", p)
            if self.known[engine].get(key, 0) >= v:
                return
            self.eng[engine].wait_ge(self.sem[p], v)
        else:
            _, q, slot, v = ev
            key = ("d", q, slot)
            if self.known[engine].get(key, 0) >= v:
                return
            self.eng[engine].wait_ge(self.dsem[q][slot], v)
        self.known[engine][key] = v
        self.nwaits += 1

    def _deps(self, engine, reads, writes):
        deps = set()
        for b in reads:
            deps |= b.lw
        for b in writes:
            deps |= b.lw
            deps |= b.rd
        best = {}
        for ev in deps:
            key = ev[:-1]
            if key not in best or best[key][-1] < ev[-1]:
                best[key] = ev
        for key in sorted(best, key=str):
            self._wait(engine, best[key])

    def _commit(self, ev, reads, writes):
        for b in writes:
            b.lw = {ev}
            b.rd = set()
        for b in reads:
            if b not in writes:
                b.rd.add(ev)

    def op(self, engine, fn, reads=(), writes=()):
        self._deps(engine, reads, writes)
        ins = fn(self.eng[engine])
        self.cnt[engine] += 1
        ins.then_inc(self.sem[engine], 1)
        self._commit(("c", engine, self.cnt[engine]), reads, writes)
        self.ninstr += 1

    def dma(self, engine, out, in_, reads=(), writes=(), **kw):
        self._deps(engine, reads, writes)
        n = self.dcnt[engine]
        self.dcnt[engine] += 1
        slot = n % NDS
        ins = self.eng[engine].dma_start(out=out, in_=in_, **kw)
        ins.then_inc(self.dsem[engine][slot], 16)
        self._commit(("d", engine, slot, 16 * (n // NDS + 1)), reads, writes)
        self.ninstr += 1

    def barrier(self):
        evs = []
        for p in ENGS:
            if self.cnt[p] > 0:
                evs.append(("c", p, self.cnt[p]))
        for q in self.dsem:
            n = self.dcnt[q]
            for slot in range(NDS):
                k = (n - slot + NDS - 1) // NDS if n > slot else 0
                if k > 0:
                    evs.append(("d", q, slot, 16 * k))
        for e in ENGS:
            for ev in evs:
                self._wait(e, ev)


class Ctx:
    def __init__(self, nc):
        self.nc = nc
        self.stack = []
        self.k = 0

    def sb(self, shape, dt, name=None):
        self.k += 1
        cm = self.nc.sbuf_tensor("%s_%d" % (name or "t", self.k), list(shape), dt)
        t = cm.__enter__()
        self.stack.append(cm)
        return Buf(t)

    def ps(self, shape, dt, name=None):
        self.k += 1
        cm = self.nc.psum_tensor("%s_%d" % (name or "p", self.k), list(shape), dt)
        t = cm.__enter__()
        self.stack.append(cm)
        return Buf(t)

    def mark(self):
        return len(self.stack)

    def release(self, mark):
        while len(self.stack) > mark:
            self.stack.pop().__exit__(None, None, None)


class RR:
    def __init__(self, items):
        self.items = list(items)
        self.i = 0

    def next(self):
        x = self.items[self.i % len(self.items)]
        self.i += 1
        return x


def own_tile_index(hh, i):
    g, o = divmod(i, 2)
    if hh == 0:
        return 4 * g + (0 if o == 0 else 3)
    return 4 * g + (1 if o == 0 else 2)


def nchunks_for(i):
    g, o = divmod(i, 2)
    return 4 * g + (2 if o == 0 else 4)


STAGE = int(os.environ.get("MK_STAGE", "9"))
_INPUT_NAMES = []
_LAST = None


def build_program():
    del _INPUT_NAMES[:]
    nc = bass.Bass("TRN2", target_bir_lowering=False)
    P = Prog(nc)
    P.open()
    C = Ctx(nc)
    T = {}

    def din(name, shape, dt=F32):
        _INPUT_NAMES.append(name)
        T[name] = Buf(nc.dram_tensor(name, list(shape), dt, kind="ExternalInput").ap())
        return T[name]

    def dout(name, shape, dt=F32):
        T[name] = Buf(nc.dram_tensor(name, list(shape), dt, kind="ExternalOutput").ap())
        return T[name]

    def dscr(name, shape, dt=BF16):
        kind = "ExternalOutput" if os.environ.get("MK_DEBUG") else "Internal"
        T[name] = Buf(nc.dram_tensor(name, list(shape), dt, kind=kind).ap())
        return T[name]

    xk = din("xk", [SEQ, D])
    xq = din("xq", [NTOK, D])
    cT = din("cT", [128, 96])
    w_ada = din("w_ada", [D, 6 * D])
    badaT = din("badaT", [128, 192])
    gmixT = din("gmixT", [128, 32])
    gffnT = din("gffnT", [128, 32])
    gfinB = din("gfinB", [128, D])
    w_in = din("w_in", [D, C_END])
    cosk = din("cosk", [SEQ, 128])
    sink = din("sink", [SEQ, 128])
    cosq = din("cosq", [NTOK, 128])
    sinq = din("sinq", [NTOK, 128])
    ident_in = din("ident", [128, 128])
    c_k = din("c_k", [2, PAST, 512])
    c_v = din("c_v", [2, PAST, 512])
    c_ki = din("c_ki", [2, PAST, 128])
    c_dk = din("c_dk", [2, PAST, 2048])
    c_dv = din("c_dv", [2, PAST, 2048])
    kcl_in = din("kcl", [128, 512])
    qrel_in = din("qrel", [NTOK, 1])
    lamv_in = din("lamv", [128, 4, 128])
    gsub_in = din("gsub", [128, 256])
    w_out = din("w_out", [D, D])
    pwq_in = din("pwq", [D, 2048])
    pkeys_in = din("pkeys", [8, 2, 128, 128])
    iota_in = din("iota", [128, 128])
    pu_in = din("pu", [16384, D])
    pv_in = din("pv", [16384, D])
    o_kp = dout("o_kp", [SEQ, 512])
    o_vp = dout("o_vp", [SEQ, 512])
    o_kip = dout("o_kip", [SEQ, 128])
    o_dkp = dout("o_dkp", [SEQ, 2048])
    o_dvp = dout("o_dvp", [SEQ, 2048])
    o_ks = dout("o_ks", [64, 512])
    o_vs = dout("o_vs", [64, 512])
    o_kis = dout("o_kis", [64, 128])
    o_dks = dout("o_dks", [64, 2048])
    o_dvs = dout("o_dvs", [64, 2048])
    o_y = dout("o_y", [NTOK, D])
    kT_s = dscr("kT_s", [4, 128, SEQ])
    v_s = dscr("v_s", [SEQ, 512])
    kiT_s = dscr("kiT_s", [128, SEQ])
    dkT_s = dscr("dkT_s", [16, 128, SEQ])
    dv_s = dscr("dv_s", [SEQ, 2048])
    skT_s = dscr("skT_s", [2, 4, 128, LS])
    sv_s = dscr("sv_s", [2, LS, 512])
    skiT_s = dscr("skiT_s", [2, 128, LS])
    sdkT_s = dscr("sdkT_s", [2, 16, 128, LS])
    sdv_s = dscr("sdv_s", [2, LS, 2048])
    qT_s = dscr("qT_s", [16, 128, NTOK])
    qiT_s = dscr("qiT_s", [16, 128, NTOK])
    dqT_s = dscr("dqT_s", [16, 128, NTOK])
    wi_s = dscr("wi_s", [NTOK, 16], F32)
    ga_s = dscr("ga_s", [4, 128, D], F32)

    attnT_s = dscr("attnT_s", [128, 32, NTOK])
    x1_s = dscr("x1_s", [NTOK, D], F32)
    h2T_s = dscr("h2T_s", [128, 32, NTOK])
    s_s = dscr("s_s", [NTOK, 2048], F32)
    G_s = dscr("G_s", [17, 128, 128, 128])
    uT_c = dscr("uT_c", [128, 128, D])
    v_c = dscr("v_c", [128, 128, D])
    ident = C.sb([128, 128], F32, "ident")
    identb = C.sb([128, 128], BF16, "identb")
    modT = C.sb([128, 192, 3], F32, "modT")
    A1 = C.sb([128, 32, 3], F32, "A1")
    A2 = C.sb([128, 32, 3], F32, "A2")
    P.dma("sync", ident[:, :], ident_in[:, :], [ident_in], [ident])
    P.op("vector", lambda e: e.tensor_copy(out=identb[:, :], in_=ident[:, :]), [ident], [identb])

    mk = C.mark()
    psF = [C.ps([128, 512], F32, "psF") for _ in range(6)]
    psB = [C.ps([128, 1024], BF16, "psB") for _ in range(2)]
    scT = C.sb([128, 96], F32, "scT")
    wst = [C.sb([128, 12288], F32, "wst") for _ in range(2)]
    pm = [psF[0], psF[1]]
    bada = C.sb([128, 192], F32, "bada")
    gm = C.sb([128, 32], F32, "gm")
    gf = C.sb([128, 32], F32, "gf")
    P.dma("sync", scT[:, :], cT[:, :], [cT], [scT])
    P.dma("sync", bada[:, :], badaT[:, :], [badaT], [bada])
    P.dma("sync", gm[:, :], gmixT[:, :], [gmixT], [gm])
    P.dma("sync", gf[:, :], gffnT[:, :], [gffnT], [gf])
    sg = C.sb([128, 96], F32, "sg")
    P.op("scalar", lambda e: e.activation(out=sg[:, :], in_=scT[:, :], func=AF.Exp, scale=-1.0), [scT], [sg])
    P.op("vector", lambda e: e.tensor_scalar(out=sg[:, :], in0=sg[:, :], scalar1=1.0, scalar2=None, op0=ALU.add), [sg], [sg])
    P.op("vector", lambda e: e.reciprocal(out=sg[:, :], in_=sg[:, :]), [sg], [sg])
    P.op("vector", lambda e: e.tensor_tensor(out=scT[:, :], in0=scT[:, :], in1=sg[:, :], op=ALU.mult), [scT, sg], [scT])
    scTb = C.sb([128, 96], BF16, "scTb")
    wbA = [C.sb([128, 12288], BF16, "wbA") for _ in range(2)]
    wbS = [[Buf(wb_.t[:, i_ * 4096:(i_ + 1) * 4096]) for i_ in range(3)] for wb_ in wbA]
    P.op("vector", lambda e: e.tensor_copy(out=scTb[:, :], in_=scT[:, :]), [scT], [scTb])
    k = 0
    for cblk in range(64):
        buf = wst[k % 2]
        sls = wbS[k % 2]
        k += 1
        P.dma("sync", buf[:, 0:32 * 384].rearrange("p (c w) -> p c w", w=384),
              w_ada[:, cblk * 384:(cblk + 1) * 384].rearrange("(c p) w -> p c w", p=128), [w_ada], [buf])
        for i_, en in enumerate(("vector", "scalar", "gpsimd")):
            sb_ = sls[i_]
            if en == "scalar":
                P.op(en, lambda e, i_=i_, sb_=sb_, buf=buf: e.copy(out=sb_[:, :], in_=buf[:, i_ * 4096:(i_ + 1) * 4096]), [buf], [sb_])
            else:
                P.op(en, lambda e, i_=i_, sb_=sb_, buf=buf: e.tensor_copy(out=sb_[:, :], in_=buf[:, i_ * 4096:(i_ + 1) * 4096]),
                     [buf], [sb_])
        for c3 in range(3):
            cb = cblk * 3 + c3
            half, cbl = divmod(cb, 96)
            for dch in range(32):
                col = dch * 384 + c3 * 128
                sb_ = sls[col // 4096]
                lc = col % 4096
                P.op("tensor", lambda e, sb_=sb_, lc=lc, cbl=cbl, half=half, dch=dch: e.matmul(
                    out=pm[half][:, cbl * 3:cbl * 3 + 3], lhsT=sb_[:, lc:lc + 128],
                    rhs=scTb[:, dch * 3:dch * 3 + 3], start=(dch == 0), stop=(dch == 31)),
                    [sb_, scTb], [pm[half]])
    for half in range(2):
        P.op("vector", lambda e, half=half: e.tensor_tensor(
            out=modT[:, half * 96:(half + 1) * 96, :],
            in0=pm[half][:, 0:288].rearrange("p (c r) -> p c r", r=3),
            in1=bada[:, half * 96:(half + 1) * 96].unsqueeze(2).to_broadcast([128, 96, 3]), op=ALU.add),
            [pm[half], bada], [modT])
    for (Ax, base, g) in ((A1, 32, gm), (A2, 128, gf)):
        P.op("vector", lambda e, Ax=Ax, base=base: e.tensor_scalar(
            out=Ax[:, :, :], in0=modT[:, base:base + 32, :], scalar1=1.0, scalar2=None, op0=ALU.add), [modT], [Ax])
        P.op("vector", lambda e, Ax=Ax, g=g: e.tensor_tensor(
            out=Ax[:, :, :], in0=Ax[:, :, :], in1=g[:, :].unsqueeze(2).to_broadcast([128, 32, 3]), op=ALU.mult),
            [Ax, g], [Ax])
    Dm = [C.sb([128, D], F32, "Dm") for _ in range(2)]
    L = C.sb([128, 3, 128], F32, "L")
    gat = [C.sb([128, 512], F32, "gat") for _ in range(2)]
    P.op("vector", lambda e: e.memset(L[:, 0, :], 1.0), [], [L])
    P.op("vector", lambda e: e.memset(L[:, 1:3, :], 0.0), [L], [L])
    P.op("vector", lambda e: e.memset(L[:, 1, 0:32], 1.0), [L], [L])
    P.op("vector", lambda e: e.memset(L[:, 2, 32:64], 1.0), [L], [L])
    eng2 = RR(["vector", "gpsimd"])
    kk = 0
    for which, base in ((0, 64), (1, 160)):
        for grp in range(2):
            rs = [0] if grp == 0 else [1, 2]
            for ri, r in enumerate(rs):
                for ch in range(32):
                    P.op(eng2.next(), lambda e, ri=ri, r=r, ch=ch, base=base: e.tensor_scalar(
                        out=Dm[ri][:, ch * 128:(ch + 1) * 128], in0=ident[:, :],
                        scalar1=modT[:, base + ch, r:r + 1], scalar2=None, op0=ALU.mult),
                        [ident, modT], [Dm[ri]])
            for blk in range(8):
                pb = psF[2 + kk % 2]
                gt = gat[kk % 2]
                kk += 1
                for ri, r in enumerate(rs):
                    P.op("tensor", lambda e, ri=ri, r=r, blk=blk, pb=pb, n=len(rs): e.matmul(
                        out=pb[:, :], lhsT=L[:, r, :], rhs=Dm[ri][:, blk * 512:(blk + 1) * 512],
                        start=(ri == 0), stop=(ri == n - 1)), [L, Dm[ri]], [pb])
                P.op("scalar", lambda e, pb=pb, gt=gt: e.copy(out=gt[:, :], in_=pb[:, :]), [pb], [gt])
                P.dma("gpsimd", ga_s[grp * 2 + which, :, blk * 512:(blk + 1) * 512], gt[:, :], [gt], [ga_s])
    P.barrier()
    C.release(mk)

    mk = C.mark()
    psF = [C.ps([128, 512], F32, "psF") for _ in range(6)]
    psB = [C.ps([128, 1024], BF16, "psB") for _ in range(2)]
    xts = RR([C.sb([128, D], F32, "xt") for _ in range(2)])
    sst = RR([C.sb([128, 2], F32, "ss") for _ in range(2)])
    hTs = [C.sb([128, 32, 128], BF16, "hT") for _ in range(9)]
    wsts = RR([C.sb([128, 4, 512], F32, "wst") for _ in range(2)])
    wbfs = RR([C.sb([128, 32, 512], BF16, "wbf") for _ in range(2)])
    ofs = RR([C.sb([128, 512], F32, "of") for _ in range(2)])
    tmps = RR([C.sb([128, 512], F32, "tmp") for _ in range(2)])
    obs = RR([C.sb([128, 512], BF16, "ob") for _ in range(3)])
    tbs = RR([C.sb([128, 4, 128], BF16, "tb") for _ in range(3)])
    tabs = RR([(C.sb([128, 128], F32, "cs"), C.sb([128, 128], F32, "sn")) for _ in range(3)])
    ps_proj = RR(psF[0:3])
    ps_xT = RR(psF[3:6])
    ps_hT = RR(psB)
    evac = RR(["vector", "scalar"])
    castq = RR(["gpsimd"])

    def weight_stream(specs):
        nxt = load_weights(*specs[0]) if specs else None
        for i_ in range(len(specs)):
            cur = nxt
            nxt = load_weights(*specs[i_ + 1]) if i_ + 1 < len(specs) else None
            yield cur

    def load_norm_T(src, row0, n, hT, prompt, Ax, shbase):
        xt = xts.next()
        ss = sst.next()
        P.dma("sync", xt[0:n, :], src[row0:row0 + n, :], [src], [xt])
        P.op("vector", lambda e: e.memset(ss[:, :], 0.0), [], [ss])
        P.op("scalar", lambda e: e.activation(out=hT[0:n, :, :].rearrange("p c t -> p (c t)"), in_=xt[0:n, :], func=AF.Square,
                                              accum_out=ss[0:n, 0:1]), [xt], [hT, ss])
        P.op("vector", lambda e: e.tensor_scalar(out=ss[0:n, 1:2], in0=ss[0:n, 0:1], scalar1=1.0 / D, scalar2=EPS,
                                                 op0=ALU.mult, op1=ALU.add), [ss], [ss])
        P.op("scalar", lambda e: e.activation(out=ss[0:n, 1:2], in_=ss[0:n, 1:2], func=AF.Sqrt), [ss], [ss])
        P.op("vector", lambda e: e.reciprocal(out=ss[0:n, 1:2], in_=ss[0:n, 1:2]), [ss], [ss])
        P.op("scalar", lambda e: e.activation(out=xt[0:n, :], in_=xt[0:n, :], func=AF.Copy, scale=ss[0:n, 1:2]), [xt, ss], [xt])
        for c4 in range(8):
            pb = ps_xT.next()
            for j in range(4):
                ch = c4 * 4 + j
                P.op("tensor", lambda e, ch=ch, j=j, pb=pb: e.transpose(
                    out=pb[:, j * 128:j * 128 + n], in_=xt[0:n, ch * 128:(ch + 1) * 128], identity=ident[0:n, 0:n]),
                    [xt, ident], [pb])
            for j in range(4):
                ch = c4 * 4 + j
                segs = [(0, n, 0)] if prompt else [(0, 32, 1), (32, 64, 2)]
                for (a, b_, r) in segs:
                    en = evac.next()
                    if en == "vector":
                        P.op("vector", lambda e, ch=ch, j=j, pb=pb, a=a, b_=b_, r=r: e.tensor_scalar(
                            out=hT[:, ch, a:b_], in0=pb[:, j * 128 + a:j * 128 + b_], scalar1=Ax[:, ch, r:r + 1],
                            scalar2=modT[:, shbase + ch, r:r + 1], op0=ALU.mult, op1=ALU.add),
                            [pb, Ax, modT], [hT])
                    else:
                        P.op("scalar", lambda e, ch=ch, j=j, pb=pb, a=a, b_=b_, r=r: e.activation(
                            out=hT[:, ch, a:b_], in_=pb[:, j * 128 + a:j * 128 + b_], func=AF.Identity,
                            scale=Ax[:, ch, r:r + 1], bias=modT[:, shbase + ch, r:r + 1]),
                            [pb, Ax, modT], [hT])
        return xt

    def load_weights(wsrc, c0, W):
        wbf = wbfs.next()
        for p4 in range(8):
            ws = wsts.next()
            P.dma("sync", ws[:, :, 0:W],
                  wsrc[p4 * 512:(p4 + 1) * 512, c0:c0 + W].rearrange("(c p) w -> p c w", p=128), [wsrc], [ws])
            en = castq.next()
            if en == "scalar":
                P.op("scalar", lambda e, ws=ws, p4=p4: e.copy(out=wbf[:, p4 * 4:(p4 + 1) * 4, 0:W], in_=ws[:, :, 0:W]),
                     [ws], [wbf])
            else:
                P.op("gpsimd", lambda e, ws=ws, p4=p4: e.tensor_copy(out=wbf[:, p4 * 4:(p4 + 1) * 4, 0:W], in_=ws[:, :, 0:W]),
                     [ws], [wbf])
        return wbf

    def project(hT, n, wbf, W):
        pb = ps_proj.next()
        for ch in range(32):
            P.op("tensor", lambda e, ch=ch: e.matmul(out=pb[0:n, 0:W], lhsT=hT[:, ch, 0:n], rhs=wbf[:, ch, 0:W],
                                                     start=(ch == 0), stop=(ch == 31)), [hT, wbf], [pb])
        return pb

    def epilogue(src, sbuf, n, W, rope=None, f32dst=None, tokdst=None, featdst=None):
        H = max(W // 128, 1)
        of = ofs.next()
        if rope is not None:
            cs, sn = rope
            tmp = tmps.next()
            s3 = src.rearrange("p (h d) -> p h d", d=128)
            o3 = of[0:n, 0:W].rearrange("p (h d) -> p h d", d=128)
            t3 = tmp[0:n, 0:W].rearrange("p (h d) -> p h d", d=128)
            P.op("vector", lambda e: e.tensor_tensor(out=o3, in0=s3, in1=cs[0:n, :].unsqueeze(1).to_broadcast([n, H, 128]),
                                                     op=ALU.mult), [sbuf, cs], [of])
            P.op("vector", lambda e: e.tensor_tensor(out=t3[:, :, 0:64], in0=s3[:, :, 64:128],
                                                     in1=sn[0:n, 0:64].unsqueeze(1).to_broadcast([n, H, 64]),
                                                     op=ALU.mult), [sbuf, sn], [tmp])
            P.op("vector", lambda e: e.tensor_tensor(out=t3[:, :, 64:128], in0=s3[:, :, 0:64],
                                                     in1=sn[0:n, 64:128].unsqueeze(1).to_broadcast([n, H, 64]),
                                                     op=ALU.mult), [sbuf, sn], [tmp])
            P.op("vector", lambda e: e.tensor_tensor(out=of[0:n, 0:W], in0=of[0:n, 0:W], in1=tmp[0:n, 0:W], op=ALU.add),
                 [of, tmp], [of])
            cur, curb = of[0:n, 0:W], of
        elif f32dst is not None:
            P.op("scalar", lambda e: e.copy(out=of[0:n, 0:W], in_=src), [sbuf], [of])
            cur, curb = of[0:n, 0:W], of
        else:
            cur, curb = src, sbuf
        if f32dst is not None:
            P.dma("scalar", f32dst[1], cur, [curb], [f32dst[0]])
        if tokdst is None and featdst is None:
            return
        ob = obs.next()
        P.op("scalar", lambda e: e.copy(out=ob[0:n, 0:W], in_=cur), [curb], [ob])
        if tokdst is not None:
            for (db, dap, r0, r1) in tokdst:
                P.dma("scalar", dap, ob[r0:r1, 0:W], [ob], [db])
        if featdst is not None:
            pb = ps_hT.next()
            tb = tbs.next()
            for h in range(H):
                P.op("tensor", lambda e, h=h: e.transpose(out=pb[:, h * 128:h * 128 + n], in_=ob[0:n, h * 128:(h + 1) * 128],
                                                          identity=identb[0:n, 0:n]), [ob, identb], [pb])
            en = evac.next()
            pv = pb[:, 0:H * 128].rearrange("p (h t) -> p h t", t=128)[:, :, 0:n]
            if en == "vector":
                P.op("vector", lambda e: e.tensor_copy(out=tb[:, 0:H, 0:n], in_=pv), [pb], [tb])
            else:
                P.op("scalar", lambda e: e.copy(out=tb[:, 0:H, 0:n], in_=pv), [pb], [tb])
            for (db, apfn, c0, c1) in featdst:
                P.dma("scalar", apfn(H), tb[:, 0:H, c0:c1], [tb], [db])

    def featT(buf, h0, t0, ncols, lead=None):
        def fn(H):
            base = buf.t if lead is None else buf.t[lead]
            return base[h0:h0 + H, :, t0:t0 + ncols].rearrange("h d t -> d h t")
        return fn

    def featT1(buf, t0, ncols, lead=None):
        def fn(H):
            base = buf.t if lead is None else buf.t[lead]
            return base[:, t0:t0 + ncols].unsqueeze(1)
        return fn

    KBLOCKS = [("k", C_K, 512, 0), ("v", C_V, 512, 0), ("ki", C_KI, 144, 0)] + \
              [("dk", C_DK + 512 * i, 512, 4 * i) for i in range(4)] + [("dv", C_DV + 512 * i, 512, 4 * i) for i in range(4)]
    QBLOCKS = [("q", C_Q + 512 * i, 512, 4 * i) for i in range(4)] + [("qi", C_QI + 512 * i, 512, 4 * i) for i in range(4)] + \
              [("ki", C_KI, 144, 0)] + [("dq", C_DQ + 512 * i, 512, 4 * i) for i in range(4)]

    def load_tabs(cb, sb_, row0, n):
        cs, sn = tabs.next()
        P.dma("sync", cs[0:n, :], cb[row0:row0 + n, :], [cb], [cs])
        P.dma("sync", sn[0:n, :], sb_[row0:row0 + n, :], [sb_], [sn])
        return cs, sn

    def k_epilogue(kind, pb, n, W, h0, tb_, prompt, tok0):
        src = pb[0:n, 0:W]
        if kind == "ki":
            src = pb[0:n, 0:128]
            W = 128
        ci = C_DK if kind == "dk" else C_DV
        if prompt:
            if kind == "k":
                epilogue(src, pb, n, W, rope=tb_, f32dst=(o_kp, o_kp[tok0:tok0 + n, :]),
                         featdst=[(kT_s, featT(kT_s, 0, tok0, n), 0, n)])
            elif kind == "v":
                epilogue(src, pb, n, W, f32dst=(o_vp, o_vp[tok0:tok0 + n, :]), tokdst=[(v_s, v_s[tok0:tok0 + n, :], 0, n)])
            elif kind == "ki":
                epilogue(src, pb, n, W, rope=tb_, f32dst=(o_kip, o_kip[tok0:tok0 + n, :]),
                         featdst=[(kiT_s, featT1(kiT_s, tok0, n), 0, n)])
            elif kind == "dk":
                epilogue(src, pb, n, W, rope=tb_, f32dst=(o_dkp, o_dkp[tok0:tok0 + n, h0 * 128:h0 * 128 + W]),
                         featdst=[(dkT_s, featT(dkT_s, h0, tok0, n), 0, n)])
            elif kind == "dv":
                epilogue(src, pb, n, W, f32dst=(o_dvp, o_dvp[tok0:tok0 + n, h0 * 128:h0 * 128 + W]),
                         tokdst=[(dv_s, dv_s[tok0:tok0 + n, h0 * 128:h0 * 128 + W], 0, n)])
        else:
            if kind == "k":
                epilogue(src, pb, n, W, rope=tb_, f32dst=(o_ks, o_ks[:, :]),
                         featdst=[(skT_s, featT(skT_s, 0, PAST, 32, lead=s), 32 * s, 32 * s + 32) for s in range(2)])
            elif kind == "v":
                epilogue(src, pb, n, W, f32dst=(o_vs, o_vs[:, :]),
                         tokdst=[(sv_s, sv_s[s, PAST:LS, :], 32 * s, 32 * s + 32) for s in range(2)])
            elif kind == "ki":
                epilogue(src, pb, n, W, rope=tb_, f32dst=(o_kis, o_kis[:, :]),
                         featdst=[(skiT_s, featT1(skiT_s, PAST, 32, lead=s), 32 * s, 32 * s + 32) for s in range(2)])
            elif kind == "dk":
                epilogue(src, pb, n, W, rope=tb_, f32dst=(o_dks, o_dks[:, h0 * 128:h0 * 128 + W]),
                         featdst=[(sdkT_s, featT(sdkT_s, h0, PAST, 32, lead=s), 32 * s, 32 * s + 32) for s in range(2)])
            elif kind == "dv":
                epilogue(src, pb, n, W, f32dst=(o_dvs, o_dvs[:, h0 * 128:h0 * 128 + W]),
                         tokdst=[(sdv_s, sdv_s[s, PAST:LS, h0 * 128:h0 * 128 + W], 32 * s, 32 * s + 32) for s in range(2)])

    def q_epilogue(kind, pb, n, W, h0, tb_, tok0):
        if kind == "ki":
            P.op("scalar", lambda e: e.copy(out=wit[0:n, :], in_=pb[0:n, 128:144]), [pb], [wit])
            P.dma("gpsimd", wi_s[tok0:tok0 + n, :], wit[0:n, :], [wit], [wi_s])
            return
        dst = {"q": qT_s, "qi": qiT_s, "dq": dqT_s}[kind]
        epilogue(pb[0:n, 0:W], pb, n, W, rope=tb_, featdst=[(dst, featT(dst, h0, tok0, n), 0, n)])

    wit = C.sb([128, 16], F32, "wit")

    NKT = int(os.environ.get("MK_NKT", "32"))
    for g0 in range(0, NKT, 8):
        tiles = list(range(g0, min(g0 + 8, NKT)))
        tabl = {}
        for si, j in enumerate(tiles):
            load_norm_T(xk, j * 128, 128, hTs[si], True, A1, 0)
        for (kind, c0, W, h0), wbf in zip(KBLOCKS, weight_stream([(w_in, b_[1], b_[2]) for b_ in KBLOCKS])):
            pend = None
            for si, j in enumerate(tiles):
                pb = project(hTs[si], 128, wbf, W)
                if pend is not None:
                    pend()
                tb_ = load_tabs(cosk, sink, j * 128, 128) if kind in ("k", "ki", "dk") else None
                pend = (lambda kind=kind, pb=pb, W=W, h0=h0, tb_=tb_, j=j: k_epilogue(kind, pb, 128, W, h0, tb_, True, j * 128))
            pend()
    NQ = int(os.environ.get("MK_NQT", str(NQT)))
    for grp in range(2):
        tiles = list(range(grp * 8, min(grp * 8 + 8, NQ)))
        slots = [(si, i, 128, True) for si, i in enumerate(tiles)]
        if grp == 1:
            slots.append((8, NQT, 64, False))
        for (si, i, n, prompt) in slots:
            load_norm_T(xq, i * 128, n, hTs[si], prompt, A1, 0)
        for (kind, c0, W, h0), wbf in zip(QBLOCKS, weight_stream([(w_in, b_[1], b_[2]) for b_ in QBLOCKS])):
            pend = None
            for (si, i, n, prompt) in slots:
                pb = project(hTs[si], n, wbf, W)
                if pend is not None:
                    pend()
                tb_ = load_tabs(cosq, sinq, i * 128, n) if kind != "ki" or not prompt else None

                def pend(kind=kind, pb=pb, n=n, W=W, h0=h0, tb_=tb_, i=i, prompt=prompt):
                    q_epilogue(kind, pb, n, W, h0, tb_, i * 128)
                    if kind == "ki" and not prompt:
                        k_epilogue("ki", pb, n, W, 0, tb_, False, 0)
            pend()
        if grp == 1:
            for (kind, c0, W, h0) in KBLOCKS:
                if kind == "ki":
                    continue
                wbf = load_weights(w_in, c0, W)
                pb = project(hTs[8], 64, wbf, W)
                tb_ = load_tabs(cosq, sinq, NQT * 128, 64) if kind in ("k", "dk") else None
                k_epilogue(kind, pb, 64, W, h0, tb_, False, 0)
    if STAGE >= 2:
        cin = RR([C.sb([128, 512], F32, "cin") for _ in range(1)])
        for s in range(2):
            for t in range(PAST // 128):
                t0 = t * 128

                def ing(srcb, sap, W, **kw):
                    cb = cin.next()
                    P.dma("sync", cb[:, 0:W], sap, [srcb], [cb])
                    epilogue(cb[:, 0:W], cb, 128, W, **kw)
                ing(c_k, c_k[s, t0:t0 + 128, :], 512, featdst=[(skT_s, featT(skT_s, 0, t0, 128, lead=s), 0, 128)])
                ing(c_v, c_v[s, t0:t0 + 128, :], 512, tokdst=[(sv_s, sv_s[s, t0:t0 + 128, :], 0, 128)])
                ing(c_ki, c_ki[s, t0:t0 + 128, :], 128, featdst=[(skiT_s, featT1(skiT_s, t0, 128, lead=s), 0, 128)])
                for hb in range(4):
                    ing(c_dk, c_dk[s, t0:t0 + 128, hb * 512:(hb + 1) * 512], 512,
                        featdst=[(sdkT_s, featT(sdkT_s, hb * 4, t0, 128, lead=s), 0, 128)])
                    ing(c_dv, c_dv[s, t0:t0 + 128, hb * 512:(hb + 1) * 512], 512,
                        tokdst=[(sdv_s, sdv_s[s, t0:t0 + 128, hb * 512:(hb + 1) * 512], 0, 128)])
    P.barrier()
    C.release(mk)
    mk = C.mark()
    psI = RR([C.ps([128, 512], F32, "psI") for _ in range(3)])
    psO = [C.ps([128, 512], F32, "psO") for _ in range(4)]
    psT = RR([C.ps([128, 1024], BF16, "psT") for _ in range(1)])
    kcl = C.sb([128, 512], F32, "kcl")
    gsubB = C.sb([128, 256], F32, "gsubB")
    lamv = C.sb([128, 4, 128], F32, "lamv")
    lamt = C.sb([128, 4], F32, "lamt")
    neglam = C.sb([128, 1], F32, "neglam")
    P.dma("sync", kcl[:, :], kcl_in[:, :], [kcl_in], [kcl])
    P.dma("sync", gsubB[:, :], gsub_in[:, :], [gsub_in], [gsubB])
    P.dma("sync", lamv[:, :, :], lamv_in[:, :, :], [lamv_in], [lamv])
    P.op("vector", lambda e: e.tensor_scalar(out=gsubB[:, :], in0=gsubB[:, :], scalar1=1.0 - LAM_INIT, scalar2=None,
                                             op0=ALU.mult), [gsubB], [gsubB])
    P.op("vector", lambda e: e.tensor_tensor(out=lamv[:, 0, :], in0=lamv[:, 0, :], in1=lamv[:, 1, :], op=ALU.mult), [lamv], [lamv])
    P.op("vector", lambda e: e.tensor_tensor(out=lamv[:, 2, :], in0=lamv[:, 2, :], in1=lamv[:, 3, :], op=ALU.mult), [lamv], [lamv])
    P.op("vector", lambda e: e.reduce_sum(out=lamt[:, 0:1], in_=lamv[:, 0, :], axis=AX.X), [lamv], [lamt])
    P.op("vector", lambda e: e.reduce_sum(out=lamt[:, 1:2], in_=lamv[:, 2, :], axis=AX.X), [lamv], [lamt])
    P.op("scalar", lambda e: e.activation(out=lamt[:, 2:4], in_=lamt[:, 0:2], func=AF.Exp), [lamt], [lamt])
    P.op("vector", lambda e: e.tensor_tensor(out=neglam[:, :], in0=lamt[:, 3:4], in1=lamt[:, 2:3], op=ALU.subtract), [lamt], [neglam])
    P.op("vector", lambda e: e.tensor_scalar(out=neglam[:, :], in0=neglam[:, :], scalar1=-LAM_INIT, scalar2=None, op0=ALU.add),
         [neglam], [neglam])

    kiT = C.sb([128, SEQ], BF16, "kiT")
    qiT = C.sb([128, 16, 128], BF16, "qiT")
    wiq = C.sb([128, 16], F32, "wiq")
    qrel = C.sb([128, 1], F32, "qrel")
    Iw = C.sb([128, SEQ], F32, "Iw")
    wk = C.sb([128, SEQ], F32, "wk")
    rls = RR([C.sb([128, 512], F32, "rl") for _ in range(2)])
    pen = C.sb([128, 512], F32, "pen")
    m8 = C.sb([128, 8], F32, "m8")
    thr = C.sb([128, 1], F32, "thr")
    maskb = C.sb([128, SEQ], BF16, "maskb")
    maskT = C.sb([128, 32, 128], BF16, "maskT")
    visb = C.sb([128, 512], BF16, "visb")
    visT = C.sb([128, 4, 128], BF16, "visT")
    kTs = RR([C.sb([128, SEQ], BF16, "kTg") for _ in range(2)])
    vts = [C.sb([128, 32, 129], BF16, "vt") for _ in range(2)]
    qgs = RR([C.sb([128, 4, 128], BF16, "qg") for _ in range(2)])
    Pts = RR([C.sb([128, 4, 128], BF16, "Pt") for _ in range(3)])
    Pms = RR([C.sb([128, 4, 128], BF16, "Pm") for _ in range(3)])
    dkTs = RR([C.sb([128, 2, SEQ], BF16, "dkT") for _ in range(2)])
    dvts = [C.sb([128, 32, 257], BF16, "dvt") for _ in range(2)]
    dqs = RR([C.sb([128, 2, 128], BF16, "dq") for _ in range(2)])
    rden = C.sb([128, 8], F32, "rden")
    Osb = [C.sb([128, 129], F32, "Osb") for _ in range(4)]
    Od = [C.sb([128, 257], F32, "Od") for _ in range(2)]
    negc = C.sb([128, 2], F32, "negc")
    P.op("vector", lambda e: e.memset(negc[:, 0:1], -1.0), [], [negc])
    P.op("vector", lambda e: e.memset(negc[:, 1:2], -0.5), [negc], [negc])
    tmpd = C.sb([128, 256], F32, "tmpd")
    dof = C.sb([128, 256], F32, "dof")
    junkd = C.sb([128, 256], BF16, "junkd")
    ssd = C.sb([128, 2], F32, "ssd")
    attn = C.sb([128, D], BF16, "attn")
    tbc = RR([C.sb([128, 8, 128], BF16, "tbc") for _ in range(2)])
    for vt in vts:
        P.op("vector", lambda e, vt=vt: e.memset(vt[:, :, 128:129], 1.0), [], [vt])
    for dvt in dvts:
        P.op("vector", lambda e, dvt=dvt: e.memset(dvt[:, :, 256:257], 1.0), [], [dvt])
    vti = [0]
    dvi = [0]

    def transposes_to(srcbuf, src_fn, dstbuf, dst_fn, chunks, nq):
        for b0 in range(0, len(chunks), 8):
            grp = chunks[b0:b0 + 8]
            pb = psT.next()
            for s_, (k0, kl) in enumerate(grp):
                P.op("tensor", lambda e, s_=s_, k0=k0, kl=kl: e.transpose(
                    out=pb[0:kl, s_ * 128:s_ * 128 + nq], in_=src_fn(k0, kl), identity=identb[0:nq, 0:nq]),
                    [srcbuf, identb], [pb])
            full = [x for x in grp if x[1] == 128]
            if full:
                nf = len(full)
                P.op("scalar", lambda e, b0=b0, nf=nf: e.copy(
                    out=dst_fn(b0, nf, 128), in_=pb[:, 0:nf * 128].rearrange("p (c t) -> p c t", t=128)[:, :, 0:nq]),
                    [pb], [dstbuf])
            for s_, (k0, kl) in enumerate(grp):
                if kl != 128:
                    P.op("scalar", lambda e, s_=s_, kl=kl, b0=b0: e.copy(
                        out=dst_fn(b0 + s_, 1, kl), in_=pb[0:kl, s_ * 128:s_ * 128 + nq].unsqueeze(1)), [pb], [dstbuf])

    def geom(nkeys, prompt):
        chunks = [(k0, min(128, nkeys - k0)) for k0 in range(0, nkeys, 128)]
        nchk = len(chunks)
        nfull = nkeys // 128
        nv = min(4, nchk) if prompt else 0
        return chunks, nchk, nfull, nv

    def idx_topk(tok0, nq, nkeys, prompt, S):
        chunks, nchk, nfull, nv = geom(nkeys, prompt)
        P.dma("sync", qiT[:, :, 0:nq], qiT_s[:, :, tok0:tok0 + nq].rearrange("h d t -> d h t"), [qiT_s], [qiT])
        P.dma("sync", wiq[0:nq, :], wi_s[tok0:tok0 + nq, :], [wi_s], [wiq])
        for kb0 in range(0, nkeys, 512):
            kw = min(512, nkeys - kb0)
            for h in range(16):
                pb = psI.next()
                P.op("tensor", lambda e, h=h, pb=pb: e.matmul(out=pb[0:nq, 0:kw], lhsT=qiT[:, h, 0:nq], rhs=kiT[:, kb0:kb0 + kw],
                                                              start=True, stop=True), [qiT, kiT], [pb])
                rl = rls.next()
                P.op("scalar", lambda e, pb=pb, rl=rl: e.activation(out=rl[0:nq, 0:kw], in_=pb[0:nq, 0:kw], func=AF.Relu), [pb], [rl])
                if h == 0:
                    P.op("vector", lambda e, rl=rl: e.tensor_scalar(out=Iw[0:nq, kb0:kb0 + kw], in0=rl[0:nq, 0:kw],
                                                                    scalar1=wiq[0:nq, 0:1], scalar2=None, op0=ALU.mult),
                         [rl, wiq], [Iw])
                else:
                    P.op("vector", lambda e, rl=rl, h=h: e.scalar_tensor_tensor(
                        out=Iw[0:nq, kb0:kb0 + kw], in0=rl[0:nq, 0:kw], scalar=wiq[0:nq, h:h + 1], in1=Iw[0:nq, kb0:kb0 + kw],
                        op0=ALU.mult, op1=ALU.add), [rl, wiq, Iw], [Iw])
        if prompt:
            v0 = nkeys - nv * 128
            P.dma("sync", qrel[0:nq, :], qrel_in[tok0:tok0 + nq, :], [qrel_in], [qrel])
            P.op("vector", lambda e: e.tensor_scalar(out=pen[0:nq, 0:nv * 128], in0=kcl[0:nq, 0:nv * 128], scalar1=qrel[0:nq, 0:1],
                                                     scalar2=NEG, op0=ALU.is_gt, op1=ALU.mult), [kcl, qrel], [pen])
            P.op("vector", lambda e: e.tensor_tensor(out=Iw[0:nq, v0:nkeys], in0=Iw[0:nq, v0:nkeys], in1=pen[0:nq, 0:nv * 128],
                                                     op=ALU.add), [Iw, pen], [Iw])
            P.op("vector", lambda e: e.tensor_scalar(out=visb[0:nq, 0:nv * 128], in0=kcl[0:nq, 0:nv * 128], scalar1=qrel[0:nq, 0:1],
                                                     scalar2=None, op0=ALU.is_le), [kcl, qrel], [visb])
        if nkeys > 256:
            cur = Iw
            for r in range(32):
                P.op("vector", lambda e, cur=cur: e.max(out=m8[0:nq, :], in_=cur[0:nq, 0:nkeys]), [cur], [m8])
                if r < 31:
                    P.op("vector", lambda e, cur=cur: e.match_replace(out=wk[0:nq, 0:nkeys], in_to_replace=m8[0:nq, :],
                                                                      in_values=cur[0:nq, 0:nkeys], imm_value=-3.0e38),
                         [cur, m8], [wk])
                    cur = wk
            P.op("vector", lambda e: e.tensor_scalar(out=thr[0:nq, :], in0=m8[0:nq, 7:8], scalar1=-1.0e29, scalar2=None,
                                                     op0=ALU.max), [m8], [thr])
        else:
            P.op("vector", lambda e: e.memset(thr[:, :], -1.0e29), [], [thr])
        P.op("vector", lambda e: e.tensor_scalar(out=maskb[0:nq, 0:nkeys], in0=Iw[0:nq, 0:nkeys], scalar1=thr[0:nq, 0:1],
                                                 scalar2=None, op0=ALU.is_ge), [Iw, thr], [maskb])

    def mask_T(tok0, nq, nkeys, prompt, S):
        chunks, nchk, nfull, nv = geom(nkeys, prompt)
        transposes_to(maskb, lambda k0, kl: maskb[0:nq, k0:k0 + kl], maskT,
                      lambda c0, n_, kl: maskT[0:kl, c0:c0 + n_, 0:nq], chunks, nq)
        if prompt:
            transposes_to(visb, lambda k0, kl: visb[0:nq, k0:k0 + kl], visT,
                          lambda c0, n_, kl: visT[0:kl, c0:c0 + n_, 0:nq], [(c * 128, 128) for c in range(nv)], nq)
        P.op("gpsimd", lambda e: e.tensor_scalar(out=maskT[:, 0:nchk, 0:nq], in0=maskT[:, 0:nchk, 0:nq], scalar1=30000.0,
                                                 scalar2=-30000.0, op0=ALU.mult, op1=ALU.add), [maskT], [maskT])

    def attend(tok0, nq, nkeys, prompt, S):
        chunks, nchk, nfull, nv = geom(nkeys, prompt)
        for g in range(4):
            kT = kTs.next()
            vt = vts[vti[0] % 2]
            vti[0] += 1
            qg = qgs.next()
            P.dma("sync", kT[:, 0:nkeys], S["kT"](g), [S["kTb"]], [kT])
            if nfull:
                P.dma("sync", vt[:, 0:nfull, 0:128], S["v"](0, nfull * 128, g).rearrange("(c p) d -> p c d", p=128), [S["vb"]], [vt])
            if nkeys > nfull * 128:
                P.dma("sync", vt[0:nkeys - nfull * 128, nfull, 0:128], S["v"](nfull * 128, nkeys, g), [S["vb"]], [vt])
            P.dma("sync", qg[:, :, 0:nq], qT_s[4 * g:4 * g + 4, :, tok0:tok0 + nq].rearrange("h d t -> d h t"), [qT_s], [qg])

            def s_mm(c):
                k0, kl = chunks[c]
                pb = psI.next()
                P.op("tensor", lambda e: e.matmul(out=pb[0:kl, :].rearrange("p (r t) -> p r t", t=128)[:, :, 0:nq],
                                                  lhsT=kT[:, k0:k0 + kl], rhs=qg[:, :, 0:nq], start=True, stop=False),
                     [kT, qg], [pb])
                P.op("tensor", lambda e: e.matmul(out=pb[0:kl, :].rearrange("p (r t) -> p r t", t=128)[:, :, 0:nq],
                                                  lhsT=identb[0:kl, 0:kl],
                                                  rhs=maskT[0:kl, c, 0:nq].unsqueeze(1).to_broadcast([kl, 4, nq]),
                                                  start=False, stop=True), [identb, maskT], [pb])
                return pb
            pbq = [s_mm(0)] + ([s_mm(1)] if nchk > 1 else [])
            for c, (k0, kl) in enumerate(chunks):
                pb = pbq.pop(0)
                if c + 2 < nchk:
                    pbq.append(s_mm(c + 2))
                Pm = Pms.next()
                P.op("scalar", lambda e, pb=pb, Pm=Pm, kl=kl: e.activation(
                    out=Pm[0:kl, :, 0:nq], in_=pb[0:kl, :].rearrange("p (r t) -> p r t", t=128)[:, :, 0:nq], func=AF.Exp,
                    scale=float(128 ** -0.5)), [pb], [Pm])
                for r in range(4):
                    P.op("tensor", lambda e, r=r, Pm=Pm, kl=kl, c=c: e.matmul(
                        out=psO[r][0:nq, 0:129], lhsT=Pm[0:kl, r, 0:nq], rhs=vt[0:kl, c, 0:129],
                        start=(c == 0), stop=(c == nchk - 1)), [Pm, vt], [psO[r]])
            for r in range(4):
                ob_ = Osb[r]
                col = (4 * g + r) * 128
                P.op("scalar", lambda e, r=r, ob_=ob_: e.copy(out=ob_[0:nq, 0:129], in_=psO[r][0:nq, 0:129]), [psO[r]], [ob_])
                P.op("gpsimd", lambda e, ob_=ob_, r=r: e.tensor_tensor(out=rden[0:nq, r:r + 1], in0=ob_[0:nq, 128:129],
                                                                       in1=negc[0:nq, 0:1], op=ALU.pow), [ob_, negc], [rden])
                P.op("gpsimd", lambda e, ob_=ob_, col=col, r=r: e.tensor_scalar(out=attn[0:nq, col:col + 128], in0=ob_[0:nq, 0:128],
                                                                                scalar1=rden[0:nq, r:r + 1], scalar2=None, op0=ALU.mult),
                     [ob_, rden], [attn])
        for hd in range(8):
            dkT = dkTs.next()
            dvt = dvts[dvi[0] % 2]
            dvi[0] += 1
            dq2 = dqs.next()
            P.dma("sync", dkT[:, :, 0:nkeys], S["dkT"](hd), [S["dkTb"]], [dkT])
            if nfull:
                P.dma("sync", dvt[:, 0:nfull, 0:256], S["dv"](0, nfull * 128, hd).rearrange("(c p) d -> p c d", p=128),
                      [S["dvb"]], [dvt])
            if nkeys > nfull * 128:
                P.dma("sync", dvt[0:nkeys - nfull * 128, nfull, 0:256], S["dv"](nfull * 128, nkeys, hd), [S["dvb"]], [dvt])
            P.dma("sync", dq2[:, :, 0:nq], dqT_s[2 * hd:2 * hd + 2, :, tok0:tok0 + nq].rearrange("m d t -> d m t"), [dqT_s], [dq2])

            def d_mm(c):
                k0, kl = chunks[c]
                pb = psI.next()
                for m in range(2):
                    P.op("tensor", lambda e, m=m: e.matmul(out=pb[0:kl, m * 128:m * 128 + nq], lhsT=dkT[:, m, k0:k0 + kl],
                                                           rhs=dq2[:, m, 0:nq], start=True, stop=True), [dkT, dq2], [pb])
                return pb
            pbq = [d_mm(0)] + ([d_mm(1)] if nchk > 1 else [])
            for c, (k0, kl) in enumerate(chunks):
                pb = pbq.pop(0)
                if c + 2 < nchk:
                    pbq.append(d_mm(c + 2))
                Pt = Pts.next()
                P.op("scalar", lambda e, pb=pb, Pt=Pt, kl=kl: e.activation(
                    out=Pt[0:kl, 0:2, 0:nq], in_=pb[0:kl, 0:256].rearrange("p (r t) -> p r t", t=128)[:, :, 0:nq], func=AF.Exp,
                    scale=float(128 ** -0.5)), [pb], [Pt])
                if prompt and c >= nchk - nv:
                    P.op("gpsimd", lambda e, Pt=Pt, kl=kl, c=c: e.tensor_tensor(
                        out=Pt[0:kl, 0:2, 0:nq], in0=Pt[0:kl, 0:2, 0:nq],
                        in1=visT[0:kl, c - (nchk - nv), 0:nq].unsqueeze(1).to_broadcast([kl, 2, nq]), op=ALU.mult),
                        [Pt, visT], [Pt])
                for m in range(2):
                    P.op("tensor", lambda e, m=m, Pt=Pt, kl=kl, c=c: e.matmul(
                        out=psO[m][0:nq, 0:257], lhsT=Pt[0:kl, m, 0:nq], rhs=dvt[0:kl, c, 0:257],
                        start=(c == 0), stop=(c == nchk - 1)), [Pt, dvt], [psO[m]])
            P.op("scalar", lambda e: e.copy(out=Od[0][0:nq, :], in_=psO[0][0:nq, 0:257]), [psO[0]], [Od[0]])
            P.op("scalar", lambda e: e.copy(out=Od[1][0:nq, :], in_=psO[1][0:nq, 0:257]), [psO[1]], [Od[1]])
            P.op("gpsimd", lambda e: e.tensor_tensor(out=rden[0:nq, 4:5], in0=Od[0][0:nq, 256:257], in1=negc[0:nq, 0:1], op=ALU.pow),
                 [Od[0], negc], [rden])
            P.op("gpsimd", lambda e: e.tensor_tensor(out=rden[0:nq, 5:6], in0=Od[1][0:nq, 256:257], in1=negc[0:nq, 0:1], op=ALU.pow),
                 [Od[1], negc], [rden])
            P.op("gpsimd", lambda e: e.tensor_tensor(out=rden[0:nq, 5:6], in0=rden[0:nq, 5:6], in1=neglam[0:nq, 0:1], op=ALU.mult),
                 [rden, neglam], [rden])
            P.op("gpsimd", lambda e: e.tensor_scalar(out=tmpd[0:nq, :], in0=Od[0][0:nq, 0:256], scalar1=rden[0:nq, 4:5],
                                                     scalar2=None, op0=ALU.mult), [Od[0], rden], [tmpd])
            P.op("gpsimd", lambda e: e.tensor_scalar(out=dof[0:nq, :], in0=Od[1][0:nq, 0:256], scalar1=rden[0:nq, 5:6],
                                                     scalar2=None, op0=ALU.mult), [Od[1], rden], [dof])
            P.op("gpsimd", lambda e: e.tensor_tensor(out=dof[0:nq, :], in0=dof[0:nq, :], in1=tmpd[0:nq, :], op=ALU.add), [dof, tmpd], [dof])
            P.op("gpsimd", lambda e: e.memset(ssd[:, :], 0.0), [], [ssd])
            P.op("scalar", lambda e: e.activation(out=junkd[0:nq, :], in_=dof[0:nq, :], func=AF.Square, accum_out=ssd[0:nq, 0:1]),
                 [dof], [junkd, ssd])
            P.op("gpsimd", lambda e: e.tensor_scalar(out=ssd[0:nq, 1:2], in0=ssd[0:nq, 0:1], scalar1=1.0 / 256, scalar2=EPS,
                                                     op0=ALU.mult, op1=ALU.add), [ssd], [ssd])
            P.op("gpsimd", lambda e: e.tensor_tensor(out=ssd[0:nq, 1:2], in0=ssd[0:nq, 1:2], in1=negc[0:nq, 1:2], op=ALU.pow),
                 [ssd, negc], [ssd])
            col = 2048 + hd * 256
            P.op("gpsimd", lambda e: e.tensor_scalar(out=tmpd[0:nq, :], in0=dof[0:nq, :], scalar1=ssd[0:nq, 1:2], scalar2=None,
                                                     op0=ALU.mult), [dof, ssd], [tmpd])
            P.op("gpsimd", lambda e, col=col: e.tensor_tensor(out=attn[0:nq, col:col + 256], in0=tmpd[0:nq, :], in1=gsubB[0:nq, :],
                                                              op=ALU.mult), [tmpd, gsubB], [attn])
        for c8 in range(4):
            pb = psT.next()
            tb = tbc.next()
            for j in range(8):
                ch = c8 * 8 + j
                P.op("tensor", lambda e, j=j, ch=ch: e.transpose(out=pb[:, j * 128:j * 128 + nq], in_=attn[0:nq, ch * 128:(ch + 1) * 128],
                                                                 identity=identb[0:nq, 0:nq]), [attn, identb], [pb])
            P.op("scalar", lambda e: e.copy(out=tb[:, :, 0:nq], in_=pb[:, :].rearrange("p (c t) -> p c t", t=128)[:, :, 0:nq]),
                 [pb], [tb])
            P.dma("gpsimd", attnT_s[:, c8 * 8:(c8 + 1) * 8, tok0:tok0 + nq], tb[:, :, 0:nq], [tb], [attnT_s])

    NA = int(os.environ.get("MK_NAT", str(NQT)))
    jobs = []
    for i in range(NA):
        nkeys = nchunks_for(i) * 128
        S = {
            "kT": (lambda g, nkeys=nkeys: kT_s[g, :, 0:nkeys]), "kTb": kT_s,
            "v": (lambda a, b_, g: v_s[a:b_, g * 128:(g + 1) * 128]), "vb": v_s,
            "dkT": (lambda hd, nkeys=nkeys: dkT_s[2 * hd:2 * hd + 2, :, 0:nkeys].rearrange("m d t -> d m t")), "dkTb": dkT_s,
            "dv": (lambda a, b_, hd: dv_s[a:b_, hd * 256:(hd + 1) * 256]), "dvb": dv_s,
            "ki": None,
        }
        jobs.append((i * 128, 128, nkeys, True, S))
    for s in range(2):
        S = {
            "kT": (lambda g, s=s: skT_s[s, g, :, :]), "kTb": skT_s,
            "v": (lambda a, b_, g, s=s: sv_s[s, a:b_, g * 128:(g + 1) * 128]), "vb": sv_s,
            "dkT": (lambda hd, s=s: sdkT_s[s, 2 * hd:2 * hd + 2, :, :].rearrange("m d t -> d m t")), "dkTb": sdkT_s,
            "dv": (lambda a, b_, hd, s=s: sdv_s[s, a:b_, hd * 256:(hd + 1) * 256]), "dvb": sdv_s,
            "ki": s,
        }
        jobs.append((NQT * 128 + 32 * s, 32, LS, False, S))

    def stage1(job):
        if job[4]["ki"] is not None:
            s_ = job[4]["ki"]
            P.dma("sync", kiT[:, 0:LS], skiT_s[s_, :, :], [skiT_s], [kiT])
        idx_topk(*job)

    P.dma("sync", kiT[:, :], kiT_s[:, :], [kiT_s], [kiT])
    stage1(jobs[0])
    mask_T(*jobs[0])
    for ji, job in enumerate(jobs):
        if ji + 1 < len(jobs):
            stage1(jobs[ji + 1])
        attend(*job)
        if ji + 1 < len(jobs):
            mask_T(*jobs[ji + 1])
    P.barrier()
    C.release(mk)

    mk = C.mark()
    psF = [C.ps([128, 512], F32, "psF") for _ in range(4)]
    ps_proj = RR(psF[0:3])
    hTs = [C.sb([128, 32, 128], BF16, "hT") for _ in range(9)]
    wsts = RR([C.sb([128, 4, 512], F32, "wst") for _ in range(2)])
    wbfs = RR([C.sb([128, 32, 512], BF16, "wbf") for _ in range(2)])
    gaP = C.sb([128, D], F32, "gaP")
    gaS = C.sb([128, D], F32, "gaS")
    xbs = RR([C.sb([128, 512], F32, "xb") for _ in range(3)])
    tms = RR([C.sb([128, 512], F32, "tm") for _ in range(3)])
    P.dma("sync", gaP[:, :], ga_s[0, :, :], [ga_s], [gaP])
    P.dma("sync", gaS[:, :], ga_s[2, :, :], [ga_s], [gaS])
    for grp in range(2):
        slots = [(si, i, 128, True) for si, i in enumerate(range(grp * 8, grp * 8 + 8))]
        if grp == 1:
            slots.append((8, NQT, 64, False))
        for (si, i, n, prompt) in slots:
            P.dma("sync", hTs[si][:, :, 0:n], attnT_s[:, :, i * 128:i * 128 + n], [attnT_s], [hTs[si]])
        for cb, wbf in zip(range(8), weight_stream([(w_out, cb_ * 512, 512) for cb_ in range(8)])):
            for (si, i, n, prompt) in slots:
                pb = project(hTs[si], n, wbf, 512)
                xb = xbs.next()
                tm = tms.next()
                ga = gaP if prompt else gaS
                P.dma("sync", xb[0:n, :], xq[i * 128:i * 128 + n, cb * 512:(cb + 1) * 512], [xq], [xb])
                P.op("vector", lambda e, pb=pb, tm=tm, ga=ga, n=n, cb=cb: e.tensor_tensor(
                    out=tm[0:n, :], in0=pb[0:n, :], in1=ga[0:n, cb * 512:(cb + 1) * 512], op=ALU.mult), [pb, ga], [tm])
                P.op("gpsimd", lambda e, tm=tm, xb=xb, n=n: e.tensor_tensor(out=tm[0:n, :], in0=tm[0:n, :], in1=xb[0:n, :], op=ALU.add),
                     [tm, xb], [tm])
                P.dma("gpsimd", x1_s[i * 128:i * 128 + n, cb * 512:(cb + 1) * 512], tm[0:n, :], [tm], [x1_s])
    P.barrier()
    C.release(mk)
    mk = C.mark()
    psF = [C.ps([128, 512], F32, "psF") for _ in range(6)]
    psB = [C.ps([128, 1024], BF16, "psB") for _ in range(2)]
    ps_proj = RR(psF[0:2])
    ps_xT = RR(psF[2:4])
    ps_s = RR(psF[4:6])
    xts = RR([C.sb([128, D], F32, "xt") for _ in range(1)])
    sst = RR([C.sb([128, 2], F32, "ss") for _ in range(2)])
    hTs = [C.sb([128, 32, 128], BF16, "hT") for _ in range(9)]
    wsts = RR([C.sb([128, 4, 512], F32, "wst") for _ in range(2)])
    wbfs = RR([C.sb([128, 32, 512], BF16, "wbf") for _ in range(2)])
    kraw = C.sb([128, 16, 128], F32, "kraw")
    keysT = C.sb([128, 16, 128], F32, "keysT")
    qsbs = RR([C.sb([128, 512], F32, "qsb") for _ in range(2)])
    qT4s = RR([C.sb([128, 4, 128], F32, "qT4") for _ in range(2)])
    sos = RR([C.sb([128, 512], F32, "so") for _ in range(2)])
    P.dma("sync", kraw[:, :, :], pkeys_in[:, :, :, :].rearrange("h c n d -> n (h c) d"), [pkeys_in], [kraw])
    for b4 in range(4):
        pb = ps_xT.next()
        for j in range(4):
            P.op("tensor", lambda e, j=j, b4=b4: e.transpose(out=pb[:, j * 128:(j + 1) * 128], in_=kraw[:, b4 * 4 + j, :],
                                                             identity=ident[:, :]), [kraw, ident], [pb])
        P.op("vector", lambda e, b4=b4: e.tensor_copy(out=keysT[:, b4 * 4:(b4 + 1) * 4, :],
                                                      in_=pb[:, :].rearrange("p (c t) -> p c t", t=128)), [pb], [keysT])
    for grp in range(2):
        slots = [(si, i, 128, True) for si, i in enumerate(range(grp * 8, grp * 8 + 8))]
        if grp == 1:
            slots.append((8, NQT, 64, False))
        for (si, i, n, prompt) in slots:
            load_norm_T(x1_s, i * 128, n, hTs[si], prompt, A2, 96)
            P.dma("gpsimd", h2T_s[:, :, i * 128:i * 128 + n], hTs[si][:, :, 0:n], [hTs[si]], [h2T_s])
        for cb, wbf in zip(range(4), weight_stream([(pwq_in, cb_ * 512, 512) for cb_ in range(4)])):
            for (si, i, n, prompt) in slots:
                pb = project(hTs[si], n, wbf, 512)
                qsb = qsbs.next()
                P.op("scalar", lambda e, pb=pb, qsb=qsb, n=n: e.copy(out=qsb[0:n, :], in_=pb[0:n, :]), [pb], [qsb])
                pt = ps_xT.next()
                for j in range(4):
                    P.op("tensor", lambda e, j=j, n=n, qsb=qsb, pt=pt: e.transpose(
                        out=pt[:, j * 128:j * 128 + n], in_=qsb[0:n, j * 128:(j + 1) * 128], identity=ident[0:n, 0:n]),
                        [qsb, ident], [pt])
                qT4 = qT4s.next()
                P.op("vector", lambda e, pt=pt, qT4=qT4, n=n: e.tensor_copy(
                    out=qT4[:, :, 0:n], in_=pt[:, :].rearrange("p (c t) -> p c t", t=128)[:, :, 0:n]), [pt], [qT4])
                po = ps_s.next()
                for j in range(4):
                    P.op("tensor", lambda e, j=j, n=n, qT4=qT4, po=po, cb=cb: e.matmul(
                        out=po[0:n, j * 128:(j + 1) * 128], lhsT=qT4[:, j, 0:n], rhs=keysT[:, cb * 4 + j, :],
                        start=True, stop=True), [qT4, keysT], [po])
                so = sos.next()
                P.op("scalar", lambda e, po=po, so=so, n=n: e.copy(out=so[0:n, :], in_=po[0:n, :]), [po], [so])
                P.dma("gpsimd", s_s[i * 128:i * 128 + n, cb * 512:(cb + 1) * 512], so[0:n, :], [so], [s_s])
    P.barrier()
    C.release(mk)

    mk = C.mark()
    psA = RR([C.ps([128, 1024], BF16, "psA") for _ in range(3)])
    psG = RR([C.ps([128, 512], F32, "psG") for _ in range(3)])
    sal = C.sb([128, 16, 128], F32, "sal")
    wk2 = C.sb([128, 16, 128], F32, "wk2")
    top = C.sb([128, 16, 16], F32, "top")
    idx = C.sb([128, 8, 16], U32, "idx")
    idxf = C.sb([128, 8, 16], F32, "idxf")
    cand = C.sb([128, 8, 256], F32, "cand")
    cw = C.sb([128, 8, 256], F32, "cw")
    best = C.sb([128, 8, 16], F32, "best")
    eb = C.sb([128, 8, 16], F32, "eb")
    Zs = C.sb([128, 8], F32, "Zs")
    e1z = C.sb([128, 8, 16], F32, "e1z")
    cth = C.sb([128, 8, 16], F32, "cth")
    e2 = C.sb([128, 8, 128], F32, "e2")
    iot = C.sb([128, 128], F32, "iot")
    ABa = C.sb([128, 128, 128], BF16, "ABa")
    ABb = C.sb([128, 128, 128], BF16, "ABb")
    AT = C.sb([128, 128, 128], BF16, "AT")
    BT = C.sb([128, 128, 128], BF16, "BT")
    Gall = C.sb([128, 128, 128], BF16, "Gall")
    P.dma("sync", iot[:, :], iota_in[:, :], [iota_in], [iot])
    NE2 = int(os.environ.get("MK_NE2", "17"))
    for ti in range(NE2):
        n = 128 if ti < NQT else 64
        tok0 = ti * 128
        P.dma("sync", sal[0:n, :, :], s_s[tok0:tok0 + n, :].rearrange("t (k m) -> t k m", m=128), [s_s], [sal])
        for hc in range(16):
            h, c = divmod(hc, 2)
            P.op("vector", lambda e, hc=hc: e.max(out=top[0:n, hc, 0:8], in_=sal[0:n, hc, :]), [sal], [top])
            if c == 0:
                P.op("vector", lambda e, hc=hc, h=h: e.max_index(out=idx[0:n, h, 0:8], in_max=top[0:n, hc, 0:8],
                                                                 in_values=sal[0:n, hc, :]), [sal, top], [idx])
            P.op("vector", lambda e, hc=hc: e.match_replace(out=wk2[0:n, hc, :], in_to_replace=top[0:n, hc, 0:8],
                                                            in_values=sal[0:n, hc, :], imm_value=-3.0e38), [sal, top], [wk2])
            P.op("vector", lambda e, hc=hc: e.max(out=top[0:n, hc, 8:16], in_=wk2[0:n, hc, :]), [wk2], [top])
            if c == 0:
                P.op("vector", lambda e, hc=hc, h=h: e.max_index(out=idx[0:n, h, 8:16], in_max=top[0:n, hc, 8:16],
                                                                 in_values=wk2[0:n, hc, :]), [wk2, top], [idx])
        top4 = top[0:n, :, :].rearrange("p (h c) k -> p h c k", c=2)
        sal4 = sal[0:n, :, :].rearrange("p (h c) k -> p h c k", c=2)
        P.op("vector", lambda e: e.tensor_copy(out=idxf[0:n, :, :], in_=idx[0:n, :, :]), [idx], [idxf])
        P.op("vector", lambda e: e.tensor_tensor(
            out=cand[0:n, :, :].rearrange("p h (i j) -> p h i j", j=16),
            in0=top4[:, :, 0, :].unsqueeze(3).to_broadcast([n, 8, 16, 16]),
            in1=top4[:, :, 1, :].unsqueeze(2).to_broadcast([n, 8, 16, 16]), op=ALU.add), [top], [cand])
        for h in range(8):
            P.op("vector", lambda e, h=h: e.max(out=best[0:n, h, 0:8], in_=cand[0:n, h, :]), [cand], [best])
            P.op("vector", lambda e, h=h: e.match_replace(out=cw[0:n, h, :], in_to_replace=best[0:n, h, 0:8],
                                                          in_values=cand[0:n, h, :], imm_value=-3.0e38), [cand, best], [cw])
            P.op("vector", lambda e, h=h: e.max(out=best[0:n, h, 8:16], in_=cw[0:n, h, :]), [cw], [best])
        P.op("vector", lambda e: e.tensor_tensor(out=eb[0:n, :, :], in0=best[0:n, :, :],
                                                 in1=best[0:n, :, 0:1].to_broadcast([n, 8, 16]), op=ALU.subtract), [best], [eb])
        P.op("scalar", lambda e: e.activation(out=eb[0:n, :, :], in_=eb[0:n, :, :], func=AF.Exp), [eb], [eb])
        P.op("vector", lambda e: e.reduce_sum(out=Zs[0:n, :], in_=eb[0:n, :, :], axis=AX.X), [eb], [Zs])
        P.op("vector", lambda e: e.reciprocal(out=Zs[0:n, :], in_=Zs[0:n, :]), [Zs], [Zs])
        P.op("vector", lambda e: e.tensor_tensor(out=e1z[0:n, :, :], in0=top4[:, :, 0, :],
                                                 in1=top4[:, :, 0, 0:1].to_broadcast([n, 8, 16]), op=ALU.subtract), [top], [e1z])
        P.op("scalar", lambda e: e.activation(out=e1z[0:n, :, :], in_=e1z[0:n, :, :], func=AF.Exp), [e1z], [e1z])
        P.op("vector", lambda e: e.tensor_tensor(out=e1z[0:n, :, :], in0=e1z[0:n, :, :],
                                                 in1=Zs[0:n, :].unsqueeze(2).to_broadcast([n, 8, 16]), op=ALU.mult), [e1z, Zs], [e1z])
        P.op("vector", lambda e: e.tensor_tensor(out=cth[0:n, :, :], in0=best[0:n, :, 15:16].to_broadcast([n, 8, 16]),
                                                 in1=top4[:, :, 0, :], op=ALU.subtract), [best, top], [cth])
        P.op("vector", lambda e: e.tensor_scalar(out=cth[0:n, :, :], in0=cth[0:n, :, :], scalar1=-4.0e-6, scalar2=None,
                                                 op0=ALU.add), [cth], [cth])
        P.op("vector", lambda e: e.tensor_tensor(out=e2[0:n, :, :], in0=sal4[:, :, 1, :],
                                                 in1=top4[:, :, 1, 0:1].to_broadcast([n, 8, 128]), op=ALU.subtract), [sal, top], [e2])
        P.op("scalar", lambda e: e.activation(out=e2[0:n, :, :], in_=e2[0:n, :, :], func=AF.Exp), [e2], [e2])
        for which in range(2):
            XT = AT if which == 0 else BT
            AB = ABa if which == 0 else ABb
            if which == 0:
                P.op("vector", lambda e, AB=AB: e.tensor_tensor(
                    out=AB[0:n, :, :], in0=iot[0:n, :].unsqueeze(1).to_broadcast([n, 128, 128]),
                    in1=idxf[0:n, :, :].rearrange("p h i -> p (h i)").unsqueeze(2).to_broadcast([n, 128, 128]),
                    op=ALU.is_equal), [iot, idxf], [AB])
                P.op("gpsimd", lambda e, AB=AB: e.tensor_tensor(
                    out=AB[0:n, :, :], in0=AB[0:n, :, :],
                    in1=e1z[0:n, :, :].rearrange("p h i -> p (h i)").unsqueeze(2).to_broadcast([n, 128, 128]),
                    op=ALU.mult), [AB, e1z], [AB])
            else:
                P.op("vector", lambda e, AB=AB: e.tensor_tensor(
                    out=AB[0:n, :, :].rearrange("p (h i) k -> p h i k", i=16),
                    in0=sal4[:, :, 1, :].unsqueeze(2).to_broadcast([n, 8, 16, 128]),
                    in1=cth[0:n, :, :].unsqueeze(3).to_broadcast([n, 8, 16, 128]), op=ALU.is_ge), [sal, cth], [AB])
                P.op("gpsimd", lambda e, AB=AB: e.tensor_tensor(
                    out=AB[0:n, :, :].rearrange("p (h i) k -> p h i k", i=16),
                    in0=AB[0:n, :, :].rearrange("p (h i) k -> p h i k", i=16),
                    in1=e2[0:n, :, :].unsqueeze(2).to_broadcast([n, 8, 16, 128]), op=ALU.mult), [AB, e2], [AB])
            for b8 in range(16):
                pb = psA.next()
                for j in range(8):
                    col = b8 * 8 + j
                    P.op("tensor", lambda e, j=j, col=col, pb=pb, AB=AB: e.transpose(
                        out=pb[:, j * 128:j * 128 + n], in_=AB[0:n, :, col], identity=identb[0:n, 0:n]), [AB, identb], [pb])
                en = "scalar" if b8 % 2 == 0 else "vector"
                src_v = pb[:, :].rearrange("p (c t) -> p c t", t=128)[:, :, 0:n]
                if en == "scalar":
                    P.op("scalar", lambda e, b8=b8, src_v=src_v, XT=XT: e.copy(out=XT[:, b8 * 8:(b8 + 1) * 8, 0:n], in_=src_v), [pb], [XT])
                else:
                    P.op("vector", lambda e, b8=b8, src_v=src_v, XT=XT: e.tensor_copy(out=XT[:, b8 * 8:(b8 + 1) * 8, 0:n], in_=src_v),
                         [pb], [XT])
        for t4 in range(0, n, 4):
            pg = psG.next()
            for j in range(4):
                P.op("tensor", lambda e, j=j, t4=t4, pg=pg: e.matmul(out=pg[:, j * 128:(j + 1) * 128], lhsT=AT[:, :, t4 + j],
                                                                     rhs=BT[:, :, t4 + j], start=True, stop=True), [AT, BT], [pg])
            en = "scalar" if (t4 // 4) % 2 == 0 else "vector"
            src_v = pg[:, :].rearrange("p (t i) -> p i t", i=128)
            if en == "scalar":
                P.op("scalar", lambda e, t4=t4, src_v=src_v: e.copy(out=Gall[:, :, t4:t4 + 4], in_=src_v), [pg], [Gall])
            else:
                P.op("vector", lambda e, t4=t4, src_v=src_v: e.tensor_copy(out=Gall[:, :, t4:t4 + 4], in_=src_v), [pg], [Gall])
        P.dma("gpsimd", G_s[ti, :, :, :], Gall[:, :, :], [Gall], [G_s])
    P.barrier()
    C.release(mk)
    mk = C.mark()
    psAct = RR([C.ps([128, 512], F32, "psAct") for _ in range(2)])
    psUT = RR([C.ps([128, 1024], BF16, "psUT") for _ in range(2)])
    psO2 = RR([C.ps([128, 512], F32, "psO2") for _ in range(3)])
    h2T = C.sb([128, 32, 512], BF16, "h2T")
    oacc = [C.sb([128, D], F32, "oacc") for _ in range(4)]
    stg = RR([C.sb([128, D], F32, "stg") for _ in range(2)])
    ubfs = RR([C.sb([128, D], BF16, "ubf") for _ in range(2)])
    uTs = RR([C.sb([128, 32, 128], BF16, "uT") for _ in range(2)])
    vbfs = [C.sb([128, D], BF16, "vbf") for _ in range(4)]
    WTs = [C.sb([128, 512], BF16, "WT") for _ in range(4)]
    Gcs = RR([C.sb([128, 4, 128], BF16, "Gc") for _ in range(2)])
    gts = RR([C.sb([128, 512], BF16, "gt") for _ in range(2)])
    _s0 = stg.items[0].t
    xbs = RR([Buf(_s0[:, 0:512]), Buf(_s0[:, 512:1024])])
    gbs = RR([Buf(_s0[:, 1024:1536]), Buf(_s0[:, 1536:2048])])
    fbs = RR([Buf(_s0[:, 2048:2560]), Buf(_s0[:, 2560:3072])])
    ybs = RR([Buf(_s0[:, 3072:3584]), Buf(_s0[:, 3584:4096])])
    jk = C.sb([128, 512], BF16, "jk")
    ss8 = C.sb([128, 10], F32, "ss8")
    u3 = pu_in[:, :].rearrange("(a b) d -> a b d", b=128)
    v3 = pv_in[:, :].rearrange("(a b) d -> a b d", b=128)
    uT_cb = [Buf(uT_c.t[c_]) for c_ in range(128)]
    v_cb = [Buf(v_c.t[c_]) for c_ in range(128)]
    BLOCKS = [[13, 14, 15, 16], [0, 1, 2, 3], [4, 5, 6, 7], [8, 9, 10, 11], [12]]
    NBLK = int(os.environ.get("MK_NBLK", "5"))
    NCH = int(os.environ.get("MK_NCH", "128"))
    for bi, blk in enumerate(BLOCKS[:NBLK]):
        tiles = [(ti, ti * 128, 128 if ti < NQT else 64) for ti in blk]
        tok0 = tiles[0][1]
        T = sum(n for (_, _, n) in tiles)
        P.dma("sync", h2T[:, :, 0:T], h2T_s[:, :, tok0:tok0 + T], [h2T_s], [h2T])
        for g0 in range(0, NCH, 4):
            for k_ in range(4):
                c = g0 + k_
                uT = uTs.next()
                if bi == 0:
                    st = stg.next()
                    P.dma("sync", st[:, :], u3[:, c, :], [pu_in], [st])
                    ubf = ubfs.next()
                    P.op("vector", lambda e, st=st, ubf=ubf: e.tensor_copy(out=ubf[:, :], in_=st[:, :]), [st], [ubf])
                    for b8 in range(4):
                        pb = psUT.next()
                        for j in range(8):
                            ch = b8 * 8 + j
                            P.op("tensor", lambda e, j=j, ch=ch, pb=pb, ubf=ubf: e.transpose(out=pb[:, j * 128:(j + 1) * 128],
                                                                                             in_=ubf[:, ch * 128:(ch + 1) * 128],
                                                                                             identity=identb[:, :]), [ubf, identb], [pb])
                        P.op("scalar", lambda e, b8=b8, pb=pb, uT=uT: e.copy(
                            out=uT[:, b8 * 8:(b8 + 1) * 8, :], in_=pb[:, :].rearrange("p (c t) -> p c t", t=128)), [pb], [uT])
                    P.dma("gpsimd", uT_cb[c][:, :], uT[:, :, :].rearrange("p c t -> p (c t)"), [uT], [uT_cb[c]])
                else:
                    P.dma("sync", uT[:, :, :].rearrange("p c t -> p (c t)"), uT_cb[c][:, :], [uT_cb[c]], [uT])
                Gc = Gcs.next()
                P.dma("sync", Gc[:, 0:len(tiles), :], G_s[blk[0]:blk[0] + len(tiles), :, c, :].rearrange("a p t -> p a t"), [G_s], [Gc])
                pa = psAct.next()
                for ch in range(32):
                    P.op("tensor", lambda e, ch=ch, pa=pa, uT=uT: e.matmul(out=pa[:, 0:T], lhsT=uT[:, ch, :], rhs=h2T[:, ch, 0:T],
                                                                    start=(ch == 0), stop=(ch == 31)), [uT, h2T], [pa])
                gt = gts.next()
                P.op("scalar", lambda e, pa=pa, gt=gt: e.activation(out=gt[:, 0:T], in_=pa[:, 0:T], func=AF.Gelu_apprx_tanh), [pa], [gt])
                WT = WTs[k_]
                P.op("gpsimd", lambda e, gt=gt, WT=WT, Gc=Gc: e.tensor_tensor(
                    out=WT[:, 0:T], in0=gt[:, 0:T], in1=Gc[:, :, :].rearrange("p a t -> p (a t)")[:, 0:T], op=ALU.mult),
                    [gt, Gc], [WT])
                if bi == 0:
                    st = stg.next()
                    P.dma("sync", st[:, :], v3[:, c, :], [pv_in], [st])
                    P.op("scalar", lambda e, st=st, k_=k_: e.copy(out=vbfs[k_][:, :], in_=st[:, :]), [st], [vbfs[k_]])
                    P.dma("gpsimd", v_cb[c][:, :], vbfs[k_][:, :], [vbfs[k_]], [v_cb[c]])
                else:
                    P.dma("sync", vbfs[k_][:, :], v_cb[c][:, :], [v_cb[c]], [vbfs[k_]])
            off = 0
            for si, (ti, tk0, n) in enumerate(tiles):
                for db in range(8):
                    po = psO2.next()
                    for k_ in range(4):
                        P.op("tensor", lambda e, k_=k_, po=po, off=off, n=n, db=db: e.matmul(
                            out=po[0:n, :], lhsT=WTs[k_][:, off:off + n], rhs=vbfs[k_][:, db * 512:(db + 1) * 512],
                            start=(k_ == 0), stop=(k_ == 3)), [WTs[k_], vbfs[k_]], [po])
                    if g0 == 0:
                        P.op("vector", lambda e, po=po, si=si, n=n, db=db: e.tensor_copy(
                            out=oacc[si][0:n, db * 512:(db + 1) * 512], in_=po[0:n, :]), [po], [oacc[si]])
                    else:
                        P.op("vector", lambda e, po=po, si=si, n=n, db=db: e.tensor_tensor(
                            out=oacc[si][0:n, db * 512:(db + 1) * 512], in0=po[0:n, :], in1=oacc[si][0:n, db * 512:(db + 1) * 512],
                            op=ALU.add), [po, oacc[si]], [oacc[si]])
                off += n
        P.barrier()
        for si, (ti, tk0, n) in enumerate(tiles):
            prompt = ti < NQT
            P.op("vector", lambda e: e.memset(ss8[:, :], 0.0), [], [ss8])
            for db in range(8):
                xb = xbs.next()
                gb = gbs.next()
                sl = slice(db * 512, (db + 1) * 512)
                P.dma("sync", xb[0:n, :], x1_s[tk0:tk0 + n, sl], [x1_s], [xb])
                P.dma("sync", gb[0:n, :], ga_s[1 if prompt else 3, 0:n, sl], [ga_s], [gb])
                P.op("vector", lambda e, si=si, sl=sl, gb=gb, n=n: e.tensor_tensor(out=oacc[si][0:n, sl], in0=oacc[si][0:n, sl],
                                                                                   in1=gb[0:n, :], op=ALU.mult), [oacc[si], gb], [oacc[si]])
                P.op("gpsimd", lambda e, si=si, sl=sl, xb=xb, n=n: e.tensor_tensor(out=oacc[si][0:n, sl], in0=oacc[si][0:n, sl],
                                                                                   in1=xb[0:n, :], op=ALU.add), [oacc[si], xb], [oacc[si]])
                P.op("scalar", lambda e, si=si, sl=sl, n=n, db=db: e.activation(out=jk[0:n, :], in_=oacc[si][0:n, sl], func=AF.Square,
                                                                                accum_out=ss8[0:n, db:db + 1]), [oacc[si]], [jk, ss8])
            P.op("vector", lambda e, n=n: e.reduce_sum(out=ss8[0:n, 8:9], in_=ss8[0:n, 0:8], axis=AX.X), [ss8], [ss8])
            P.op("vector", lambda e, n=n: e.tensor_scalar(out=ss8[0:n, 9:10], in0=ss8[0:n, 8:9], scalar1=1.0 / D, scalar2=EPS,
                                                          op0=ALU.mult, op1=ALU.add), [ss8], [ss8])
            P.op("scalar", lambda e, n=n: e.activation(out=ss8[0:n, 9:10], in_=ss8[0:n, 9:10], func=AF.Sqrt), [ss8], [ss8])
            P.op("vector", lambda e, n=n: e.reciprocal(out=ss8[0:n, 9:10], in_=ss8[0:n, 9:10]), [ss8], [ss8])
            for db in range(8):
                fb = fbs.next()
                yb = ybs.next()
                sl = slice(db * 512, (db + 1) * 512)
                P.dma("sync", fb[0:n, :], gfinB[0:n, sl], [gfinB], [fb])
                P.op("vector", lambda e, si=si, sl=sl, fb=fb, yb=yb, n=n: e.scalar_tensor_tensor(
                    out=yb[0:n, :], in0=oacc[si][0:n, sl], scalar=ss8[0:n, 9:10], in1=fb[0:n, :], op0=ALU.mult, op1=ALU.mult),
                    [oacc[si], ss8, fb], [yb])
                P.dma("gpsimd", o_y[tk0:tk0 + n, sl], yb[0:n, :], [yb], [o_y])
        P.barrier()
    P.barrier()
    C.release(mk)
    P.barrier()
    P.close()
    return nc


_ROPE_CACHE = {}


def _rope_tabs(pos):
    half = 64
    inv = (np.float32(10000.0) ** (-np.arange(half, dtype=np.float32) / np.float32(half))).astype(np.float32)
    ang = pos.astype(np.float32)[:, None] * inv[None, :]
    cos, sin = np.cos(ang).astype(np.float32), np.sin(ang).astype(np.float32)
    return (np.ascontiguousarray(np.concatenate([cos, cos], axis=1)),
            np.ascontiguousarray(np.concatenate([-sin, sin], axis=1)))


def _fp(v):
    return np.ascontiguousarray(v.reshape(-1, 128).T)


def kernel(x_prompt, x_sample, cache_dsa_k, cache_dsa_v, cache_idx_k, cache_diff_k, cache_diff_v,
           c_prompt, c_sample, w_ada, b_ada, g_norm_mix, g_norm_ffn, w_in,
           diff_lambda_q1, diff_lambda_k1, diff_lambda_q2, diff_lambda_k2, g_diff_subln, w_out,
           peer_w_query, peer_sub_keys, peer_u, peer_v, g_final):
    f = np.float32
    A = lambda a: np.ascontiguousarray(np.asarray(a, dtype=f))
    x_prompt, x_sample = A(x_prompt), A(x_sample)
    nc = build_program()
    cosk, sink = _rope_tabs(np.arange(SEQ))
    ident = np.eye(128, dtype=f)
    in_maps = []
    own = {}
    shared = {
        "w_ada": A(w_ada[0]), "badaT": _fp(A(b_ada[0])), "gmixT": _fp(A(g_norm_mix[0])), "gffnT": _fp(A(g_norm_ffn[0])),
        "gfinB": np.ascontiguousarray(np.broadcast_to(A(g_final)[None, :], (128, D))),
        "w_in": A(w_in[0]), "cosk": cosk, "sink": sink, "ident": ident,
        "kcl": np.ascontiguousarray(np.broadcast_to((np.arange(512) // 64).astype(f)[None, :], (128, 512))),
        "lamv": np.ascontiguousarray(np.broadcast_to(np.stack([A(diff_lambda_q1[0]), A(diff_lambda_k1[0]),
                                                                A(diff_lambda_q2[0]), A(diff_lambda_k2[0])])[None], (128, 4, 128))),
        "gsub": np.ascontiguousarray(np.broadcast_to(A(g_diff_subln[0])[None, :], (128, 256))),
        "w_out": A(w_out[0]), "pwq": A(peer_w_query[0]), "pkeys": A(peer_sub_keys[0]), "pu": A(peer_u[0]), "pv": A(peer_v[0]),
        "iota": np.ascontiguousarray(np.broadcast_to(np.arange(128, dtype=f)[None, :], (128, 128))),
    }
    for c in range(8):
        b, hh = divmod(c, 2)
        js = [own_tile_index(hh, i) for i in range(NQT)]
        own[c] = js
        xq = np.concatenate([x_prompt[b, j * 128:(j + 1) * 128] for j in js] + [x_sample[2 * c], x_sample[2 * c + 1]], axis=0)
        posq = np.concatenate([np.arange(j * 128, (j + 1) * 128) for j in js] + [np.arange(PAST, LS), np.arange(PAST, LS)])
        cosq, sinq = _rope_tabs(posq)
        qrel = np.zeros((NTOK, 1), f)
        for i, j in enumerate(js):
            nch = nchunks_for(i)
            base64 = 2 * (nch - min(4, nch))
            qrel[i * 128:(i + 1) * 128, 0] = (np.arange(j * 128, (j + 1) * 128) // 64) - base64
        cv = np.stack([A(c_prompt)[b], A(c_sample)[2 * c], A(c_sample)[2 * c + 1]], axis=0)
        cT = np.ascontiguousarray(cv.reshape(3, 32, 128).transpose(2, 1, 0).reshape(128, 96))
        m = dict(shared)
        m.update({
            "xk": x_prompt[b], "xq": np.ascontiguousarray(xq), "cT": cT, "cosq": cosq, "sinq": sinq, "qrel": qrel,
            "c_k": A(cache_dsa_k[0, 2 * c:2 * c + 2]).reshape(2, PAST, 512),
            "c_v": A(cache_dsa_v[0, 2 * c:2 * c + 2]).reshape(2, PAST, 512),
            "c_ki": A(cache_idx_k[0, 2 * c:2 * c + 2]).reshape(2, PAST, 128),
            "c_dk": A(cache_diff_k[0, 2 * c:2 * c + 2]).reshape(2, PAST, 2048),
            "c_dv": A(cache_diff_v[0, 2 * c:2 * c + 2]).reshape(2, PAST, 2048),
        })
        in_maps.append(m)
    names = set(_INPUT_NAMES)
    in_maps = [{k: v for k, v in m.items() if k in names} for m in in_maps]
    ncore = int(os.environ.get("MK_CORES", "8"))
    res = run_bass_kernel_spmd(nc, in_maps[:ncore], core_ids=list(range(ncore)))
    R = list(res.results)
    while len(R) < 8:
        R.append(R[0])
    global _LAST
    _LAST = R
    y_prompt = np.zeros((4, SEQ, D), f)
    y_sample = np.zeros((16, 32, D), f)
    for c in range(8):
        b = c // 2
        oy = R[c]["o_y"]
        for i, j in enumerate(own[c]):
            y_prompt[b, j * 128:(j + 1) * 128] = oy[i * 128:(i + 1) * 128]
        y_sample[2 * c] = oy[NQT * 128:NQT * 128 + 32]
        y_sample[2 * c + 1] = oy[NQT * 128 + 32:NQT * 128 + 64]

    def pk(name, shp):
        return np.stack([R[2 * b][name] for b in range(4)], axis=0).reshape((1, 4, SEQ) + shp)

    def sk(name, shp):
        return np.concatenate([R[c][name].reshape((2, 32) + shp) for c in range(8)], axis=0)[None]

    return (y_prompt, y_sample,
            pk("o_kp", (4, 128)), pk("o_vp", (4, 128)), pk("o_kip", (128,)), pk("o_dkp", (8, 2, 128)), pk("o_dvp", (8, 256)),
            sk("o_ks", (4, 128)), sk("o_vs", (4, 128)), sk("o_kis", (128,)), sk("o_dks", (8, 2, 128)), sk("o_dvs", (8, 256)))
```
